# Optimizing a Trainium2 kernel written in Bass

```python
import math
import jax, jax.numpy as jnp
from jax import lax
import numpy as np

D_MODEL = 1024
BATCH = 2
SEQ = 16384
DEPTH = 2

N_MIXERS = 2
N_HEADS = 16
HEAD_DIM = D_MODEL // N_HEADS
MOBA_BLOCK = 256
MOBA_TOPK = 3
Q_CHUNK = 64
CONV_WIDTH = 31
D_FF = 2816
N_SUB = 3
N_ATTN = (DEPTH + 1) // 2
N_CONV = DEPTH // 2
EPS = 1e-6

kernel_name = "moba_conformer_conv_hybrid_adaln"


def rms_norm(x, g):
    xf = x.astype(jnp.float32)
    y = xf * lax.rsqrt(jnp.mean(xf * xf, axis=-1, keepdims=True) + EPS)
    return (y * g.astype(jnp.float32)).astype(x.dtype)


def layer_norm(x, g, b):
    xf = x.astype(jnp.float32)
    mu = jnp.mean(xf, axis=-1, keepdims=True)
    var = jnp.mean(jnp.square(xf - mu), axis=-1, keepdims=True)
    y = (xf - mu) * lax.rsqrt(var + EPS)
    return (y * g.astype(jnp.float32) + b.astype(jnp.float32)).astype(x.dtype)


def swiglu_ffn(h, w_in, w_out):
    u = h @ w_in
    a, b = jnp.split(u, 2, axis=-1)
    return (jax.nn.silu(a) * b) @ w_out


def alibi_slopes(n_heads):
    return jnp.asarray([2.0 ** (-8.0 * (h + 1) / n_heads) for h in range(n_heads)], dtype=jnp.float32)


def moba_attention(h, w_qkv, g_q, g_k, w_o):
    B, S, _ = h.shape
    H, Dh, BLK, C = N_HEADS, HEAD_DIM, MOBA_BLOCK, Q_CHUNK
    qkv = (h @ w_qkv).reshape(B, S, 3, H, Dh)
    q = rms_norm(qkv[:, :, 0], g_q).transpose(0, 2, 1, 3)
    k = rms_norm(qkv[:, :, 1], g_k).transpose(0, 2, 1, 3)
    v = qkv[:, :, 2].transpose(0, 2, 1, 3)
    s_pad = ((S + BLK - 1) // BLK) * BLK
    pad = [(0, 0), (0, 0), (0, s_pad - S), (0, 0)]
    q, k, v = jnp.pad(q, pad), jnp.pad(k, pad), jnp.pad(v, pad)
    nb = s_pad // BLK
    k_sel = min(MOBA_TOPK, nb)
    kb = k.reshape(B, H, nb, BLK, Dh)
    vb = v.reshape(B, H, nb, BLK, Dh)
    kbar = jnp.mean(kb.astype(jnp.float32), axis=3)
    slopes = alibi_slopes(H)
    scale = 1.0 / math.sqrt(Dh)
    b_idx = jnp.arange(B)[:, None, None, None]
    h_idx = jnp.arange(H)[None, :, None, None]
    neg = jnp.float32(-jnp.inf)

    def chunk_fn(ci):
        t0 = ci * C
        own = t0 // BLK
        qc = lax.dynamic_slice_in_dim(q, t0, C, axis=2)
        t = t0 + jnp.arange(C)
        gate = jnp.einsum('bhcd,bhnd->bhcn', qc.astype(jnp.float32), kbar)
        past = jnp.arange(nb) < own
        gate = jnp.where(past[None, None, None, :], gate, neg)
        _, idx = lax.top_k(gate, k_sel)
        valid = idx < own
        ks = kb[b_idx, h_idx, idx]
        vs = vb[b_idx, h_idx, idx]
        pos_sel = idx[..., None] * BLK + jnp.arange(BLK)
        dist_sel = (t[None, None, :, None, None] - pos_sel).astype(jnp.float32)
        s_sel = jnp.einsum('bhcd,bhckld->bhckl', qc, ks).astype(jnp.float32) * scale
        s_sel = s_sel - slopes[None, :, None, None, None] * jnp.abs(dist_sel)
        s_sel = jnp.where(valid[..., None], s_sel, neg)
        ko = lax.dynamic_index_in_dim(kb, own, axis=2, keepdims=False)
        vo = lax.dynamic_index_in_dim(vb, own, axis=2, keepdims=False)
        pos_own = own * BLK + jnp.arange(BLK)
        dist_own = (t[:, None] - pos_own[None, :]).astype(jnp.float32)
        s_own = jnp.einsum('bhcd,bhld->bhcl', qc, ko).astype(jnp.float32) * scale
        s_own = s_own - slopes[None, :, None, None] * jnp.abs(dist_own)[None, None]
        s_own = jnp.where((dist_own >= 0)[None, None], s_own, neg)
        s_all = jnp.concatenate([s_sel.reshape(B, H, C, k_sel * BLK), s_own], axis=-1)
        p = jax.nn.softmax(s_all, axis=-1).astype(v.dtype)
        p_sel = p[..., :k_sel * BLK].reshape(B, H, C, k_sel, BLK)
        p_own = p[..., k_sel * BLK:]
        return (jnp.einsum('bhckl,bhckld->bhcd', p_sel, vs)
                + jnp.einsum('bhcl,bhld->bhcd', p_own, vo))

    outs = lax.map(chunk_fn, jnp.arange(s_pad // C))
    o = outs.transpose(1, 0, 3, 2, 4).reshape(B, s_pad, H * Dh)[:, :S]
    return o @ w_o


def conformer_conv(h, w_pw1, b_pw1, w_dw, b_dw, ln_g, ln_b, w_pw2, b_pw2):
    D = h.shape[-1]
    u = h @ w_pw1 + b_pw1
    a, g = jnp.split(u, 2, axis=-1)
    u = a * jax.nn.sigmoid(g)
    u = lax.conv_general_dilated(
        u, w_dw[:, None, :].astype(u.dtype), window_strides=(1,),
        padding=[(CONV_WIDTH - 1, 0)],
        dimension_numbers=('NWC', 'WIO', 'NWC'),
        feature_group_count=D) + b_dw
    u = jax.nn.silu(layer_norm(u, ln_g, ln_b))
    return u @ w_pw2 + b_pw2


def setup_inputs(seed: int = 0) -> dict:
    key = jax.random.key(seed)
    ks = jax.random.split(key, 24)
    D = D_MODEL
    nrm = lambda k, shape, fan_in: jax.random.normal(k, shape, jnp.float32) * (fan_in ** -0.5)
    small = lambda k, shape, s: jax.random.normal(k, shape, jnp.float32) * s
    return {
        "x": jax.random.normal(ks[0], (BATCH, SEQ, D), jnp.float32),
        "c": jax.random.normal(ks[1], (BATCH, D), jnp.float32),
        "norm_g": 1.0 + small(ks[2], (DEPTH, N_SUB, D), 0.02),
        "ada_w": nrm(ks[3], (DEPTH, D, N_SUB * 3 * D), D),
        "ada_b": small(ks[4], (DEPTH, N_SUB * 3 * D), 0.02),
        "ffn_w_in": nrm(ks[5], (DEPTH, 2, D, 2 * D_FF), D),
        "ffn_w_out": nrm(ks[6], (DEPTH, 2, D_FF, D), D_FF),
        "attn_w_qkv": nrm(ks[7], (N_ATTN, D, 3 * N_HEADS * HEAD_DIM), D),
        "attn_g_q": 1.0 + small(ks[8], (N_ATTN, HEAD_DIM), 0.02),
        "attn_g_k": 1.0 + small(ks[9], (N_ATTN, HEAD_DIM), 0.02),
        "attn_w_o": nrm(ks[10], (N_ATTN, N_HEADS * HEAD_DIM, D), N_HEADS * HEAD_DIM),
        "conv_w_pw1": nrm(ks[11], (N_CONV, D, 2 * D), D),
        "conv_b_pw1": small(ks[12], (N_CONV, 2 * D), 0.02),
        "conv_w_dw": nrm(ks[13], (N_CONV, CONV_WIDTH, D), CONV_WIDTH),
        "conv_b_dw": small(ks[14], (N_CONV, D), 0.02),
        "conv_ln_g": 1.0 + small(ks[15], (N_CONV, D), 0.02),
        "conv_ln_b": small(ks[16], (N_CONV, D), 0.02),
        "conv_w_pw2": nrm(ks[17], (N_CONV, D, D), D),
        "conv_b_pw2": small(ks[18], (N_CONV, D), 0.02),
    }


def reference(x, c, norm_g, ada_w, ada_b, ffn_w_in, ffn_w_out,
              attn_w_qkv, attn_g_q, attn_g_k, attn_w_o,
              conv_w_pw1, conv_b_pw1, conv_w_dw, conv_b_dw, conv_ln_g, conv_ln_b,
              conv_w_pw2, conv_b_pw2):
    B = x.shape[0]
    c_act = jax.nn.silu(c)
    for i in range(DEPTH):
        mod = (c_act @ ada_w[i] + ada_b[i]).reshape(B, N_SUB, 3, D_MODEL)
        shift, scl, gate = mod[:, :, 0, None, :], mod[:, :, 1, None, :], mod[:, :, 2, None, :]

        def pre(z, j):
            return rms_norm(z, norm_g[i, j]) * (1.0 + scl[:, j]) + shift[:, j]

        x = x + 0.5 * gate[:, 0] * swiglu_ffn(pre(x, 0), ffn_w_in[i, 0], ffn_w_out[i, 0])
        h = pre(x, 1)
        m = i // N_MIXERS
        if i % N_MIXERS == 0:
            y = moba_attention(h, attn_w_qkv[m], attn_g_q[m], attn_g_k[m], attn_w_o[m])
        else:
            y = conformer_conv(h, conv_w_pw1[m], conv_b_pw1[m], conv_w_dw[m], conv_b_dw[m],
                               conv_ln_g[m], conv_ln_b[m], conv_w_pw2[m], conv_b_pw2[m])
        x = x + gate[:, 1] * y
        x = x + 0.5 * gate[:, 2] * swiglu_ffn(pre(x, 2), ffn_w_in[i, 1], ffn_w_out[i, 1])
    return x
```

```python
import math
import numpy as np
import concourse.bass as bass
import concourse.mybir as mybir
from concourse.bass_utils import run_bass_kernel_spmd

F32 = mybir.dt.float32
BF16 = mybir.dt.bfloat16
AF = mybir.ActivationFunctionType
ALU = mybir.AluOpType
AX = mybir.AxisListType

D = 1024
KC = 8
DFF = 2816
FC = 22
NH = 16
DH = 64
BLK = 256
NB = 64
SEQ = 16384
BATCH = 2
TOK = 4096
NLB = 16
EPS = 1e-6
NEG = -30000.0
CW = 31


class Buf:
    __slots__ = ("name", "last_w", "readers")

    def __init__(self, name):
        self.name = name
        self.last_w = None
        self.readers = []


class Sched:
    DMA_ENGS = ("sp", "pool_dma")

    def __init__(self, nc, n_dma_sems=12):
        self.nc = nc
        self.ops = []
        self.engs = {"pe": nc.tensor, "act": nc.scalar, "dve": nc.vector,
                     "pool": nc.gpsimd, "sp": nc.sync, "pool_dma": nc.gpsimd}
        self.stream = {"pe": "pe", "act": "act", "dve": "dve", "pool": "pool",
                       "sp": "sp", "pool_dma": "pool"}
        self.n_dma_sems = n_dma_sems
        self._stack = []
        self.bar_list = []
        cm = nc.sbuf_tensor("bar_t", [128, 8], F32)
        self.bar_t = cm.__enter__()

    def sb(self, name, shape, dt):
        self._uid = getattr(self, "_uid", 0) + 1
        cm = self.nc.sbuf_tensor("s%d_%s" % (self._uid, name), shape, dt)
        t = cm.__enter__()
        self._stack.append(cm)
        return t

    def ps(self, name, shape, dt=F32):
        cm = self.nc.psum_tensor("p_" + name, shape, dt)
        t = cm.__enter__()
        self._stack.append(cm)
        return t

    def barrier_free(self, n_free):
        nc = self.nc
        bb = Buf("barrier%d" % len(self.ops))
        prev = list(range(len(self.ops)))
        i0 = self.op("pool", lambda: nc.gpsimd.memset(self.bar_t[:, 0:1], 0.0), writes=[bb])
        self.ops[i0]["deps"] = set(prev)
        for e, fn in (("dve", lambda: nc.vector.memset(self.bar_t[:, 1:2], 0.0)),
                      ("act", lambda: nc.scalar.activation(out=self.bar_t[:, 2:3], in_=self.bar_t[:, 0:1], func=AF.Copy)),
                      ("pe", None),
                      ("sp", lambda: nc.sync.dma_start(out=self.bar_t[:, 4:5], in_=self.bar_t[:, 0:1]))):
            if fn is None:
                continue
            self.op(e, fn, reads=[bb])
        self.bar_after = i0
        self.bar_list.append(i0)
        for _ in range(n_free):
            cm = self._stack.pop()
            cm.__exit__(None, None, None)

    def op(self, eng, fn, reads=(), writes=()):
        deps = set()
        idx = len(self.ops)
        for b in reads:
            if b.last_w is not None:
                deps.add(b.last_w)
        for b in writes:
            if b.last_w is not None:
                deps.add(b.last_w)
            for r in b.readers:
                deps.add(r)
        for b in reads:
            b.readers.append(idx)
        for b in writes:
            b.last_w = idx
            b.readers = []
        deps.discard(idx)
        if self.bar_list:
            deps.add(self.bar_list[-1])
        self.ops.append({"eng": eng, "fn": fn, "deps": deps, "dma": eng in self.DMA_ENGS})
        return idx

    def emit(self):
        nc = self.nc
        ops = self.ops
        n = len(ops)
        need = [False] * n
        for i, o in enumerate(ops):
            if o["dma"]:
                need[i] = True
            for d in o["deps"]:
                if ops[d]["dma"]:
                    continue
                sd, si = self.stream[ops[d]["eng"]], self.stream[o["eng"]]
                if sd != si or sd != "pe":
                    need[d] = True
        sem_cm = []

        def mksem(name):
            cm = nc.semaphore(name)
            s = cm.__enter__()
            sem_cm.append(cm)
            return s

        eng_sem = {k: mksem("s_" + k) for k in ("pe", "act", "dve", "pool")}
        eng_cnt = {k: 0 for k in eng_sem}
        dma_sems = {q: [mksem("d_%s_%d" % (q, i)) for i in range(self.n_dma_sems)] for q in self.DMA_ENGS}
        dma_cnt = {q: [0] * self.n_dma_sems for q in self.DMA_ENGS}
        dma_last = {q: [None] * self.n_dma_sems for q in self.DMA_ENGS}
        dma_rr = {q: 0 for q in self.DMA_ENGS}
        ev = [None] * n
        waited = {s: {} for s in ("pe", "act", "dve", "pool", "sp")}
        for i, o in enumerate(ops):
            eng = o["eng"]
            st = self.stream[eng]
            e = self.engs[eng]
            waits = {}
            for d in o["deps"]:
                if ev[d] is None:
                    continue
                sem, val, key = ev[d]
                if key not in waits or waits[key][1] < val:
                    waits[key] = (sem, val)
            my = None
            if o["dma"]:
                k = dma_rr[eng]
                dma_rr[eng] = (k + 1) % self.n_dma_sems
                prev = dma_last[eng][k]
                if prev is not None:
                    sem, val, key = ev[prev]
                    if key not in waits or waits[key][1] < val:
                        waits[key] = (sem, val)
                dma_cnt[eng][k] += 16
                my = (dma_sems[eng][k], dma_cnt[eng][k], (eng, k))
                dma_last[eng][k] = i
            elif need[i]:
                eng_cnt[st] += 1
                my = (eng_sem[st], eng_cnt[st], st)
            for key, (sem, val) in waits.items():
                if waited[st].get(key, 0) >= val:
                    continue
                e.wait_ge(sem, val)
                waited[st][key] = val
            inst = o["fn"]()
            if my is not None:
                inst.then_inc(my[0], 16 if o["dma"] else 1)
                ev[i] = my
            o["fn"] = None
        sp = nc.sync
        for q in self.DMA_ENGS:
            for k in range(self.n_dma_sems):
                if dma_cnt[q][k] > 0:
                    sp.wait_ge(dma_sems[q][k], dma_cnt[q][k])
        for k in eng_sem:
            if eng_cnt[k] > 0:
                sp.wait_ge(eng_sem[k], eng_cnt[k])
        self.max_counts = dict(eng_cnt)


def snake_blocks(j):
    out = []
    for i in range(8):
        out.append(8 * i + j)
        out.append(8 * i + 7 - j)
    return out


class Ctx:
    pass


def setup_common(S, cx):
    nc = S.nc
    cx.ones_bf = S.sb("ones_bf", [128, 128], BF16)
    cx.b_ones = Buf("ones")
    S.op("dve", lambda: nc.vector.memset(cx.ones_bf[:], 1.0), writes=[cx.b_ones])
    cx.psum = [S.ps("psb%d" % i, [128, 512], F32) for i in range(8)]
    cx.b_psum = [Buf("psb%d" % i) for i in range(8)]


def emit_mod_vectors(S, cx, modT, normg, layer_slot, j):
    nc = S.nc
    A = S.sb("modA_%s" % layer_slot, [128, 8], F32)
    b = Buf("modA_%s" % layer_slot)
    base = j * 24
    S.op("dve", lambda: nc.vector.scalar_tensor_tensor(
        out=A[:], in0=modT[:, base + 8:base + 16], scalar=1.0, in1=normg,
        op0=ALU.add, op1=ALU.mult), reads=[cx.b_mod], writes=[b])
    return A, modT[:, base:base + 8], modT[:, base + 16:base + 24], b


def emit_ffn_phase(S, cx, x_in, b_xin, x_out, b_xout, w_in_d, w_out_d, A, Bv, G, gscale, b_mod, ntiles, tag):
    nc = S.nc
    win = cx.win
    wout = cx.wout
    HF = FC // 2
    for kc in range(KC):
        S.op("pool_dma", (lambda kc=kc: nc.gpsimd.dma_start(out=win[:, kc, :], in_=w_in_d[kc * 128:(kc + 1) * 128, :])),
             writes=[cx.b_win[kc]])
    for f in range(FC):
        S.op("pool_dma", (lambda f=f: nc.gpsimd.dma_start(out=wout[:, f, :], in_=w_out_d[f * 128:(f + 1) * 128, :])),
             writes=[cx.b_wout[f]])
    Gs = S.sb("Gs_" + tag, [128, 8], F32)
    b_Gs = Buf("Gs_" + tag)
    S.op("dve", lambda: nc.vector.tensor_scalar(out=Gs[:], in0=G, scalar1=float(gscale), scalar2=None, op0=ALU.mult),
         reads=[b_mod], writes=[b_Gs])
    xv_in = x_in.rearrange("(c p) t -> p c t", p=128)
    xv_out = x_out.rearrange("(c p) t -> p c t", p=128)
    for t in range(ntiles):
        xt = cx.xt[t % 2]
        bx = cx.b_xt[t % 2]
        S.op("sp", (lambda xt=xt, t=t: nc.sync.dma_start(out=xt[:], in_=xv_in[:, :, t * 512:(t + 1) * 512])),
             reads=[b_xin[t]], writes=[bx])
        emit_norm_mod(S, cx, xt, bx, A, Bv, b_mod)
        for hf in range(2):
            for fl in range(HF):
                f = hf * HF + fl
                pa = 1 + (f % 2) * 2
                pb = pa + 1

                def mm_a(f=f, pa=pa):
                    for kc in range(KC):
                        m = nc.tensor.matmul(cx.psum[pa][:], lhsT=win[:, kc, f * 128:(f + 1) * 128], rhs=cx.h[:, kc, :],
                                             start=(kc == 0), stop=(kc == KC - 1))
                    return m

                def mm_b(f=f, pb=pb):
                    for kc in range(KC):
                        m = nc.tensor.matmul(cx.psum[pb][:], lhsT=win[:, kc, DFF + f * 128:DFF + (f + 1) * 128],
                                             rhs=cx.h[:, kc, :], start=(kc == 0), stop=(kc == KC - 1))
                    return m
                S.op("pe", mm_a, reads=cx.b_win + [cx.b_h], writes=[cx.b_psum[pa]])
                S.op("pe", mm_b, reads=cx.b_win + [cx.b_h], writes=[cx.b_psum[pb]])
                sa = cx.sa[f % 2]
                S.op("act", (lambda sa=sa, pa=pa: nc.scalar.activation(out=sa[:], in_=cx.psum[pa][:], func=AF.Silu)),
                     reads=[cx.b_psum[pa]], writes=[cx.b_sa[f % 2]])
                S.op("dve", (lambda sa=sa, pb=pb, fl=fl: nc.vector.tensor_tensor(out=cx.u[:, fl, :], in0=sa[:], in1=cx.psum[pb][:], op=ALU.mult)),
                     reads=[cx.b_sa[f % 2], cx.b_psum[pb]], writes=[cx.b_u[fl]])
            for oc in range(KC):
                py = 5 + (oc % 2)

                def mm_y(oc=oc, py=py, hf=hf):
                    for fl in range(HF):
                        m = nc.tensor.matmul(cx.psum[py][:], lhsT=wout[:, hf * HF + fl, oc * 128:(oc + 1) * 128], rhs=cx.u[:, fl, :],
                                             start=(fl == 0), stop=(fl == HF - 1))
                    return m
                S.op("pe", mm_y, reads=cx.b_wout + cx.b_u, writes=[cx.b_psum[py]])
                S.op("dve", (lambda oc=oc, py=py, xt=xt: nc.vector.scalar_tensor_tensor(
                    out=xt[:, oc, :], in0=cx.psum[py][:], scalar=Gs[:, oc:oc + 1], in1=xt[:, oc, :],
                    op0=ALU.mult, op1=ALU.add)), reads=[cx.b_psum[py], b_Gs], writes=[bx])
        S.op("sp", (lambda xt=xt, t=t: nc.sync.dma_start(out=xv_out[:, :, t * 512:(t + 1) * 512], in_=xt[:])),
             reads=[bx], writes=[b_xout[t]])


def emit_norm_mod(S, cx, xt, bx, A, Bv, b_mod, n=512):
    nc = S.nc
    for c in range(KC):
        S.op("act", (lambda c=c: nc.scalar.activation(out=cx.sq[c % 2][:, :n], in_=xt[:, c, :n], func=AF.Square)),
             reads=[bx], writes=[cx.b_sq[c % 2]])
        S.op("pe", (lambda c=c: nc.tensor.matmul(cx.psum[0][:, :n], lhsT=cx.ones_bf[:], rhs=cx.sq[c % 2][:, :n],
                                                 start=(c == 0), stop=(c == KC - 1))),
             reads=[cx.b_sq[c % 2], cx.b_ones], writes=[cx.b_psum[0]])
    S.op("dve", lambda: nc.vector.tensor_scalar(out=cx.rstd[:, :n], in0=cx.psum[0][:, :n], scalar1=1.0 / D, scalar2=EPS,
                                                op0=ALU.mult, op1=ALU.add), reads=[cx.b_psum[0]], writes=[cx.b_rstd])
    S.op("act", lambda: nc.scalar.activation(out=cx.rstd[:, :n], in_=cx.rstd[:, :n], func=AF.Sqrt), reads=[cx.b_rstd], writes=[cx.b_rstd])
    S.op("dve", lambda: nc.vector.reciprocal(out=cx.rstd[:, :n], in_=cx.rstd[:, :n]), reads=[cx.b_rstd], writes=[cx.b_rstd])
    for c in range(KC):
        tmp = cx.tmp[c % 2]
        S.op("dve", (lambda c=c, tmp=tmp: nc.vector.tensor_tensor(out=tmp[:, :n], in0=xt[:, c, :n], in1=cx.rstd[:, :n], op=ALU.mult)),
             reads=[bx, cx.b_rstd], writes=[cx.b_tmp[c % 2]])
        S.op("act", (lambda c=c, tmp=tmp: nc.scalar.activation(out=cx.h[:, c, :n], in_=tmp[:, :n], func=AF.Identity,
                                                               bias=Bv[:, c:c + 1], scale=A[:, c:c + 1])),
             reads=[cx.b_tmp[c % 2], b_mod], writes=[cx.b_h])


def alloc_tile_bufs(S, cx):
    cx.xt = [S.sb("xt%d" % i, [128, KC, 512], F32) for i in range(2)]
    cx.b_xt = [Buf("xt%d" % i) for i in range(2)]
    cx.sq = [S.sb("sq%d" % i, [128, 512], BF16) for i in range(2)]
    cx.b_sq = [Buf("sq%d" % i) for i in range(2)]
    cx.h = S.sb("h", [128, KC, 512], BF16)
    cx.b_h = Buf("h")
    cx.rstd = S.sb("rstd", [128, 512], F32)
    cx.b_rstd = Buf("rstd")
    cx.tmp = [S.sb("tmp%d" % i, [128, 512], F32) for i in range(2)]
    cx.b_tmp = [Buf("tmp%d" % i) for i in range(2)]
    cx.sa = [S.sb("sa%d" % i, [128, 512], F32) for i in range(2)]
    cx.b_sa = [Buf("sa%d" % i) for i in range(2)]


def alloc_ffn_bufs(S, cx):
    cx.win = S.sb("win", [128, KC, 2 * DFF], BF16)
    cx.wout = S.sb("wout", [128, FC, D], BF16)
    cx.b_win = [Buf("win%d" % i) for i in range(KC)]
    cx.b_wout = [Buf("wout%d" % i) for i in range(FC)]
    cx.u = S.sb("u", [128, FC // 2, 512], BF16)
    cx.b_u = [Buf("u%d" % i) for i in range(FC // 2)]


def _fm(v, n=None):
    v = np.asarray(v, np.float32).reshape(-1, 128)
    return np.ascontiguousarray(v.T)


SP_OFF = {}


def _sp_layout():
    off = 0
    for name, w in (("c", 8), ("ada_b0", 72), ("ada_b1", 72),
                    ("ng00", 8), ("ng01", 8), ("ng02", 8), ("ng10", 8), ("ng11", 8), ("ng12", 8),
                    ("gq", 1), ("gk", 1), ("b_pw1", 16), ("b_dw", 8), ("ln_g", 8), ("ln_b", 8),
                    ("b_pw2", 8), ("w_dw", 8 * CW)):
        SP_OFF[name] = (off, w)
        off += w
    return off


SP_N = _sp_layout()


def pack_small(inp, b):
    sp = np.zeros((128, SP_N), np.float32)

    def put(name, arr):
        o, w = SP_OFF[name]
        assert arr.shape == (128, w), (name, arr.shape, w)
        sp[:, o:o + w] = arr
    put("c", _fm(inp["c"][b]))
    put("ada_b0", _fm(inp["ada_b"][0]))
    put("ada_b1", _fm(inp["ada_b"][1]))
    for i in range(2):
        for j in range(3):
            put("ng%d%d" % (i, j), _fm(inp["norm_g"][i, j]))
    put("gq", np.tile(np.asarray(inp["attn_g_q"][0], np.float32), 2).reshape(128, 1))
    put("gk", np.tile(np.asarray(inp["attn_g_k"][0], np.float32), 2).reshape(128, 1))
    put("b_pw1", _fm(inp["conv_b_pw1"][0]))
    put("b_dw", _fm(inp["conv_b_dw"][0]))
    put("ln_g", _fm(inp["conv_ln_g"][0]))
    put("ln_b", _fm(inp["conv_ln_b"][0]))
    put("b_pw2", _fm(inp["conv_b_pw2"][0]))
    wd = np.asarray(inp["conv_w_dw"][0], np.float32)
    put("w_dw", np.ascontiguousarray(wd.reshape(CW, 8, 128).transpose(2, 1, 0)).reshape(128, 8 * CW))
    return sp


def spc(cx, name):
    o, w = SP_OFF[name]
    return cx.spt[:, o:o + w]


def emit_load_small(S, cx, sp_d):
    nc = S.nc
    cx.spt = S.sb("spt", [128, SP_N], F32)
    cx.b_sp = Buf("spt")
    S.op("sp", lambda: nc.sync.dma_start(out=cx.spt[:], in_=sp_d[:, :]), writes=[cx.b_sp])


def emit_adaln(S, cx, ada_w_d, modT, b_modT):
    nc = S.nc
    NCOL = 1152
    stg = [S.sb("adastg%d" % i, [128, KC, NCOL], F32) for i in range(2)]
    b_stg = [Buf("adastg%d" % i) for i in range(2)]
    cact = S.sb("cact", [128, 8], F32)
    b_cact = Buf("cact")
    S.op("act", lambda: nc.scalar.activation(out=cact[:], in_=spc(cx, "c"), func=AF.Silu), reads=[cx.b_sp], writes=[b_cact])
    k = 0
    for i in range(2):
        wv = ada_w_d[i].rearrange("(kc p) n -> p kc n", p=128)
        for pc in range(9216 // NCOL):
            st = stg[k % 2]
            bs = b_stg[k % 2]
            k += 1
            S.op("sp", (lambda st=st, wv=wv, pc=pc: nc.sync.dma_start(out=st[:], in_=wv[:, :, pc * NCOL:(pc + 1) * NCOL])), writes=[bs])

            def mm(st=st, pc=pc, i=i):
                for m in range(NCOL // 128):
                    col = pc * (NCOL // 128) + m
                    for kc in range(KC):
                        r = nc.tensor.matmul(cx.psum[7][:, col:col + 1], lhsT=st[:, kc, m * 128:(m + 1) * 128], rhs=cact[:, kc:kc + 1],
                                             start=(kc == 0), stop=(kc == KC - 1))
                return r
            S.op("pe", mm, reads=[bs, b_cact], writes=[cx.b_psum[7]])
        S.op("dve", (lambda i=i: nc.vector.tensor_tensor(out=modT[i][:], in0=cx.psum[7][:, 0:72], in1=spc(cx, "ada_b%d" % i), op=ALU.add)),
             reads=[cx.b_psum[7], cx.b_sp], writes=[b_modT[i]])
    return stg


def joint_mod(S, cx, i, j, modT, b_modT):
    nc = S.nc
    A, Bv, G, bA = emit_mod_vectors_i(S, cx, modT[i], b_modT[i], spc(cx, "ng%d%d" % (i, j)), "l%ds%d" % (i, j), j)
    return A, Bv, G, bA


def emit_mod_vectors_i(S, cx, modT, b_modT, normg, tag, j):
    nc = S.nc
    AB = S.sb("modAB_" + tag, [128, 24], F32)
    b = Buf("modAB_" + tag)
    base = j * 24
    S.op("dve", lambda: nc.vector.tensor_copy(out=AB[:], in_=modT[:, base:base + 24]), reads=[b_modT, cx.b_sp], writes=[b])
    S.op("dve", lambda: nc.vector.scalar_tensor_tensor(
        out=AB[:, 8:16], in0=modT[:, base + 8:base + 16], scalar=1.0, in1=normg,
        op0=ALU.add, op1=ALU.mult), reads=[b_modT, cx.b_sp], writes=[b])
    return AB[:, 8:16], AB[:, 0:8], AB[:, 16:24], b


def load_x_tile(S, cx, xv, b_x, t, n=512):
    nc = S.nc
    xt = cx.xt[t % 2]
    bx = cx.b_xt[t % 2]
    S.op("sp", (lambda: nc.sync.dma_start(out=xt[:, :, :n], in_=xv[:, :, t * n:(t + 1) * n])), reads=[b_x[t]], writes=[bx])
    return xt, bx


def emit_qkv_phase(S, cx, x_d, b_x, wqkv_d, A, Bv, b_mod, QT_d, KT_d, kbar_d, V_d):
    nc = S.nc
    wq = S.sb("wqkv_sb", [128, KC, 3072], BF16)
    b_wq = [Buf("wqkv%d" % k) for k in range(KC)]
    for kc in range(KC):
        S.op("pool_dma", (lambda kc=kc: nc.gpsimd.dma_start(out=wq[:, kc, :], in_=wqkv_d[kc * 128:(kc + 1) * 128, :])), writes=[b_wq[kc]])
    bo = S.sb("blkones", [128, 128], BF16)
    b_bo = Buf("blkones")
    S.op("dve", lambda: nc.vector.memset(bo[:], 0.0), writes=[b_bo])
    S.op("dve", lambda: nc.vector.memset(bo[0:64, 0:64], 1.0), writes=[b_bo])
    S.op("dve", lambda: nc.vector.memset(bo[64:128, 64:128], 1.0), writes=[b_bo])
    g8 = S.sb("g8", [128, 2], F32)
    b_g8 = Buf("g8")
    S.op("dve", lambda: nc.vector.tensor_scalar(out=g8[:, 0:1], in0=spc(cx, "gq"), scalar1=0.125, scalar2=None, op0=ALU.mult),
         reads=[cx.b_sp], writes=[b_g8])
    S.op("dve", lambda: nc.vector.tensor_copy(out=g8[:, 1:2], in_=spc(cx, "gk")), reads=[cx.b_sp], writes=[b_g8])
    qo = [S.sb("qo%d" % i, [128, 512], BF16) for i in range(2)]
    b_qo = [Buf("qo%d" % i) for i in range(2)]
    kb = [S.sb("kb%d" % i, [128, 2], F32) for i in range(2)]
    b_kb = [Buf("kb%d" % i) for i in range(2)]
    vt = [S.sb("vt%d" % i, [128, 1024], BF16) for i in range(2)]
    b_vt = [Buf("vt%d" % i) for i in range(2)]
    xv = x_d.rearrange("(c p) t -> p c t", p=128)
    QTv = QT_d.rearrange("h d t -> (h d) t")
    KTv = KT_d.rearrange("h d t -> (h d) t")
    kbv = kbar_d.rearrange("h d l -> (h d) l")
    cnt = 0
    for t in range(8):
        xt, bx = load_x_tile(S, cx, xv, b_x, t)
        emit_norm_mod(S, cx, xt, bx, A, Bv, b_mod)
        for which in range(2):
            goff = which * 1024
            outv = QTv if which == 0 else KTv
            for hp in range(8):
                k = cnt % 2
                cnt += 1
                pq = cx.psum[1 + k]
                pss = cx.psum[3 + k]

                def mm(hp=hp, pq=pq, goff=goff):
                    for kc in range(KC):
                        m = nc.tensor.matmul(pq[:], lhsT=wq[:, kc, goff + hp * 128:goff + (hp + 1) * 128], rhs=cx.h[:, kc, :],
                                             start=(kc == 0), stop=(kc == KC - 1))
                    return m
                S.op("pe", mm, reads=b_wq + [cx.b_h], writes=[cx.b_psum[1 + k]])
                S.op("act", (lambda k=k, pq=pq: nc.scalar.activation(out=cx.sq[k][:], in_=pq[:], func=AF.Square)),
                     reads=[cx.b_psum[1 + k]], writes=[cx.b_sq[k]])
                S.op("pe", (lambda k=k, pss=pss: nc.tensor.matmul(pss[:], lhsT=bo[:], rhs=cx.sq[k][:], start=True, stop=True)),
                     reads=[cx.b_sq[k], b_bo], writes=[cx.b_psum[3 + k]])
                r1 = cx.tmp[k]
                S.op("dve", (lambda r1=r1, pss=pss: nc.vector.tensor_scalar(out=r1[:], in0=pss[:], scalar1=1.0 / DH, scalar2=EPS,
                                                                            op0=ALU.mult, op1=ALU.add)),
                     reads=[cx.b_psum[3 + k]], writes=[cx.b_tmp[k]])
                S.op("act", (lambda r1=r1: nc.scalar.activation(out=r1[:], in_=r1[:], func=AF.Sqrt)), reads=[cx.b_tmp[k]], writes=[cx.b_tmp[k]])
                S.op("dve", (lambda r1=r1: nc.vector.reciprocal(out=r1[:], in_=r1[:])), reads=[cx.b_tmp[k]], writes=[cx.b_tmp[k]])
                S.op("dve", (lambda r1=r1, pq=pq: nc.vector.tensor_tensor(out=r1[:], in0=pq[:], in1=r1[:], op=ALU.mult)),
                     reads=[cx.b_tmp[k], cx.b_psum[1 + k]], writes=[cx.b_tmp[k]])
                sa = cx.sa[k]
                S.op("act", (lambda r1=r1, sa=sa, which=which: nc.scalar.activation(out=sa[:], in_=r1[:], func=AF.Copy,
                                                                                  scale=g8[:, which:which + 1])),
                     reads=[cx.b_tmp[k], b_g8], writes=[cx.b_sa[k]])
                S.op("pool", (lambda k=k, sa=sa: nc.gpsimd.tensor_copy(out=qo[k][:], in_=sa[:])), reads=[cx.b_sa[k]], writes=[b_qo[k]])
                S.op("sp", (lambda k=k, hp=hp, t=t, outv=outv: nc.sync.dma_start(out=outv[hp * 128:(hp + 1) * 128, t * 512:(t + 1) * 512], in_=qo[k][:])),
                     reads=[b_qo[k]])
                if which == 1:
                    S.op("dve", (lambda k=k, sa=sa: nc.vector.tensor_reduce(out=kb[k][:], in_=sa[:].rearrange("p (b t) -> p b t", b=2),
                                                                          axis=AX.X, op=ALU.add)),
                         reads=[cx.b_sa[k]], writes=[b_kb[k]])
                    S.op("dve", (lambda k=k: nc.vector.tensor_scalar(out=kb[k][:], in0=kb[k][:], scalar1=1.0 / BLK, scalar2=None, op0=ALU.mult)),
                         reads=[b_kb[k]], writes=[b_kb[k]])
                    S.op("sp", (lambda k=k, hp=hp, t=t: nc.sync.dma_start(out=kbv[hp * 128:(hp + 1) * 128, 2 * t:2 * t + 2], in_=kb[k][:])),
                         reads=[b_kb[k]])
        for tg in range(4):
            v = vt[tg % 2]
            for ns in range(2):
                pv = cx.psum[5 + ns]

                def mmv(tg=tg, ns=ns, pv=pv):
                    for kc in range(KC):
                        m = nc.tensor.matmul(pv[:], lhsT=cx.h[:, kc, tg * 128:(tg + 1) * 128], rhs=wq[:, kc, 2048 + ns * 512:2048 + (ns + 1) * 512],
                                             start=(kc == 0), stop=(kc == KC - 1))
                    return m
                S.op("pe", mmv, reads=b_wq + [cx.b_h], writes=[cx.b_psum[5 + ns]])
                if ns == 0:
                    S.op("act", (lambda v=v, pv=pv: nc.scalar.copy(out=v[:, 0:512], in_=pv[:])), reads=[cx.b_psum[5]], writes=[b_vt[tg % 2]])
                else:
                    S.op("dve", (lambda v=v, pv=pv: nc.vector.tensor_copy(out=v[:, 512:1024], in_=pv[:])), reads=[cx.b_psum[6]], writes=[b_vt[tg % 2]])
            S.op("sp", (lambda v=v, tg=tg, t=t: nc.sync.dma_start(out=V_d[t * 512 + tg * 128:t * 512 + (tg + 1) * 128, :], in_=v[:])),
                 reads=[b_vt[tg % 2]])


def build_L1():
    nc = bass.Bass("TRN2", target_bir_lowering=False)
    sp_d = nc.dram_tensor("sp", [128, SP_N], F32, kind="ExternalInput").ap()
    ada_w = nc.dram_tensor("ada_w", [2, 1024, 9216], F32, kind="ExternalInput").ap()
    xin = nc.dram_tensor("xin", [1024, TOK], F32, kind="ExternalInput").ap()
    w_in = nc.dram_tensor("w_in", [1024, 2 * DFF], F32, kind="ExternalInput").ap()
    w_out = nc.dram_tensor("w_out", [DFF, 1024], F32, kind="ExternalInput").ap()
    wqkv = nc.dram_tensor("wqkv", [1024, 3072], F32, kind="ExternalInput").ap()
    x1 = nc.dram_tensor("x1", [1024, TOK], F32, kind="ExternalOutput").ap()
    modo = nc.dram_tensor("modo", [128, 144], F32, kind="ExternalOutput").ap()
    QT = nc.dram_tensor("QT", [NH, DH, TOK], BF16, kind="ExternalOutput").ap()
    KT = nc.dram_tensor("KT", [NH, DH, TOK], BF16, kind="ExternalOutput").ap()
    kbar = nc.dram_tensor("kbar", [NH, DH, NLB], F32, kind="ExternalOutput").ap()
    V = nc.dram_tensor("V", [TOK, 1024], BF16, kind="ExternalOutput").ap()
    S = Sched(nc)
    cx = Ctx()
    setup_common(S, cx)
    emit_load_small(S, cx, sp_d)
    modT = [S.sb("modT%d" % i, [128, 72], F32) for i in range(2)]
    b_modT = [Buf("modT%d" % i) for i in range(2)]
    A0, B0, G0, bm0 = None, None, None, None
    AB0 = joint_mod
    emit_adaln_scoped(S, cx, ada_w, modT, b_modT)
    for i in range(2):
        S.op("sp", (lambda i=i: nc.sync.dma_start(out=modo[:, i * 72:(i + 1) * 72], in_=modT[i][:])), reads=[b_modT[i]])
    A, Bv, G, bA = emit_mod_vectors_i(S, cx, modT[0], b_modT[0], spc(cx, "ng00"), "l0s0", 0)
    A1, Bv1, G1, bA1 = emit_mod_vectors_i(S, cx, modT[0], b_modT[0], spc(cx, "ng01"), "l0s1", 1)
    alloc_tile_bufs(S, cx)
    alloc_ffn_bufs(S, cx)
    b_xin = [Buf("xin%d" % t) for t in range(8)]
    b_x1 = [Buf("x1_%d" % t) for t in range(8)]
    emit_ffn_phase(S, cx, xin, b_xin, x1, b_x1, w_in, w_out, A, Bv, G, 0.5, bA, 8, "f00")
    S.barrier_free(3)
    emit_qkv_phase(S, cx, x1, b_x1, wqkv, A1, Bv1, bA1, QT, KT, kbar, V)
    S.emit()
    return nc


def emit_adaln_scoped(S, cx, ada_w, modT, b_modT):
    n0 = len(S._stack)
    emit_adaln(S, cx, ada_w, modT, b_modT)
    S.barrier_free(len(S._stack) - n0)


SLOPES = [2.0 ** (-8.0 * (h + 1) / NH) for h in range(NH)]


def emit_attn_phase(S, cx, d):
    nc = S.nc
    KTa = [S.sb("KTa%d" % i, [128, SEQ], BF16) for i in range(2)]
    Va = [S.sb("Va%d" % i, [128, 128, 65], BF16) for i in range(2)]
    Qa = [S.sb("Qa%d" % i, [128, TOK], BF16) for i in range(2)]
    Ko = [S.sb("Ko%d" % i, [128, TOK], BF16) for i in range(2)]
    Vo = [S.sb("Vo%d" % i, [128, 32, 65], BF16) for i in range(2)]
    bT = [S.sb("bT%d" % i, [128, 1024], F32) for i in range(2)]
    dgb = [S.sb("dgb%d" % i, [128, 512], F32) for i in range(2)]
    cq = [S.sb("cq%d" % i, [128, 256], F32) for i in range(2)]
    b_KT = [Buf("KTa%d" % i) for i in range(2)]
    b_Va = [Buf("Va%d" % i) for i in range(2)]
    b_Qa = [Buf("Qa%d" % i) for i in range(2)]
    b_Qm = [[Buf("Qm%d_%d" % (i, l)) for l in range(NLB)] for i in range(2)]
    b_Ko = [Buf("Ko%d" % i) for i in range(2)]
    b_Vo = [Buf("Vo%d" % i) for i in range(2)]
    b_hd = [Buf("hd%d" % i) for i in range(2)]
    cst = S.sb("cst", [128, CST_N], F32)
    b_cst = Buf("cst")
    S.op("sp", lambda: nc.sync.dma_start(out=cst[:], in_=d["cst"][:, :]), writes=[b_cst])

    def C(name):
        o, w = CST_OFF[name]
        return cst[:, o:o + w]
    kb32 = S.sb("kb32", [64, NH * NB], F32)
    kbh = S.sb("kbh", [64, NH * NB], BF16)
    kbl = S.sb("kbl", [64, NH * NB], BF16)
    kbt = S.sb("kbt", [64, NH * NB], F32)
    b_kb = Buf("kbhl")
    S.op("sp", lambda: nc.sync.dma_start(out=kb32[:], in_=d["kbar"].rearrange("d h m -> d (h m)")), writes=[b_kb])
    S.op("dve", lambda: nc.vector.tensor_copy(out=kbh[:], in_=kb32[:]), reads=[b_kb], writes=[b_kb])
    S.op("dve", lambda: nc.vector.tensor_copy(out=kbt[:], in_=kbh[:]), reads=[b_kb], writes=[b_kb])
    S.op("dve", lambda: nc.vector.tensor_tensor(out=kbt[:], in0=kb32[:], in1=kbt[:], op=ALU.subtract), reads=[b_kb], writes=[b_kb])
    S.op("dve", lambda: nc.vector.tensor_copy(out=kbl[:], in_=kbt[:]), reads=[b_kb], writes=[b_kb])
    mbp = [S.sb("mbp%d" % i, [128, 128], F32) for i in range(2)]
    b_mbp = [Buf("mbp%d" % i) for i in range(2)]
    gmt = [S.sb("gmt%d" % i, [128, 64], F32) for i in range(2)]
    b_gmt = [Buf("gmt%d" % i) for i in range(2)]
    t8 = [S.sb("t8_%d" % i, [128, 8], F32) for i in range(2)]
    pt = [S.sb("pt%d" % i, [128, 512], BF16) for i in range(2)]
    b_pt = [Buf("pt%d" % i) for i in range(2)]
    pt2 = S.sb("pt2", [128, 512], BF16)
    b_pt2 = Buf("pt2")
    dtmp = S.sb("dtmp", [128, 512], F32)
    b_dtmp = Buf("dtmp")
    o1 = S.sb("o1", [128, 256], F32)
    o2 = S.sb("o2", [128, 256], F32)
    rd = S.sb("rd", [128, 256], F32)
    ob = [S.sb("ob%d" % i, [128, 256], BF16) for i in range(2)]
    b_o = Buf("o12")
    b_ob = [Buf("ob%d" % i) for i in range(2)]
    for i in range(2):
        S.op("sp", (lambda i=i: nc.sync.dma_start(out=KTa[i][64:128, :], in_=d["ind"][:, :])), writes=[b_KT[i]])
        S.op("dve", (lambda i=i: nc.vector.memset(Va[i][:, :, 64:65], 1.0)), writes=[b_Va[i]])
        S.op("dve", (lambda i=i: nc.vector.memset(Vo[i][:, :, 64:65], 1.0)), writes=[b_Vo[i]])
        S.op("dve", (lambda i=i: nc.vector.memset(mbp[i][:], 0.0)), writes=[b_mbp[i]])
    ps = cx.psum
    bp = cx.b_psum
    scnt = 0
    gcnt = 0
    for h in range(NH):
        cur = h % 2
        sl = SLOPES[h]
        es = math.exp(sl)
        S.op("sp", (lambda h=h, cur=cur: nc.sync.dma_start(out=KTa[cur][0:64, :], in_=d["KT"][h])), writes=[b_KT[cur]])
        for q8 in range(8):
            S.op("sp", (lambda h=h, cur=cur, q8=q8: nc.sync.dma_start(out=Va[cur][:, q8 * 16:(q8 + 1) * 16, 0:64], in_=d["Vd"][h, :, q8 * 16:(q8 + 1) * 16, :])),
                 writes=[b_Va[cur]])
        S.op("sp", (lambda h=h, cur=cur: nc.sync.dma_start(out=Qa[cur][0:64, :], in_=d["QT"][h])), writes=[b_Qa[cur]] + b_Qm[cur])
        S.op("sp", (lambda h=h, cur=cur: nc.sync.dma_start(out=Ko[cur][0:64, :], in_=d["KTo"][h])), writes=[b_Ko[cur]])
        for q2 in range(2):
            S.op("sp", (lambda h=h, cur=cur, q2=q2: nc.sync.dma_start(out=Vo[cur][:, q2 * 16:(q2 + 1) * 16, 0:64], in_=d["Vo"][h, :, q2 * 16:(q2 + 1) * 16, :])),
                 writes=[b_Vo[cur]])
        for (VV, bV) in ((Va[cur], b_Va[cur]), (Vo[cur], b_Vo[cur])):
            Vodd = VV[:].rearrange("p (n two) e -> p n two e", two=2)[:, :, 1, :]
            S.op("dve", (lambda Vodd=Vodd, es=es: nc.vector.tensor_scalar(out=Vodd[:, :, 0:64], in0=Vodd[:, :, 0:64], scalar1=es, scalar2=None, op0=ALU.mult)),
                 reads=[bV], writes=[bV])
            S.op("dve", (lambda Vodd=Vodd, es=es: nc.vector.memset(Vodd[:, :, 64:65], es)), reads=[bV], writes=[bV])
        S.op("dve", (lambda cur=cur, sl=sl: nc.vector.tensor_scalar(out=bT[cur][:], in0=C("base"), scalar1=sl, scalar2=None, op0=ALU.mult)),
             reads=[b_cst], writes=[b_hd[cur]])
        S.op("dve", (lambda cur=cur, sl=sl: nc.vector.scalar_tensor_tensor(out=dgb[cur][:], in0=C("Dm"), scalar=-sl, in1=C("causal"),
                                                                         op0=ALU.mult, op1=ALU.add)), reads=[b_cst], writes=[b_hd[cur]])
        S.op("act", (lambda cur=cur, sl=sl: nc.scalar.activation(out=cq[cur][:], in_=C("irow"), func=AF.Exp, scale=-sl)),
             reads=[b_cst], writes=[b_hd[cur]])
        for l in range(NLB):
            for hq in range(2):
                g = gcnt % 2
                gcnt += 1
                q0 = l * 256 + hq * 128

                def mmg(cur=cur, q0=q0, h=h):
                    nc.tensor.matmul(ps[6][:, 0:64], lhsT=Qa[cur][0:64, q0:q0 + 128], rhs=kbh[:, h * 64:(h + 1) * 64], start=True, stop=False)
                    return nc.tensor.matmul(ps[6][:, 0:64], lhsT=Qa[cur][0:64, q0:q0 + 128], rhs=kbl[:, h * 64:(h + 1) * 64], start=False, stop=True)
                S.op("pe", mmg, reads=[b_Qa[cur], b_kb], writes=[bp[6]])
                pb = C("past")[:, l * 64:(l + 1) * 64]
                S.op("dve", (lambda g=g, pb=pb: nc.vector.tensor_tensor(out=gmt[g][:], in0=ps[6][:, 0:64], in1=pb, op=ALU.add)),
                     reads=[bp[6], b_cst], writes=[b_gmt[g]])
                S.op("dve", (lambda g=g: nc.vector.max(out=t8[g][:], in_=gmt[g][:])), reads=[b_gmt[g]], writes=[b_gmt[g]])
                S.op("dve", (lambda g=g: nc.vector.tensor_scalar(out=gmt[g][:], in0=gmt[g][:], scalar1=t8[g][:, 2:3], scalar2=-NEG,
                                                                 op0=ALU.is_ge, op1=ALU.mult)), reads=[b_gmt[g]], writes=[b_gmt[g]])
                S.op("dve", (lambda g=g, pb=pb: nc.vector.scalar_tensor_tensor(out=mbp[g][:, 64:128], in0=gmt[g][:], scalar=NEG, in1=pb,
                                                                             op0=ALU.add, op1=ALU.add)),
                     reads=[b_gmt[g], b_cst], writes=[b_mbp[g]])
                S.op("pe", (lambda g=g: nc.tensor.transpose(ps[7][:, 0:128], mbp[g][:], C("ident"))), reads=[b_mbp[g], b_cst], writes=[bp[7]])
                S.op("act", (lambda cur=cur, q0=q0: nc.scalar.copy(out=Qa[cur][64:128, q0:q0 + 128], in_=ps[7][64:128, 0:128])),
                     reads=[bp[7]], writes=[b_Qm[cur][l]])
        for i in range(8):
            nk = 8 * i + 7
            for X in (2 * i, 2 * i + 1):
                po = 2 + (X % 2)
                for n in range(nk):
                    sb_ = scnt % 2
                    scnt += 1

                    def mms(cur=cur, X=X, n=n, sb_=sb_):
                        for c in range(2):
                            m = nc.tensor.matmul(ps[sb_][:, c * 256:(c + 1) * 256], lhsT=KTa[cur][:, n * 256 + c * 128:n * 256 + (c + 1) * 128],
                                                 rhs=Qa[cur][:, X * 256:(X + 1) * 256], start=True, stop=True)
                        return m
                    S.op("pe", mms, reads=[b_KT[cur], b_Qa[cur], b_Qm[cur][X]], writes=[bp[sb_]])
                    S.op("act", (lambda cur=cur, X=X, n=n, sb_=sb_: nc.scalar.activation(out=pt[sb_][:], in_=ps[sb_][:], func=AF.Exp,
                                                                                        bias=bT[cur][:, X * 64 + n:X * 64 + n + 1])),
                         reads=[bp[sb_], b_hd[cur]], writes=[b_pt[sb_]])

                    def mmpv(cur=cur, n=n, sb_=sb_, po=po, nk=nk):
                        for c in range(2):
                            m = nc.tensor.matmul(ps[po][0:65, 0:256], lhsT=Va[cur][:, 2 * n + c, :], rhs=pt[sb_][:, c * 256:(c + 1) * 256],
                                                 start=(n == 0 and c == 0), stop=(n == nk - 1 and c == 1))
                        return m
                    S.op("pe", mmpv, reads=[b_Va[cur], b_pt[sb_]], writes=[bp[po]])

                def mmd(cur=cur, X=X):
                    for c in range(2):
                        m = nc.tensor.matmul(ps[4][:, c * 256:(c + 1) * 256], lhsT=Ko[cur][0:64, X * 256 + c * 128:X * 256 + (c + 1) * 128],
                                             rhs=Qa[cur][0:64, X * 256:(X + 1) * 256], start=True, stop=True)
                    return m
                S.op("pe", mmd, reads=[b_Ko[cur], b_Qa[cur]], writes=[bp[4]])
                S.op("dve", (lambda cur=cur: nc.vector.tensor_tensor(out=dtmp[:], in0=ps[4][:], in1=dgb[cur][:], op=ALU.add)),
                     reads=[bp[4], b_hd[cur]], writes=[b_dtmp])
                S.op("act", (lambda: nc.scalar.activation(out=pt2[:], in_=dtmp[:], func=AF.Exp)), reads=[b_dtmp], writes=[b_pt2])

                def mmpo(cur=cur, X=X):
                    for c in range(2):
                        m = nc.tensor.matmul(ps[5][0:65, 0:256], lhsT=Vo[cur][:, 2 * X + c, :], rhs=pt2[:, c * 256:(c + 1) * 256],
                                             start=(c == 0), stop=(c == 1))
                    return m
                S.op("pe", mmpo, reads=[b_Vo[cur], b_pt2], writes=[bp[5]])
                S.op("dve", (lambda cur=cur, po=po: nc.vector.tensor_tensor(out=o1[0:65, :], in0=ps[po][0:65, 0:256], in1=cq[cur][0:65, :], op=ALU.mult)),
                     reads=[bp[po], b_hd[cur]], writes=[b_o])
                S.op("dve", (lambda: nc.vector.tensor_tensor(out=o2[0:65, :], in0=o1[0:65, :], in1=ps[5][0:65, 0:256], op=ALU.add)),
                     reads=[bp[5], b_o], writes=[b_o])
                S.op("pe", (lambda: nc.tensor.matmul(ps[6][0:64, 0:256], lhsT=C("sel65")[0:65, :], rhs=o2[0:65, :], start=True, stop=True)),
                     reads=[b_o, b_cst], writes=[bp[6]])
                S.op("dve", (lambda: nc.vector.reciprocal(out=rd[0:64, :], in_=ps[6][0:64, 0:256])), reads=[bp[6]], writes=[b_o])
                k = X % 2
                S.op("dve", (lambda k=k: nc.vector.tensor_tensor(out=ob[k][0:64, :], in0=o2[0:64, :], in1=rd[0:64, :], op=ALU.mult)),
                     reads=[b_o], writes=[b_ob[k]])
                S.op("sp", (lambda k=k, h=h, X=X: nc.sync.dma_start(out=d["OT"][h, :, X * 256:(X + 1) * 256], in_=ob[k][0:64, :])),
                     reads=[b_ob[k]], writes=[d["b_OT"][X // 2]])


CST_OFF = {}


def _cst_layout():
    off = 0
    for name, w in (("past", 1024), ("base", 1024), ("irow", 256), ("Dm", 512), ("causal", 512), ("ident", 128), ("sel65", 64)):
        CST_OFF[name] = (off, w)
        off += w
    return off


CST_N = _cst_layout()


def make_cst(j):
    blocks = snake_blocks(j)
    c = np.zeros((128, CST_N), np.float32)
    p = np.arange(128, dtype=np.float32)[:, None]
    past = np.zeros((16, 64), np.float32)
    base = np.zeros((128, 16, 64), np.float32)
    for l, own in enumerate(blocks):
        m = np.arange(64)
        past[l] = np.where(m < own, 0.0, NEG)
        dist = np.where(m < own, own - m, 1).astype(np.float32)
        base[:, l, :] = 2.0 * p - 256.0 * dist[None, :]
    o, w = CST_OFF["past"]
    c[:, o:o + w] = np.tile(past.reshape(1, 1024), (128, 1))
    o, w = CST_OFF["base"]
    c[:, o:o + w] = base.reshape(128, 1024)
    o, w = CST_OFF["irow"]
    c[:, o:o + w] = np.arange(256, dtype=np.float32)[None, :]
    i = np.arange(256, dtype=np.float32)[None, None, :]
    pp = np.arange(128, dtype=np.float32)[:, None, None]
    cc = np.arange(2, dtype=np.float32)[None, :, None]
    Dm = np.broadcast_to(i - 2 * pp, (128, 2, 256))
    causal = np.where(i >= 2 * pp + cc, 0.0, NEG)
    o, w = CST_OFF["Dm"]
    c[:, o:o + w] = Dm.reshape(128, 512)
    o, w = CST_OFF["causal"]
    c[:, o:o + w] = causal.reshape(128, 512)
    o, w = CST_OFF["ident"]
    c[:, o:o + w] = np.eye(128, dtype=np.float32)
    o, w = CST_OFF["sel65"]
    c[64, o:o + w] = 1.0
    return c


def emit_proj_res_phase(S, cx, x_in, b_xin, x_out, b_xout, src_d, b_src, w_d, G, bias, b_mod, tag, src_is_heads, ntiles=8):
    nc = S.nc
    w = S.sb("w_" + tag, [128, KC, D], BF16)
    b_w = Buf("w_" + tag)
    for kc in range(KC):
        S.op("pool_dma", (lambda kc=kc: nc.gpsimd.dma_start(out=w[:, kc, :], in_=w_d[kc * 128:(kc + 1) * 128, :])), writes=[b_w])
    sv = src_d.rearrange("h d t -> (h d) t") if src_is_heads else src_d
    sv = sv.rearrange("(c p) t -> p c t", p=128)
    xv_in = x_in.rearrange("(c p) t -> p c t", p=128)
    xv_out = x_out.rearrange("(c p) t -> p c t", p=128)
    for t in range(ntiles):
        xt, bx = load_x_tile(S, cx, xv_in, b_xin, t)
        S.op("sp", (lambda t=t: nc.sync.dma_start(out=cx.h[:], in_=sv[:, :, t * 512:(t + 1) * 512])), reads=[b_src[t]], writes=[cx.b_h])
        for oc in range(KC):
            py = 5 + (oc % 2)

            def mm(oc=oc, py=py):
                for kc in range(KC):
                    m = nc.tensor.matmul(cx.psum[py][:], lhsT=w[:, kc, oc * 128:(oc + 1) * 128], rhs=cx.h[:, kc, :], start=(kc == 0), stop=(kc == KC - 1))
                return m
            S.op("pe", mm, reads=[b_w, cx.b_h], writes=[cx.b_psum[py]])
            if bias is None:
                S.op("dve", (lambda oc=oc, py=py, xt=xt: nc.vector.scalar_tensor_tensor(
                    out=xt[:, oc, :], in0=cx.psum[py][:], scalar=G[:, oc:oc + 1], in1=xt[:, oc, :], op0=ALU.mult, op1=ALU.add)),
                    reads=[cx.b_psum[py], b_mod], writes=[bx])
            else:
                tmp = cx.tmp[oc % 2]
                S.op("act", (lambda oc=oc, py=py, tmp=tmp: nc.scalar.activation(out=tmp[:], in_=cx.psum[py][:], func=AF.Identity, bias=bias[:, oc:oc + 1])),
                     reads=[cx.b_psum[py], cx.b_sp], writes=[cx.b_tmp[oc % 2]])
                S.op("dve", (lambda oc=oc, tmp=tmp, xt=xt: nc.vector.scalar_tensor_tensor(
                    out=xt[:, oc, :], in0=tmp[:], scalar=G[:, oc:oc + 1], in1=xt[:, oc, :], op0=ALU.mult, op1=ALU.add)),
                    reads=[cx.b_tmp[oc % 2], b_mod], writes=[bx])
        S.op("sp", (lambda xt=xt, t=t: nc.sync.dma_start(out=xv_out[:, :, t * 512:(t + 1) * 512], in_=xt[:])), reads=[bx], writes=[b_xout[t]])


def emit_glu_phase(S, cx, x_in, b_xin, w_d, A, Bv, b_mod, u_out, ntiles=8, hmask=None, b_hmask=None, b_uout=None):
    nc = S.nc
    w = S.sb("w_pw1", [128, KC, 2 * D], BF16)
    b_w = Buf("w_pw1")
    for kc in range(KC):
        S.op("pool_dma", (lambda kc=kc: nc.gpsimd.dma_start(out=w[:, kc, :], in_=w_d[kc * 128:(kc + 1) * 128, :])), writes=[b_w])
    uo = [S.sb("uo%d" % i, [128, 512], BF16) for i in range(2)]
    b_uo = [Buf("uo%d" % i) for i in range(2)]
    xv_in = x_in.rearrange("(c p) t -> p c t", p=128)
    uv = u_out.rearrange("(c p) t -> p c t", p=128)
    bp1 = spc(cx, "b_pw1")
    for t in range(ntiles):
        xt, bx = load_x_tile(S, cx, xv_in, b_xin, t)
        emit_norm_mod(S, cx, xt, bx, A, Bv, b_mod)
        for oc in range(KC):
            k = oc % 2
            pa = 1 + k * 2
            pg = pa + 1

            def mma(oc=oc, pa=pa):
                for kc in range(KC):
                    m = nc.tensor.matmul(cx.psum[pa][:], lhsT=w[:, kc, oc * 128:(oc + 1) * 128], rhs=cx.h[:, kc, :], start=(kc == 0), stop=(kc == KC - 1))
                return m

            def mmg(oc=oc, pg=pg):
                for kc in range(KC):
                    m = nc.tensor.matmul(cx.psum[pg][:], lhsT=w[:, kc, D + oc * 128:D + (oc + 1) * 128], rhs=cx.h[:, kc, :], start=(kc == 0), stop=(kc == KC - 1))
                return m
            S.op("pe", mma, reads=[b_w, cx.b_h], writes=[cx.b_psum[pa]])
            S.op("pe", mmg, reads=[b_w, cx.b_h], writes=[cx.b_psum[pg]])
            sa = cx.sa[k]
            S.op("act", (lambda oc=oc, pg=pg, sa=sa: nc.scalar.activation(out=sa[:], in_=cx.psum[pg][:], func=AF.Sigmoid, bias=bp1[:, 8 + oc:9 + oc])),
                 reads=[cx.b_psum[pg], cx.b_sp], writes=[cx.b_sa[k]])
            S.op("dve", (lambda oc=oc, pa=pa, sa=sa, k=k: nc.vector.scalar_tensor_tensor(
                out=uo[k][:], in0=cx.psum[pa][:], scalar=bp1[:, oc:oc + 1], in1=sa[:], op0=ALU.add, op1=ALU.mult)),
                reads=[cx.b_psum[pa], cx.b_sa[k], cx.b_sp], writes=[b_uo[k]])
            if hmask is not None and t == 8:
                S.op("pool", (lambda k=k: nc.gpsimd.tensor_tensor(out=uo[k][:], in0=uo[k][:], in1=hmask, op=ALU.mult)),
                     reads=[b_uo[k], b_hmask], writes=[b_uo[k]])
            S.op("sp", (lambda oc=oc, k=k, t=t: nc.sync.dma_start(out=uv[:, oc, t * 512:(t + 1) * 512], in_=uo[k][:])), reads=[b_uo[k]],
                 writes=([b_uout[t]] if b_uout is not None else []))


def build_L2():
    nc = bass.Bass("TRN2", target_bir_lowering=False)
    din = lambda n, s, dt: nc.dram_tensor(n, s, dt, kind="ExternalInput").ap()
    sp_d = din("sp", [128, SP_N], F32)
    mod_d = din("modi", [128, 144], F32)
    x1 = din("x1", [1024, TOK], F32)
    d = {"KT": din("KTf", [NH, DH, SEQ], BF16), "ind": din("ind", [64, SEQ], BF16), "Vd": din("Vd", [NH, 128, 128, DH], BF16),
         "QT": din("QT", [NH, DH, TOK], BF16), "KTo": din("KTo", [NH, DH, TOK], BF16), "Vo": din("Vo", [NH, 128, 32, DH], BF16),
         "kbar": din("kbarf", [DH, NH, NB], F32), "cst": din("cst", [128, CST_N], F32)}
    w_o = din("w_o", [1024, 1024], F32)
    w_in1 = din("w_in1", [1024, 2 * DFF], F32)
    w_out1 = din("w_out1", [DFF, 1024], F32)
    w_in2 = din("w_in2", [1024, 2 * DFF], F32)
    w_out2 = din("w_out2", [DFF, 1024], F32)
    w_pw1 = din("w_pw1", [1024, 2048], F32)
    OT = nc.dram_tensor("OT", [NH, DH, TOK], BF16).ap()
    xa = nc.dram_tensor("xa", [1024, TOK], F32).ap()
    xb = nc.dram_tensor("xb", [1024, TOK], F32).ap()
    x4 = nc.dram_tensor("x4", [1024, TOK], F32, kind="ExternalOutput").ap()
    uT = nc.dram_tensor("uT", [1024, TOK], BF16, kind="ExternalOutput").ap()
    d["OT"] = OT
    d["b_OT"] = [Buf("OT%d" % t) for t in range(8)]
    S = Sched(nc)
    cx = Ctx()
    setup_common(S, cx)
    emit_load_small(S, cx, sp_d)
    modT = [S.sb("modT%d" % i, [128, 72], F32) for i in range(2)]
    b_modT = [Buf("modT%d" % i) for i in range(2)]
    for i in range(2):
        S.op("sp", (lambda i=i: nc.sync.dma_start(out=modT[i][:], in_=mod_d[:, i * 72:(i + 1) * 72])), writes=[b_modT[i]])
    m01 = emit_mod_vectors_i(S, cx, modT[0], b_modT[0], spc(cx, "ng01"), "l0s1", 1)
    m02 = emit_mod_vectors_i(S, cx, modT[0], b_modT[0], spc(cx, "ng02"), "l0s2", 2)
    m10 = emit_mod_vectors_i(S, cx, modT[1], b_modT[1], spc(cx, "ng10"), "l1s0", 0)
    m11 = emit_mod_vectors_i(S, cx, modT[1], b_modT[1], spc(cx, "ng11"), "l1s1", 1)
    n0 = len(S._stack)
    emit_attn_phase(S, cx, d)
    S.barrier_free(len(S._stack) - n0)
    alloc_tile_bufs(S, cx)
    b_x1 = [Buf("x1_%d" % t) for t in range(8)]
    b_xa = [Buf("xa_%d" % t) for t in range(8)]
    b_xb = [Buf("xb_%d" % t) for t in range(8)]
    b_x4 = [Buf("x4_%d" % t) for t in range(8)]
    n1 = len(S._stack)
    emit_proj_res_phase(S, cx, x1, b_x1, xa, b_xa, OT, d["b_OT"], w_o, m01[2], None, m01[3], "wo", True)
    S.barrier_free(len(S._stack) - n1)
    alloc_ffn_bufs(S, cx)
    emit_ffn_phase(S, cx, xa, b_xa, xb, b_xb, w_in1, w_out1, m02[0], m02[1], m02[2], 0.5, m02[3], 8, "f01")
    emit_ffn_phase(S, cx, xb, b_xb, x4, b_x4, w_in2, w_out2, m10[0], m10[1], m10[2], 0.5, m10[3], 8, "f10")
    S.barrier_free(len(S._stack) - n1)
    emit_glu_phase(S, cx, x4, b_x4, w_pw1, m11[0], m11[1], m11[3], uT)
    S.emit()
    return nc


def emit_conv_phase(S, cx, uext_d, x_in, b_xin, x_out, b_xout, w2_d, ident_d, G, b_mod, u_scr=None, b_u=None):
    nc = S.nc
    w2 = S.sb("w_pw2", [128, KC, D], BF16)
    b_w2 = Buf("w_pw2")
    for kc in range(KC):
        S.op("pool_dma", (lambda kc=kc: nc.gpsimd.dma_start(out=w2[:, kc, :], in_=w2_d[kc * 128:(kc + 1) * 128, :])), writes=[b_w2])
    ident = S.sb("identc", [128, 128], F32)
    b_id = Buf("identc")
    S.op("sp", lambda: nc.sync.dma_start(out=ident[:], in_=ident_d[:, :]), writes=[b_id])
    dg = S.sb("dg", [128, KC * CW, 128], BF16)
    b_dg = Buf("dg")
    wdw = spc(cx, "w_dw")
    for ck in range(KC * CW):
        S.op("dve", (lambda ck=ck: nc.vector.tensor_scalar(out=dg[:, ck, :], in0=ident[:], scalar1=wdw[:, ck:ck + 1], scalar2=None, op0=ALU.mult)),
             reads=[b_id, cx.b_sp], writes=[b_dg])
    ue = [S.sb("ue%d" % i, [128, KC, 288], BF16) for i in range(2)]
    b_ue = [Buf("ue%d" % i) for i in range(2)]
    vs = S.sb("vs", [128, KC, 256], F32)
    b_vs = [Buf("vs%d" % c) for c in range(KC)]
    vb = [S.sb("vb%d" % i, [128, 256], BF16) for i in range(2)]
    b_vb = [Buf("vb%d" % i) for i in range(2)]
    vq = [S.sb("vq%d" % i, [128, 256], BF16) for i in range(2)]
    b_vq = [Buf("vq%d" % i) for i in range(2)]
    mu = S.sb("mu", [128, 256], F32)
    rs = S.sb("rs", [128, 256], F32)
    b_st = Buf("lnstat")
    uv = uext_d.rearrange("(c p) l e -> p c l e", p=128) if uext_d is not None else None
    usv = u_scr.rearrange("(c p) t -> p c t", p=128) if u_scr is not None else None
    xv_in = x_in.rearrange("(c p) t -> p c t", p=128)
    xv_out = x_out.rearrange("(c p) t -> p c t", p=128)
    for l in range(NLB):
        u_ = ue[l % 2]
        if usv is None:
            S.op("sp", (lambda u_=u_, l=l: nc.sync.dma_start(out=u_[:], in_=uv[:, :, l, :])), writes=[b_ue[l % 2]])
        else:
            S.op("sp", (lambda u_=u_, l=l: nc.sync.dma_start(out=u_[:, :, 32:288], in_=usv[:, :, l * 256:(l + 1) * 256])),
                 reads=[b_u[l // 2]], writes=[b_ue[l % 2]])
            S.op("sp", (lambda u_=u_, l=l: nc.sync.dma_start(out=u_[:, :, 0:32], in_=usv[:, :, TOK + l * 32:TOK + (l + 1) * 32])),
                 reads=[b_u[8]], writes=[b_ue[l % 2]])
        xt, bx = load_x_tile(S, cx, xv_in, b_xin, l, n=256)
        for c in range(KC):
            k2 = c % 2
            pc = cx.psum[1 + k2]

            def mmc(c=c, pc=pc, u_=u_):
                for k in range(CW):
                    m = nc.tensor.matmul(pc[:, 0:256], lhsT=dg[:, c * CW + k, :], rhs=u_[:, c, 2 + k:2 + k + 256], start=(k == 0), stop=(k == CW - 1))
                return m
            S.op("pe", mmc, reads=[b_dg, b_ue[l % 2]], writes=[cx.b_psum[1 + k2]])
            S.op("act", (lambda c=c, pc=pc: nc.scalar.activation(out=vs[:, c, :], in_=pc[:, 0:256], func=AF.Identity, bias=spc(cx, "b_dw")[:, c:c + 1])),
                 reads=[cx.b_psum[1 + k2], cx.b_sp], writes=[b_vs[c]])
            S.op("pool", (lambda c=c, k2=k2: nc.gpsimd.tensor_copy(out=vb[k2][:], in_=vs[:, c, :])), reads=[b_vs[c]], writes=[b_vb[k2]])
            S.op("act", (lambda c=c, k2=k2: nc.scalar.activation(out=vq[k2][:], in_=vs[:, c, :], func=AF.Square)), reads=[b_vs[c]], writes=[b_vq[k2]])
            S.op("pe", (lambda c=c, k2=k2: nc.tensor.matmul(cx.psum[3][:, 0:256], lhsT=cx.ones_bf[:], rhs=vb[k2][:], start=(c == 0), stop=(c == KC - 1))),
                 reads=[b_vb[k2], cx.b_ones], writes=[cx.b_psum[3]])
            S.op("pe", (lambda c=c, k2=k2: nc.tensor.matmul(cx.psum[4][:, 0:256], lhsT=cx.ones_bf[:], rhs=vq[k2][:], start=(c == 0), stop=(c == KC - 1))),
                 reads=[b_vq[k2], cx.b_ones], writes=[cx.b_psum[4]])
        S.op("dve", lambda: nc.vector.tensor_scalar(out=mu[:], in0=cx.psum[3][:, 0:256], scalar1=1.0 / D, scalar2=None, op0=ALU.mult),
             reads=[cx.b_psum[3]], writes=[b_st])
        S.op("dve", lambda: nc.vector.tensor_tensor(out=rs[:], in0=mu[:], in1=mu[:], op=ALU.mult), reads=[b_st], writes=[b_st])
        S.op("dve", lambda: nc.vector.scalar_tensor_tensor(out=rs[:], in0=cx.psum[4][:, 0:256], scalar=1.0 / D, in1=rs[:], op0=ALU.mult, op1=ALU.subtract),
             reads=[cx.b_psum[4], b_st], writes=[b_st])
        S.op("dve", lambda: nc.vector.tensor_scalar(out=rs[:], in0=rs[:], scalar1=EPS, scalar2=None, op0=ALU.add), reads=[b_st], writes=[b_st])
        S.op("act", lambda: nc.scalar.activation(out=rs[:], in_=rs[:], func=AF.Sqrt), reads=[b_st], writes=[b_st])
        S.op("dve", lambda: nc.vector.reciprocal(out=rs[:], in_=rs[:]), reads=[b_st], writes=[b_st])
        for c in range(KC):
            tmp = cx.tmp[c % 2]
            S.op("dve", (lambda c=c, tmp=tmp: nc.vector.tensor_tensor(out=tmp[:, 0:256], in0=vs[:, c, :], in1=mu[:], op=ALU.subtract)),
                 reads=[b_vs[c], b_st], writes=[cx.b_tmp[c % 2]])
            S.op("dve", (lambda c=c, tmp=tmp: nc.vector.tensor_tensor(out=tmp[:, 0:256], in0=tmp[:, 0:256], in1=rs[:], op=ALU.mult)),
                 reads=[b_st, cx.b_tmp[c % 2]], writes=[cx.b_tmp[c % 2]])
            S.op("act", (lambda c=c, tmp=tmp: nc.scalar.activation(out=cx.h[:, c, 0:256], in_=tmp[:, 0:256], func=AF.Silu,
                                                                   bias=spc(cx, "ln_b")[:, c:c + 1], scale=spc(cx, "ln_g")[:, c:c + 1])),
                 reads=[cx.b_tmp[c % 2], cx.b_sp], writes=[cx.b_h])
        for oc in range(KC):
            py = 5 + (oc % 2)

            def mm(oc=oc, py=py):
                for kc in range(KC):
                    m = nc.tensor.matmul(cx.psum[py][:, 0:256], lhsT=w2[:, kc, oc * 128:(oc + 1) * 128], rhs=cx.h[:, kc, 0:256], start=(kc == 0), stop=(kc == KC - 1))
                return m
            S.op("pe", mm, reads=[b_w2, cx.b_h], writes=[cx.b_psum[py]])
            sa = cx.sa[oc % 2]
            S.op("act", (lambda oc=oc, py=py, sa=sa: nc.scalar.activation(out=sa[:, 0:256], in_=cx.psum[py][:, 0:256], func=AF.Identity,
                                                                        bias=spc(cx, "b_pw2")[:, oc:oc + 1])),
                 reads=[cx.b_psum[py], cx.b_sp], writes=[cx.b_sa[oc % 2]])
            S.op("dve", (lambda oc=oc, sa=sa, xt=xt: nc.vector.scalar_tensor_tensor(
                out=xt[:, oc, 0:256], in0=sa[:, 0:256], scalar=G[:, oc:oc + 1], in1=xt[:, oc, 0:256], op0=ALU.mult, op1=ALU.add)),
                reads=[cx.b_sa[oc % 2], b_mod], writes=[bx])
        S.op("sp", (lambda xt=xt, l=l: nc.sync.dma_start(out=xv_out[:, :, l * 256:(l + 1) * 256], in_=xt[:, :, 0:256])),
             reads=[bx], writes=[b_xout[l // 2]])


def build_L3():
    nc = bass.Bass("TRN2", target_bir_lowering=False)
    din = lambda n, s, dt: nc.dram_tensor(n, s, dt, kind="ExternalInput").ap()
    sp_d = din("sp", [128, SP_N], F32)
    mod_d = din("modi", [128, 144], F32)
    x4 = din("x4", [1024, TOK], F32)
    uext = din("uext", [1024, NLB, 288], BF16)
    ident_d = din("ident", [128, 128], F32)
    w_pw2 = din("w_pw2", [1024, 1024], F32)
    w_in3 = din("w_in3", [1024, 2 * DFF], F32)
    w_out3 = din("w_out3", [DFF, 1024], F32)
    x5 = nc.dram_tensor("x5", [1024, TOK], F32).ap()
    xo = nc.dram_tensor("xo", [1024, TOK], F32, kind="ExternalOutput").ap()
    S = Sched(nc)
    cx = Ctx()
    setup_common(S, cx)
    emit_load_small(S, cx, sp_d)
    modT = [S.sb("modT%d" % i, [128, 72], F32) for i in range(2)]
    b_modT = [Buf("modT%d" % i) for i in range(2)]
    for i in range(2):
        S.op("sp", (lambda i=i: nc.sync.dma_start(out=modT[i][:], in_=mod_d[:, i * 72:(i + 1) * 72])), writes=[b_modT[i]])
    m11 = emit_mod_vectors_i(S, cx, modT[1], b_modT[1], spc(cx, "ng11"), "l1s1", 1)
    m12 = emit_mod_vectors_i(S, cx, modT[1], b_modT[1], spc(cx, "ng12"), "l1s2", 2)
    alloc_tile_bufs(S, cx)
    b_x4 = [Buf("x4_%d" % t) for t in range(16)]
    b_x5 = [Buf("x5_%d" % t) for t in range(8)]
    b_xo = [Buf("xo_%d" % t) for t in range(8)]
    n1 = len(S._stack)
    emit_conv_phase(S, cx, uext, x4, b_x4, x5, b_x5, w_pw2, ident_d, m11[2], m11[3])
    S.barrier_free(len(S._stack) - n1)
    alloc_ffn_bufs(S, cx)
    emit_ffn_phase(S, cx, x5, b_x5, xo, b_xo, w_in3, w_out3, m12[0], m12[1], m12[2], 0.5, m12[3], 8, "f11")
    S.emit()
    return nc


_NC = {}


def _get(name, fn):
    if name not in _NC:
        _NC[name] = fn()
    return _NC[name]


def _deint(a):
    sh = a.shape
    return np.ascontiguousarray(a.reshape(sh[:-1] + (sh[-1] // 256, 128, 2)).swapaxes(-1, -2)).reshape(sh)


def kernel_unfused(**inp):
    import ml_dtypes
    bf = ml_dtypes.bfloat16
    inp = {k: np.asarray(v) for k, v in inp.items()}
    cores = list(range(8))
    toks = []
    for core in cores:
        blocks = snake_blocks(core % 4)
        toks.append(np.concatenate([np.arange(g * 256, (g + 1) * 256) for g in blocks]))
    sps = [pack_small(inp, c // 4) for c in cores]
    nc1 = _get("L1", build_L1)
    maps = [{"sp": sps[c], "ada_w": inp["ada_w"], "xin": np.ascontiguousarray(inp["x"][c // 4][toks[c]].T),
             "w_in": inp["ffn_w_in"][0, 0], "w_out": inp["ffn_w_out"][0, 0], "wqkv": inp["attn_w_qkv"][0]} for c in cores]
    r1 = run_bass_kernel_spmd(nc1, maps, core_ids=cores).results
    ind = np.zeros((64, SEQ), bf)
    for m in range(64):
        ind[m, m * 256:(m + 1) * 256] = 1
    ident = np.eye(128, dtype=np.float32)
    maps2 = []
    per_batch = {}
    for b in range(2):
        KTf = np.zeros((NH, DH, SEQ), bf)
        Vf = np.zeros((SEQ, D), bf)
        kbf = np.zeros((DH, NH, NB), np.float32)
        for j in range(4):
            c = 4 * b + j
            blocks = snake_blocks(j)
            KT = np.asarray(r1[c]["KT"])
            V = np.asarray(r1[c]["V"])
            kb = np.asarray(r1[c]["kbar"])
            for l, g in enumerate(blocks):
                KTf[:, :, g * 256:(g + 1) * 256] = KT[:, :, l * 256:(l + 1) * 256]
                Vf[g * 256:(g + 1) * 256] = V[l * 256:(l + 1) * 256]
                kbf[:, :, g] = kb[:, :, l].T
        KTf = _deint(KTf)
        Vd = np.ascontiguousarray(Vf.reshape(NB, 128, 2, NH, DH).transpose(3, 1, 0, 2, 4)).reshape(NH, 128, 128, DH)
        per_batch[b] = (KTf, Vd, kbf)
    for c in cores:
        b = c // 4
        KTf, Vd, kbf = per_batch[b]
        KTo = _deint(np.asarray(r1[c]["KT"]))
        V = np.asarray(r1[c]["V"])
        Vo = np.ascontiguousarray(V.reshape(NLB, 128, 2, NH, DH).transpose(3, 1, 0, 2, 4)).reshape(NH, 128, 32, DH)
        maps2.append({"sp": sps[c], "modi": r1[c]["modo"], "x1": r1[c]["x1"], "KTf": KTf, "ind": ind, "Vd": Vd,
                      "QT": r1[c]["QT"], "KTo": KTo, "Vo": Vo, "kbarf": kbf, "cst": make_cst(c % 4),
                      "w_o": inp["attn_w_o"][0], "w_in1": inp["ffn_w_in"][0, 1], "w_out1": inp["ffn_w_out"][0, 1],
                      "w_in2": inp["ffn_w_in"][1, 0], "w_out2": inp["ffn_w_out"][1, 0], "w_pw1": inp["conv_w_pw1"][0]})
    nc2 = _get("L2", build_L2)
    r2 = run_bass_kernel_spmd(nc2, maps2, core_ids=cores).results
    maps3 = []
    for b in range(2):
        Uf = np.zeros((D, SEQ), bf)
        for j in range(4):
            c = 4 * b + j
            Uf[:, toks[c]] = np.asarray(r2[c]["uT"])
        for j in range(4):
            c = 4 * b + j
            ue = np.zeros((D, NLB, 288), bf)
            for l, g in enumerate(snake_blocks(j)):
                ue[:, l, 32:] = Uf[:, g * 256:(g + 1) * 256]
                if g > 0:
                    ue[:, l, :32] = Uf[:, g * 256 - 32:g * 256]
            maps3.append({"sp": sps[c], "modi": r1[c]["modo"], "x4": r2[c]["x4"], "uext": ue, "ident": ident,
                          "w_pw2": inp["conv_w_pw2"][0], "w_in3": inp["ffn_w_in"][1, 1], "w_out3": inp["ffn_w_out"][1, 1]})
    nc3 = _get("L3", build_L3)
    r3 = run_bass_kernel_spmd(nc3, maps3, core_ids=cores).results
    out = np.zeros((BATCH, SEQ, D), np.float32)
    for c in cores:
        out[c // 4][toks[c]] = np.asarray(r3[c]["xo"]).T
    return out


NPOS = 80
NT_ALL = 40
TOKX = TOK + 512


def pos_order(j):
    real = []
    own = snake_blocks(j)
    for i in range(8):
        A, B = 8 * i + j, 8 * i + 7 - j
        real += [A, B] + [g for g in range(8 * i, 8 * i + 8) if g not in (A, B)]
    dup = [(g - 1 if g > 0 else None) for g in own]
    return real, dup


def emit_kvq_phase(S, cx, x_d, b_x, wqkv_d, A, Bv, b_mod, QT_d, KT_d, kbar_d, V_d, x1o_d, b_x1o, b_kv):
    nc = S.nc
    wq = S.sb("wqkv_sb", [128, KC, 3072], BF16)
    b_wq = [Buf("wqkv%d" % k) for k in range(KC)]
    for kc in range(KC):
        S.op("pool_dma", (lambda kc=kc: nc.gpsimd.dma_start(out=wq[:, kc, :], in_=wqkv_d[kc * 128:(kc + 1) * 128, :])), writes=[b_wq[kc]])
    bo = S.sb("blkones", [128, 128], BF16)
    b_bo = Buf("blkones")
    S.op("dve", lambda: nc.vector.memset(bo[:], 0.0), writes=[b_bo])
    S.op("dve", lambda: nc.vector.memset(bo[0:64, 0:64], 1.0), writes=[b_bo])
    S.op("dve", lambda: nc.vector.memset(bo[64:128, 64:128], 1.0), writes=[b_bo])
    g8 = S.sb("g8", [128, 2], F32)
    b_g8 = Buf("g8")
    S.op("dve", lambda: nc.vector.tensor_scalar(out=g8[:, 0:1], in0=spc(cx, "gq"), scalar1=0.125, scalar2=None, op0=ALU.mult),
         reads=[cx.b_sp], writes=[b_g8])
    S.op("dve", lambda: nc.vector.tensor_copy(out=g8[:, 1:2], in_=spc(cx, "gk")), reads=[cx.b_sp], writes=[b_g8])
    qo = [S.sb("qo%d" % i, [128, 512], BF16) for i in range(2)]
    b_qo = [Buf("qo%d" % i) for i in range(2)]
    kb = [S.sb("kb%d" % i, [128, 2], F32) for i in range(2)]
    b_kb = [Buf("kb%d" % i) for i in range(2)]
    vt = [S.sb("vt%d" % i, [128, 1024], BF16) for i in range(2)]
    b_vt = [Buf("vt%d" % i) for i in range(2)]
    xv = x_d.rearrange("(c p) t -> p c t", p=128)
    xov = x1o_d.rearrange("(c p) t -> p c t", p=128)
    QTv = QT_d.rearrange("h d t -> (h d) t")
    KTv = KT_d.rearrange("h d t -> (h d) t")
    cnt = 0
    for t in range(NT_ALL):
        is_own = (t < 32 and t % 4 == 0)
        is_dup = t >= 32
        xt, bx = load_x_tile(S, cx, xv, b_x, t)
        if is_own:
            S.op("sp", (lambda xt=xt, t=t: nc.sync.dma_start(out=xov[:, :, (t // 4) * 512:(t // 4 + 1) * 512], in_=xt[:])),
                 reads=[bx], writes=[b_x1o[t // 4]])
        if is_dup:
            i2 = t - 32
            for k2 in range(2):
                S.op("sp", (lambda xt=xt, i2=i2, k2=k2: nc.sync.dma_start(
                    out=xov[:, :, TOK + (2 * i2 + k2) * 32:TOK + (2 * i2 + k2 + 1) * 32], in_=xt[:, :, k2 * 256 + 224:k2 * 256 + 256])),
                    reads=[bx], writes=[b_x1o[8]])
        emit_norm_mod(S, cx, xt, bx, A, Bv, b_mod)
        for which in range(2):
            if which == 0 and not (is_own or is_dup):
                continue
            goff = which * 1024
            for hp in range(8):
                k = cnt % 2
                cnt += 1
                pq = cx.psum[1 + k]
                pss = cx.psum[3 + k]

                def mm(hp=hp, pq=pq, goff=goff):
                    for kc in range(KC):
                        m = nc.tensor.matmul(pq[:], lhsT=wq[:, kc, goff + hp * 128:goff + (hp + 1) * 128], rhs=cx.h[:, kc, :],
                                             start=(kc == 0), stop=(kc == KC - 1))
                    return m
                S.op("pe", mm, reads=b_wq + [cx.b_h], writes=[cx.b_psum[1 + k]])
                S.op("act", (lambda k=k, pq=pq: nc.scalar.activation(out=cx.sq[k][:], in_=pq[:], func=AF.Square)),
                     reads=[cx.b_psum[1 + k]], writes=[cx.b_sq[k]])
                S.op("pe", (lambda k=k, pss=pss: nc.tensor.matmul(pss[:], lhsT=bo[:], rhs=cx.sq[k][:], start=True, stop=True)),
                     reads=[cx.b_sq[k], b_bo], writes=[cx.b_psum[3 + k]])
                r1 = cx.tmp[k]
                S.op("dve", (lambda r1=r1, pss=pss: nc.vector.tensor_scalar(out=r1[:], in0=pss[:], scalar1=1.0 / DH, scalar2=EPS,
                                                                            op0=ALU.mult, op1=ALU.add)),
                     reads=[cx.b_psum[3 + k]], writes=[cx.b_tmp[k]])
                S.op("act", (lambda r1=r1: nc.scalar.activation(out=r1[:], in_=r1[:], func=AF.Sqrt)), reads=[cx.b_tmp[k]], writes=[cx.b_tmp[k]])
                S.op("dve", (lambda r1=r1: nc.vector.reciprocal(out=r1[:], in_=r1[:])), reads=[cx.b_tmp[k]], writes=[cx.b_tmp[k]])
                S.op("dve", (lambda r1=r1, pq=pq: nc.vector.tensor_tensor(out=r1[:], in0=pq[:], in1=r1[:], op=ALU.mult)),
                     reads=[cx.b_tmp[k], cx.b_psum[1 + k]], writes=[cx.b_tmp[k]])
                sa = cx.sa[k]
                S.op("act", (lambda r1=r1, sa=sa, which=which: nc.scalar.activation(out=sa[:], in_=r1[:], func=AF.Copy,
                                                                                  scale=g8[:, which:which + 1])),
                     reads=[cx.b_tmp[k], b_g8], writes=[cx.b_sa[k]])
                if which == 0:
                    S.op("pool", (lambda k=k, sa=sa: nc.gpsimd.tensor_copy(out=qo[k][:], in_=sa[:])), reads=[cx.b_sa[k]], writes=[b_qo[k]])
                    if is_own:
                        S.op("sp", (lambda k=k, hp=hp, t=t: nc.sync.dma_start(
                            out=QTv[hp * 128:(hp + 1) * 128, (t // 4) * 512:(t // 4 + 1) * 512], in_=qo[k][:])), reads=[b_qo[k]], writes=[b_kv])
                    else:
                        i2 = t - 32
                        for k2 in range(2):
                            S.op("sp", (lambda k=k, hp=hp, i2=i2, k2=k2: nc.sync.dma_start(
                                out=QTv[hp * 128:(hp + 1) * 128, TOK + (2 * i2 + k2) * 32:TOK + (2 * i2 + k2 + 1) * 32],
                                in_=qo[k][:, k2 * 256 + 224:k2 * 256 + 256])), reads=[b_qo[k]], writes=[b_kv])
                else:
                    S.op("pool", (lambda k=k, sa=sa: nc.gpsimd.tensor_copy(
                        out=qo[k][:].rearrange("q (b c p) -> q b c p", b=2, c=2),
                        in_=sa[:].rearrange("q (b p c) -> q b c p", b=2, c=2))), reads=[cx.b_sa[k]], writes=[b_qo[k]])
                    S.op("sp", (lambda k=k, hp=hp, t=t: nc.sync.dma_start(out=KTv[hp * 128:(hp + 1) * 128, t * 512:(t + 1) * 512], in_=qo[k][:])),
                         reads=[b_qo[k]], writes=[b_kv])
                    S.op("dve", (lambda k=k, sa=sa: nc.vector.tensor_reduce(out=kb[k][:], in_=sa[:].rearrange("p (b t) -> p b t", b=2),
                                                                          axis=AX.X, op=ALU.add)),
                         reads=[cx.b_sa[k]], writes=[b_kb[k]])
                    S.op("dve", (lambda k=k: nc.vector.tensor_scalar(out=kb[k][:], in0=kb[k][:], scalar1=1.0 / BLK, scalar2=None, op0=ALU.mult)),
                         reads=[b_kb[k]], writes=[b_kb[k]])
                    S.op("sp", (lambda k=k, hp=hp, t=t: nc.sync.dma_start(out=kbar_d[hp * 128:(hp + 1) * 128, 2 * t:2 * t + 2], in_=kb[k][:])),
                         reads=[b_kb[k]], writes=[b_kv])
        for tg in range(4):
            v = vt[tg % 2]
            for ns in range(2):
                pv = cx.psum[5 + ns]

                def mmv(tg=tg, ns=ns, pv=pv):
                    for kc in range(KC):
                        m = nc.tensor.matmul(pv[:], lhsT=cx.h[:, kc, tg * 128:(tg + 1) * 128], rhs=wq[:, kc, 2048 + ns * 512:2048 + (ns + 1) * 512],
                                             start=(kc == 0), stop=(kc == KC - 1))
                    return m
                S.op("pe", mmv, reads=b_wq + [cx.b_h], writes=[cx.b_psum[5 + ns]])
                if ns == 0:
                    S.op("act", (lambda v=v, pv=pv: nc.scalar.copy(out=v[:, 0:512], in_=pv[:])), reads=[cx.b_psum[5]], writes=[b_vt[tg % 2]])
                else:
                    S.op("dve", (lambda v=v, pv=pv: nc.vector.tensor_copy(out=v[:, 512:1024], in_=pv[:])), reads=[cx.b_psum[6]], writes=[b_vt[tg % 2]])
            blk = 2 * t + tg // 2
            hf = tg % 2
            for c2 in range(2):
                dst = V_d[:, hf * 64:(hf + 1) * 64, 2 * blk + c2, :].rearrange("h pp d -> pp h d")
                S.op("sp", (lambda v=v, dst=dst, c2=c2: nc.sync.dma_start(out=dst, in_=v[c2:128:2, :].rearrange("p (h d) -> p h d", h=NH))),
                     reads=[b_vt[tg % 2]], writes=[b_kv])


CST2_OFF = {}


def _cst2_layout():
    off = 0
    for name, w in (("past", 2048), ("base", 2048), ("irow", 256), ("irowH", 32), ("Dm", 512), ("causal", 512),
                    ("DmH", 64), ("causalH", 64), ("ident", 128), ("sel65", 64)):
        CST2_OFF[name] = (off, w)
        off += w
    return off


CST2_N = _cst2_layout()


def make_cst2(j):
    real, dup = pos_order(j)
    own = snake_blocks(j)
    c = np.zeros((128, CST2_N), np.float32)
    p = np.arange(128, dtype=np.float32)[:, None]
    gpos = np.array(real)
    past = np.zeros((32, 64), np.float32)
    base = np.zeros((128, 32, 64), np.float32)
    for row in range(32):
        o = own[row] if row < 16 else own[row - 16] - 1
        valid = gpos < o
        past[row] = np.where(valid, 0.0, NEG)
        dist = np.where(valid, o - gpos, 1).astype(np.float32)
        base[:, row, :] = 2.0 * p - 256.0 * dist[None, :]

    def put(name, arr):
        o_, w = CST2_OFF[name]
        c[:, o_:o_ + w] = arr.reshape(arr.shape[0], -1)
    put("past", np.tile(past.reshape(1, 2048), (128, 1)))
    put("base", base.reshape(128, 2048))
    put("irow", np.tile(np.arange(256, dtype=np.float32)[None, :], (128, 1)))
    put("irowH", np.tile((224 + np.arange(32, dtype=np.float32))[None, :], (128, 1)))
    pp = np.arange(128, dtype=np.float32)[:, None, None]
    cc = np.arange(2, dtype=np.float32)[None, :, None]
    i = np.arange(256, dtype=np.float32)[None, None, :]
    put("Dm", np.broadcast_to(i - 2 * pp, (128, 2, 256)).copy())
    put("causal", np.where(i >= 2 * pp + cc, 0.0, NEG).astype(np.float32))
    ih = 224 + np.arange(32, dtype=np.float32)[None, None, :]
    put("DmH", np.broadcast_to(ih - 2 * pp, (128, 2, 32)).copy())
    put("causalH", np.where(ih >= 2 * pp + cc, 0.0, NEG).astype(np.float32))
    put("ident", np.eye(128, dtype=np.float32))
    s65 = np.zeros((128, 64), np.float32)
    s65[64] = 1.0
    put("sel65", s65)
    hmask = np.ones((128, 512), np.float32)
    for l in range(16):
        if dup[l] is None:
            hmask[:, l * 32:(l + 1) * 32] = 0.0
    return c, hmask


def emit_attn2(S, cx, d):
    nc = S.nc
    cst = S.sb("cst2", [128, CST2_N], F32)
    b_cst = Buf("cst2")
    S.op("sp", lambda: nc.sync.dma_start(out=cst[:], in_=d["cst"][:, :]), writes=[b_cst])

    def C(name):
        o, w = CST2_OFF[name]
        return cst[:, o:o + w]
    kbh = S.sb("kbh", [64, NH * NB], BF16)
    kbl = S.sb("kbl", [64, NH * NB], BF16)
    b_kb = Buf("kbhl")
    n0 = len(S._stack)
    kb32 = S.sb("kb32", [64, NH * NB], F32)
    kbt = S.sb("kbt", [64, NH * NB], F32)
    S.op("sp", lambda: nc.sync.dma_start(out=kb32[:].rearrange("d (h m) -> d h m", h=NH),
                                         in_=d["kbar"].rearrange("(h d) m -> d h m", h=NH)[:, :, 0:NB]), reads=[d["b_kv"]], writes=[b_kb])
    S.op("dve", lambda: nc.vector.tensor_copy(out=kbh[:], in_=kb32[:]), reads=[b_kb], writes=[b_kb])
    S.op("dve", lambda: nc.vector.tensor_copy(out=kbt[:], in_=kbh[:]), reads=[b_kb], writes=[b_kb])
    S.op("dve", lambda: nc.vector.tensor_tensor(out=kbt[:], in0=kb32[:], in1=kbt[:], op=ALU.subtract), reads=[b_kb], writes=[b_kb])
    S.op("dve", lambda: nc.vector.tensor_copy(out=kbl[:], in_=kbt[:]), reads=[b_kb], writes=[b_kb])
    S.barrier_free(len(S._stack) - n0)
    KTa = [S.sb("KTa%d" % i, [128, SEQ], BF16) for i in range(2)]
    Va = [S.sb("Va%d" % i, [128, 128, 65], BF16) for i in range(2)]
    Qa = [S.sb("Qa%d" % i, [128, TOKX], BF16) for i in range(2)]
    Kd = [S.sb("Kd%d" % i, [128, TOK], BF16) for i in range(2)]
    Vdd = [S.sb("Vdd%d" % i, [128, 32, 65], BF16) for i in range(2)]
    bT = S.sb("bT", [128, 2048], F32)
    dgb = [S.sb("dgb%d" % i, [128, 512], F32) for i in range(2)]
    dgbH = [S.sb("dgbH%d" % i, [128, 64], F32) for i in range(2)]
    cq = [S.sb("cq%d" % i, [128, 288], F32) for i in range(2)]
    b_KT = [Buf("KTa%d" % i) for i in range(2)]
    b_Va = [Buf("Va%d" % i) for i in range(2)]
    b_Qa = [Buf("Qa%d" % i) for i in range(2)]
    b_Qm = [[Buf("Qm%d_%d" % (i, l)) for l in range(32)] for i in range(2)]
    b_Kd = [Buf("Kd%d" % i) for i in range(2)]
    b_Vdd = [Buf("Vdd%d" % i) for i in range(2)]
    b_hd = [Buf("hd%d" % i) for i in range(2)]
    b_bT = Buf("bT")
    mbp = [S.sb("mbp%d" % i, [128, 128], F32) for i in range(2)]
    b_mbp = [Buf("mbp%d" % i) for i in range(2)]
    gmt = [S.sb("gmt%d" % i, [128, 64], F32) for i in range(2)]
    b_gmt = [Buf("gmt%d" % i) for i in range(2)]
    t8 = [S.sb("t8_%d" % i, [128, 8], F32) for i in range(2)]
    pt = [S.sb("pt%d" % i, [128, 512], BF16) for i in range(2)]
    b_pt = [Buf("pt%d" % i) for i in range(2)]
    pt2 = S.sb("pt2", [128, 512], BF16)
    b_pt2 = Buf("pt2")
    dtmp = S.sb("dtmp", [128, 512], F32)
    b_dtmp = Buf("dtmp")
    o1 = S.sb("o1", [128, 256], F32)
    o2 = S.sb("o2", [128, 256], F32)
    rd = S.sb("rd", [128, 256], F32)
    ob = [S.sb("ob%d" % i, [128, 256], BF16) for i in range(2)]
    b_o = Buf("o12")
    b_ob = [Buf("ob%d" % i) for i in range(2)]
    for i in range(2):
        S.op("sp", (lambda i=i: nc.sync.dma_start(out=KTa[i][64:128, :], in_=d["ind"][:, :])), writes=[b_KT[i]])
        S.op("dve", (lambda i=i: nc.vector.memset(Va[i][:, :, 64:65], 1.0)), writes=[b_Va[i]])
        S.op("dve", (lambda i=i: nc.vector.memset(Vdd[i][:, :, 64:65], 1.0)), writes=[b_Vdd[i]])
        S.op("dve", (lambda i=i: nc.vector.memset(mbp[i][:], 0.0)), writes=[b_mbp[i]])
    ps = cx.psum
    bp = cx.b_psum
    scnt = 0
    gcnt = 0
    ocnt = 0
    for h in range(NH):
        cur = h % 2
        sl = SLOPES[h]
        es = math.exp(sl)
        S.op("sp", (lambda h=h, cur=cur: nc.sync.dma_start(out=KTa[cur][0:64, :], in_=d["KT"][h, :, 0:SEQ])), reads=[d["b_kv"]], writes=[b_KT[cur]])
        for q8 in range(8):
            S.op("sp", (lambda h=h, cur=cur, q8=q8: nc.sync.dma_start(out=Va[cur][:, q8 * 16:(q8 + 1) * 16, 0:64], in_=d["Vd"][h, :, q8 * 16:(q8 + 1) * 16, :])),
                 reads=[d["b_kv"]], writes=[b_Va[cur]])
        S.op("sp", (lambda h=h, cur=cur: nc.sync.dma_start(out=Qa[cur][0:64, :], in_=d["QT"][h])), reads=[d["b_kv"]], writes=[b_Qa[cur]] + b_Qm[cur])
        S.op("sp", (lambda h=h, cur=cur: nc.sync.dma_start(out=Kd[cur][0:64, :], in_=d["KT"][h, :, SEQ:SEQ + TOK])), reads=[d["b_kv"]], writes=[b_Kd[cur]])
        for q2 in range(2):
            S.op("sp", (lambda h=h, cur=cur, q2=q2: nc.sync.dma_start(out=Vdd[cur][:, q2 * 16:(q2 + 1) * 16, 0:64],
                                                                     in_=d["Vd"][h, :, 128 + q2 * 16:128 + (q2 + 1) * 16, :])),
                 reads=[d["b_kv"]], writes=[b_Vdd[cur]])
        for (VV, bV) in ((Va[cur], b_Va[cur]), (Vdd[cur], b_Vdd[cur])):
            Vodd = VV[:].rearrange("p (n two) e -> p n two e", two=2)[:, :, 1, :]
            S.op("dve", (lambda Vodd=Vodd, es=es: nc.vector.tensor_scalar(out=Vodd[:, :, 0:64], in0=Vodd[:, :, 0:64], scalar1=es, scalar2=None, op0=ALU.mult)),
                 reads=[bV], writes=[bV])
            S.op("dve", (lambda Vodd=Vodd, es=es: nc.vector.memset(Vodd[:, :, 64:65], es)), reads=[bV], writes=[bV])
        S.op("dve", (lambda sl=sl: nc.vector.tensor_scalar(out=bT[:], in0=C("base"), scalar1=sl, scalar2=None, op0=ALU.mult)),
             reads=[b_cst], writes=[b_bT])
        S.op("dve", (lambda cur=cur, sl=sl: nc.vector.scalar_tensor_tensor(out=dgb[cur][:], in0=C("Dm"), scalar=-sl, in1=C("causal"),
                                                                         op0=ALU.mult, op1=ALU.add)), reads=[b_cst], writes=[b_hd[cur]])
        S.op("dve", (lambda cur=cur, sl=sl: nc.vector.scalar_tensor_tensor(out=dgbH[cur][:], in0=C("DmH"), scalar=-sl, in1=C("causalH"),
                                                                         op0=ALU.mult, op1=ALU.add)), reads=[b_cst], writes=[b_hd[cur]])
        S.op("act", (lambda cur=cur, sl=sl: nc.scalar.activation(out=cq[cur][:, 0:256], in_=C("irow"), func=AF.Exp, scale=-sl)),
             reads=[b_cst], writes=[b_hd[cur]])
        S.op("act", (lambda cur=cur, sl=sl: nc.scalar.activation(out=cq[cur][:, 256:288], in_=C("irowH"), func=AF.Exp, scale=-sl)),
             reads=[b_cst], writes=[b_hd[cur]])
        groups = []
        for l in range(NLB):
            groups.append((l * 256, 128, l, l))
            groups.append((l * 256 + 128, 128, l, l))
        for l in range(NLB):
            groups.append((TOK + l * 32, 32, 16 + l, 16 + l))
        for (q0, nr, row, qmi) in groups:
            g = gcnt % 2
            gcnt += 1

            def mmg(cur=cur, q0=q0, nr=nr, h=h):
                nc.tensor.matmul(ps[6][0:nr, 0:64], lhsT=Qa[cur][0:64, q0:q0 + nr], rhs=kbh[:, h * 64:(h + 1) * 64], start=True, stop=False)
                return nc.tensor.matmul(ps[6][0:nr, 0:64], lhsT=Qa[cur][0:64, q0:q0 + nr], rhs=kbl[:, h * 64:(h + 1) * 64], start=False, stop=True)
            S.op("pe", mmg, reads=[b_Qa[cur], b_kb], writes=[bp[6]])
            pb = C("past")[0:nr, row * 64:(row + 1) * 64]
            S.op("dve", (lambda g=g, pb=pb, nr=nr: nc.vector.tensor_tensor(out=gmt[g][0:nr, :], in0=ps[6][0:nr, 0:64], in1=pb, op=ALU.add)),
                 reads=[bp[6], b_cst], writes=[b_gmt[g]])
            S.op("dve", (lambda g=g, nr=nr: nc.vector.max(out=t8[g][0:nr, :], in_=gmt[g][0:nr, :])), reads=[b_gmt[g]], writes=[b_gmt[g]])
            S.op("dve", (lambda g=g, nr=nr: nc.vector.tensor_scalar(out=gmt[g][0:nr, :], in0=gmt[g][0:nr, :], scalar1=t8[g][0:nr, 2:3], scalar2=-NEG,
                                                                    op0=ALU.is_ge, op1=ALU.mult)), reads=[b_gmt[g]], writes=[b_gmt[g]])
            S.op("dve", (lambda g=g, pb=pb, nr=nr: nc.vector.scalar_tensor_tensor(out=mbp[g][0:nr, 64:128], in0=gmt[g][0:nr, :], scalar=NEG, in1=pb,
                                                                               op0=ALU.add, op1=ALU.add)),
                 reads=[b_gmt[g], b_cst], writes=[b_mbp[g]])
            S.op("pe", (lambda g=g, nr=nr: nc.tensor.transpose(ps[7][:, 0:nr], mbp[g][0:nr, :], C("ident")[0:nr, 0:nr])),
                 reads=[b_mbp[g], b_cst], writes=[bp[7]])
            S.op("act", (lambda cur=cur, q0=q0, nr=nr: nc.scalar.copy(out=Qa[cur][64:128, q0:q0 + nr], in_=ps[7][64:128, 0:nr])),
                 reads=[bp[7]], writes=[b_Qm[cur][qmi]])
        for i in range(8):
            nk = 8 * i + 8
            qbs = []
            for k in range(2):
                l = 2 * i + k
                qbs.append(dict(q0=l * 256, nq=256, row=l, qm=l, K=KTa[cur], koff=(8 * i + k) * 256, V=Va[cur], ch0=2 * (8 * i + k),
                                bK=b_KT[cur], bV=b_Va[cur], dg=dgb[cur], cqv=cq[cur][:, 0:256], ot=l // 2))
            for k in range(2):
                l = 2 * i + k
                qbs.append(dict(q0=TOK + l * 32, nq=32, row=16 + l, qm=16 + l, K=Kd[cur], koff=l * 256, V=Vdd[cur], ch0=2 * l,
                                bK=b_Kd[cur], bV=b_Vdd[cur], dg=dgbH[cur], cqv=cq[cur][:, 256:288], ot=8))
            for qi, qb in enumerate(qbs):
                q0, nq = qb["q0"], qb["nq"]
                po = 2 + (qi % 2)
                for n in range(nk):
                    sb_ = scnt % 2
                    scnt += 1

                    def mms(cur=cur, q0=q0, nq=nq, n=n, sb_=sb_):
                        for c in range(2):
                            m = nc.tensor.matmul(ps[sb_][:, c * nq:(c + 1) * nq], lhsT=KTa[cur][:, n * 256 + c * 128:n * 256 + (c + 1) * 128],
                                                 rhs=Qa[cur][:, q0:q0 + nq], start=True, stop=True)
                        return m
                    S.op("pe", mms, reads=[b_KT[cur], b_Qa[cur], b_Qm[cur][qb["qm"]]], writes=[bp[sb_]])
                    S.op("act", (lambda row=qb["row"], nq=nq, n=n, sb_=sb_: nc.scalar.activation(out=pt[sb_][:, 0:2 * nq], in_=ps[sb_][:, 0:2 * nq], func=AF.Exp,
                                                                                                bias=bT[:, row * 64 + n:row * 64 + n + 1])),
                         reads=[bp[sb_], b_bT], writes=[b_pt[sb_]])

                    def mmpv(cur=cur, n=n, sb_=sb_, po=po, nk=nk, nq=nq):
                        for c in range(2):
                            m = nc.tensor.matmul(ps[po][0:65, 0:nq], lhsT=Va[cur][:, 2 * n + c, :], rhs=pt[sb_][:, c * nq:(c + 1) * nq],
                                                 start=(n == 0 and c == 0), stop=(n == nk - 1 and c == 1))
                        return m
                    S.op("pe", mmpv, reads=[b_Va[cur], b_pt[sb_]], writes=[bp[po]])

                def mmd(qb=qb, cur=cur, q0=q0, nq=nq):
                    for c in range(2):
                        m = nc.tensor.matmul(ps[4][:, c * nq:(c + 1) * nq], lhsT=qb["K"][0:64, qb["koff"] + c * 128:qb["koff"] + (c + 1) * 128],
                                             rhs=Qa[cur][0:64, q0:q0 + nq], start=True, stop=True)
                    return m
                S.op("pe", mmd, reads=[qb["bK"], b_Qa[cur]], writes=[bp[4]])
                S.op("dve", (lambda qb=qb, nq=nq: nc.vector.tensor_tensor(out=dtmp[:, 0:2 * nq], in0=ps[4][:, 0:2 * nq], in1=qb["dg"][:, 0:2 * nq], op=ALU.add)),
                     reads=[bp[4], b_hd[cur]], writes=[b_dtmp])
                S.op("act", (lambda nq=nq: nc.scalar.activation(out=pt2[:, 0:2 * nq], in_=dtmp[:, 0:2 * nq], func=AF.Exp)), reads=[b_dtmp], writes=[b_pt2])

                def mmpo(qb=qb, nq=nq):
                    for c in range(2):
                        m = nc.tensor.matmul(ps[5][0:65, 0:nq], lhsT=qb["V"][:, qb["ch0"] + c, :], rhs=pt2[:, c * nq:(c + 1) * nq],
                                             start=(c == 0), stop=(c == 1))
                    return m
                S.op("pe", mmpo, reads=[qb["bV"], b_pt2], writes=[bp[5]])
                S.op("dve", (lambda qb=qb, po=po, nq=nq: nc.vector.tensor_tensor(out=o1[0:65, 0:nq], in0=ps[po][0:65, 0:nq], in1=qb["cqv"][0:65, :], op=ALU.mult)),
                     reads=[bp[po], b_hd[cur]], writes=[b_o])
                S.op("dve", (lambda nq=nq: nc.vector.tensor_tensor(out=o2[0:65, 0:nq], in0=o1[0:65, 0:nq], in1=ps[5][0:65, 0:nq], op=ALU.add)),
                     reads=[bp[5], b_o], writes=[b_o])
                S.op("pe", (lambda nq=nq: nc.tensor.matmul(ps[6][0:64, 0:nq], lhsT=C("sel65")[0:65, :], rhs=o2[0:65, 0:nq], start=True, stop=True)),
                     reads=[b_o, b_cst], writes=[bp[6]])
                S.op("dve", (lambda nq=nq: nc.vector.reciprocal(out=rd[0:64, 0:nq], in_=ps[6][0:64, 0:nq])), reads=[bp[6]], writes=[b_o])
                k = ocnt % 2
                ocnt += 1
                S.op("dve", (lambda k=k, nq=nq: nc.vector.tensor_tensor(out=ob[k][0:64, 0:nq], in0=o2[0:64, 0:nq], in1=rd[0:64, 0:nq], op=ALU.mult)),
                     reads=[b_o], writes=[b_ob[k]])
                S.op("sp", (lambda k=k, h=h, q0=q0, nq=nq: nc.sync.dma_start(out=d["OT"][h, :, q0:q0 + nq], in_=ob[k][0:64, 0:nq])),
                     reads=[b_ob[k]], writes=[d["b_OT"][qb["ot"]]])


def build_fused():
    nc = bass.Bass("TRN2", target_bir_lowering=False)
    din = lambda n, s, dt: nc.dram_tensor(n, s, dt, kind="ExternalInput").ap()
    scr = lambda n, s, dt: nc.dram_tensor(n, s, dt).ap()
    sp_d = din("sp", [128, SP_N], F32)
    ada_w = din("ada_w", [2, 1024, 9216], F32)
    xall = din("xall", [1024, NT_ALL * 512], F32)
    w_in = [din("w_in%d" % i, [1024, 2 * DFF], F32) for i in range(4)]
    w_out = [din("w_out%d" % i, [DFF, 1024], F32) for i in range(4)]
    wqkv = din("wqkv", [1024, 3072], F32)
    w_o = din("w_o", [1024, 1024], F32)
    w_pw1 = din("w_pw1", [1024, 2048], F32)
    w_pw2 = din("w_pw2", [1024, 1024], F32)
    ind_d = din("ind", [64, SEQ], BF16)
    cst_d = din("cst", [128, CST2_N], F32)
    hm_d = din("hmask", [128, 512], F32)
    ident_d = din("ident", [128, 128], F32)
    xo = nc.dram_tensor("xo", [1024, TOK], F32, kind="ExternalOutput").ap()
    x1all = scr("x1all", [1024, NT_ALL * 512], F32)
    x1o = scr("x1o", [1024, TOKX], F32)
    xa = scr("xa", [1024, TOKX], F32)
    xb = scr("xb", [1024, TOKX], F32)
    x4 = scr("x4", [1024, TOKX], F32)
    x5 = scr("x5", [1024, TOK], F32)
    uT = scr("uT", [1024, TOKX], BF16)
    KT = scr("KTs", [NH, DH, NPOS * 256], BF16)
    Vd = scr("Vds", [NH, 128, 2 * NPOS, DH], BF16)
    kbar = scr("kbars", [NH * DH, NPOS], F32)
    QT = scr("QTs", [NH, DH, TOKX], BF16)
    OT = scr("OTs", [NH, DH, TOKX], BF16)
    S = Sched(nc)
    cx = Ctx()
    setup_common(S, cx)
    emit_load_small(S, cx, sp_d)
    hmask = S.sb("hmask", [128, 512], F32)
    b_hm = Buf("hmask")
    S.op("sp", lambda: nc.sync.dma_start(out=hmask[:], in_=hm_d[:, :]), writes=[b_hm])
    modT = [S.sb("modT%d" % i, [128, 72], F32) for i in range(2)]
    b_modT = [Buf("modT%d" % i) for i in range(2)]
    emit_adaln_scoped(S, cx, ada_w, modT, b_modT)
    M = {}
    for i in range(2):
        for j in range(3):
            M[(i, j)] = emit_mod_vectors_i(S, cx, modT[i], b_modT[i], spc(cx, "ng%d%d" % (i, j)), "l%ds%d" % (i, j), j)
    nbase = len(S._stack)
    mk = lambda name, n: [Buf("%s_%d" % (name, t)) for t in range(n)]
    b_xall, b_x1all, b_x1o = mk("xall", NT_ALL), mk("x1all", NT_ALL), mk("x1o", 9)
    b_xa, b_xb, b_x4, b_u, b_x5, b_xo = mk("xa", 9), mk("xb", 9), mk("x4", 16), mk("u", 9), mk("x5", 8), mk("xo", 8)
    b_x4t = b_x4[:9]
    alloc_tile_bufs(S, cx)
    ntile = len(S._stack)
    alloc_ffn_bufs(S, cx)
    m = M[(0, 0)]
    emit_ffn_phase(S, cx, xall, b_xall, x1all, b_x1all, w_in[0], w_out[0], m[0], m[1], m[2], 0.5, m[3], NT_ALL, "f00")
    S.barrier_free(len(S._stack) - ntile)
    m = M[(0, 1)]
    b_kv = Buf("kvq")
    emit_kvq_phase(S, cx, x1all, b_x1all, wqkv, m[0], m[1], m[3], QT, KT, kbar, Vd, x1o, b_x1o, b_kv)
    S.barrier_free(len(S._stack) - nbase)
    b_OT = mk("OT", 9)
    emit_attn2(S, cx, {"KT": KT, "ind": ind_d, "Vd": Vd, "QT": QT, "kbar": kbar, "cst": cst_d, "OT": OT, "b_OT": b_OT, "b_kv": b_kv})
    S.barrier_free(len(S._stack) - nbase)
    alloc_tile_bufs(S, cx)
    emit_proj_res_phase(S, cx, x1o, b_x1o, xa, b_xa, OT, b_OT, w_o, m[2], None, m[3], "wo", True, ntiles=9)
    S.barrier_free(len(S._stack) - ntile)
    alloc_ffn_bufs(S, cx)
    m = M[(0, 2)]
    emit_ffn_phase(S, cx, xa, b_xa, xb, b_xb, w_in[1], w_out[1], m[0], m[1], m[2], 0.5, m[3], 9, "f01")
    m = M[(1, 0)]
    emit_ffn_phase(S, cx, xb, b_xb, x4, b_x4t, w_in[2], w_out[2], m[0], m[1], m[2], 0.5, m[3], 9, "f10")
    S.barrier_free(len(S._stack) - ntile)
    m = M[(1, 1)]
    emit_glu_phase(S, cx, x4, b_x4t, w_pw1, m[0], m[1], m[3], uT, ntiles=9, hmask=hmask[:], b_hmask=b_hm, b_uout=b_u)
    S.barrier_free(len(S._stack) - ntile)
    b_x4c = [Buf("x4c_%d" % t) for t in range(16)]
    emit_conv_phase(S, cx, None, x4, b_x4c, x5, b_x5, w_pw2, ident_d, m[2], m[3], u_scr=uT, b_u=b_u)
    S.barrier_free(len(S._stack) - ntile)
    alloc_ffn_bufs(S, cx)
    m = M[(1, 2)]
    emit_ffn_phase(S, cx, x5, b_x5, xo, b_xo, w_in[3], w_out[3], m[0], m[1], m[2], 0.5, m[3], 8, "f11")
    S.emit()
    return nc


def kernel(**inp):
    import ml_dtypes
    bf = ml_dtypes.bfloat16
    inp = {k: np.asarray(v) for k, v in inp.items()}
    cores = list(range(8))
    ind = np.zeros((64, SEQ), bf)
    for m in range(64):
        ind[m, m * 256:(m + 1) * 256] = 1
    ident = np.eye(128, dtype=np.float32)
    maps = []
    toks = []
    for c in cores:
        b, j = c // 4, c % 4
        real, dup = pos_order(j)
        xb_ = inp["x"][b]
        xall = np.zeros((NPOS * 256, D), np.float32)
        for pi, g in enumerate(real + dup):
            if g is not None:
                xall[pi * 256:(pi + 1) * 256] = xb_[g * 256:(g + 1) * 256]
        cst, hmask = make_cst2(j)
        toks.append(np.concatenate([np.arange(g * 256, (g + 1) * 256) for g in snake_blocks(j)]))
        mp = {"sp": pack_small(inp, b), "ada_w": inp["ada_w"], "xall": np.ascontiguousarray(xall.T),
              "wqkv": inp["attn_w_qkv"][0], "w_o": inp["attn_w_o"][0], "w_pw1": inp["conv_w_pw1"][0], "w_pw2": inp["conv_w_pw2"][0],
              "ind": ind, "cst": cst, "hmask": hmask, "ident": ident}
        for k, (i, w) in enumerate(((0, 0), (0, 1), (1, 0), (1, 1))):
            mp["w_in%d" % k] = inp["ffn_w_in"][i, w]
            mp["w_out%d" % k] = inp["ffn_w_out"][i, w]
        maps.append(mp)
    nc = _get("fused", build_fused)
    res = run_bass_kernel_spmd(nc, maps, core_ids=cores).results
    out = np.zeros((BATCH, SEQ, D), np.float32)
    for c in cores:
        out[c // 4][toks[c]] = np.asarray(res[c]["xo"]).T
    return out
```

```python
import math
import numpy as np
import concourse.bass as bass
import concourse.mybir as mybir
from concourse.bass_utils import run_bass_kernel_spmd

F32 = mybir.dt.float32
BF16 = mybir.dt.bfloat16
AF = mybir.ActivationFunctionType
ALU = mybir.AluOpType
AX = mybir.AxisListType

D = 1024
KC = 8
DFF = 2816
FC = 22
NH = 16
DH = 64
BLK = 256
NB = 64
SEQ = 16384
BATCH = 2
TOK = 4096
NLB = 16
EPS = 1e-6
NEG = -30000.0
CW = 31


class Buf:
    __slots__ = ("name", "last_w", "readers")

    def __init__(self, name):
        self.name = name
        self.last_w = None
        self.readers = []


class Sched:
    DMA_ENGS = ("sp", "pool_dma")

    def __init__(self, nc, n_dma_sems=12):
        self.nc = nc
        self.ops = []
        self.engs = {"pe": nc.tensor, "act": nc.scalar, "dve": nc.vector,
                     "pool": nc.gpsimd, "sp": nc.sync, "pool_dma": nc.gpsimd}
        self.stream = {"pe": "pe", "act": "act", "dve": "dve", "pool": "pool",
                       "sp": "sp", "pool_dma": "pool"}
        self.n_dma_sems = n_dma_sems
        self._stack = []
        self.bar_list = []
        cm = nc.sbuf_tensor("bar_t", [128, 8], F32)
        self.bar_t = cm.__enter__()

    def sb(self, name, shape, dt):
        self._uid = getattr(self, "_uid", 0) + 1
        cm = self.nc.sbuf_tensor("s%d_%s" % (self._uid, name), shape, dt)
        t = cm.__enter__()
        self._stack.append(cm)
        return t

    def ps(self, name, shape, dt=F32):
        cm = self.nc.psum_tensor("p_" + name, shape, dt)
        t = cm.__enter__()
        self._stack.append(cm)
        return t

    def barrier_free(self, n_free):
        nc = self.nc
        bb = Buf("barrier%d" % len(self.ops))
        prev = list(range(len(self.ops)))
        i0 = self.op("pool", lambda: nc.gpsimd.memset(self.bar_t[:, 0:1], 0.0), writes=[bb])
        self.ops[i0]["deps"] = set(prev)
        for e, fn in (("dve", lambda: nc.vector.memset(self.bar_t[:, 1:2], 0.0)),
                      ("act", lambda: nc.scalar.activation(out=self.bar_t[:, 2:3], in_=self.bar_t[:, 0:1], func=AF.Copy)),
                      ("pe", None),
                      ("sp", lambda: nc.sync.dma_start(out=self.bar_t[:, 4:5], in_=self.bar_t[:, 0:1]))):
            if fn is None:
                continue
            self.op(e, fn, reads=[bb])
        self.bar_after = i0
        self.bar_list.append(i0)
        for _ in range(n_free):
            cm = self._stack.pop()
            cm.__exit__(None, None, None)

    def op(self, eng, fn, reads=(), writes=()):
        deps = set()
        idx = len(self.ops)
        for b in reads:
            if b.last_w is not None:
                deps.add(b.last_w)
        for b in writes:
            if b.last_w is not None:
                deps.add(b.last_w)
            for r in b.readers:
                deps.add(r)
        for b in reads:
            b.readers.append(idx)
        for b in writes:
            b.last_w = idx
            b.readers = []
        deps.discard(idx)
        if self.bar_list:
            deps.add(self.bar_list[-1])
        self.ops.append({"eng": eng, "fn": fn, "deps": deps, "dma": eng in self.DMA_ENGS})
        return idx

    def emit(self):
        nc = self.nc
        ops = self.ops
        n = len(ops)
        need = [False] * n
        for i, o in enumerate(ops):
            if o["dma"]:
                need[i] = True
            for d in o["deps"]:
                if ops[d]["dma"]:
                    continue
                sd, si = self.stream[ops[d]["eng"]], self.stream[o["eng"]]
                if sd != si or sd != "pe":
                    need[d] = True
        sem_cm = []

        def mksem(name):
            cm = nc.semaphore(name)
            s = cm.__enter__()
            sem_cm.append(cm)
            return s

        eng_sem = {k: mksem("s_" + k) for k in ("pe", "act", "dve", "pool")}
        eng_cnt = {k: 0 for k in eng_sem}
        dma_sems = {q: [mksem("d_%s_%d" % (q, i)) for i in range(self.n_dma_sems)] for q in self.DMA_ENGS}
        dma_cnt = {q: [0] * self.n_dma_sems for q in self.DMA_ENGS}
        dma_last = {q: [None] * self.n_dma_sems for q in self.DMA_ENGS}
        dma_rr = {q: 0 for q in self.DMA_ENGS}
        ev = [None] * n
        waited = {s: {} for s in ("pe", "act", "dve", "pool", "sp")}
        for i, o in enumerate(ops):
            eng = o["eng"]
            st = self.stream[eng]
            e = self.engs[eng]
            waits = {}
            for d in o["deps"]:
                if ev[d] is None:
                    continue
                sem, val, key = ev[d]
                if key not in waits or waits[key][1] < val:
                    waits[key] = (sem, val)
            my = None
            if o["dma"]:
                k = dma_rr[eng]
                dma_rr[eng] = (k + 1) % self.n_dma_sems
                prev = dma_last[eng][k]
                if prev is not None:
                    sem, val, key = ev[prev]
                    if key not in waits or waits[key][1] < val:
                        waits[key] = (sem, val)
                dma_cnt[eng][k] += 16
                my = (dma_sems[eng][k], dma_cnt[eng][k], (eng, k))
                dma_last[eng][k] = i
            elif need[i]:
                eng_cnt[st] += 1
                my = (eng_sem[st], eng_cnt[st], st)
            for key, (sem, val) in waits.items():
                if waited[st].get(key, 0) >= val:
                    continue
                e.wait_ge(sem, val)
                waited[st][key] = val
            inst = o["fn"]()
            if my is not None:
                inst.then_inc(my[0], 16 if o["dma"] else 1)
                ev[i] = my
            o["fn"] = None
        sp = nc.sync
        for q in self.DMA_ENGS:
            for k in range(self.n_dma_sems):
                if dma_cnt[q][k] > 0:
                    sp.wait_ge(dma_sems[q][k], dma_cnt[q][k])
        for k in eng_sem:
            if eng_cnt[k] > 0:
                sp.wait_ge(eng_sem[k], eng_cnt[k])
        self.max_counts = dict(eng_cnt)


def snake_blocks(j):
    out = []
    for i in range(8):
        out.append(8 * i + j)
        out.append(8 * i + 7 - j)
    return out


class Ctx:
    pass


def setup_common(S, cx):
    nc = S.nc
    cx.ones_bf = S.sb("ones_bf", [128, 128], BF16)
    cx.b_ones = Buf("ones")
    S.op("dve", lambda: nc.vector.memset(cx.ones_bf[:], 1.0), writes=[cx.b_ones])
    cx.psum = [S.ps("psb%d" % i, [128, 512], F32) for i in range(8)]
    cx.b_psum = [Buf("psb%d" % i) for i in range(8)]


def emit_mod_vectors(S, cx, modT, normg, layer_slot, j):
    nc = S.nc
    A = S.sb("modA_%s" % layer_slot, [128, 8], F32)
    b = Buf("modA_%s" % layer_slot)
    base = j * 24
    S.op("dve", lambda: nc.vector.scalar_tensor_tensor(
        out=A[:], in0=modT[:, base + 8:base + 16], scalar=1.0, in1=normg,
        op0=ALU.add, op1=ALU.mult), reads=[cx.b_mod], writes=[b])
    return A, modT[:, base:base + 8], modT[:, base + 16:base + 24], b


def emit_ffn_phase(S, cx, x_in, b_xin, x_out, b_xout, w_in_d, w_out_d, A, Bv, G, gscale, b_mod, ntiles, tag):
    nc = S.nc
    win = cx.win
    wout = cx.wout
    HF = FC // 2
    for kc in range(KC):
        S.op("pool_dma", (lambda kc=kc: nc.gpsimd.dma_start(out=win[:, kc, :], in_=w_in_d[kc * 128:(kc + 1) * 128, :])),
             writes=[cx.b_win[kc]])
    for f in range(FC):
        S.op("pool_dma", (lambda f=f: nc.gpsimd.dma_start(out=wout[:, f, :], in_=w_out_d[f * 128:(f + 1) * 128, :])),
             writes=[cx.b_wout[f]])
    Gs = S.sb("Gs_" + tag, [128, 8], F32)
    b_Gs = Buf("Gs_" + tag)
    S.op("dve", lambda: nc.vector.tensor_scalar(out=Gs[:], in0=G, scalar1=float(gscale), scalar2=None, op0=ALU.mult),
         reads=[b_mod], writes=[b_Gs])
    xv_in = x_in.rearrange("(c p) t -> p c t", p=128)
    xv_out = x_out.rearrange("(c p) t -> p c t", p=128)
    for t in range(ntiles):
        xt = cx.xt[t % 2]
        bx = cx.b_xt[t % 2]
        S.op("sp", (lambda xt=xt, t=t: nc.sync.dma_start(out=xt[:], in_=xv_in[:, :, t * 512:(t + 1) * 512])),
             reads=[b_xin[t]], writes=[bx])
        emit_norm_mod(S, cx, xt, bx, A, Bv, b_mod)
        for hf in range(2):
            for fl in range(HF):
                f = hf * HF + fl
                pa = 1 + (f % 2) * 2
                pb = pa + 1

                def mm_a(f=f, pa=pa):
                    for kc in range(KC):
                        m = nc.tensor.matmul(cx.psum[pa][:], lhsT=win[:, kc, f * 128:(f + 1) * 128], rhs=cx.h[:, kc, :],
                                             start=(kc == 0), stop=(kc == KC - 1))
                    return m

                def mm_b(f=f, pb=pb):
                    for kc in range(KC):
                        m = nc.tensor.matmul(cx.psum[pb][:], lhsT=win[:, kc, DFF + f * 128:DFF + (f + 1) * 128],
                                             rhs=cx.h[:, kc, :], start=(kc == 0), stop=(kc == KC - 1))
                    return m
                S.op("pe", mm_a, reads=cx.b_win + [cx.b_h], writes=[cx.b_psum[pa]])
                S.op("pe", mm_b, reads=cx.b_win + [cx.b_h], writes=[cx.b_psum[pb]])
                sa = cx.sa[f % 2]
                S.op("act", (lambda sa=sa, pa=pa: nc.scalar.activation(out=sa[:], in_=cx.psum[pa][:], func=AF.Silu)),
                     reads=[cx.b_psum[pa]], writes=[cx.b_sa[f % 2]])
                S.op("dve", (lambda sa=sa, pb=pb, fl=fl: nc.vector.tensor_tensor(out=cx.u[:, fl, :], in0=sa[:], in1=cx.psum[pb][:], op=ALU.mult)),
                     reads=[cx.b_sa[f % 2], cx.b_psum[pb]], writes=[cx.b_u[fl]])
            for oc in range(KC):
                py = 5 + (oc % 2)

                def mm_y(oc=oc, py=py, hf=hf):
                    for fl in range(HF):
                        m = nc.tensor.matmul(cx.psum[py][:], lhsT=wout[:, hf * HF + fl, oc * 128:(oc + 1) * 128], rhs=cx.u[:, fl, :],
                                             start=(fl == 0), stop=(fl == HF - 1))
                    return m
                S.op("pe", mm_y, reads=cx.b_wout + cx.b_u, writes=[cx.b_psum[py]])
                S.op("dve", (lambda oc=oc, py=py, xt=xt: nc.vector.scalar_tensor_tensor(
                    out=xt[:, oc, :], in0=cx.psum[py][:], scalar=Gs[:, oc:oc + 1], in1=xt[:, oc, :],
                    op0=ALU.mult, op1=ALU.add)), reads=[cx.b_psum[py], b_Gs], writes=[bx])
        S.op("sp", (lambda xt=xt, t=t: nc.sync.dma_start(out=xv_out[:, :, t * 512:(t + 1) * 512], in_=xt[:])),
             reads=[bx], writes=[b_xout[t]])


def emit_norm_mod(S, cx, xt, bx, A, Bv, b_mod, n=512):
    nc = S.nc
    for c in range(KC):
        S.op("act", (lambda c=c: nc.scalar.activation(out=cx.sq[c % 2][:, :n], in_=xt[:, c, :n], func=AF.Square)),
             reads=[bx], writes=[cx.b_sq[c % 2]])
        S.op("pe", (lambda c=c: nc.tensor.matmul(cx.psum[0][:, :n], lhsT=cx.ones_bf[:], rhs=cx.sq[c % 2][:, :n],
                                                 start=(c == 0), stop=(c == KC - 1))),
             reads=[cx.b_sq[c % 2], cx.b_ones], writes=[cx.b_psum[0]])
    S.op("dve", lambda: nc.vector.tensor_scalar(out=cx.rstd[:, :n], in0=cx.psum[0][:, :n], scalar1=1.0 / D, scalar2=EPS,
                                                op0=ALU.mult, op1=ALU.add), reads=[cx.b_psum[0]], writes=[cx.b_rstd])
    S.op("act", lambda: nc.scalar.activation(out=cx.rstd[:, :n], in_=cx.rstd[:, :n], func=AF.Sqrt), reads=[cx.b_rstd], writes=[cx.b_rstd])
    S.op("dve", lambda: nc.vector.reciprocal(out=cx.rstd[:, :n], in_=cx.rstd[:, :n]), reads=[cx.b_rstd], writes=[cx.b_rstd])
    for c in range(KC):
        tmp = cx.tmp[c % 2]
        S.op("dve", (lambda c=c, tmp=tmp: nc.vector.tensor_tensor(out=tmp[:, :n], in0=xt[:, c, :n], in1=cx.rstd[:, :n], op=ALU.mult)),
             reads=[bx, cx.b_rstd], writes=[cx.b_tmp[c % 2]])
        S.op("act", (lambda c=c, tmp=tmp: nc.scalar.activation(out=cx.h[:, c, :n], in_=tmp[:, :n], func=AF.Identity,
                                                               bias=Bv[:, c:c + 1], scale=A[:, c:c + 1])),
             reads=[cx.b_tmp[c % 2], b_mod], writes=[cx.b_h])


def alloc_tile_bufs(S, cx):
    cx.xt = [S.sb("xt%d" % i, [128, KC, 512], F32) for i in range(2)]
    cx.b_xt = [Buf("xt%d" % i) for i in range(2)]
    cx.sq = [S.sb("sq%d" % i, [128, 512], BF16) for i in range(2)]
    cx.b_sq = [Buf("sq%d" % i) for i in range(2)]
    cx.h = S.sb("h", [128, KC, 512], BF16)
    cx.b_h = Buf("h")
    cx.rstd = S.sb("rstd", [128, 512], F32)
    cx.b_rstd = Buf("rstd")
    cx.tmp = [S.sb("tmp%d" % i, [128, 512], F32) for i in range(2)]
    cx.b_tmp = [Buf("tmp%d" % i) for i in range(2)]
    cx.sa = [S.sb("sa%d" % i, [128, 512], F32) for i in range(2)]
    cx.b_sa = [Buf("sa%d" % i) for i in range(2)]


def alloc_ffn_bufs(S, cx):
    cx.win = S.sb("win", [128, KC, 2 * DFF], BF16)
    cx.wout = S.sb("wout", [128, FC, D], BF16)
    cx.b_win = [Buf("win%d" % i) for i in range(KC)]
    cx.b_wout = [Buf("wout%d" % i) for i in range(FC)]
    cx.u = S.sb("u", [128, FC // 2, 512], BF16)
    cx.b_u = [Buf("u%d" % i) for i in range(FC // 2)]


def _fm(v, n=None):
    v = np.asarray(v, np.float32).reshape(-1, 128)
    return np.ascontiguousarray(v.T)


SP_OFF = {}


def _sp_layout():
    off = 0
    for name, w in (("c", 8), ("ada_b0", 72), ("ada_b1", 72),
                    ("ng00", 8), ("ng01", 8), ("ng02", 8), ("ng10", 8), ("ng11", 8), ("ng12", 8),
                    ("gq", 1), ("gk", 1), ("b_pw1", 16), ("b_dw", 8), ("ln_g", 8), ("ln_b", 8),
                    ("b_pw2", 8), ("w_dw", 8 * CW)):
        SP_OFF[name] = (off, w)
        off += w
    return off


SP_N = _sp_layout()


def pack_small(inp, b):
    sp = np.zeros((128, SP_N), np.float32)

    def put(name, arr):
        o, w = SP_OFF[name]
        assert arr.shape == (128, w), (name, arr.shape, w)
        sp[:, o:o + w] = arr
    put("c", _fm(inp["c"][b]))
    put("ada_b0", _fm(inp["ada_b"][0]))
    put("ada_b1", _fm(inp["ada_b"][1]))
    for i in range(2):
        for j in range(3):
            put("ng%d%d" % (i, j), _fm(inp["norm_g"][i, j]))
    put("gq", np.tile(np.asarray(inp["attn_g_q"][0], np.float32), 2).reshape(128, 1))
    put("gk", np.tile(np.asarray(inp["attn_g_k"][0], np.float32), 2).reshape(128, 1))
    put("b_pw1", _fm(inp["conv_b_pw1"][0]))
    put("b_dw", _fm(inp["conv_b_dw"][0]))
    put("ln_g", _fm(inp["conv_ln_g"][0]))
    put("ln_b", _fm(inp["conv_ln_b"][0]))
    put("b_pw2", _fm(inp["conv_b_pw2"][0]))
    wd = np.asarray(inp["conv_w_dw"][0], np.float32)
    put("w_dw", np.ascontiguousarray(wd.reshape(CW, 8, 128).transpose(2, 1, 0)).reshape(128, 8 * CW))
    return sp


def spc(cx, name):
    o, w = SP_OFF[name]
    return cx.spt[:, o:o + w]


def emit_load_small(S, cx, sp_d):
    nc = S.nc
    cx.spt = S.sb("spt", [128, SP_N], F32)
    cx.b_sp = Buf("spt")
    S.op("sp", lambda: nc.sync.dma_start(out=cx.spt[:], in_=sp_d[:, :]), writes=[cx.b_sp])


def emit_adaln(S, cx, ada_w_d, modT, b_modT):
    nc = S.nc
    NCOL = 1152
    stg = [S.sb("adastg%d" % i, [128, KC, NCOL], F32) for i in range(2)]
    b_stg = [Buf("adastg%d" % i) for i in range(2)]
    cact = S.sb("cact", [128, 8], F32)
    b_cact = Buf("cact")
    S.op("act", lambda: nc.scalar.activation(out=cact[:], in_=spc(cx, "c"), func=AF.Silu), reads=[cx.b_sp], writes=[b_cact])
    k = 0
    for i in range(2):
        wv = ada_w_d[i].rearrange("(kc p) n -> p kc n", p=128)
        for pc in range(9216 // NCOL):
            st = stg[k % 2]
            bs = b_stg[k % 2]
            k += 1
            S.op("sp", (lambda st=st, wv=wv, pc=pc: nc.sync.dma_start(out=st[:], in_=wv[:, :, pc * NCOL:(pc + 1) * NCOL])), writes=[bs])

            def mm(st=st, pc=pc, i=i):
                for m in range(NCOL // 128):
                    col = pc * (NCOL // 128) + m
                    for kc in range(KC):
                        r = nc.tensor.matmul(cx.psum[7][:, col:col + 1], lhsT=st[:, kc, m * 128:(m + 1) * 128], rhs=cact[:, kc:kc + 1],
                                             start=(kc == 0), stop=(kc == KC - 1))
                return r
            S.op("pe", mm, reads=[bs, b_cact], writes=[cx.b_psum[7]])
        S.op("dve", (lambda i=i: nc.vector.tensor_tensor(out=modT[i][:], in0=cx.psum[7][:, 0:72], in1=spc(cx, "ada_b%d" % i), op=ALU.add)),
             reads=[cx.b_psum[7], cx.b_sp], writes=[b_modT[i]])
    return stg


def joint_mod(S, cx, i, j, modT, b_modT):
    nc = S.nc
    A, Bv, G, bA = emit_mod_vectors_i(S, cx, modT[i], b_modT[i], spc(cx, "ng%d%d" % (i, j)), "l%ds%d" % (i, j), j)
    return A, Bv, G, bA


def emit_mod_vectors_i(S, cx, modT, b_modT, normg, tag, j):
    nc = S.nc
    AB = S.sb("modAB_" + tag, [128, 24], F32)
    b = Buf("modAB_" + tag)
    base = j * 24
    S.op("dve", lambda: nc.vector.tensor_copy(out=AB[:], in_=modT[:, base:base + 24]), reads=[b_modT, cx.b_sp], writes=[b])
    S.op("dve", lambda: nc.vector.scalar_tensor_tensor(
        out=AB[:, 8:16], in0=modT[:, base + 8:base + 16], scalar=1.0, in1=normg,
        op0=ALU.add, op1=ALU.mult), reads=[b_modT, cx.b_sp], writes=[b])
    return AB[:, 8:16], AB[:, 0:8], AB[:, 16:24], b


def load_x_tile(S, cx, xv, b_x, t, n=512):
    nc = S.nc
    xt = cx.xt[t % 2]
    bx = cx.b_xt[t % 2]
    S.op("sp", (lambda: nc.sync.dma_start(out=xt[:, :, :n], in_=xv[:, :, t * n:(t + 1) * n])), reads=[b_x[t]], writes=[bx])
    return xt, bx


def emit_qkv_phase(S, cx, x_d, b_x, wqkv_d, A, Bv, b_mod, QT_d, KT_d, kbar_d, V_d):
    nc = S.nc
    wq = S.sb("wqkv_sb", [128, KC, 3072], BF16)
    b_wq = [Buf("wqkv%d" % k) for k in range(KC)]
    for kc in range(KC):
        S.op("pool_dma", (lambda kc=kc: nc.gpsimd.dma_start(out=wq[:, kc, :], in_=wqkv_d[kc * 128:(kc + 1) * 128, :])), writes=[b_wq[kc]])
    bo = S.sb("blkones", [128, 128], BF16)
    b_bo = Buf("blkones")
    S.op("dve", lambda: nc.vector.memset(bo[:], 0.0), writes=[b_bo])
    S.op("dve", lambda: nc.vector.memset(bo[0:64, 0:64], 1.0), writes=[b_bo])
    S.op("dve", lambda: nc.vector.memset(bo[64:128, 64:128], 1.0), writes=[b_bo])
    g8 = S.sb("g8", [128, 2], F32)
    b_g8 = Buf("g8")
    S.op("dve", lambda: nc.vector.tensor_scalar(out=g8[:, 0:1], in0=spc(cx, "gq"), scalar1=0.125, scalar2=None, op0=ALU.mult),
         reads=[cx.b_sp], writes=[b_g8])
    S.op("dve", lambda: nc.vector.tensor_copy(out=g8[:, 1:2], in_=spc(cx, "gk")), reads=[cx.b_sp], writes=[b_g8])
    qo = [S.sb("qo%d" % i, [128, 512], BF16) for i in range(2)]
    b_qo = [Buf("qo%d" % i) for i in range(2)]
    kb = [S.sb("kb%d" % i, [128, 2], F32) for i in range(2)]
    b_kb = [Buf("kb%d" % i) for i in range(2)]
    vt = [S.sb("vt%d" % i, [128, 1024], BF16) for i in range(2)]
    b_vt = [Buf("vt%d" % i) for i in range(2)]
    xv = x_d.rearrange("(c p) t -> p c t", p=128)
    QTv = QT_d.rearrange("h d t -> (h d) t")
    KTv = KT_d.rearrange("h d t -> (h d) t")
    kbv = kbar_d.rearrange("h d l -> (h d) l")
    cnt = 0
    for t in range(8):
        xt, bx = load_x_tile(S, cx, xv, b_x, t)
        emit_norm_mod(S, cx, xt, bx, A, Bv, b_mod)
        for which in range(2):
            goff = which * 1024
            outv = QTv if which == 0 else KTv
            for hp in range(8):
                k = cnt % 2
                cnt += 1
                pq = cx.psum[1 + k]
                pss = cx.psum[3 + k]

                def mm(hp=hp, pq=pq, goff=goff):
                    for kc in range(KC):
                        m = nc.tensor.matmul(pq[:], lhsT=wq[:, kc, goff + hp * 128:goff + (hp + 1) * 128], rhs=cx.h[:, kc, :],
                                             start=(kc == 0), stop=(kc == KC - 1))
                    return m
                S.op("pe", mm, reads=b_wq + [cx.b_h], writes=[cx.b_psum[1 + k]])
                S.op("act", (lambda k=k, pq=pq: nc.scalar.activation(out=cx.sq[k][:], in_=pq[:], func=AF.Square)),
                     reads=[cx.b_psum[1 + k]], writes=[cx.b_sq[k]])
                S.op("pe", (lambda k=k, pss=pss: nc.tensor.matmul(pss[:], lhsT=bo[:], rhs=cx.sq[k][:], start=True, stop=True)),
                     reads=[cx.b_sq[k], b_bo], writes=[cx.b_psum[3 + k]])
                r1 = cx.tmp[k]
                S.op("dve", (lambda r1=r1, pss=pss: nc.vector.tensor_scalar(out=r1[:], in0=pss[:], scalar1=1.0 / DH, scalar2=EPS,
                                                                            op0=ALU.mult, op1=ALU.add)),
                     reads=[cx.b_psum[3 + k]], writes=[cx.b_tmp[k]])
                S.op("act", (lambda r1=r1: nc.scalar.activation(out=r1[:], in_=r1[:], func=AF.Sqrt)), reads=[cx.b_tmp[k]], writes=[cx.b_tmp[k]])
                S.op("dve", (lambda r1=r1: nc.vector.reciprocal(out=r1[:], in_=r1[:])), reads=[cx.b_tmp[k]], writes=[cx.b_tmp[k]])
                S.op("dve", (lambda r1=r1, pq=pq: nc.vector.tensor_tensor(out=r1[:], in0=pq[:], in1=r1[:], op=ALU.mult)),
                     reads=[cx.b_tmp[k], cx.b_psum[1 + k]], writes=[cx.b_tmp[k]])
                sa = cx.sa[k]
                S.op("act", (lambda r1=r1, sa=sa, which=which: nc.scalar.activation(out=sa[:], in_=r1[:], func=AF.Copy,
                                                                                  scale=g8[:, which:which + 1])),
                     reads=[cx.b_tmp[k], b_g8], writes=[cx.b_sa[k]])
                S.op("pool", (lambda k=k, sa=sa: nc.gpsimd.tensor_copy(out=qo[k][:], in_=sa[:])), reads=[cx.b_sa[k]], writes=[b_qo[k]])
                S.op("sp", (lambda k=k, hp=hp, t=t, outv=outv: nc.sync.dma_start(out=outv[hp * 128:(hp + 1) * 128, t * 512:(t + 1) * 512], in_=qo[k][:])),
                     reads=[b_qo[k]])
                if which == 1:
                    S.op("dve", (lambda k=k, sa=sa: nc.vector.tensor_reduce(out=kb[k][:], in_=sa[:].rearrange("p (b t) -> p b t", b=2),
                                                                          axis=AX.X, op=ALU.add)),
                         reads=[cx.b_sa[k]], writes=[b_kb[k]])
                    S.op("dve", (lambda k=k: nc.vector.tensor_scalar(out=kb[k][:], in0=kb[k][:], scalar1=1.0 / BLK, scalar2=None, op0=ALU.mult)),
                         reads=[b_kb[k]], writes=[b_kb[k]])
                    S.op("sp", (lambda k=k, hp=hp, t=t: nc.sync.dma_start(out=kbv[hp * 128:(hp + 1) * 128, 2 * t:2 * t + 2], in_=kb[k][:])),
                         reads=[b_kb[k]])
        for tg in range(4):
            v = vt[tg % 2]
            for ns in range(2):
                pv = cx.psum[5 + ns]

                def mmv(tg=tg, ns=ns, pv=pv):
                    for kc in range(KC):
                        m = nc.tensor.matmul(pv[:], lhsT=cx.h[:, kc, tg * 128:(tg + 1) * 128], rhs=wq[:, kc, 2048 + ns * 512:2048 + (ns + 1) * 512],
                                             start=(kc == 0), stop=(kc == KC - 1))
                    return m
                S.op("pe", mmv, reads=b_wq + [cx.b_h], writes=[cx.b_psum[5 + ns]])
                if ns == 0:
                    S.op("act", (lambda v=v, pv=pv: nc.scalar.copy(out=v[:, 0:512], in_=pv[:])), reads=[cx.b_psum[5]], writes=[b_vt[tg % 2]])
                else:
                    S.op("dve", (lambda v=v, pv=pv: nc.vector.tensor_copy(out=v[:, 512:1024], in_=pv[:])), reads=[cx.b_psum[6]], writes=[b_vt[tg % 2]])
            S.op("sp", (lambda v=v, tg=tg, t=t: nc.sync.dma_start(out=V_d[t * 512 + tg * 128:t * 512 + (tg + 1) * 128, :], in_=v[:])),
                 reads=[b_vt[tg % 2]])


def build_L1():
    nc = bass.Bass("TRN2", target_bir_lowering=False)
    sp_d = nc.dram_tensor("sp", [128, SP_N], F32, kind="ExternalInput").ap()
    ada_w = nc.dram_tensor("ada_w", [2, 1024, 9216], F32, kind="ExternalInput").ap()
    xin = nc.dram_tensor("xin", [1024, TOK], F32, kind="ExternalInput").ap()
    w_in = nc.dram_tensor("w_in", [1024, 2 * DFF], F32, kind="ExternalInput").ap()
    w_out = nc.dram_tensor("w_out", [DFF, 1024], F32, kind="ExternalInput").ap()
    wqkv = nc.dram_tensor("wqkv", [1024, 3072], F32, kind="ExternalInput").ap()
    x1 = nc.dram_tensor("x1", [1024, TOK], F32, kind="ExternalOutput").ap()
    modo = nc.dram_tensor("modo", [128, 144], F32, kind="ExternalOutput").ap()
    QT = nc.dram_tensor("QT", [NH, DH, TOK], BF16, kind="ExternalOutput").ap()
    KT = nc.dram_tensor("KT", [NH, DH, TOK], BF16, kind="ExternalOutput").ap()
    kbar = nc.dram_tensor("kbar", [NH, DH, NLB], F32, kind="ExternalOutput").ap()
    V = nc.dram_tensor("V", [TOK, 1024], BF16, kind="ExternalOutput").ap()
    S = Sched(nc)
    cx = Ctx()
    setup_common(S, cx)
    emit_load_small(S, cx, sp_d)
    modT = [S.sb("modT%d" % i, [128, 72], F32) for i in range(2)]
    b_modT = [Buf("modT%d" % i) for i in range(2)]
    A0, B0, G0, bm0 = None, None, None, None
    AB0 = joint_mod
    emit_adaln_scoped(S, cx, ada_w, modT, b_modT)
    for i in range(2):
        S.op("sp", (lambda i=i: nc.sync.dma_start(out=modo[:, i * 72:(i + 1) * 72], in_=modT[i][:])), reads=[b_modT[i]])
    A, Bv, G, bA = emit_mod_vectors_i(S, cx, modT[0], b_modT[0], spc(cx, "ng00"), "l0s0", 0)
    A1, Bv1, G1, bA1 = emit_mod_vectors_i(S, cx, modT[0], b_modT[0], spc(cx, "ng01"), "l0s1", 1)
    alloc_tile_bufs(S, cx)
    alloc_ffn_bufs(S, cx)
    b_xin = [Buf("xin%d" % t) for t in range(8)]
    b_x1 = [Buf("x1_%d" % t) for t in range(8)]
    emit_ffn_phase(S, cx, xin, b_xin, x1, b_x1, w_in, w_out, A, Bv, G, 0.5, bA, 8, "f00")
    S.barrier_free(3)
    emit_qkv_phase(S, cx, x1, b_x1, wqkv, A1, Bv1, bA1, QT, KT, kbar, V)
    S.emit()
    return nc


def emit_adaln_scoped(S, cx, ada_w, modT, b_modT):
    n0 = len(S._stack)
    emit_adaln(S, cx, ada_w, modT, b_modT)
    S.barrier_free(len(S._stack) - n0)


SLOPES = [2.0 ** (-8.0 * (h + 1) / NH) for h in range(NH)]


def emit_attn_phase(S, cx, d):
    nc = S.nc
    KTa = [S.sb("KTa%d" % i, [128, SEQ], BF16) for i in range(2)]
    Va = [S.sb("Va%d" % i, [128, 128, 65], BF16) for i in range(2)]
    Qa = [S.sb("Qa%d" % i, [128, TOK], BF16) for i in range(2)]
    Ko = [S.sb("Ko%d" % i, [128, TOK], BF16) for i in range(2)]
    Vo = [S.sb("Vo%d" % i, [128, 32, 65], BF16) for i in range(2)]
    bT = [S.sb("bT%d" % i, [128, 1024], F32) for i in range(2)]
    dgb = [S.sb("dgb%d" % i, [128, 512], F32) for i in range(2)]
    cq = [S.sb("cq%d" % i, [128, 256], F32) for i in range(2)]
    b_KT = [Buf("KTa%d" % i) for i in range(2)]
    b_Va = [Buf("Va%d" % i) for i in range(2)]
    b_Qa = [Buf("Qa%d" % i) for i in range(2)]
    b_Qm = [[Buf("Qm%d_%d" % (i, l)) for l in range(NLB)] for i in range(2)]
    b_Ko = [Buf("Ko%d" % i) for i in range(2)]
    b_Vo = [Buf("Vo%d" % i) for i in range(2)]
    b_hd = [Buf("hd%d" % i) for i in range(2)]
    cst = S.sb("cst", [128, CST_N], F32)
    b_cst = Buf("cst")
    S.op("sp", lambda: nc.sync.dma_start(out=cst[:], in_=d["cst"][:, :]), writes=[b_cst])

    def C(name):
        o, w = CST_OFF[name]
        return cst[:, o:o + w]
    kb32 = S.sb("kb32", [64, NH * NB], F32)
    kbh = S.sb("kbh", [64, NH * NB], BF16)
    kbl = S.sb("kbl", [64, NH * NB], BF16)
    kbt = S.sb("kbt", [64, NH * NB], F32)
    b_kb = Buf("kbhl")
    S.op("sp", lambda: nc.sync.dma_start(out=kb32[:], in_=d["kbar"].rearrange("d h m -> d (h m)")), writes=[b_kb])
    S.op("dve", lambda: nc.vector.tensor_copy(out=kbh[:], in_=kb32[:]), reads=[b_kb], writes=[b_kb])
    S.op("dve", lambda: nc.vector.tensor_copy(out=kbt[:], in_=kbh[:]), reads=[b_kb], writes=[b_kb])
    S.op("dve", lambda: nc.vector.tensor_tensor(out=kbt[:], in0=kb32[:], in1=kbt[:], op=ALU.subtract), reads=[b_kb], writes=[b_kb])
    S.op("dve", lambda: nc.vector.tensor_copy(out=kbl[:], in_=kbt[:]), reads=[b_kb], writes=[b_kb])
    mbp = [S.sb("mbp%d" % i, [128, 128], F32) for i in range(2)]
    b_mbp = [Buf("mbp%d" % i) for i in range(2)]
    gmt = [S.sb("gmt%d" % i, [128, 64], F32) for i in range(2)]
    b_gmt = [Buf("gmt%d" % i) for i in range(2)]
    t8 = [S.sb("t8_%d" % i, [128, 8], F32) for i in range(2)]
    pt = [S.sb("pt%d" % i, [128, 512], BF16) for i in range(2)]
    b_pt = [Buf("pt%d" % i) for i in range(2)]
    pt2 = S.sb("pt2", [128, 512], BF16)
    b_pt2 = Buf("pt2")
    dtmp = S.sb("dtmp", [128, 512], F32)
    b_dtmp = Buf("dtmp")
    o1 = S.sb("o1", [128, 256], F32)
    o2 = S.sb("o2", [128, 256], F32)
    rd = S.sb("rd", [128, 256], F32)
    ob = [S.sb("ob%d" % i, [128, 256], BF16) for i in range(2)]
    b_o = Buf("o12")
    b_ob = [Buf("ob%d" % i) for i in range(2)]
    for i in range(2):
        S.op("sp", (lambda i=i: nc.sync.dma_start(out=KTa[i][64:128, :], in_=d["ind"][:, :])), writes=[b_KT[i]])
        S.op("dve", (lambda i=i: nc.vector.memset(Va[i][:, :, 64:65], 1.0)), writes=[b_Va[i]])
        S.op("dve", (lambda i=i: nc.vector.memset(Vo[i][:, :, 64:65], 1.0)), writes=[b_Vo[i]])
        S.op("dve", (lambda i=i: nc.vector.memset(mbp[i][:], 0.0)), writes=[b_mbp[i]])
    ps = cx.psum
    bp = cx.b_psum
    scnt = 0
    gcnt = 0
    for h in range(NH):
        cur = h % 2
        sl = SLOPES[h]
        es = math.exp(sl)
        S.op("sp", (lambda h=h, cur=cur: nc.sync.dma_start(out=KTa[cur][0:64, :], in_=d["KT"][h])), writes=[b_KT[cur]])
        for q8 in range(8):
            S.op("sp", (lambda h=h, cur=cur, q8=q8: nc.sync.dma_start(out=Va[cur][:, q8 * 16:(q8 + 1) * 16, 0:64], in_=d["Vd"][h, :, q8 * 16:(q8 + 1) * 16, :])),
                 writes=[b_Va[cur]])
        S.op("sp", (lambda h=h, cur=cur: nc.sync.dma_start(out=Qa[cur][0:64, :], in_=d["QT"][h])), writes=[b_Qa[cur]] + b_Qm[cur])
        S.op("sp", (lambda h=h, cur=cur: nc.sync.dma_start(out=Ko[cur][0:64, :], in_=d["KTo"][h])), writes=[b_Ko[cur]])
        for q2 in range(2):
            S.op("sp", (lambda h=h, cur=cur, q2=q2: nc.sync.dma_start(out=Vo[cur][:, q2 * 16:(q2 + 1) * 16, 0:64], in_=d["Vo"][h, :, q2 * 16:(q2 + 1) * 16, :])),
                 writes=[b_Vo[cur]])
        for (VV, bV) in ((Va[cur], b_Va[cur]), (Vo[cur], b_Vo[cur])):
            Vodd = VV[:].rearrange("p (n two) e -> p n two e", two=2)[:, :, 1, :]
            S.op("dve", (lambda Vodd=Vodd, es=es: nc.vector.tensor_scalar(out=Vodd[:, :, 0:64], in0=Vodd[:, :, 0:64], scalar1=es, scalar2=None, op0=ALU.mult)),
                 reads=[bV], writes=[bV])
            S.op("dve", (lambda Vodd=Vodd, es=es: nc.vector.memset(Vodd[:, :, 64:65], es)), reads=[bV], writes=[bV])
        S.op("dve", (lambda cur=cur, sl=sl: nc.vector.tensor_scalar(out=bT[cur][:], in0=C("base"), scalar1=sl, scalar2=None, op0=ALU.mult)),
             reads=[b_cst], writes=[b_hd[cur]])
        S.op("dve", (lambda cur=cur, sl=sl: nc.vector.scalar_tensor_tensor(out=dgb[cur][:], in0=C("Dm"), scalar=-sl, in1=C("causal"),
                                                                         op0=ALU.mult, op1=ALU.add)), reads=[b_cst], writes=[b_hd[cur]])
        S.op("act", (lambda cur=cur, sl=sl: nc.scalar.activation(out=cq[cur][:], in_=C("irow"), func=AF.Exp, scale=-sl)),
             reads=[b_cst], writes=[b_hd[cur]])
        for l in range(NLB):
            for hq in range(2):
                g = gcnt % 2
                gcnt += 1
                q0 = l * 256 + hq * 128

                def mmg(cur=cur, q0=q0, h=h):
                    nc.tensor.matmul(ps[6][:, 0:64], lhsT=Qa[cur][0:64, q0:q0 + 128], rhs=kbh[:, h * 64:(h + 1) * 64], start=True, stop=False)
                    return nc.tensor.matmul(ps[6][:, 0:64], lhsT=Qa[cur][0:64, q0:q0 + 128], rhs=kbl[:, h * 64:(h + 1) * 64], start=False, stop=True)
                S.op("pe", mmg, reads=[b_Qa[cur], b_kb], writes=[bp[6]])
                pb = C("past")[:, l * 64:(l + 1) * 64]
                S.op("dve", (lambda g=g, pb=pb: nc.vector.tensor_tensor(out=gmt[g][:], in0=ps[6][:, 0:64], in1=pb, op=ALU.add)),
                     reads=[bp[6], b_cst], writes=[b_gmt[g]])
                S.op("dve", (lambda g=g: nc.vector.max(out=t8[g][:], in_=gmt[g][:])), reads=[b_gmt[g]], writes=[b_gmt[g]])
                S.op("dve", (lambda g=g: nc.vector.tensor_scalar(out=gmt[g][:], in0=gmt[g][:], scalar1=t8[g][:, 2:3], scalar2=-NEG,
                                                                 op0=ALU.is_ge, op1=ALU.mult)), reads=[b_gmt[g]], writes=[b_gmt[g]])
                S.op("dve", (lambda g=g, pb=pb: nc.vector.scalar_tensor_tensor(out=mbp[g][:, 64:128], in0=gmt[g][:], scalar=NEG, in1=pb,
                                                                             op0=ALU.add, op1=ALU.add)),
                     reads=[b_gmt[g], b_cst], writes=[b_mbp[g]])
                S.op("pe", (lambda g=g: nc.tensor.transpose(ps[7][:, 0:128], mbp[g][:], C("ident"))), reads=[b_mbp[g], b_cst], writes=[bp[7]])
                S.op("act", (lambda cur=cur, q0=q0: nc.scalar.copy(out=Qa[cur][64:128, q0:q0 + 128], in_=ps[7][64:128, 0:128])),
                     reads=[bp[7]], writes=[b_Qm[cur][l]])
        for i in range(8):
            nk = 8 * i + 7
            for X in (2 * i, 2 * i + 1):
                po = 2 + (X % 2)
                for n in range(nk):
                    sb_ = scnt % 2
                    scnt += 1

                    def mms(cur=cur, X=X, n=n, sb_=sb_):
                        for c in range(2):
                            m = nc.tensor.matmul(ps[sb_][:, c * 256:(c + 1) * 256], lhsT=KTa[cur][:, n * 256 + c * 128:n * 256 + (c + 1) * 128],
                                                 rhs=Qa[cur][:, X * 256:(X + 1) * 256], start=True, stop=True)
                        return m
                    S.op("pe", mms, reads=[b_KT[cur], b_Qa[cur], b_Qm[cur][X]], writes=[bp[sb_]])
                    S.op("act", (lambda cur=cur, X=X, n=n, sb_=sb_: nc.scalar.activation(out=pt[sb_][:], in_=ps[sb_][:], func=AF.Exp,
                                                                                        bias=bT[cur][:, X * 64 + n:X * 64 + n + 1])),
                         reads=[bp[sb_], b_hd[cur]], writes=[b_pt[sb_]])

                    def mmpv(cur=cur, n=n, sb_=sb_, po=po, nk=nk):
                        for c in range(2):
                            m = nc.tensor.matmul(ps[po][0:65, 0:256], lhsT=Va[cur][:, 2 * n + c, :], rhs=pt[sb_][:, c * 256:(c + 1) * 256],
                                                 start=(n == 0 and c == 0), stop=(n == nk - 1 and c == 1))
                        return m
                    S.op("pe", mmpv, reads=[b_Va[cur], b_pt[sb_]], writes=[bp[po]])

                def mmd(cur=cur, X=X):
                    for c in range(2):
                        m = nc.tensor.matmul(ps[4][:, c * 256:(c + 1) * 256], lhsT=Ko[cur][0:64, X * 256 + c * 128:X * 256 + (c + 1) * 128],
                                             rhs=Qa[cur][0:64, X * 256:(X + 1) * 256], start=True, stop=True)
                    return m
                S.op("pe", mmd, reads=[b_Ko[cur], b_Qa[cur]], writes=[bp[4]])
                S.op("dve", (lambda cur=cur: nc.vector.tensor_tensor(out=dtmp[:], in0=ps[4][:], in1=dgb[cur][:], op=ALU.add)),
                     reads=[bp[4], b_hd[cur]], writes=[b_dtmp])
                S.op("act", (lambda: nc.scalar.activation(out=pt2[:], in_=dtmp[:], func=AF.Exp)), reads=[b_dtmp], writes=[b_pt2])

                def mmpo(cur=cur, X=X):
                    for c in range(2):
                        m = nc.tensor.matmul(ps[5][0:65, 0:256], lhsT=Vo[cur][:, 2 * X + c, :], rhs=pt2[:, c * 256:(c + 1) * 256],
                                             start=(c == 0), stop=(c == 1))
                    return m
                S.op("pe", mmpo, reads=[b_Vo[cur], b_pt2], writes=[bp[5]])
                S.op("dve", (lambda cur=cur, po=po: nc.vector.tensor_tensor(out=o1[0:65, :], in0=ps[po][0:65, 0:256], in1=cq[cur][0:65, :], op=ALU.mult)),
                     reads=[bp[po], b_hd[cur]], writes=[b_o])
                S.op("dve", (lambda: nc.vector.tensor_tensor(out=o2[0:65, :], in0=o1[0:65, :], in1=ps[5][0:65, 0:256], op=ALU.add)),
                     reads=[bp[5], b_o], writes=[b_o])
                S.op("pe", (lambda: nc.tensor.matmul(ps[6][0:64, 0:256], lhsT=C("sel65")[0:65, :], rhs=o2[0:65, :], start=True, stop=True)),
                     reads=[b_o, b_cst], writes=[bp[6]])
                S.op("dve", (lambda: nc.vector.reciprocal(out=rd[0:64, :], in_=ps[6][0:64, 0:256])), reads=[bp[6]], writes=[b_o])
                k = X % 2
                S.op("dve", (lambda k=k: nc.vector.tensor_tensor(out=ob[k][0:64, :], in0=o2[0:64, :], in1=rd[0:64, :], op=ALU.mult)),
                     reads=[b_o], writes=[b_ob[k]])
                S.op("sp", (lambda k=k, h=h, X=X: nc.sync.dma_start(out=d["OT"][h, :, X * 256:(X + 1) * 256], in_=ob[k][0:64, :])),
                     reads=[b_ob[k]], writes=[d["b_OT"][X // 2]])


CST_OFF = {}


def _cst_layout():
    off = 0
    for name, w in (("past", 1024), ("base", 1024), ("irow", 256), ("Dm", 512), ("causal", 512), ("ident", 128), ("sel65", 64)):
        CST_OFF[name] = (off, w)
        off += w
    return off


CST_N = _cst_layout()


def make_cst(j):
    blocks = snake_blocks(j)
    c = np.zeros((128, CST_N), np.float32)
    p = np.arange(128, dtype=np.float32)[:, None]
    past = np.zeros((16, 64), np.float32)
    base = np.zeros((128, 16, 64), np.float32)
    for l, own in enumerate(blocks):
        m = np.arange(64)
        past[l] = np.where(m < own, 0.0, NEG)
        dist = np.where(m < own, own - m, 1).astype(np.float32)
        base[:, l, :] = 2.0 * p - 256.0 * dist[None, :]
    o, w = CST_OFF["past"]
    c[:, o:o + w] = np.tile(past.reshape(1, 1024), (128, 1))
    o, w = CST_OFF["base"]
    c[:, o:o + w] = base.reshape(128, 1024)
    o, w = CST_OFF["irow"]
    c[:, o:o + w] = np.arange(256, dtype=np.float32)[None, :]
    i = np.arange(256, dtype=np.float32)[None, None, :]
    pp = np.arange(128, dtype=np.float32)[:, None, None]
    cc = np.arange(2, dtype=np.float32)[None, :, None]
    Dm = np.broadcast_to(i - 2 * pp, (128, 2, 256))
    causal = np.where(i >= 2 * pp + cc, 0.0, NEG)
    o, w = CST_OFF["Dm"]
    c[:, o:o + w] = Dm.reshape(128, 512)
    o, w = CST_OFF["causal"]
    c[:, o:o + w] = causal.reshape(128, 512)
    o, w = CST_OFF["ident"]
    c[:, o:o + w] = np.eye(128, dtype=np.float32)
    o, w = CST_OFF["sel65"]
    c[64, o:o + w] = 1.0
    return c


def emit_proj_res_phase(S, cx, x_in, b_xin, x_out, b_xout, src_d, b_src, w_d, G, bias, b_mod, tag, src_is_heads, ntiles=8):
    nc = S.nc
    w = S.sb("w_" + tag, [128, KC, D], BF16)
    b_w = Buf("w_" + tag)
    for kc in range(KC):
        S.op("pool_dma", (lambda kc=kc: nc.gpsimd.dma_start(out=w[:, kc, :], in_=w_d[kc * 128:(kc + 1) * 128, :])), writes=[b_w])
    sv = src_d.rearrange("h d t -> (h d) t") if src_is_heads else src_d
    sv = sv.rearrange("(c p) t -> p c t", p=128)
    xv_in = x_in.rearrange("(c p) t -> p c t", p=128)
    xv_out = x_out.rearrange("(c p) t -> p c t", p=128)
    for t in range(ntiles):
        xt, bx = load_x_tile(S, cx, xv_in, b_xin, t)
        S.op("sp", (lambda t=t: nc.sync.dma_start(out=cx.h[:], in_=sv[:, :, t * 512:(t + 1) * 512])), reads=[b_src[t]], writes=[cx.b_h])
        for oc in range(KC):
            py = 5 + (oc % 2)

            def mm(oc=oc, py=py):
                for kc in range(KC):
                    m = nc.tensor.matmul(cx.psum[py][:], lhsT=w[:, kc, oc * 128:(oc + 1) * 128], rhs=cx.h[:, kc, :], start=(kc == 0), stop=(kc == KC - 1))
                return m
            S.op("pe", mm, reads=[b_w, cx.b_h], writes=[cx.b_psum[py]])
            if bias is None:
                S.op("dve", (lambda oc=oc, py=py, xt=xt: nc.vector.scalar_tensor_tensor(
                    out=xt[:, oc, :], in0=cx.psum[py][:], scalar=G[:, oc:oc + 1], in1=xt[:, oc, :], op0=ALU.mult, op1=ALU.add)),
                    reads=[cx.b_psum[py], b_mod], writes=[bx])
            else:
                tmp = cx.tmp[oc % 2]
                S.op("act", (lambda oc=oc, py=py, tmp=tmp: nc.scalar.activation(out=tmp[:], in_=cx.psum[py][:], func=AF.Identity, bias=bias[:, oc:oc + 1])),
                     reads=[cx.b_psum[py], cx.b_sp], writes=[cx.b_tmp[oc % 2]])
                S.op("dve", (lambda oc=oc, tmp=tmp, xt=xt: nc.vector.scalar_tensor_tensor(
                    out=xt[:, oc, :], in0=tmp[:], scalar=G[:, oc:oc + 1], in1=xt[:, oc, :], op0=ALU.mult, op1=ALU.add)),
                    reads=[cx.b_tmp[oc % 2], b_mod], writes=[bx])
        S.op("sp", (lambda xt=xt, t=t: nc.sync.dma_start(out=xv_out[:, :, t * 512:(t + 1) * 512], in_=xt[:])), reads=[bx], writes=[b_xout[t]])


def emit_glu_phase(S, cx, x_in, b_xin, w_d, A, Bv, b_mod, u_out, ntiles=8, hmask=None, b_hmask=None, b_uout=None):
    nc = S.nc
    w = S.sb("w_pw1", [128, KC, 2 * D], BF16)
    b_w = Buf("w_pw1")
    for kc in range(KC):
        S.op("pool_dma", (lambda kc=kc: nc.gpsimd.dma_start(out=w[:, kc, :], in_=w_d[kc * 128:(kc + 1) * 128, :])), writes=[b_w])
    uo = [S.sb("uo%d" % i, [128, 512], BF16) for i in range(2)]
    b_uo = [Buf("uo%d" % i) for i in range(2)]
    xv_in = x_in.rearrange("(c p) t -> p c t", p=128)
    uv = u_out.rearrange("(c p) t -> p c t", p=128)
    bp1 = spc(cx, "b_pw1")
    for t in range(ntiles):
        xt, bx = load_x_tile(S, cx, xv_in, b_xin, t)
        emit_norm_mod(S, cx, xt, bx, A, Bv, b_mod)
        for oc in range(KC):
            k = oc % 2
            pa = 1 + k * 2
            pg = pa + 1

            def mma(oc=oc, pa=pa):
                for kc in range(KC):
                    m = nc.tensor.matmul(cx.psum[pa][:], lhsT=w[:, kc, oc * 128:(oc + 1) * 128], rhs=cx.h[:, kc, :], start=(kc == 0), stop=(kc == KC - 1))
                return m

            def mmg(oc=oc, pg=pg):
                for kc in range(KC):
                    m = nc.tensor.matmul(cx.psum[pg][:], lhsT=w[:, kc, D + oc * 128:D + (oc + 1) * 128], rhs=cx.h[:, kc, :], start=(kc == 0), stop=(kc == KC - 1))
                return m
            S.op("pe", mma, reads=[b_w, cx.b_h], writes=[cx.b_psum[pa]])
            S.op("pe", mmg, reads=[b_w, cx.b_h], writes=[cx.b_psum[pg]])
            sa = cx.sa[k]
            S.op("act", (lambda oc=oc, pg=pg, sa=sa: nc.scalar.activation(out=sa[:], in_=cx.psum[pg][:], func=AF.Sigmoid, bias=bp1[:, 8 + oc:9 + oc])),
                 reads=[cx.b_psum[pg], cx.b_sp], writes=[cx.b_sa[k]])
            S.op("dve", (lambda oc=oc, pa=pa, sa=sa, k=k: nc.vector.scalar_tensor_tensor(
                out=uo[k][:], in0=cx.psum[pa][:], scalar=bp1[:, oc:oc + 1], in1=sa[:], op0=ALU.add, op1=ALU.mult)),
                reads=[cx.b_psum[pa], cx.b_sa[k], cx.b_sp], writes=[b_uo[k]])
            if hmask is not None and t == 8:
                S.op("pool", (lambda k=k: nc.gpsimd.tensor_tensor(out=uo[k][:], in0=uo[k][:], in1=hmask, op=ALU.mult)),
                     reads=[b_uo[k], b_hmask], writes=[b_uo[k]])
            S.op("sp", (lambda oc=oc, k=k, t=t: nc.sync.dma_start(out=uv[:, oc, t * 512:(t + 1) * 512], in_=uo[k][:])), reads=[b_uo[k]],
                 writes=([b_uout[t]] if b_uout is not None else []))


def build_L2():
    nc = bass.Bass("TRN2", target_bir_lowering=False)
    din = lambda n, s, dt: nc.dram_tensor(n, s, dt, kind="ExternalInput").ap()
    sp_d = din("sp", [128, SP_N], F32)
    mod_d = din("modi", [128, 144], F32)
    x1 = din("x1", [1024, TOK], F32)
    d = {"KT": din("KTf", [NH, DH, SEQ], BF16), "ind": din("ind", [64, SEQ], BF16), "Vd": din("Vd", [NH, 128, 128, DH], BF16),
         "QT": din("QT", [NH, DH, TOK], BF16), "KTo": din("KTo", [NH, DH, TOK], BF16), "Vo": din("Vo", [NH, 128, 32, DH], BF16),
         "kbar": din("kbarf", [DH, NH, NB], F32), "cst": din("cst", [128, CST_N], F32)}
    w_o = din("w_o", [1024, 1024], F32)
    w_in1 = din("w_in1", [1024, 2 * DFF], F32)
    w_out1 = din("w_out1", [DFF, 1024], F32)
    w_in2 = din("w_in2", [1024, 2 * DFF], F32)
    w_out2 = din("w_out2", [DFF, 1024], F32)
    w_pw1 = din("w_pw1", [1024, 2048], F32)
    OT = nc.dram_tensor("OT", [NH, DH, TOK], BF16).ap()
    xa = nc.dram_tensor("xa", [1024, TOK], F32).ap()
    xb = nc.dram_tensor("xb", [1024, TOK], F32).ap()
    x4 = nc.dram_tensor("x4", [1024, TOK], F32, kind="ExternalOutput").ap()
    uT = nc.dram_tensor("uT", [1024, TOK], BF16, kind="ExternalOutput").ap()
    d["OT"] = OT
    d["b_OT"] = [Buf("OT%d" % t) for t in range(8)]
    S = Sched(nc)
    cx = Ctx()
    setup_common(S, cx)
    emit_load_small(S, cx, sp_d)
    modT = [S.sb("modT%d" % i, [128, 72], F32) for i in range(2)]
    b_modT = [Buf("modT%d" % i) for i in range(2)]
    for i in range(2):
        S.op("sp", (lambda i=i: nc.sync.dma_start(out=modT[i][:], in_=mod_d[:, i * 72:(i + 1) * 72])), writes=[b_modT[i]])
    m01 = emit_mod_vectors_i(S, cx, modT[0], b_modT[0], spc(cx, "ng01"), "l0s1", 1)
    m02 = emit_mod_vectors_i(S, cx, modT[0], b_modT[0], spc(cx, "ng02"), "l0s2", 2)
    m10 = emit_mod_vectors_i(S, cx, modT[1], b_modT[1], spc(cx, "ng10"), "l1s0", 0)
    m11 = emit_mod_vectors_i(S, cx, modT[1], b_modT[1], spc(cx, "ng11"), "l1s1", 1)
    n0 = len(S._stack)
    emit_attn_phase(S, cx, d)
    S.barrier_free(len(S._stack) - n0)
    alloc_tile_bufs(S, cx)
    b_x1 = [Buf("x1_%d" % t) for t in range(8)]
    b_xa = [Buf("xa_%d" % t) for t in range(8)]
    b_xb = [Buf("xb_%d" % t) for t in range(8)]
    b_x4 = [Buf("x4_%d" % t) for t in range(8)]
    n1 = len(S._stack)
    emit_proj_res_phase(S, cx, x1, b_x1, xa, b_xa, OT, d["b_OT"], w_o, m01[2], None, m01[3], "wo", True)
    S.barrier_free(len(S._stack) - n1)
    alloc_ffn_bufs(S, cx)
    emit_ffn_phase(S, cx, xa, b_xa, xb, b_xb, w_in1, w_out1, m02[0], m02[1], m02[2], 0.5, m02[3], 8, "f01")
    emit_ffn_phase(S, cx, xb, b_xb, x4, b_x4, w_in2, w_out2, m10[0], m10[1], m10[2], 0.5, m10[3], 8, "f10")
    S.barrier_free(len(S._stack) - n1)
    emit_glu_phase(S, cx, x4, b_x4, w_pw1, m11[0], m11[1], m11[3], uT)
    S.emit()
    return nc


def emit_conv_phase(S, cx, uext_d, x_in, b_xin, x_out, b_xout, w2_d, ident_d, G, b_mod, u_scr=None, b_u=None):
    nc = S.nc
    w2 = S.sb("w_pw2", [128, KC, D], BF16)
    b_w2 = Buf("w_pw2")
    for kc in range(KC):
        S.op("pool_dma", (lambda kc=kc: nc.gpsimd.dma_start(out=w2[:, kc, :], in_=w2_d[kc * 128:(kc + 1) * 128, :])), writes=[b_w2])
    ident = S.sb("identc", [128, 128], F32)
    b_id = Buf("identc")
    S.op("sp", lambda: nc.sync.dma_start(out=ident[:], in_=ident_d[:, :]), writes=[b_id])
    dg = S.sb("dg", [128, KC * CW, 128], BF16)
    b_dg = Buf("dg")
    wdw = spc(cx, "w_dw")
    for ck in range(KC * CW):
        S.op("dve", (lambda ck=ck: nc.vector.tensor_scalar(out=dg[:, ck, :], in0=ident[:], scalar1=wdw[:, ck:ck + 1], scalar2=None, op0=ALU.mult)),
             reads=[b_id, cx.b_sp], writes=[b_dg])
    ue = [S.sb("ue%d" % i, [128, KC, 288], BF16) for i in range(2)]
    b_ue = [Buf("ue%d" % i) for i in range(2)]
    vs = S.sb("vs", [128, KC, 256], F32)
    b_vs = [Buf("vs%d" % c) for c in range(KC)]
    vb = [S.sb("vb%d" % i, [128, 256], BF16) for i in range(2)]
    b_vb = [Buf("vb%d" % i) for i in range(2)]
    vq = [S.sb("vq%d" % i, [128, 256], BF16) for i in range(2)]
    b_vq = [Buf("vq%d" % i) for i in range(2)]
    mu = S.sb("mu", [128, 256], F32)
    rs = S.sb("rs", [128, 256], F32)
    b_st = Buf("lnstat")
    uv = uext_d.rearrange("(c p) l e -> p c l e", p=128) if uext_d is not None else None
    usv = u_scr.rearrange("(c p) t -> p c t", p=128) if u_scr is not None else None
    xv_in = x_in.rearrange("(c p) t -> p c t", p=128)
    xv_out = x_out.rearrange("(c p) t -> p c t", p=128)
    for l in range(NLB):
        u_ = ue[l % 2]
        if usv is None:
            S.op("sp", (lambda u_=u_, l=l: nc.sync.dma_start(out=u_[:], in_=uv[:, :, l, :])), writes=[b_ue[l % 2]])
        else:
            S.op("sp", (lambda u_=u_, l=l: nc.sync.dma_start(out=u_[:, :, 32:288], in_=usv[:, :, l * 256:(l + 1) * 256])),
                 reads=[b_u[l // 2]], writes=[b_ue[l % 2]])
            S.op("sp", (lambda u_=u_, l=l: nc.sync.dma_start(out=u_[:, :, 0:32], in_=usv[:, :, TOK + l * 32:TOK + (l + 1) * 32])),
                 reads=[b_u[8]], writes=[b_ue[l % 2]])
        xt, bx = load_x_tile(S, cx, xv_in, b_xin, l, n=256)
        for c in range(KC):
            k2 = c % 2
            pc = cx.psum[1 + k2]

            def mmc(c=c, pc=pc, u_=u_):
                for k in range(CW):
                    m = nc.tensor.matmul(pc[:, 0:256], lhsT=dg[:, c * CW + k, :], rhs=u_[:, c, 2 + k:2 + k + 256], start=(k == 0), stop=(k == CW - 1))
                return m
            S.op("pe", mmc, reads=[b_dg, b_ue[l % 2]], writes=[cx.b_psum[1 + k2]])
            S.op("act", (lambda c=c, pc=pc: nc.scalar.activation(out=vs[:, c, :], in_=pc[:, 0:256], func=AF.Identity, bias=spc(cx, "b_dw")[:, c:c + 1])),
                 reads=[cx.b_psum[1 + k2], cx.b_sp], writes=[b_vs[c]])
            S.op("pool", (lambda c=c, k2=k2: nc.gpsimd.tensor_copy(out=vb[k2][:], in_=vs[:, c, :])), reads=[b_vs[c]], writes=[b_vb[k2]])
            S.op("act", (lambda c=c, k2=k2: nc.scalar.activation(out=vq[k2][:], in_=vs[:, c, :], func=AF.Square)), reads=[b_vs[c]], writes=[b_vq[k2]])
            S.op("pe", (lambda c=c, k2=k2: nc.tensor.matmul(cx.psum[3][:, 0:256], lhsT=cx.ones_bf[:], rhs=vb[k2][:], start=(c == 0), stop=(c == KC - 1))),
                 reads=[b_vb[k2], cx.b_ones], writes=[cx.b_psum[3]])
            S.op("pe", (lambda c=c, k2=k2: nc.tensor.matmul(cx.psum[4][:, 0:256], lhsT=cx.ones_bf[:], rhs=vq[k2][:], start=(c == 0), stop=(c == KC - 1))),
                 reads=[b_vq[k2], cx.b_ones], writes=[cx.b_psum[4]])
        S.op("dve", lambda: nc.vector.tensor_scalar(out=mu[:], in0=cx.psum[3][:, 0:256], scalar1=1.0 / D, scalar2=None, op0=ALU.mult),
             reads=[cx.b_psum[3]], writes=[b_st])
        S.op("dve", lambda: nc.vector.tensor_tensor(out=rs[:], in0=mu[:], in1=mu[:], op=ALU.mult), reads=[b_st], writes=[b_st])
        S.op("dve", lambda: nc.vector.scalar_tensor_tensor(out=rs[:], in0=cx.psum[4][:, 0:256], scalar=1.0 / D, in1=rs[:], op0=ALU.mult, op1=ALU.subtract),
             reads=[cx.b_psum[4], b_st], writes=[b_st])
        S.op("dve", lambda: nc.vector.tensor_scalar(out=rs[:], in0=rs[:], scalar1=EPS, scalar2=None, op0=ALU.add), reads=[b_st], writes=[b_st])
        S.op("act", lambda: nc.scalar.activation(out=rs[:], in_=rs[:], func=AF.Sqrt), reads=[b_st], writes=[b_st])
        S.op("dve", lambda: nc.vector.reciprocal(out=rs[:], in_=rs[:]), reads=[b_st], writes=[b_st])
        for c in range(KC):
            tmp = cx.tmp[c % 2]
            S.op("dve", (lambda c=c, tmp=tmp: nc.vector.tensor_tensor(out=tmp[:, 0:256], in0=vs[:, c, :], in1=mu[:], op=ALU.subtract)),
                 reads=[b_vs[c], b_st], writes=[cx.b_tmp[c % 2]])
            S.op("dve", (lambda c=c, tmp=tmp: nc.vector.tensor_tensor(out=tmp[:, 0:256], in0=tmp[:, 0:256], in1=rs[:], op=ALU.mult)),
                 reads=[b_st, cx.b_tmp[c % 2]], writes=[cx.b_tmp[c % 2]])
            S.op("act", (lambda c=c, tmp=tmp: nc.scalar.activation(out=cx.h[:, c, 0:256], in_=tmp[:, 0:256], func=AF.Silu,
                                                                   bias=spc(cx, "ln_b")[:, c:c + 1], scale=spc(cx, "ln_g")[:, c:c + 1])),
                 reads=[cx.b_tmp[c % 2], cx.b_sp], writes=[cx.b_h])
        for oc in range(KC):
            py = 5 + (oc % 2)

            def mm(oc=oc, py=py):
                for kc in range(KC):
                    m = nc.tensor.matmul(cx.psum[py][:, 0:256], lhsT=w2[:, kc, oc * 128:(oc + 1) * 128], rhs=cx.h[:, kc, 0:256], start=(kc == 0), stop=(kc == KC - 1))
                return m
            S.op("pe", mm, reads=[b_w2, cx.b_h], writes=[cx.b_psum[py]])
            sa = cx.sa[oc % 2]
            S.op("act", (lambda oc=oc, py=py, sa=sa: nc.scalar.activation(out=sa[:, 0:256], in_=cx.psum[py][:, 0:256], func=AF.Identity,
                                                                        bias=spc(cx, "b_pw2")[:, oc:oc + 1])),
                 reads=[cx.b_psum[py], cx.b_sp], writes=[cx.b_sa[oc % 2]])
            S.op("dve", (lambda oc=oc, sa=sa, xt=xt: nc.vector.scalar_tensor_tensor(
                out=xt[:, oc, 0:256], in0=sa[:, 0:256], scalar=G[:, oc:oc + 1], in1=xt[:, oc, 0:256], op0=ALU.mult, op1=ALU.add)),
                reads=[cx.b_sa[oc % 2], b_mod], writes=[bx])
        S.op("sp", (lambda xt=xt, l=l: nc.sync.dma_start(out=xv_out[:, :, l * 256:(l + 1) * 256], in_=xt[:, :, 0:256])),
             reads=[bx], writes=[b_xout[l // 2]])


def build_L3():
    nc = bass.Bass("TRN2", target_bir_lowering=False)
    din = lambda n, s, dt: nc.dram_tensor(n, s, dt, kind="ExternalInput").ap()
    sp_d = din("sp", [128, SP_N], F32)
    mod_d = din("modi", [128, 144], F32)
    x4 = din("x4", [1024, TOK], F32)
    uext = din("uext", [1024, NLB, 288], BF16)
    ident_d = din("ident", [128, 128], F32)
    w_pw2 = din("w_pw2", [1024, 1024], F32)
    w_in3 = din("w_in3", [1024, 2 * DFF], F32)
    w_out3 = din("w_out3", [DFF, 1024], F32)
    x5 = nc.dram_tensor("x5", [1024, TOK], F32).ap()
    xo = nc.dram_tensor("xo", [1024, TOK], F32, kind="ExternalOutput").ap()
    S = Sched(nc)
    cx = Ctx()
    setup_common(S, cx)
    emit_load_small(S, cx, sp_d)
    modT = [S.sb("modT%d" % i, [128, 72], F32) for i in range(2)]
    b_modT = [Buf("modT%d" % i) for i in range(2)]
    for i in range(2):
        S.op("sp", (lambda i=i: nc.sync.dma_start(out=modT[i][:], in_=mod_d[:, i * 72:(i + 1) * 72])), writes=[b_modT[i]])
    m11 = emit_mod_vectors_i(S, cx, modT[1], b_modT[1], spc(cx, "ng11"), "l1s1", 1)
    m12 = emit_mod_vectors_i(S, cx, modT[1], b_modT[1], spc(cx, "ng12"), "l1s2", 2)
    alloc_tile_bufs(S, cx)
    b_x4 = [Buf("x4_%d" % t) for t in range(16)]
    b_x5 = [Buf("x5_%d" % t) for t in range(8)]
    b_xo = [Buf("xo_%d" % t) for t in range(8)]
    n1 = len(S._stack)
    emit_conv_phase(S, cx, uext, x4, b_x4, x5, b_x5, w_pw2, ident_d, m11[2], m11[3])
    S.barrier_free(len(S._stack) - n1)
    alloc_ffn_bufs(S, cx)
    emit_ffn_phase(S, cx, x5, b_x5, xo, b_xo, w_in3, w_out3, m12[0], m12[1], m12[2], 0.5, m12[3], 8, "f11")
    S.emit()
    return nc


_NC = {}


def _get(name, fn):
    if name not in _NC:
        _NC[name] = fn()
    return _NC[name]


def _deint(a):
    sh = a.shape
    return np.ascontiguousarray(a.reshape(sh[:-1] + (sh[-1] // 256, 128, 2)).swapaxes(-1, -2)).reshape(sh)


def kernel_unfused(**inp):
    import ml_dtypes
    bf = ml_dtypes.bfloat16
    inp = {k: np.asarray(v) for k, v in inp.items()}
    cores = list(range(8))
    toks = []
    for core in cores:
        blocks = snake_blocks(core % 4)
        toks.append(np.concatenate([np.arange(g * 256, (g + 1) * 256) for g in blocks]))
    sps = [pack_small(inp, c // 4) for c in cores]
    nc1 = _get("L1", build_L1)
    maps = [{"sp": sps[c], "ada_w": inp["ada_w"], "xin": np.ascontiguousarray(inp["x"][c // 4][toks[c]].T),
             "w_in": inp["ffn_w_in"][0, 0], "w_out": inp["ffn_w_out"][0, 0], "wqkv": inp["attn_w_qkv"][0]} for c in cores]
    r1 = run_bass_kernel_spmd(nc1, maps, core_ids=cores).results
    ind = np.zeros((64, SEQ), bf)
    for m in range(64):
        ind[m, m * 256:(m + 1) * 256] = 1
    ident = np.eye(128, dtype=np.float32)
    maps2 = []
    per_batch = {}
    for b in range(2):
        KTf = np.zeros((NH, DH, SEQ), bf)
        Vf = np.zeros((SEQ, D), bf)
        kbf = np.zeros((DH, NH, NB), np.float32)
        for j in range(4):
            c = 4 * b + j
            blocks = snake_blocks(j)
            KT = np.asarray(r1[c]["KT"])
            V = np.asarray(r1[c]["V"])
            kb = np.asarray(r1[c]["kbar"])
            for l, g in enumerate(blocks):
                KTf[:, :, g * 256:(g + 1) * 256] = KT[:, :, l * 256:(l + 1) * 256]
                Vf[g * 256:(g + 1) * 256] = V[l * 256:(l + 1) * 256]
                kbf[:, :, g] = kb[:, :, l].T
        KTf = _deint(KTf)
        Vd = np.ascontiguousarray(Vf.reshape(NB, 128, 2, NH, DH).transpose(3, 1, 0, 2, 4)).reshape(NH, 128, 128, DH)
        per_batch[b] = (KTf, Vd, kbf)
    for c in cores:
        b = c // 4
        KTf, Vd, kbf = per_batch[b]
        KTo = _deint(np.asarray(r1[c]["KT"]))
        V = np.asarray(r1[c]["V"])
        Vo = np.ascontiguousarray(V.reshape(NLB, 128, 2, NH, DH).transpose(3, 1, 0, 2, 4)).reshape(NH, 128, 32, DH)
        maps2.append({"sp": sps[c], "modi": r1[c]["modo"], "x1": r1[c]["x1"], "KTf": KTf, "ind": ind, "Vd": Vd,
                      "QT": r1[c]["QT"], "KTo": KTo, "Vo": Vo, "kbarf": kbf, "cst": make_cst(c % 4),
                      "w_o": inp["attn_w_o"][0], "w_in1": inp["ffn_w_in"][0, 1], "w_out1": inp["ffn_w_out"][0, 1],
                      "w_in2": inp["ffn_w_in"][1, 0], "w_out2": inp["ffn_w_out"][1, 0], "w_pw1": inp["conv_w_pw1"][0]})
    nc2 = _get("L2", build_L2)
    r2 = run_bass_kernel_spmd(nc2, maps2, core_ids=cores).results
    maps3 = []
    for b in range(2):
        Uf = np.zeros((D, SEQ), bf)
        for j in range(4):
            c = 4 * b + j
            Uf[:, toks[c]] = np.asarray(r2[c]["uT"])
        for j in range(4):
            c = 4 * b + j
            ue = np.zeros((D, NLB, 288), bf)
            for l, g in enumerate(snake_blocks(j)):
                ue[:, l, 32:] = Uf[:, g * 256:(g + 1) * 256]
                if g > 0:
                    ue[:, l, :32] = Uf[:, g * 256 - 32:g * 256]
            maps3.append({"sp": sps[c], "modi": r1[c]["modo"], "x4": r2[c]["x4"], "uext": ue, "ident": ident,
                          "w_pw2": inp["conv_w_pw2"][0], "w_in3": inp["ffn_w_in"][1, 1], "w_out3": inp["ffn_w_out"][1, 1]})
    nc3 = _get("L3", build_L3)
    r3 = run_bass_kernel_spmd(nc3, maps3, core_ids=cores).results
    out = np.zeros((BATCH, SEQ, D), np.float32)
    for c in cores:
        out[c // 4][toks[c]] = np.asarray(r3[c]["xo"]).T
    return out


NPOS = 80
NT_ALL = 40
TOKX = TOK + 512


def pos_order(j):
    real = []
    own = snake_blocks(j)
    for i in range(8):
        A, B = 8 * i + j, 8 * i + 7 - j
        real += [A, B] + [g for g in range(8 * i, 8 * i + 8) if g not in (A, B)]
    dup = [(g - 1 if g > 0 else None) for g in own]
    return real, dup


def emit_kvq_phase(S, cx, x_d, b_x, wqkv_d, A, Bv, b_mod, QT_d, KT_d, kbar_d, V_d, x1o_d, b_x1o, b_kv):
    nc = S.nc
    wq = S.sb("wqkv_sb", [128, KC, 3072], BF16)
    b_wq = [Buf("wqkv%d" % k) for k in range(KC)]
    for kc in range(KC):
        S.op("pool_dma", (lambda kc=kc: nc.gpsimd.dma_start(out=wq[:, kc, :], in_=wqkv_d[kc * 128:(kc + 1) * 128, :])), writes=[b_wq[kc]])
    bo = S.sb("blkones", [128, 128], BF16)
    b_bo = Buf("blkones")
    S.op("dve", lambda: nc.vector.memset(bo[:], 0.0), writes=[b_bo])
    S.op("dve", lambda: nc.vector.memset(bo[0:64, 0:64], 1.0), writes=[b_bo])
    S.op("dve", lambda: nc.vector.memset(bo[64:128, 64:128], 1.0), writes=[b_bo])
    g8 = S.sb("g8", [128, 2], F32)
    b_g8 = Buf("g8")
    S.op("dve", lambda: nc.vector.tensor_scalar(out=g8[:, 0:1], in0=spc(cx, "gq"), scalar1=0.125, scalar2=None, op0=ALU.mult),
         reads=[cx.b_sp], writes=[b_g8])
    S.op("dve", lambda: nc.vector.tensor_copy(out=g8[:, 1:2], in_=spc(cx, "gk")), reads=[cx.b_sp], writes=[b_g8])
    qo = [S.sb("qo%d" % i, [128, 512], BF16) for i in range(2)]
    b_qo = [Buf("qo%d" % i) for i in range(2)]
    kb = [S.sb("kb%d" % i, [128, 2], F32) for i in range(2)]
    b_kb = [Buf("kb%d" % i) for i in range(2)]
    vt = [S.sb("vt%d" % i, [128, 1024], BF16) for i in range(2)]
    b_vt = [Buf("vt%d" % i) for i in range(2)]
    xv = x_d.rearrange("(c p) t -> p c t", p=128)
    xov = x1o_d.rearrange("(c p) t -> p c t", p=128)
    QTv = QT_d.rearrange("h d t -> (h d) t")
    KTv = KT_d.rearrange("h d t -> (h d) t")
    cnt = 0
    for t in range(NT_ALL):
        is_own = (t < 32 and t % 4 == 0)
        is_dup = t >= 32
        xt, bx = load_x_tile(S, cx, xv, b_x, t)
        if is_own:
            S.op("sp", (lambda xt=xt, t=t: nc.sync.dma_start(out=xov[:, :, (t // 4) * 512:(t // 4 + 1) * 512], in_=xt[:])),
                 reads=[bx], writes=[b_x1o[t // 4]])
        if is_dup:
            i2 = t - 32
            for k2 in range(2):
                S.op("sp", (lambda xt=xt, i2=i2, k2=k2: nc.sync.dma_start(
                    out=xov[:, :, TOK + (2 * i2 + k2) * 32:TOK + (2 * i2 + k2 + 1) * 32], in_=xt[:, :, k2 * 256 + 224:k2 * 256 + 256])),
                    reads=[bx], writes=[b_x1o[8]])
        emit_norm_mod(S, cx, xt, bx, A, Bv, b_mod)
        items = [(w_, hp) for w_ in range(2) for hp in range(8) if not (w_ == 0 and not (is_own or is_dup))]
        base_cnt = cnt
        cnt += len(items)

        def st_A(ii, base_cnt=base_cnt, items=items):
            which, hp = items[ii]
            k = (base_cnt + ii) % 2
            goff = which * 1024
            pq = cx.psum[1 + k]

            def mm():
                for kc in range(KC):
                    m = nc.tensor.matmul(pq[:], lhsT=wq[:, kc, goff + hp * 128:goff + (hp + 1) * 128], rhs=cx.h[:, kc, :],
                                         start=(kc == 0), stop=(kc == KC - 1))
                return m
            S.op("pe", mm, reads=b_wq + [cx.b_h], writes=[cx.b_psum[1 + k]])
            S.op("act", (lambda: nc.scalar.activation(out=cx.sq[k][:], in_=pq[:], func=AF.Square)),
                 reads=[cx.b_psum[1 + k]], writes=[cx.b_sq[k]])

        def st_B(ii, base_cnt=base_cnt, items=items, t=t, is_own=is_own):
            which, hp = items[ii]
            k = (base_cnt + ii) % 2
            pq = cx.psum[1 + k]
            pss = cx.psum[3 + k]
            S.op("pe", (lambda: nc.tensor.matmul(pss[:], lhsT=bo[:], rhs=cx.sq[k][:], start=True, stop=True)),
                 reads=[cx.b_sq[k], b_bo], writes=[cx.b_psum[3 + k]])
            r1 = cx.tmp[k]
            S.op("dve", (lambda: nc.vector.tensor_scalar(out=r1[:], in0=pss[:], scalar1=1.0 / DH, scalar2=EPS, op0=ALU.mult, op1=ALU.add)),
                 reads=[cx.b_psum[3 + k]], writes=[cx.b_tmp[k]])
            S.op("act", (lambda: nc.scalar.activation(out=r1[:], in_=r1[:], func=AF.Sqrt)), reads=[cx.b_tmp[k]], writes=[cx.b_tmp[k]])
            S.op("dve", (lambda: nc.vector.reciprocal(out=r1[:], in_=r1[:])), reads=[cx.b_tmp[k]], writes=[cx.b_tmp[k]])
            S.op("dve", (lambda: nc.vector.tensor_tensor(out=r1[:], in0=pq[:], in1=r1[:], op=ALU.mult)),
                 reads=[cx.b_tmp[k], cx.b_psum[1 + k]], writes=[cx.b_tmp[k]])
            sa = cx.sa[k]
            S.op("act", (lambda: nc.scalar.activation(out=sa[:], in_=r1[:], func=AF.Copy, scale=g8[:, which:which + 1])),
                 reads=[cx.b_tmp[k], b_g8], writes=[cx.b_sa[k]])
            if which == 0:
                S.op("pool", (lambda: nc.gpsimd.tensor_copy(out=qo[k][:], in_=sa[:])), reads=[cx.b_sa[k]], writes=[b_qo[k]])
                if is_own:
                    S.op("sp", (lambda: nc.sync.dma_start(out=QTv[hp * 128:(hp + 1) * 128, (t // 4) * 512:(t // 4 + 1) * 512], in_=qo[k][:])),
                         reads=[b_qo[k]], writes=[b_kv])
                else:
                    i2 = t - 32
                    for k2 in range(2):
                        S.op("sp", (lambda k2=k2: nc.sync.dma_start(
                            out=QTv[hp * 128:(hp + 1) * 128, TOK + (2 * i2 + k2) * 32:TOK + (2 * i2 + k2 + 1) * 32],
                            in_=qo[k][:, k2 * 256 + 224:k2 * 256 + 256])), reads=[b_qo[k]], writes=[b_kv])
            else:
                S.op("pool", (lambda: nc.gpsimd.tensor_copy(
                    out=qo[k][:].rearrange("q (b c p) -> q b c p", b=2, c=2),
                    in_=sa[:].rearrange("q (b p c) -> q b c p", b=2, c=2))), reads=[cx.b_sa[k]], writes=[b_qo[k]])
                S.op("sp", (lambda: nc.sync.dma_start(out=KTv[hp * 128:(hp + 1) * 128, t * 512:(t + 1) * 512], in_=qo[k][:])),
                     reads=[b_qo[k]], writes=[b_kv])
                S.op("dve", (lambda: nc.vector.tensor_reduce(out=kb[k][:], in_=sa[:].rearrange("p (b t) -> p b t", b=2), axis=AX.X, op=ALU.add)),
                     reads=[cx.b_sa[k]], writes=[b_kb[k]])
                S.op("dve", (lambda: nc.vector.tensor_scalar(out=kb[k][:], in0=kb[k][:], scalar1=1.0 / BLK, scalar2=None, op0=ALU.mult)),
                     reads=[b_kb[k]], writes=[b_kb[k]])
                S.op("sp", (lambda: nc.sync.dma_start(out=kbar_d[hp * 128:(hp + 1) * 128, 2 * t:2 * t + 2], in_=kb[k][:])),
                     reads=[b_kb[k]], writes=[b_kv])
        st_A(0)
        for ii in range(len(items)):
            if ii + 1 < len(items):
                st_A(ii + 1)
            st_B(ii)
        for tg in range(4):
            v = vt[tg % 2]
            for ns in range(2):
                pv = cx.psum[5 + ns]

                def mmv(tg=tg, ns=ns, pv=pv):
                    for kc in range(KC):
                        m = nc.tensor.matmul(pv[:], lhsT=cx.h[:, kc, tg * 128:(tg + 1) * 128], rhs=wq[:, kc, 2048 + ns * 512:2048 + (ns + 1) * 512],
                                             start=(kc == 0), stop=(kc == KC - 1))
                    return m
                S.op("pe", mmv, reads=b_wq + [cx.b_h], writes=[cx.b_psum[5 + ns]])
                if ns == 0:
                    S.op("act", (lambda v=v, pv=pv: nc.scalar.copy(out=v[:, 0:512], in_=pv[:])), reads=[cx.b_psum[5]], writes=[b_vt[tg % 2]])
                else:
                    S.op("dve", (lambda v=v, pv=pv: nc.vector.tensor_copy(out=v[:, 512:1024], in_=pv[:])), reads=[cx.b_psum[6]], writes=[b_vt[tg % 2]])
            blk = 2 * t + tg // 2
            hf = tg % 2
            for c2 in range(2):
                dst = V_d[:, hf * 64:(hf + 1) * 64, 2 * blk + c2, :].rearrange("h pp d -> pp h d")
                S.op("sp", (lambda v=v, dst=dst, c2=c2: nc.sync.dma_start(out=dst, in_=v[c2:128:2, :].rearrange("p (h d) -> p h d", h=NH))),
                     reads=[b_vt[tg % 2]], writes=[b_kv])


CST2_OFF = {}


def _cst2_layout():
    off = 0
    for name, w in (("past", 2048), ("base", 2048), ("irow", 256), ("irowH", 32), ("Dm", 512), ("causal", 512),
                    ("DmH", 64), ("causalH", 64), ("ident", 128), ("sel65", 64)):
        CST2_OFF[name] = (off, w)
        off += w
    return off


CST2_N = _cst2_layout()


def make_cst2(j):
    real, dup = pos_order(j)
    own = snake_blocks(j)
    c = np.zeros((128, CST2_N), np.float32)
    p = np.arange(128, dtype=np.float32)[:, None]
    gpos = np.array(real)
    past = np.zeros((32, 64), np.float32)
    base = np.zeros((128, 32, 64), np.float32)
    for row in range(32):
        o = own[row] if row < 16 else own[row - 16] - 1
        valid = gpos < o
        past[row] = np.where(valid, 0.0, NEG)
        dist = np.where(valid, o - gpos, 1).astype(np.float32)
        base[:, row, :] = 2.0 * p - 256.0 * dist[None, :]

    def put(name, arr):
        o_, w = CST2_OFF[name]
        c[:, o_:o_ + w] = arr.reshape(arr.shape[0], -1)
    put("past", np.tile(past.reshape(1, 2048), (128, 1)))
    put("base", base.reshape(128, 2048))
    put("irow", np.tile(np.arange(256, dtype=np.float32)[None, :], (128, 1)))
    put("irowH", np.tile((224 + np.arange(32, dtype=np.float32))[None, :], (128, 1)))
    pp = np.arange(128, dtype=np.float32)[:, None, None]
    cc = np.arange(2, dtype=np.float32)[None, :, None]
    i = np.arange(256, dtype=np.float32)[None, None, :]
    put("Dm", np.broadcast_to(i - 2 * pp, (128, 2, 256)).copy())
    put("causal", np.where(i >= 2 * pp + cc, 0.0, NEG).astype(np.float32))
    ih = 224 + np.arange(32, dtype=np.float32)[None, None, :]
    put("DmH", np.broadcast_to(ih - 2 * pp, (128, 2, 32)).copy())
    put("causalH", np.where(ih >= 2 * pp + cc, 0.0, NEG).astype(np.float32))
    put("ident", np.eye(128, dtype=np.float32))
    s65 = np.zeros((128, 64), np.float32)
    s65[64] = 1.0
    put("sel65", s65)
    hmask = np.ones((128, 512), np.float32)
    for l in range(16):
        if dup[l] is None:
            hmask[:, l * 32:(l + 1) * 32] = 0.0
    return c, hmask


def emit_attn2(S, cx, d):
    nc = S.nc
    cst = S.sb("cst2", [128, CST2_N], F32)
    b_cst = Buf("cst2")
    S.op("sp", lambda: nc.sync.dma_start(out=cst[:], in_=d["cst"][:, :]), writes=[b_cst])

    def C(name):
        o, w = CST2_OFF[name]
        return cst[:, o:o + w]
    kbh = S.sb("kbh", [64, NH * NB], BF16)
    kbl = S.sb("kbl", [64, NH * NB], BF16)
    b_kb = Buf("kbhl")
    n0 = len(S._stack)
    kb32 = S.sb("kb32", [64, NH * NB], F32)
    kbt = S.sb("kbt", [64, NH * NB], F32)
    S.op("sp", lambda: nc.sync.dma_start(out=kb32[:].rearrange("d (h m) -> d h m", h=NH),
                                         in_=d["kbar"].rearrange("(h d) m -> d h m", h=NH)[:, :, 0:NB]), reads=[d["b_kv"]], writes=[b_kb])
    S.op("dve", lambda: nc.vector.tensor_copy(out=kbh[:], in_=kb32[:]), reads=[b_kb], writes=[b_kb])
    S.op("dve", lambda: nc.vector.tensor_copy(out=kbt[:], in_=kbh[:]), reads=[b_kb], writes=[b_kb])
    S.op("dve", lambda: nc.vector.tensor_tensor(out=kbt[:], in0=kb32[:], in1=kbt[:], op=ALU.subtract), reads=[b_kb], writes=[b_kb])
    S.op("dve", lambda: nc.vector.tensor_copy(out=kbl[:], in_=kbt[:]), reads=[b_kb], writes=[b_kb])
    S.barrier_free(len(S._stack) - n0)
    KTa = [S.sb("KTa%d" % i, [128, SEQ], BF16) for i in range(2)]
    Va = [S.sb("Va%d" % i, [128, 128, 65], BF16) for i in range(2)]
    Qa = [S.sb("Qa%d" % i, [128, TOKX], BF16) for i in range(2)]
    Kd = [S.sb("Kd%d" % i, [128, TOK], BF16) for i in range(2)]
    Vdd = [S.sb("Vdd%d" % i, [128, 32, 65], BF16) for i in range(2)]
    bT = S.sb("bT", [128, 2048], F32)
    dgb = [S.sb("dgb%d" % i, [128, 512], F32) for i in range(2)]
    dgbH = [S.sb("dgbH%d" % i, [128, 64], F32) for i in range(2)]
    cq = [S.sb("cq%d" % i, [128, 288], F32) for i in range(2)]
    b_KT = [Buf("KTa%d" % i) for i in range(2)]
    b_Va = [Buf("Va%d" % i) for i in range(2)]
    b_Qa = [Buf("Qa%d" % i) for i in range(2)]
    b_Qm = [[Buf("Qm%d_%d" % (i, l)) for l in range(32)] for i in range(2)]
    b_Kd = [Buf("Kd%d" % i) for i in range(2)]
    b_Vdd = [Buf("Vdd%d" % i) for i in range(2)]
    b_hd = [Buf("hd%d" % i) for i in range(2)]
    b_bT = Buf("bT")
    mbp = [S.sb("mbp%d" % i, [128, 128], F32) for i in range(2)]
    b_mbp = [Buf("mbp%d" % i) for i in range(2)]
    gmt = [S.sb("gmt%d" % i, [128, 64], F32) for i in range(2)]
    b_gmt = [Buf("gmt%d" % i) for i in range(2)]
    t8 = [S.sb("t8_%d" % i, [128, 8], F32) for i in range(2)]
    pt = [S.sb("pt%d" % i, [128, 512], BF16) for i in range(2)]
    b_pt = [Buf("pt%d" % i) for i in range(2)]
    pt2 = S.sb("pt2", [128, 512], BF16)
    b_pt2 = Buf("pt2")
    dtmp = S.sb("dtmp", [128, 512], F32)
    b_dtmp = Buf("dtmp")
    o1 = S.sb("o1", [128, 256], F32)
    o2 = S.sb("o2", [128, 256], F32)
    rd = S.sb("rd", [128, 256], F32)
    ob = [S.sb("ob%d" % i, [128, 256], BF16) for i in range(2)]
    b_o = Buf("o12")
    b_ob = [Buf("ob%d" % i) for i in range(2)]
    for i in range(2):
        S.op("sp", (lambda i=i: nc.sync.dma_start(out=KTa[i][64:128, :], in_=d["ind"][:, :])), writes=[b_KT[i]])
        S.op("dve", (lambda i=i: nc.vector.memset(Va[i][:, :, 64:65], 1.0)), writes=[b_Va[i]])
        S.op("dve", (lambda i=i: nc.vector.memset(Vdd[i][:, :, 64:65], 1.0)), writes=[b_Vdd[i]])
        S.op("dve", (lambda i=i: nc.vector.memset(mbp[i][:], 0.0)), writes=[b_mbp[i]])
    ps = cx.psum
    bp = cx.b_psum
    scnt = 0
    gcnt = 0
    ocnt = 0
    for h in range(NH):
        cur = h % 2
        sl = SLOPES[h]
        es = math.exp(sl)
        S.op("sp", (lambda h=h, cur=cur: nc.sync.dma_start(out=KTa[cur][0:64, :], in_=d["KT"][h, :, 0:SEQ])), reads=[d["b_kv"]], writes=[b_KT[cur]])
        for q8 in range(8):
            S.op("sp", (lambda h=h, cur=cur, q8=q8: nc.sync.dma_start(out=Va[cur][:, q8 * 16:(q8 + 1) * 16, 0:64], in_=d["Vd"][h, :, q8 * 16:(q8 + 1) * 16, :])),
                 reads=[d["b_kv"]], writes=[b_Va[cur]])
        S.op("sp", (lambda h=h, cur=cur: nc.sync.dma_start(out=Qa[cur][0:64, :], in_=d["QT"][h])), reads=[d["b_kv"]], writes=[b_Qa[cur]] + b_Qm[cur])
        S.op("sp", (lambda h=h, cur=cur: nc.sync.dma_start(out=Kd[cur][0:64, :], in_=d["KT"][h, :, SEQ:SEQ + TOK])), reads=[d["b_kv"]], writes=[b_Kd[cur]])
        for q2 in range(2):
            S.op("sp", (lambda h=h, cur=cur, q2=q2: nc.sync.dma_start(out=Vdd[cur][:, q2 * 16:(q2 + 1) * 16, 0:64],
                                                                     in_=d["Vd"][h, :, 128 + q2 * 16:128 + (q2 + 1) * 16, :])),
                 reads=[d["b_kv"]], writes=[b_Vdd[cur]])
        for (VV, bV) in ((Va[cur], b_Va[cur]), (Vdd[cur], b_Vdd[cur])):
            Vodd = VV[:].rearrange("p (n two) e -> p n two e", two=2)[:, :, 1, :]
            S.op("dve", (lambda Vodd=Vodd, es=es: nc.vector.tensor_scalar(out=Vodd[:, :, 0:64], in0=Vodd[:, :, 0:64], scalar1=es, scalar2=None, op0=ALU.mult)),
                 reads=[bV], writes=[bV])
            S.op("dve", (lambda Vodd=Vodd, es=es: nc.vector.memset(Vodd[:, :, 64:65], es)), reads=[bV], writes=[bV])
        S.op("dve", (lambda sl=sl: nc.vector.tensor_scalar(out=bT[:], in0=C("base"), scalar1=sl, scalar2=None, op0=ALU.mult)),
             reads=[b_cst], writes=[b_bT])
        S.op("dve", (lambda cur=cur, sl=sl: nc.vector.scalar_tensor_tensor(out=dgb[cur][:], in0=C("Dm"), scalar=-sl, in1=C("causal"),
                                                                         op0=ALU.mult, op1=ALU.add)), reads=[b_cst], writes=[b_hd[cur]])
        S.op("dve", (lambda cur=cur, sl=sl: nc.vector.scalar_tensor_tensor(out=dgbH[cur][:], in0=C("DmH"), scalar=-sl, in1=C("causalH"),
                                                                         op0=ALU.mult, op1=ALU.add)), reads=[b_cst], writes=[b_hd[cur]])
        S.op("act", (lambda cur=cur, sl=sl: nc.scalar.activation(out=cq[cur][:, 0:256], in_=C("irow"), func=AF.Exp, scale=-sl)),
             reads=[b_cst], writes=[b_hd[cur]])
        S.op("act", (lambda cur=cur, sl=sl: nc.scalar.activation(out=cq[cur][:, 256:288], in_=C("irowH"), func=AF.Exp, scale=-sl)),
             reads=[b_cst], writes=[b_hd[cur]])
        groups = []
        for l in range(NLB):
            groups.append((l * 256, 128, l, l))
            groups.append((l * 256 + 128, 128, l, l))
        for l in range(NLB):
            groups.append((TOK + l * 32, 32, 16 + l, 16 + l))
        GB = (6, 4)
        TB = (7, 5)

        def g_s1(gi, cur=cur, h=h):
            q0, nr, row, qmi = groups[gi]
            pg = ps[GB[gi % 2]]

            def mmg():
                nc.tensor.matmul(pg[0:nr, 0:64], lhsT=Qa[cur][0:64, q0:q0 + nr], rhs=kbh[:, h * 64:(h + 1) * 64], start=True, stop=False)
                return nc.tensor.matmul(pg[0:nr, 0:64], lhsT=Qa[cur][0:64, q0:q0 + nr], rhs=kbl[:, h * 64:(h + 1) * 64], start=False, stop=True)
            S.op("pe", mmg, reads=[b_Qa[cur], b_kb], writes=[bp[GB[gi % 2]]])

        def g_s2(gi):
            q0, nr, row, qmi = groups[gi]
            g = gi % 2
            pg = ps[GB[g]]
            pb = C("past")[0:nr, row * 64:(row + 1) * 64]
            S.op("dve", (lambda: nc.vector.tensor_tensor(out=gmt[g][0:nr, :], in0=pg[0:nr, 0:64], in1=pb, op=ALU.add)),
                 reads=[bp[GB[g]], b_cst], writes=[b_gmt[g]])
            S.op("dve", (lambda: nc.vector.max(out=t8[g][0:nr, :], in_=gmt[g][0:nr, :])), reads=[b_gmt[g]], writes=[b_gmt[g]])
            S.op("dve", (lambda: nc.vector.tensor_scalar(out=gmt[g][0:nr, :], in0=gmt[g][0:nr, :], scalar1=t8[g][0:nr, 2:3], scalar2=-NEG,
                                                         op0=ALU.is_ge, op1=ALU.mult)), reads=[b_gmt[g]], writes=[b_gmt[g]])
            S.op("dve", (lambda: nc.vector.scalar_tensor_tensor(out=mbp[g][0:nr, 64:128], in0=gmt[g][0:nr, :], scalar=NEG, in1=pb,
                                                                op0=ALU.add, op1=ALU.add)),
                 reads=[b_gmt[g], b_cst], writes=[b_mbp[g]])

        def g_s3(gi, cur=cur):
            q0, nr, row, qmi = groups[gi]
            g = gi % 2
            ptb = ps[TB[g]]
            S.op("pe", (lambda: nc.tensor.transpose(ptb[:, 0:nr], mbp[g][0:nr, :], C("ident")[0:nr, 0:nr])),
                 reads=[b_mbp[g], b_cst], writes=[bp[TB[g]]])
            S.op("act", (lambda: nc.scalar.copy(out=Qa[cur][64:128, q0:q0 + nr], in_=ptb[64:128, 0:nr])),
                 reads=[bp[TB[g]]], writes=[b_Qm[cur][qmi]])
        g_s1(0)
        for gi in range(len(groups)):
            g_s2(gi)
            if gi + 1 < len(groups):
                g_s1(gi + 1)
            g_s3(gi)
        for i in range(8):
            nk = 8 * i + 8
            qbs = []
            for k in range(2):
                l = 2 * i + k
                qbs.append(dict(q0=l * 256, nq=256, row=l, qm=l, K=KTa[cur], koff=(8 * i + k) * 256, V=Va[cur], ch0=2 * (8 * i + k),
                                bK=b_KT[cur], bV=b_Va[cur], dg=dgb[cur], cqv=cq[cur][:, 0:256], ot=l // 2))
            for k in range(2):
                l = 2 * i + k
                qbs.append(dict(q0=TOK + l * 32, nq=32, row=16 + l, qm=16 + l, K=Kd[cur], koff=l * 256, V=Vdd[cur], ch0=2 * l,
                                bK=b_Kd[cur], bV=b_Vdd[cur], dg=dgbH[cur], cqv=cq[cur][:, 256:288], ot=8))
            for qi, qb in enumerate(qbs):
                q0, nq = qb["q0"], qb["nq"]
                po = 2 + (qi % 2)
                sbase = scnt
                scnt += nk

                def emit_S(n, cur=cur, q0=q0, nq=nq, qb=qb, sbase=sbase):
                    sb_ = (sbase + n) % 2

                    def mms():
                        for c in range(2):
                            m = nc.tensor.matmul(ps[sb_][:, c * nq:(c + 1) * nq], lhsT=KTa[cur][:, n * 256 + c * 128:n * 256 + (c + 1) * 128],
                                                 rhs=Qa[cur][:, q0:q0 + nq], start=True, stop=True)
                        return m
                    S.op("pe", mms, reads=[b_KT[cur], b_Qa[cur], b_Qm[cur][qb["qm"]]], writes=[bp[sb_]])

                def emit_E(n, nq=nq, qb=qb, sbase=sbase):
                    sb_ = (sbase + n) % 2
                    row = qb["row"]
                    S.op("act", (lambda: nc.scalar.activation(out=pt[sb_][:, 0:2 * nq], in_=ps[sb_][:, 0:2 * nq], func=AF.Exp,
                                                              bias=bT[:, row * 64 + n:row * 64 + n + 1])),
                         reads=[bp[sb_], b_bT], writes=[b_pt[sb_]])

                def emit_PV(n, cur=cur, nq=nq, po=po, nk=nk, sbase=sbase):
                    sb_ = (sbase + n) % 2

                    def mmpv():
                        for c in range(2):
                            m = nc.tensor.matmul(ps[po][0:65, 0:nq], lhsT=Va[cur][:, 2 * n + c, :], rhs=pt[sb_][:, c * nq:(c + 1) * nq],
                                                 start=(n == 0 and c == 0), stop=(n == nk - 1 and c == 1))
                        return m
                    S.op("pe", mmpv, reads=[b_Va[cur], b_pt[sb_]], writes=[bp[po]])
                emit_S(0)
                for n in range(nk):
                    emit_E(n)
                    if n + 1 < nk:
                        emit_S(n + 1)
                    emit_PV(n)

                def mmd(qb=qb, cur=cur, q0=q0, nq=nq):
                    for c in range(2):
                        m = nc.tensor.matmul(ps[4][:, c * nq:(c + 1) * nq], lhsT=qb["K"][0:64, qb["koff"] + c * 128:qb["koff"] + (c + 1) * 128],
                                             rhs=Qa[cur][0:64, q0:q0 + nq], start=True, stop=True)
                    return m
                S.op("pe", mmd, reads=[qb["bK"], b_Qa[cur]], writes=[bp[4]])
                S.op("dve", (lambda qb=qb, nq=nq: nc.vector.tensor_tensor(out=dtmp[:, 0:2 * nq], in0=ps[4][:, 0:2 * nq], in1=qb["dg"][:, 0:2 * nq], op=ALU.add)),
                     reads=[bp[4], b_hd[cur]], writes=[b_dtmp])
                S.op("act", (lambda nq=nq: nc.scalar.activation(out=pt2[:, 0:2 * nq], in_=dtmp[:, 0:2 * nq], func=AF.Exp)), reads=[b_dtmp], writes=[b_pt2])

                def mmpo(qb=qb, nq=nq):
                    for c in range(2):
                        m = nc.tensor.matmul(ps[5][0:65, 0:nq], lhsT=qb["V"][:, qb["ch0"] + c, :], rhs=pt2[:, c * nq:(c + 1) * nq],
                                             start=(c == 0), stop=(c == 1))
                    return m
                S.op("pe", mmpo, reads=[qb["bV"], b_pt2], writes=[bp[5]])
                S.op("dve", (lambda qb=qb, po=po, nq=nq: nc.vector.tensor_tensor(out=o1[0:65, 0:nq], in0=ps[po][0:65, 0:nq], in1=qb["cqv"][0:65, :], op=ALU.mult)),
                     reads=[bp[po], b_hd[cur]], writes=[b_o])
                S.op("dve", (lambda nq=nq: nc.vector.tensor_tensor(out=o2[0:65, 0:nq], in0=o1[0:65, 0:nq], in1=ps[5][0:65, 0:nq], op=ALU.add)),
                     reads=[bp[5], b_o], writes=[b_o])
                S.op("pe", (lambda nq=nq: nc.tensor.matmul(ps[6][0:64, 0:nq], lhsT=C("sel65")[0:65, :], rhs=o2[0:65, 0:nq], start=True, stop=True)),
                     reads=[b_o, b_cst], writes=[bp[6]])
                S.op("dve", (lambda nq=nq: nc.vector.reciprocal(out=rd[0:64, 0:nq], in_=ps[6][0:64, 0:nq])), reads=[bp[6]], writes=[b_o])
                k = ocnt % 2
                ocnt += 1
                S.op("dve", (lambda k=k, nq=nq: nc.vector.tensor_tensor(out=ob[k][0:64, 0:nq], in0=o2[0:64, 0:nq], in1=rd[0:64, 0:nq], op=ALU.mult)),
                     reads=[b_o], writes=[b_ob[k]])
                S.op("sp", (lambda k=k, h=h, q0=q0, nq=nq: nc.sync.dma_start(out=d["OT"][h, :, q0:q0 + nq], in_=ob[k][0:64, 0:nq])),
                     reads=[b_ob[k]], writes=[d["b_OT"][qb["ot"]]])


def build_fused():
    nc = bass.Bass("TRN2", target_bir_lowering=False)
    din = lambda n, s, dt: nc.dram_tensor(n, s, dt, kind="ExternalInput").ap()
    scr = lambda n, s, dt: nc.dram_tensor(n, s, dt).ap()
    sp_d = din("sp", [128, SP_N], F32)
    ada_w = din("ada_w", [2, 1024, 9216], F32)
    xall = din("xall", [1024, NT_ALL * 512], F32)
    w_in = [din("w_in%d" % i, [1024, 2 * DFF], F32) for i in range(4)]
    w_out = [din("w_out%d" % i, [DFF, 1024], F32) for i in range(4)]
    wqkv = din("wqkv", [1024, 3072], F32)
    w_o = din("w_o", [1024, 1024], F32)
    w_pw1 = din("w_pw1", [1024, 2048], F32)
    w_pw2 = din("w_pw2", [1024, 1024], F32)
    ind_d = din("ind", [64, SEQ], BF16)
    cst_d = din("cst", [128, CST2_N], F32)
    hm_d = din("hmask", [128, 512], F32)
    ident_d = din("ident", [128, 128], F32)
    xo = nc.dram_tensor("xo", [1024, TOK], F32, kind="ExternalOutput").ap()
    x1all = scr("x1all", [1024, NT_ALL * 512], F32)
    x1o = scr("x1o", [1024, TOKX], F32)
    xa = scr("xa", [1024, TOKX], F32)
    xb = scr("xb", [1024, TOKX], F32)
    x4 = scr("x4", [1024, TOKX], F32)
    x5 = scr("x5", [1024, TOK], F32)
    uT = scr("uT", [1024, TOKX], BF16)
    KT = scr("KTs", [NH, DH, NPOS * 256], BF16)
    Vd = scr("Vds", [NH, 128, 2 * NPOS, DH], BF16)
    kbar = scr("kbars", [NH * DH, NPOS], F32)
    QT = scr("QTs", [NH, DH, TOKX], BF16)
    OT = scr("OTs", [NH, DH, TOKX], BF16)
    S = Sched(nc)
    cx = Ctx()
    setup_common(S, cx)
    emit_load_small(S, cx, sp_d)
    hmask = S.sb("hmask", [128, 512], F32)
    b_hm = Buf("hmask")
    S.op("sp", lambda: nc.sync.dma_start(out=hmask[:], in_=hm_d[:, :]), writes=[b_hm])
    modT = [S.sb("modT%d" % i, [128, 72], F32) for i in range(2)]
    b_modT = [Buf("modT%d" % i) for i in range(2)]
    emit_adaln_scoped(S, cx, ada_w, modT, b_modT)
    M = {}
    for i in range(2):
        for j in range(3):
            M[(i, j)] = emit_mod_vectors_i(S, cx, modT[i], b_modT[i], spc(cx, "ng%d%d" % (i, j)), "l%ds%d" % (i, j), j)
    nbase = len(S._stack)
    mk = lambda name, n: [Buf("%s_%d" % (name, t)) for t in range(n)]
    b_xall, b_x1all, b_x1o = mk("xall", NT_ALL), mk("x1all", NT_ALL), mk("x1o", 9)
    b_xa, b_xb, b_x4, b_u, b_x5, b_xo = mk("xa", 9), mk("xb", 9), mk("x4", 16), mk("u", 9), mk("x5", 8), mk("xo", 8)
    b_x4t = b_x4[:9]
    alloc_tile_bufs(S, cx)
    ntile = len(S._stack)
    alloc_ffn_bufs(S, cx)
    m = M[(0, 0)]
    emit_ffn_phase(S, cx, xall, b_xall, x1all, b_x1all, w_in[0], w_out[0], m[0], m[1], m[2], 0.5, m[3], NT_ALL, "f00")
    S.barrier_free(len(S._stack) - ntile)
    m = M[(0, 1)]
    b_kv = Buf("kvq")
    emit_kvq_phase(S, cx, x1all, b_x1all, wqkv, m[0], m[1], m[3], QT, KT, kbar, Vd, x1o, b_x1o, b_kv)
    S.barrier_free(len(S._stack) - nbase)
    b_OT = mk("OT", 9)
    emit_attn2(S, cx, {"KT": KT, "ind": ind_d, "Vd": Vd, "QT": QT, "kbar": kbar, "cst": cst_d, "OT": OT, "b_OT": b_OT, "b_kv": b_kv})
    S.barrier_free(len(S._stack) - nbase)
    alloc_tile_bufs(S, cx)
    emit_proj_res_phase(S, cx, x1o, b_x1o, xa, b_xa, OT, b_OT, w_o, m[2], None, m[3], "wo", True, ntiles=9)
    S.barrier_free(len(S._stack) - ntile)
    alloc_ffn_bufs(S, cx)
    m = M[(0, 2)]
    emit_ffn_phase(S, cx, xa, b_xa, xb, b_xb, w_in[1], w_out[1], m[0], m[1], m[2], 0.5, m[3], 9, "f01")
    m = M[(1, 0)]
    emit_ffn_phase(S, cx, xb, b_xb, x4, b_x4t, w_in[2], w_out[2], m[0], m[1], m[2], 0.5, m[3], 9, "f10")
    S.barrier_free(len(S._stack) - ntile)
    m = M[(1, 1)]
    emit_glu_phase(S, cx, x4, b_x4t, w_pw1, m[0], m[1], m[3], uT, ntiles=9, hmask=hmask[:], b_hmask=b_hm, b_uout=b_u)
    S.barrier_free(len(S._stack) - ntile)
    b_x4c = [Buf("x4c_%d" % t) for t in range(16)]
    emit_conv_phase(S, cx, None, x4, b_x4c, x5, b_x5, w_pw2, ident_d, m[2], m[3], u_scr=uT, b_u=b_u)
    S.barrier_free(len(S._stack) - ntile)
    alloc_ffn_bufs(S, cx)
    m = M[(1, 2)]
    emit_ffn_phase(S, cx, x5, b_x5, xo, b_xo, w_in[3], w_out[3], m[0], m[1], m[2], 0.5, m[3], 8, "f11")
    S.emit()
    return nc


def kernel(**inp):
    import ml_dtypes
    bf = ml_dtypes.bfloat16
    inp = {k: np.asarray(v) for k, v in inp.items()}
    cores = list(range(8))
    ind = np.zeros((64, SEQ), bf)
    for m in range(64):
        ind[m, m * 256:(m + 1) * 256] = 1
    ident = np.eye(128, dtype=np.float32)
    maps = []
    toks = []
    for c in cores:
        b, j = c // 4, c % 4
        real, dup = pos_order(j)
        xb_ = inp["x"][b]
        xall = np.zeros((NPOS * 256, D), np.float32)
        for pi, g in enumerate(real + dup):
            if g is not None:
                xall[pi * 256:(pi + 1) * 256] = xb_[g * 256:(g + 1) * 256]
        cst, hmask = make_cst2(j)
        toks.append(np.concatenate([np.arange(g * 256, (g + 1) * 256) for g in snake_blocks(j)]))
        mp = {"sp": pack_small(inp, b), "ada_w": inp["ada_w"], "xall": np.ascontiguousarray(xall.T),
              "wqkv": inp["attn_w_qkv"][0], "w_o": inp["attn_w_o"][0], "w_pw1": inp["conv_w_pw1"][0], "w_pw2": inp["conv_w_pw2"][0],
              "ind": ind, "cst": cst, "hmask": hmask, "ident": ident}
        for k, (i, w) in enumerate(((0, 0), (0, 1), (1, 0), (1, 1))):
            mp["w_in%d" % k] = inp["ffn_w_in"][i, w]
            mp["w_out%d" % k] = inp["ffn_w_out"][i, w]
        maps.append(mp)
    nc = _get("fused", build_fused)
    res = run_bass_kernel_spmd(nc, maps, core_ids=cores).results
    out = np.zeros((BATCH, SEQ, D), np.float32)
    for c in cores:
        out[c // 4][toks[c]] = np.asarray(res[c]["xo"]).T
    return out
```

```python
import math
import numpy as np
import concourse.bass as bass
import concourse.mybir as mybir
from concourse.bass_utils import run_bass_kernel_spmd

F32 = mybir.dt.float32
BF16 = mybir.dt.bfloat16
AF = mybir.ActivationFunctionType
ALU = mybir.AluOpType
AX = mybir.AxisListType

D = 1024
KC = 8
DFF = 2816
FC = 22
NH = 16
DH = 64
BLK = 256
NB = 64
SEQ = 16384
BATCH = 2
TOK = 4096
NLB = 16
EPS = 1e-6
NEG = -30000.0
CW = 31


class Buf:
    __slots__ = ("name", "last_w", "readers")

    def __init__(self, name):
        self.name = name
        self.last_w = None
        self.readers = []


class Sched:
    DMA_ENGS = ("sp", "pool_dma")

    def __init__(self, nc, n_dma_sems=12):
        self.nc = nc
        self.ops = []
        self.engs = {"pe": nc.tensor, "act": nc.scalar, "dve": nc.vector,
                     "pool": nc.gpsimd, "sp": nc.sync, "pool_dma": nc.gpsimd}
        self.stream = {"pe": "pe", "act": "act", "dve": "dve", "pool": "pool",
                       "sp": "sp", "pool_dma": "pool"}
        self.n_dma_sems = n_dma_sems
        self._stack = []
        self.bar_list = []
        cm = nc.sbuf_tensor("bar_t", [128, 8], F32)
        self.bar_t = cm.__enter__()

    def sb(self, name, shape, dt):
        self._uid = getattr(self, "_uid", 0) + 1
        cm = self.nc.sbuf_tensor("s%d_%s" % (self._uid, name), shape, dt)
        t = cm.__enter__()
        self._stack.append(cm)
        return t

    def ps(self, name, shape, dt=F32):
        cm = self.nc.psum_tensor("p_" + name, shape, dt)
        t = cm.__enter__()
        self._stack.append(cm)
        return t

    def barrier_free(self, n_free):
        nc = self.nc
        bb = Buf("barrier%d" % len(self.ops))
        prev = list(range(len(self.ops)))
        i0 = self.op("pool", lambda: nc.gpsimd.memset(self.bar_t[:, 0:1], 0.0), writes=[bb])
        self.ops[i0]["deps"] = set(prev)
        for e, fn in (("dve", lambda: nc.vector.memset(self.bar_t[:, 1:2], 0.0)),
                      ("act", lambda: nc.scalar.activation(out=self.bar_t[:, 2:3], in_=self.bar_t[:, 0:1], func=AF.Copy)),
                      ("pe", None),
                      ("sp", lambda: nc.sync.dma_start(out=self.bar_t[:, 4:5], in_=self.bar_t[:, 0:1]))):
            if fn is None:
                continue
            self.op(e, fn, reads=[bb])
        self.bar_after = i0
        self.bar_list.append(i0)
        for _ in range(n_free):
            cm = self._stack.pop()
            cm.__exit__(None, None, None)

    def op(self, eng, fn, reads=(), writes=()):
        deps = set()
        idx = len(self.ops)
        for b in reads:
            if b.last_w is not None:
                deps.add(b.last_w)
        for b in writes:
            if b.last_w is not None:
                deps.add(b.last_w)
            for r in b.readers:
                deps.add(r)
        for b in reads:
            b.readers.append(idx)
        for b in writes:
            b.last_w = idx
            b.readers = []
        deps.discard(idx)
        if self.bar_list:
            deps.add(self.bar_list[-1])
        self.ops.append({"eng": eng, "fn": fn, "deps": deps, "dma": eng in self.DMA_ENGS})
        return idx

    def emit(self):
        nc = self.nc
        ops = self.ops
        n = len(ops)
        need = [False] * n
        for i, o in enumerate(ops):
            if o["dma"]:
                need[i] = True
            for d in o["deps"]:
                if ops[d]["dma"]:
                    continue
                sd, si = self.stream[ops[d]["eng"]], self.stream[o["eng"]]
                if sd != si or sd != "pe":
                    need[d] = True
        sem_cm = []

        def mksem(name):
            cm = nc.semaphore(name)
            s = cm.__enter__()
            sem_cm.append(cm)
            return s

        eng_sem = {k: mksem("s_" + k) for k in ("pe", "act", "dve", "pool")}
        eng_cnt = {k: 0 for k in eng_sem}
        dma_sems = {q: [mksem("d_%s_%d" % (q, i)) for i in range(self.n_dma_sems)] for q in self.DMA_ENGS}
        dma_cnt = {q: [0] * self.n_dma_sems for q in self.DMA_ENGS}
        dma_last = {q: [None] * self.n_dma_sems for q in self.DMA_ENGS}
        dma_rr = {q: 0 for q in self.DMA_ENGS}
        ev = [None] * n
        waited = {s: {} for s in ("pe", "act", "dve", "pool", "sp")}
        for i, o in enumerate(ops):
            eng = o["eng"]
            st = self.stream[eng]
            e = self.engs[eng]
            waits = {}
            for d in o["deps"]:
                if ev[d] is None:
                    continue
                sem, val, key = ev[d]
                if key not in waits or waits[key][1] < val:
                    waits[key] = (sem, val)
            my = None
            if o["dma"]:
                k = dma_rr[eng]
                dma_rr[eng] = (k + 1) % self.n_dma_sems
                prev = dma_last[eng][k]
                if prev is not None:
                    sem, val, key = ev[prev]
                    if key not in waits or waits[key][1] < val:
                        waits[key] = (sem, val)
                dma_cnt[eng][k] += 16
                my = (dma_sems[eng][k], dma_cnt[eng][k], (eng, k))
                dma_last[eng][k] = i
            elif need[i]:
                eng_cnt[st] += 1
                my = (eng_sem[st], eng_cnt[st], st)
            for key, (sem, val) in waits.items():
                if waited[st].get(key, 0) >= val:
                    continue
                e.wait_ge(sem, val)
                waited[st][key] = val
            inst = o["fn"]()
            if my is not None:
                inst.then_inc(my[0], 16 if o["dma"] else 1)
                ev[i] = my
            o["fn"] = None
        sp = nc.sync
        for q in self.DMA_ENGS:
            for k in range(self.n_dma_sems):
                if dma_cnt[q][k] > 0:
                    sp.wait_ge(dma_sems[q][k], dma_cnt[q][k])
        for k in eng_sem:
            if eng_cnt[k] > 0:
                sp.wait_ge(eng_sem[k], eng_cnt[k])
        self.max_counts = dict(eng_cnt)


def snake_blocks(j):
    out = []
    for i in range(8):
        out.append(8 * i + j)
        out.append(8 * i + 7 - j)
    return out


class Ctx:
    pass


def setup_common(S, cx):
    nc = S.nc
    cx.ones_bf = S.sb("ones_bf", [128, 128], BF16)
    cx.b_ones = Buf("ones")
    S.op("dve", lambda: nc.vector.memset(cx.ones_bf[:], 1.0), writes=[cx.b_ones])
    cx.psall = S.ps("psall", [128, 8 * 512], F32)
    cx.psum = [cx.psall[:, i * 512:(i + 1) * 512] for i in range(8)]
    cx.b_psum = [Buf("psb%d" % i) for i in range(8)]


def emit_mod_vectors(S, cx, modT, normg, layer_slot, j):
    nc = S.nc
    A = S.sb("modA_%s" % layer_slot, [128, 8], F32)
    b = Buf("modA_%s" % layer_slot)
    base = j * 24
    S.op("dve", lambda: nc.vector.scalar_tensor_tensor(
        out=A[:], in0=modT[:, base + 8:base + 16], scalar=1.0, in1=normg,
        op0=ALU.add, op1=ALU.mult), reads=[cx.b_mod], writes=[b])
    return A, modT[:, base:base + 8], modT[:, base + 16:base + 24], b


def emit_ffn_phase(S, cx, x_in, b_xin, x_out, b_xout, w_in_d, w_out_d, A, Bv, G, gscale, b_mod, ntiles, tag):
    nc = S.nc
    win = cx.win
    wout = cx.wout
    HF = FC // 2
    for kc in range(KC):
        S.op("pool_dma", (lambda kc=kc: nc.gpsimd.dma_start(out=win[:, kc, :], in_=w_in_d[kc * 128:(kc + 1) * 128, :])),
             writes=[cx.b_win[kc]])
    for f in range(FC):
        S.op("pool_dma", (lambda f=f: nc.gpsimd.dma_start(out=wout[:, f, :], in_=w_out_d[f * 128:(f + 1) * 128, :])),
             writes=[cx.b_wout[f]])
    Gs = S.sb("Gs_" + tag, [128, 8], F32)
    b_Gs = Buf("Gs_" + tag)
    S.op("dve", lambda: nc.vector.tensor_scalar(out=Gs[:], in0=G, scalar1=float(gscale), scalar2=None, op0=ALU.mult),
         reads=[b_mod], writes=[b_Gs])
    xv_in = x_in.rearrange("(c p) t -> p c t", p=128)
    xv_out = x_out.rearrange("(c p) t -> p c t", p=128)
    def _ld(t):
        S.op("sp", (lambda: nc.sync.dma_start(out=cx.xt[t % 2][:], in_=xv_in[:, :, t * 512:(t + 1) * 512])),
             reads=[b_xin[t]], writes=[cx.b_xt[t % 2]])
    _ld(0)
    for t in range(ntiles):
        xt = cx.xt[t % 2]
        bx = cx.b_xt[t % 2]
        emit_norm_mod(S, cx, xt, bx, A, Bv, b_mod)
        if t + 1 < ntiles:
            _ld(t + 1)
        for hf in range(2):
            for fl in range(HF):
                f = hf * HF + fl
                pa = 1 + (f % 2) * 2
                pb = pa + 1

                def mm_a(f=f, pa=pa):
                    for kc in range(KC):
                        m = nc.tensor.matmul(cx.psum[pa][:], lhsT=win[:, kc, f * 128:(f + 1) * 128], rhs=cx.h[:, kc, :],
                                             start=(kc == 0), stop=(kc == KC - 1))
                    return m

                def mm_b(f=f, pb=pb):
                    for kc in range(KC):
                        m = nc.tensor.matmul(cx.psum[pb][:], lhsT=win[:, kc, DFF + f * 128:DFF + (f + 1) * 128],
                                             rhs=cx.h[:, kc, :], start=(kc == 0), stop=(kc == KC - 1))
                    return m
                S.op("pe", mm_a, reads=cx.b_win + [cx.b_h], writes=[cx.b_psum[pa]])
                S.op("pe", mm_b, reads=cx.b_win + [cx.b_h], writes=[cx.b_psum[pb]])
                sa = cx.sa[f % 2]
                S.op("act", (lambda sa=sa, pa=pa: nc.scalar.activation(out=sa[:], in_=cx.psum[pa][:], func=AF.Silu)),
                     reads=[cx.b_psum[pa]], writes=[cx.b_sa[f % 2]])
                S.op("dve", (lambda sa=sa, pb=pb, fl=fl: nc.vector.tensor_tensor(out=cx.u[:, fl, :], in0=sa[:], in1=cx.psum[pb][:], op=ALU.mult)),
                     reads=[cx.b_sa[f % 2], cx.b_psum[pb]], writes=[cx.b_u[fl]])
            for oc in range(KC):
                py = 5 + (oc % 2)

                def mm_y(oc=oc, py=py, hf=hf):
                    for fl in range(HF):
                        m = nc.tensor.matmul(cx.psum[py][:], lhsT=wout[:, hf * HF + fl, oc * 128:(oc + 1) * 128], rhs=cx.u[:, fl, :],
                                             start=(fl == 0), stop=(fl == HF - 1))
                    return m
                S.op("pe", mm_y, reads=cx.b_wout + cx.b_u, writes=[cx.b_psum[py]])
                S.op("dve", (lambda oc=oc, py=py, xt=xt: nc.vector.scalar_tensor_tensor(
                    out=xt[:, oc, :], in0=cx.psum[py][:], scalar=Gs[:, oc:oc + 1], in1=xt[:, oc, :],
                    op0=ALU.mult, op1=ALU.add)), reads=[cx.b_psum[py], b_Gs], writes=[bx])
        S.op("sp", (lambda xt=xt, t=t: nc.sync.dma_start(out=xv_out[:, :, t * 512:(t + 1) * 512], in_=xt[:])),
             reads=[bx], writes=[b_xout[t]])


def emit_norm_mod(S, cx, xt, bx, A, Bv, b_mod, n=512):
    nc = S.nc
    for c in range(KC):
        S.op("act", (lambda c=c: nc.scalar.activation(out=cx.sq[c % 2][:, :n], in_=xt[:, c, :n], func=AF.Square)),
             reads=[bx], writes=[cx.b_sq[c % 2]])
        S.op("pe", (lambda c=c: nc.tensor.matmul(cx.psum[0][:, :n], lhsT=cx.ones_bf[:], rhs=cx.sq[c % 2][:, :n],
                                                 start=(c == 0), stop=(c == KC - 1))),
             reads=[cx.b_sq[c % 2], cx.b_ones], writes=[cx.b_psum[0]])
    S.op("dve", lambda: nc.vector.tensor_scalar(out=cx.rstd[:, :n], in0=cx.psum[0][:, :n], scalar1=1.0 / D, scalar2=EPS,
                                                op0=ALU.mult, op1=ALU.add), reads=[cx.b_psum[0]], writes=[cx.b_rstd])
    S.op("act", lambda: nc.scalar.activation(out=cx.rstd[:, :n], in_=cx.rstd[:, :n], func=AF.Sqrt), reads=[cx.b_rstd], writes=[cx.b_rstd])
    S.op("dve", lambda: nc.vector.reciprocal(out=cx.rstd[:, :n], in_=cx.rstd[:, :n]), reads=[cx.b_rstd], writes=[cx.b_rstd])
    for c in range(KC):
        tmp = cx.tmp[c % 2]
        S.op("dve", (lambda c=c, tmp=tmp: nc.vector.tensor_tensor(out=tmp[:, :n], in0=xt[:, c, :n], in1=cx.rstd[:, :n], op=ALU.mult)),
             reads=[bx, cx.b_rstd], writes=[cx.b_tmp[c % 2]])
        S.op("act", (lambda c=c, tmp=tmp: nc.scalar.activation(out=cx.h[:, c, :n], in_=tmp[:, :n], func=AF.Identity,
                                                               bias=Bv[:, c:c + 1], scale=A[:, c:c + 1])),
             reads=[cx.b_tmp[c % 2], b_mod], writes=[cx.b_h])


def alloc_tile_bufs(S, cx):
    cx.xt = [S.sb("xt%d" % i, [128, KC, 512], F32) for i in range(2)]
    cx.b_xt = [Buf("xt%d" % i) for i in range(2)]
    cx.sq = [S.sb("sq%d" % i, [128, 512], BF16) for i in range(2)]
    cx.b_sq = [Buf("sq%d" % i) for i in range(2)]
    cx.h = S.sb("h", [128, KC, 512], BF16)
    cx.b_h = Buf("h")
    cx.rstd = S.sb("rstd", [128, 512], F32)
    cx.b_rstd = Buf("rstd")
    cx.tmp = [S.sb("tmp%d" % i, [128, 512], F32) for i in range(2)]
    cx.b_tmp = [Buf("tmp%d" % i) for i in range(2)]
    cx.sa = [S.sb("sa%d" % i, [128, 512], F32) for i in range(2)]
    cx.b_sa = [Buf("sa%d" % i) for i in range(2)]


def alloc_ffn_bufs(S, cx):
    cx.win = S.sb("win", [128, KC, 2 * DFF], BF16)
    cx.wout = S.sb("wout", [128, FC, D], BF16)
    cx.b_win = [Buf("win%d" % i) for i in range(KC)]
    cx.b_wout = [Buf("wout%d" % i) for i in range(FC)]
    cx.u = S.sb("u", [128, FC // 2, 512], BF16)
    cx.b_u = [Buf("u%d" % i) for i in range(FC // 2)]


def _fm(v, n=None):
    v = np.asarray(v, np.float32).reshape(-1, 128)
    return np.ascontiguousarray(v.T)


SP_OFF = {}


def _sp_layout():
    off = 0
    for name, w in (("c", 8), ("ada_b0", 72), ("ada_b1", 72),
                    ("ng00", 8), ("ng01", 8), ("ng02", 8), ("ng10", 8), ("ng11", 8), ("ng12", 8),
                    ("gq", 1), ("gk", 1), ("b_pw1", 16), ("b_dw", 8), ("ln_g", 8), ("ln_b", 8),
                    ("b_pw2", 8), ("w_dw", 8 * CW)):
        SP_OFF[name] = (off, w)
        off += w
    return off


SP_N = _sp_layout()


def pack_small(inp, b):
    sp = np.zeros((128, SP_N), np.float32)

    def put(name, arr):
        o, w = SP_OFF[name]
        assert arr.shape == (128, w), (name, arr.shape, w)
        sp[:, o:o + w] = arr
    put("c", _fm(inp["c"][b]))
    put("ada_b0", _fm(inp["ada_b"][0]))
    put("ada_b1", _fm(inp["ada_b"][1]))
    for i in range(2):
        for j in range(3):
            put("ng%d%d" % (i, j), _fm(inp["norm_g"][i, j]))
    put("gq", np.tile(np.asarray(inp["attn_g_q"][0], np.float32), 2).reshape(128, 1))
    put("gk", np.tile(np.asarray(inp["attn_g_k"][0], np.float32), 2).reshape(128, 1))
    put("b_pw1", _fm(inp["conv_b_pw1"][0]))
    put("b_dw", _fm(inp["conv_b_dw"][0]))
    put("ln_g", _fm(inp["conv_ln_g"][0]))
    put("ln_b", _fm(inp["conv_ln_b"][0]))
    put("b_pw2", _fm(inp["conv_b_pw2"][0]))
    wd = np.asarray(inp["conv_w_dw"][0], np.float32)
    put("w_dw", np.ascontiguousarray(wd.reshape(CW, 8, 128).transpose(2, 1, 0)).reshape(128, 8 * CW))
    return sp


def spc(cx, name):
    o, w = SP_OFF[name]
    return cx.spt[:, o:o + w]


def emit_load_small(S, cx, sp_d):
    nc = S.nc
    cx.spt = S.sb("spt", [128, SP_N], F32)
    cx.b_sp = Buf("spt")
    S.op("sp", lambda: nc.sync.dma_start(out=cx.spt[:], in_=sp_d[:, :]), writes=[cx.b_sp])


def emit_adaln(S, cx, ada_w_d, modT, b_modT):
    nc = S.nc
    NCOL = 1152
    stg = [S.sb("adastg%d" % i, [128, KC, NCOL], F32) for i in range(2)]
    b_stg = [Buf("adastg%d" % i) for i in range(2)]
    cact = S.sb("cact", [128, 8], F32)
    b_cact = Buf("cact")
    S.op("act", lambda: nc.scalar.activation(out=cact[:], in_=spc(cx, "c"), func=AF.Silu), reads=[cx.b_sp], writes=[b_cact])
    k = 0
    for i in range(2):
        wv = ada_w_d[i].rearrange("(kc p) n -> p kc n", p=128)
        for pc in range(9216 // NCOL):
            st = stg[k % 2]
            bs = b_stg[k % 2]
            k += 1
            S.op("sp", (lambda st=st, wv=wv, pc=pc: nc.sync.dma_start(out=st[:], in_=wv[:, :, pc * NCOL:(pc + 1) * NCOL])), writes=[bs])

            def mm(st=st, pc=pc, i=i):
                for m in range(NCOL // 128):
                    col = pc * (NCOL // 128) + m
                    for kc in range(KC):
                        r = nc.tensor.matmul(cx.psum[7][:, col:col + 1], lhsT=st[:, kc, m * 128:(m + 1) * 128], rhs=cact[:, kc:kc + 1],
                                             start=(kc == 0), stop=(kc == KC - 1))
                return r
            S.op("pe", mm, reads=[bs, b_cact], writes=[cx.b_psum[7]])
        S.op("dve", (lambda i=i: nc.vector.tensor_tensor(out=modT[i][:], in0=cx.psum[7][:, 0:72], in1=spc(cx, "ada_b%d" % i), op=ALU.add)),
             reads=[cx.b_psum[7], cx.b_sp], writes=[b_modT[i]])
    return stg


def joint_mod(S, cx, i, j, modT, b_modT):
    nc = S.nc
    A, Bv, G, bA = emit_mod_vectors_i(S, cx, modT[i], b_modT[i], spc(cx, "ng%d%d" % (i, j)), "l%ds%d" % (i, j), j)
    return A, Bv, G, bA


def emit_mod_vectors_i(S, cx, modT, b_modT, normg, tag, j):
    nc = S.nc
    AB = S.sb("modAB_" + tag, [128, 24], F32)
    b = Buf("modAB_" + tag)
    base = j * 24
    S.op("dve", lambda: nc.vector.tensor_copy(out=AB[:], in_=modT[:, base:base + 24]), reads=[b_modT, cx.b_sp], writes=[b])
    S.op("dve", lambda: nc.vector.scalar_tensor_tensor(
        out=AB[:, 8:16], in0=modT[:, base + 8:base + 16], scalar=1.0, in1=normg,
        op0=ALU.add, op1=ALU.mult), reads=[b_modT, cx.b_sp], writes=[b])
    return AB[:, 8:16], AB[:, 0:8], AB[:, 16:24], b


def load_x_tile(S, cx, xv, b_x, t, n=512):
    nc = S.nc
    xt = cx.xt[t % 2]
    bx = cx.b_xt[t % 2]
    S.op("sp", (lambda: nc.sync.dma_start(out=xt[:, :, :n], in_=xv[:, :, t * n:(t + 1) * n])), reads=[b_x[t]], writes=[bx])
    return xt, bx


def emit_qkv_phase(S, cx, x_d, b_x, wqkv_d, A, Bv, b_mod, QT_d, KT_d, kbar_d, V_d):
    nc = S.nc
    wq = S.sb("wqkv_sb", [128, KC, 3072], BF16)
    b_wq = [Buf("wqkv%d" % k) for k in range(KC)]
    for kc in range(KC):
        S.op("pool_dma", (lambda kc=kc: nc.gpsimd.dma_start(out=wq[:, kc, :], in_=wqkv_d[kc * 128:(kc + 1) * 128, :])), writes=[b_wq[kc]])
    bo = S.sb("blkones", [128, 128], BF16)
    b_bo = Buf("blkones")
    S.op("dve", lambda: nc.vector.memset(bo[:], 0.0), writes=[b_bo])
    S.op("dve", lambda: nc.vector.memset(bo[0:64, 0:64], 1.0), writes=[b_bo])
    S.op("dve", lambda: nc.vector.memset(bo[64:128, 64:128], 1.0), writes=[b_bo])
    g8 = S.sb("g8", [128, 2], F32)
    b_g8 = Buf("g8")
    S.op("dve", lambda: nc.vector.tensor_scalar(out=g8[:, 0:1], in0=spc(cx, "gq"), scalar1=0.125, scalar2=None, op0=ALU.mult),
         reads=[cx.b_sp], writes=[b_g8])
    S.op("dve", lambda: nc.vector.tensor_copy(out=g8[:, 1:2], in_=spc(cx, "gk")), reads=[cx.b_sp], writes=[b_g8])
    qo = [S.sb("qo%d" % i, [128, 512], BF16) for i in range(2)]
    b_qo = [Buf("qo%d" % i) for i in range(2)]
    kb = [S.sb("kb%d" % i, [128, 2], F32) for i in range(2)]
    b_kb = [Buf("kb%d" % i) for i in range(2)]
    vt = [S.sb("vt%d" % i, [128, 1024], BF16) for i in range(2)]
    b_vt = [Buf("vt%d" % i) for i in range(2)]
    xv = x_d.rearrange("(c p) t -> p c t", p=128)
    QTv = QT_d.rearrange("h d t -> (h d) t")
    KTv = KT_d.rearrange("h d t -> (h d) t")
    kbv = kbar_d.rearrange("h d l -> (h d) l")
    cnt = 0
    for t in range(8):
        xt, bx = load_x_tile(S, cx, xv, b_x, t)
        emit_norm_mod(S, cx, xt, bx, A, Bv, b_mod)
        for which in range(2):
            goff = which * 1024
            outv = QTv if which == 0 else KTv
            for hp in range(8):
                k = cnt % 2
                cnt += 1
                pq = cx.psum[1 + k]
                pss = cx.psum[3 + k]

                def mm(hp=hp, pq=pq, goff=goff):
                    for kc in range(KC):
                        m = nc.tensor.matmul(pq[:], lhsT=wq[:, kc, goff + hp * 128:goff + (hp + 1) * 128], rhs=cx.h[:, kc, :],
                                             start=(kc == 0), stop=(kc == KC - 1))
                    return m
                S.op("pe", mm, reads=b_wq + [cx.b_h], writes=[cx.b_psum[1 + k]])
                S.op("act", (lambda k=k, pq=pq: nc.scalar.activation(out=cx.sq[k][:], in_=pq[:], func=AF.Square)),
                     reads=[cx.b_psum[1 + k]], writes=[cx.b_sq[k]])
                S.op("pe", (lambda k=k, pss=pss: nc.tensor.matmul(pss[:], lhsT=bo[:], rhs=cx.sq[k][:], start=True, stop=True)),
                     reads=[cx.b_sq[k], b_bo], writes=[cx.b_psum[3 + k]])
                r1 = cx.tmp[k]
                S.op("dve", (lambda r1=r1, pss=pss: nc.vector.tensor_scalar(out=r1[:], in0=pss[:], scalar1=1.0 / DH, scalar2=EPS,
                                                                            op0=ALU.mult, op1=ALU.add)),
                     reads=[cx.b_psum[3 + k]], writes=[cx.b_tmp[k]])
                S.op("act", (lambda r1=r1: nc.scalar.activation(out=r1[:], in_=r1[:], func=AF.Sqrt)), reads=[cx.b_tmp[k]], writes=[cx.b_tmp[k]])
                S.op("dve", (lambda r1=r1: nc.vector.reciprocal(out=r1[:], in_=r1[:])), reads=[cx.b_tmp[k]], writes=[cx.b_tmp[k]])
                S.op("dve", (lambda r1=r1, pq=pq: nc.vector.tensor_tensor(out=r1[:], in0=pq[:], in1=r1[:], op=ALU.mult)),
                     reads=[cx.b_tmp[k], cx.b_psum[1 + k]], writes=[cx.b_tmp[k]])
                sa = cx.sa[k]
                S.op("act", (lambda r1=r1, sa=sa, which=which: nc.scalar.activation(out=sa[:], in_=r1[:], func=AF.Copy,
                                                                                  scale=g8[:, which:which + 1])),
                     reads=[cx.b_tmp[k], b_g8], writes=[cx.b_sa[k]])
                S.op("pool", (lambda k=k, sa=sa: nc.gpsimd.tensor_copy(out=qo[k][:], in_=sa[:])), reads=[cx.b_sa[k]], writes=[b_qo[k]])
                S.op("sp", (lambda k=k, hp=hp, t=t, outv=outv: nc.sync.dma_start(out=outv[hp * 128:(hp + 1) * 128, t * 512:(t + 1) * 512], in_=qo[k][:])),
                     reads=[b_qo[k]])
                if which == 1:
                    S.op("dve", (lambda k=k, sa=sa: nc.vector.tensor_reduce(out=kb[k][:], in_=sa[:].rearrange("p (b t) -> p b t", b=2),
                                                                          axis=AX.X, op=ALU.add)),
                         reads=[cx.b_sa[k]], writes=[b_kb[k]])
                    S.op("dve", (lambda k=k: nc.vector.tensor_scalar(out=kb[k][:], in0=kb[k][:], scalar1=1.0 / BLK, scalar2=None, op0=ALU.mult)),
                         reads=[b_kb[k]], writes=[b_kb[k]])
                    S.op("sp", (lambda k=k, hp=hp, t=t: nc.sync.dma_start(out=kbv[hp * 128:(hp + 1) * 128, 2 * t:2 * t + 2], in_=kb[k][:])),
                         reads=[b_kb[k]])
        for tg in range(4):
            v = vt[tg % 2]
            for ns in range(2):
                pv = cx.psum[5 + ns]

                def mmv(tg=tg, ns=ns, pv=pv):
                    for kc in range(KC):
                        m = nc.tensor.matmul(pv[:], lhsT=cx.h[:, kc, tg * 128:(tg + 1) * 128], rhs=wq[:, kc, 2048 + ns * 512:2048 + (ns + 1) * 512],
                                             start=(kc == 0), stop=(kc == KC - 1))
                    return m
                S.op("pe", mmv, reads=b_wq + [cx.b_h], writes=[cx.b_psum[5 + ns]])
                if ns == 0:
                    S.op("act", (lambda v=v, pv=pv: nc.scalar.copy(out=v[:, 0:512], in_=pv[:])), reads=[cx.b_psum[5]], writes=[b_vt[tg % 2]])
                else:
                    S.op("dve", (lambda v=v, pv=pv: nc.vector.tensor_copy(out=v[:, 512:1024], in_=pv[:])), reads=[cx.b_psum[6]], writes=[b_vt[tg % 2]])
            S.op("sp", (lambda v=v, tg=tg, t=t: nc.sync.dma_start(out=V_d[t * 512 + tg * 128:t * 512 + (tg + 1) * 128, :], in_=v[:])),
                 reads=[b_vt[tg % 2]])


def build_L1():
    nc = bass.Bass("TRN2", target_bir_lowering=False)
    sp_d = nc.dram_tensor("sp", [128, SP_N], F32, kind="ExternalInput").ap()
    ada_w = nc.dram_tensor("ada_w", [2, 1024, 9216], F32, kind="ExternalInput").ap()
    xin = nc.dram_tensor("xin", [1024, TOK], F32, kind="ExternalInput").ap()
    w_in = nc.dram_tensor("w_in", [1024, 2 * DFF], F32, kind="ExternalInput").ap()
    w_out = nc.dram_tensor("w_out", [DFF, 1024], F32, kind="ExternalInput").ap()
    wqkv = nc.dram_tensor("wqkv", [1024, 3072], F32, kind="ExternalInput").ap()
    x1 = nc.dram_tensor("x1", [1024, TOK], F32, kind="ExternalOutput").ap()
    modo = nc.dram_tensor("modo", [128, 144], F32, kind="ExternalOutput").ap()
    QT = nc.dram_tensor("QT", [NH, DH, TOK], BF16, kind="ExternalOutput").ap()
    KT = nc.dram_tensor("KT", [NH, DH, TOK], BF16, kind="ExternalOutput").ap()
    kbar = nc.dram_tensor("kbar", [NH, DH, NLB], F32, kind="ExternalOutput").ap()
    V = nc.dram_tensor("V", [TOK, 1024], BF16, kind="ExternalOutput").ap()
    S = Sched(nc)
    cx = Ctx()
    setup_common(S, cx)
    emit_load_small(S, cx, sp_d)
    modT = [S.sb("modT%d" % i, [128, 72], F32) for i in range(2)]
    b_modT = [Buf("modT%d" % i) for i in range(2)]
    A0, B0, G0, bm0 = None, None, None, None
    AB0 = joint_mod
    emit_adaln_scoped(S, cx, ada_w, modT, b_modT)
    for i in range(2):
        S.op("sp", (lambda i=i: nc.sync.dma_start(out=modo[:, i * 72:(i + 1) * 72], in_=modT[i][:])), reads=[b_modT[i]])
    A, Bv, G, bA = emit_mod_vectors_i(S, cx, modT[0], b_modT[0], spc(cx, "ng00"), "l0s0", 0)
    A1, Bv1, G1, bA1 = emit_mod_vectors_i(S, cx, modT[0], b_modT[0], spc(cx, "ng01"), "l0s1", 1)
    alloc_tile_bufs(S, cx)
    alloc_ffn_bufs(S, cx)
    b_xin = [Buf("xin%d" % t) for t in range(8)]
    b_x1 = [Buf("x1_%d" % t) for t in range(8)]
    emit_ffn_phase(S, cx, xin, b_xin, x1, b_x1, w_in, w_out, A, Bv, G, 0.5, bA, 8, "f00")
    S.barrier_free(3)
    emit_qkv_phase(S, cx, x1, b_x1, wqkv, A1, Bv1, bA1, QT, KT, kbar, V)
    S.emit()
    return nc


def emit_adaln_scoped(S, cx, ada_w, modT, b_modT):
    n0 = len(S._stack)
    emit_adaln(S, cx, ada_w, modT, b_modT)
    S.barrier_free(len(S._stack) - n0)


SLOPES = [2.0 ** (-8.0 * (h + 1) / NH) for h in range(NH)]


def emit_attn_phase(S, cx, d):
    nc = S.nc
    KTa = [S.sb("KTa%d" % i, [128, SEQ], BF16) for i in range(2)]
    Va = [S.sb("Va%d" % i, [128, 128, 65], BF16) for i in range(2)]
    Qa = [S.sb("Qa%d" % i, [128, TOK], BF16) for i in range(2)]
    Ko = [S.sb("Ko%d" % i, [128, TOK], BF16) for i in range(2)]
    Vo = [S.sb("Vo%d" % i, [128, 32, 65], BF16) for i in range(2)]
    bT = [S.sb("bT%d" % i, [128, 1024], F32) for i in range(2)]
    dgb = [S.sb("dgb%d" % i, [128, 512], F32) for i in range(2)]
    cq = [S.sb("cq%d" % i, [128, 256], F32) for i in range(2)]
    b_KT = [Buf("KTa%d" % i) for i in range(2)]
    b_Va = [Buf("Va%d" % i) for i in range(2)]
    b_Qa = [Buf("Qa%d" % i) for i in range(2)]
    b_Qm = [[Buf("Qm%d_%d" % (i, l)) for l in range(NLB)] for i in range(2)]
    b_Ko = [Buf("Ko%d" % i) for i in range(2)]
    b_Vo = [Buf("Vo%d" % i) for i in range(2)]
    b_hd = [Buf("hd%d" % i) for i in range(2)]
    cst = S.sb("cst", [128, CST_N], F32)
    b_cst = Buf("cst")
    S.op("sp", lambda: nc.sync.dma_start(out=cst[:], in_=d["cst"][:, :]), writes=[b_cst])

    def C(name):
        o, w = CST_OFF[name]
        return cst[:, o:o + w]
    kb32 = S.sb("kb32", [64, NH * NB], F32)
    kbh = S.sb("kbh", [64, NH * NB], BF16)
    kbl = S.sb("kbl", [64, NH * NB], BF16)
    kbt = S.sb("kbt", [64, NH * NB], F32)
    b_kb = Buf("kbhl")
    S.op("sp", lambda: nc.sync.dma_start(out=kb32[:], in_=d["kbar"].rearrange("d h m -> d (h m)")), writes=[b_kb])
    S.op("dve", lambda: nc.vector.tensor_copy(out=kbh[:], in_=kb32[:]), reads=[b_kb], writes=[b_kb])
    S.op("dve", lambda: nc.vector.tensor_copy(out=kbt[:], in_=kbh[:]), reads=[b_kb], writes=[b_kb])
    S.op("dve", lambda: nc.vector.tensor_tensor(out=kbt[:], in0=kb32[:], in1=kbt[:], op=ALU.subtract), reads=[b_kb], writes=[b_kb])
    S.op("dve", lambda: nc.vector.tensor_copy(out=kbl[:], in_=kbt[:]), reads=[b_kb], writes=[b_kb])
    mbp = [S.sb("mbp%d" % i, [128, 128], F32) for i in range(2)]
    b_mbp = [Buf("mbp%d" % i) for i in range(2)]
    gmt = [S.sb("gmt%d" % i, [128, 64], F32) for i in range(2)]
    b_gmt = [Buf("gmt%d" % i) for i in range(2)]
    t8 = [S.sb("t8_%d" % i, [128, 8], F32) for i in range(2)]
    pt = [S.sb("pt%d" % i, [128, 512], BF16) for i in range(2)]
    b_pt = [Buf("pt%d" % i) for i in range(2)]
    pt2 = S.sb("pt2", [128, 512], BF16)
    b_pt2 = Buf("pt2")
    dtmp = S.sb("dtmp", [128, 512], F32)
    b_dtmp = Buf("dtmp")
    o1 = S.sb("o1", [128, 256], F32)
    o2 = S.sb("o2", [128, 256], F32)
    rd = S.sb("rd", [128, 256], F32)
    ob = [S.sb("ob%d" % i, [128, 256], BF16) for i in range(2)]
    b_o = Buf("o12")
    b_ob = [Buf("ob%d" % i) for i in range(2)]
    for i in range(2):
        S.op("sp", (lambda i=i: nc.sync.dma_start(out=KTa[i][64:128, :], in_=d["ind"][:, :])), writes=[b_KT[i]])
        S.op("dve", (lambda i=i: nc.vector.memset(Va[i][:, :, 64:65], 1.0)), writes=[b_Va[i]])
        S.op("dve", (lambda i=i: nc.vector.memset(Vo[i][:, :, 64:65], 1.0)), writes=[b_Vo[i]])
        S.op("dve", (lambda i=i: nc.vector.memset(mbp[i][:], 0.0)), writes=[b_mbp[i]])
    ps = cx.psum
    bp = cx.b_psum
    scnt = 0
    gcnt = 0
    for h in range(NH):
        cur = h % 2
        sl = SLOPES[h]
        es = math.exp(sl)
        S.op("sp", (lambda h=h, cur=cur: nc.sync.dma_start(out=KTa[cur][0:64, :], in_=d["KT"][h])), writes=[b_KT[cur]])
        for q8 in range(8):
            S.op("sp", (lambda h=h, cur=cur, q8=q8: nc.sync.dma_start(out=Va[cur][:, q8 * 16:(q8 + 1) * 16, 0:64], in_=d["Vd"][h, :, q8 * 16:(q8 + 1) * 16, :])),
                 writes=[b_Va[cur]])
        S.op("sp", (lambda h=h, cur=cur: nc.sync.dma_start(out=Qa[cur][0:64, :], in_=d["QT"][h])), writes=[b_Qa[cur]] + b_Qm[cur])
        S.op("sp", (lambda h=h, cur=cur: nc.sync.dma_start(out=Ko[cur][0:64, :], in_=d["KTo"][h])), writes=[b_Ko[cur]])
        for q2 in range(2):
            S.op("sp", (lambda h=h, cur=cur, q2=q2: nc.sync.dma_start(out=Vo[cur][:, q2 * 16:(q2 + 1) * 16, 0:64], in_=d["Vo"][h, :, q2 * 16:(q2 + 1) * 16, :])),
                 writes=[b_Vo[cur]])
        for (VV, bV) in ((Va[cur], b_Va[cur]), (Vo[cur], b_Vo[cur])):
            Vodd = VV[:].rearrange("p (n two) e -> p n two e", two=2)[:, :, 1, :]
            S.op("dve", (lambda Vodd=Vodd, es=es: nc.vector.tensor_scalar(out=Vodd[:, :, 0:64], in0=Vodd[:, :, 0:64], scalar1=es, scalar2=None, op0=ALU.mult)),
                 reads=[bV], writes=[bV])
            S.op("dve", (lambda Vodd=Vodd, es=es: nc.vector.memset(Vodd[:, :, 64:65], es)), reads=[bV], writes=[bV])
        S.op("dve", (lambda cur=cur, sl=sl: nc.vector.tensor_scalar(out=bT[cur][:], in0=C("base"), scalar1=sl, scalar2=None, op0=ALU.mult)),
             reads=[b_cst], writes=[b_hd[cur]])
        S.op("dve", (lambda cur=cur, sl=sl: nc.vector.scalar_tensor_tensor(out=dgb[cur][:], in0=C("Dm"), scalar=-sl, in1=C("causal"),
                                                                         op0=ALU.mult, op1=ALU.add)), reads=[b_cst], writes=[b_hd[cur]])
        S.op("act", (lambda cur=cur, sl=sl: nc.scalar.activation(out=cq[cur][:], in_=C("irow"), func=AF.Exp, scale=-sl)),
             reads=[b_cst], writes=[b_hd[cur]])
        for l in range(NLB):
            for hq in range(2):
                g = gcnt % 2
                gcnt += 1
                q0 = l * 256 + hq * 128

                def mmg(cur=cur, q0=q0, h=h):
                    nc.tensor.matmul(ps[6][:, 0:64], lhsT=Qa[cur][0:64, q0:q0 + 128], rhs=kbh[:, h * 64:(h + 1) * 64], start=True, stop=False)
                    return nc.tensor.matmul(ps[6][:, 0:64], lhsT=Qa[cur][0:64, q0:q0 + 128], rhs=kbl[:, h * 64:(h + 1) * 64], start=False, stop=True)
                S.op("pe", mmg, reads=[b_Qa[cur], b_kb], writes=[bp[6]])
                pb = C("past")[:, l * 64:(l + 1) * 64]
                S.op("dve", (lambda g=g, pb=pb: nc.vector.tensor_tensor(out=gmt[g][:], in0=ps[6][:, 0:64], in1=pb, op=ALU.add)),
                     reads=[bp[6], b_cst], writes=[b_gmt[g]])
                S.op("dve", (lambda g=g: nc.vector.max(out=t8[g][:], in_=gmt[g][:])), reads=[b_gmt[g]], writes=[b_gmt[g]])
                S.op("dve", (lambda g=g: nc.vector.tensor_scalar(out=gmt[g][:], in0=gmt[g][:], scalar1=t8[g][:, 2:3], scalar2=-NEG,
                                                                 op0=ALU.is_ge, op1=ALU.mult)), reads=[b_gmt[g]], writes=[b_gmt[g]])
                S.op("dve", (lambda g=g, pb=pb: nc.vector.scalar_tensor_tensor(out=mbp[g][:, 64:128], in0=gmt[g][:], scalar=NEG, in1=pb,
                                                                             op0=ALU.add, op1=ALU.add)),
                     reads=[b_gmt[g], b_cst], writes=[b_mbp[g]])
                S.op("pe", (lambda g=g: nc.tensor.transpose(ps[7][:, 0:128], mbp[g][:], C("ident"))), reads=[b_mbp[g], b_cst], writes=[bp[7]])
                S.op("act", (lambda cur=cur, q0=q0: nc.scalar.copy(out=Qa[cur][64:128, q0:q0 + 128], in_=ps[7][64:128, 0:128])),
                     reads=[bp[7]], writes=[b_Qm[cur][l]])
        for i in range(8):
            nk = 8 * i + 7
            for X in (2 * i, 2 * i + 1):
                po = 2 + (X % 2)
                for n in range(nk):
                    sb_ = scnt % 2
                    scnt += 1

                    def mms(cur=cur, X=X, n=n, sb_=sb_):
                        for c in range(2):
                            m = nc.tensor.matmul(ps[sb_][:, c * 256:(c + 1) * 256], lhsT=KTa[cur][:, n * 256 + c * 128:n * 256 + (c + 1) * 128],
                                                 rhs=Qa[cur][:, X * 256:(X + 1) * 256], start=True, stop=True)
                        return m
                    S.op("pe", mms, reads=[b_KT[cur], b_Qa[cur], b_Qm[cur][X]], writes=[bp[sb_]])
                    S.op("act", (lambda cur=cur, X=X, n=n, sb_=sb_: nc.scalar.activation(out=pt[sb_][:], in_=ps[sb_][:], func=AF.Exp,
                                                                                        bias=bT[cur][:, X * 64 + n:X * 64 + n + 1])),
                         reads=[bp[sb_], b_hd[cur]], writes=[b_pt[sb_]])

                    def mmpv(cur=cur, n=n, sb_=sb_, po=po, nk=nk):
                        for c in range(2):
                            m = nc.tensor.matmul(ps[po][0:65, 0:256], lhsT=Va[cur][:, 2 * n + c, :], rhs=pt[sb_][:, c * 256:(c + 1) * 256],
                                                 start=(n == 0 and c == 0), stop=(n == nk - 1 and c == 1))
                        return m
                    S.op("pe", mmpv, reads=[b_Va[cur], b_pt[sb_]], writes=[bp[po]])

                def mmd(cur=cur, X=X):
                    for c in range(2):
                        m = nc.tensor.matmul(ps[4][:, c * 256:(c + 1) * 256], lhsT=Ko[cur][0:64, X * 256 + c * 128:X * 256 + (c + 1) * 128],
                                             rhs=Qa[cur][0:64, X * 256:(X + 1) * 256], start=True, stop=True)
                    return m
                S.op("pe", mmd, reads=[b_Ko[cur], b_Qa[cur]], writes=[bp[4]])
                S.op("dve", (lambda cur=cur: nc.vector.tensor_tensor(out=dtmp[:], in0=ps[4][:], in1=dgb[cur][:], op=ALU.add)),
                     reads=[bp[4], b_hd[cur]], writes=[b_dtmp])
                S.op("act", (lambda: nc.scalar.activation(out=pt2[:], in_=dtmp[:], func=AF.Exp)), reads=[b_dtmp], writes=[b_pt2])

                def mmpo(cur=cur, X=X):
                    for c in range(2):
                        m = nc.tensor.matmul(ps[5][0:65, 0:256], lhsT=Vo[cur][:, 2 * X + c, :], rhs=pt2[:, c * 256:(c + 1) * 256],
                                             start=(c == 0), stop=(c == 1))
                    return m
                S.op("pe", mmpo, reads=[b_Vo[cur], b_pt2], writes=[bp[5]])
                S.op("dve", (lambda cur=cur, po=po: nc.vector.tensor_tensor(out=o1[0:65, :], in0=ps[po][0:65, 0:256], in1=cq[cur][0:65, :], op=ALU.mult)),
                     reads=[bp[po], b_hd[cur]], writes=[b_o])
                S.op("dve", (lambda: nc.vector.tensor_tensor(out=o2[0:65, :], in0=o1[0:65, :], in1=ps[5][0:65, 0:256], op=ALU.add)),
                     reads=[bp[5], b_o], writes=[b_o])
                S.op("pe", (lambda: nc.tensor.matmul(ps[6][0:64, 0:256], lhsT=C("sel65")[0:65, :], rhs=o2[0:65, :], start=True, stop=True)),
                     reads=[b_o, b_cst], writes=[bp[6]])
                S.op("dve", (lambda: nc.vector.reciprocal(out=rd[0:64, :], in_=ps[6][0:64, 0:256])), reads=[bp[6]], writes=[b_o])
                k = X % 2
                S.op("dve", (lambda k=k: nc.vector.tensor_tensor(out=ob[k][0:64, :], in0=o2[0:64, :], in1=rd[0:64, :], op=ALU.mult)),
                     reads=[b_o], writes=[b_ob[k]])
                S.op("sp", (lambda k=k, h=h, X=X: nc.sync.dma_start(out=d["OT"][h, :, X * 256:(X + 1) * 256], in_=ob[k][0:64, :])),
                     reads=[b_ob[k]], writes=[d["b_OT"][X // 2]])


CST_OFF = {}


def _cst_layout():
    off = 0
    for name, w in (("past", 1024), ("base", 1024), ("irow", 256), ("Dm", 512), ("causal", 512), ("ident", 128), ("sel65", 64)):
        CST_OFF[name] = (off, w)
        off += w
    return off


CST_N = _cst_layout()


def make_cst(j):
    blocks = snake_blocks(j)
    c = np.zeros((128, CST_N), np.float32)
    p = np.arange(128, dtype=np.float32)[:, None]
    past = np.zeros((16, 64), np.float32)
    base = np.zeros((128, 16, 64), np.float32)
    for l, own in enumerate(blocks):
        m = np.arange(64)
        past[l] = np.where(m < own, 0.0, NEG)
        dist = np.where(m < own, own - m, 1).astype(np.float32)
        base[:, l, :] = 2.0 * p - 256.0 * dist[None, :]
    o, w = CST_OFF["past"]
    c[:, o:o + w] = np.tile(past.reshape(1, 1024), (128, 1))
    o, w = CST_OFF["base"]
    c[:, o:o + w] = base.reshape(128, 1024)
    o, w = CST_OFF["irow"]
    c[:, o:o + w] = np.arange(256, dtype=np.float32)[None, :]
    i = np.arange(256, dtype=np.float32)[None, None, :]
    pp = np.arange(128, dtype=np.float32)[:, None, None]
    cc = np.arange(2, dtype=np.float32)[None, :, None]
    Dm = np.broadcast_to(i - 2 * pp, (128, 2, 256))
    causal = np.where(i >= 2 * pp + cc, 0.0, NEG)
    o, w = CST_OFF["Dm"]
    c[:, o:o + w] = Dm.reshape(128, 512)
    o, w = CST_OFF["causal"]
    c[:, o:o + w] = causal.reshape(128, 512)
    o, w = CST_OFF["ident"]
    c[:, o:o + w] = np.eye(128, dtype=np.float32)
    o, w = CST_OFF["sel65"]
    c[64, o:o + w] = 1.0
    return c


def emit_proj_res_phase(S, cx, x_in, b_xin, x_out, b_xout, src_d, b_src, w_d, G, bias, b_mod, tag, src_is_heads, ntiles=8):
    nc = S.nc
    w = S.sb("w_" + tag, [128, KC, D], BF16)
    b_w = Buf("w_" + tag)
    for kc in range(KC):
        S.op("pool_dma", (lambda kc=kc: nc.gpsimd.dma_start(out=w[:, kc, :], in_=w_d[kc * 128:(kc + 1) * 128, :])), writes=[b_w])
    sv = src_d.rearrange("h d t -> (h d) t") if src_is_heads else src_d
    sv = sv.rearrange("(c p) t -> p c t", p=128)
    xv_in = x_in.rearrange("(c p) t -> p c t", p=128)
    xv_out = x_out.rearrange("(c p) t -> p c t", p=128)
    for t in range(ntiles):
        xt, bx = load_x_tile(S, cx, xv_in, b_xin, t)
        S.op("sp", (lambda t=t: nc.sync.dma_start(out=cx.h[:], in_=sv[:, :, t * 512:(t + 1) * 512])), reads=[b_src[t]], writes=[cx.b_h])
        for oc in range(KC):
            py = 5 + (oc % 2)

            def mm(oc=oc, py=py):
                for kc in range(KC):
                    m = nc.tensor.matmul(cx.psum[py][:], lhsT=w[:, kc, oc * 128:(oc + 1) * 128], rhs=cx.h[:, kc, :], start=(kc == 0), stop=(kc == KC - 1))
                return m
            S.op("pe", mm, reads=[b_w, cx.b_h], writes=[cx.b_psum[py]])
            if bias is None:
                S.op("dve", (lambda oc=oc, py=py, xt=xt: nc.vector.scalar_tensor_tensor(
                    out=xt[:, oc, :], in0=cx.psum[py][:], scalar=G[:, oc:oc + 1], in1=xt[:, oc, :], op0=ALU.mult, op1=ALU.add)),
                    reads=[cx.b_psum[py], b_mod], writes=[bx])
            else:
                tmp = cx.tmp[oc % 2]
                S.op("act", (lambda oc=oc, py=py, tmp=tmp: nc.scalar.activation(out=tmp[:], in_=cx.psum[py][:], func=AF.Identity, bias=bias[:, oc:oc + 1])),
                     reads=[cx.b_psum[py], cx.b_sp], writes=[cx.b_tmp[oc % 2]])
                S.op("dve", (lambda oc=oc, tmp=tmp, xt=xt: nc.vector.scalar_tensor_tensor(
                    out=xt[:, oc, :], in0=tmp[:], scalar=G[:, oc:oc + 1], in1=xt[:, oc, :], op0=ALU.mult, op1=ALU.add)),
                    reads=[cx.b_tmp[oc % 2], b_mod], writes=[bx])
        S.op("sp", (lambda xt=xt, t=t: nc.sync.dma_start(out=xv_out[:, :, t * 512:(t + 1) * 512], in_=xt[:])), reads=[bx], writes=[b_xout[t]])


def emit_glu_phase(S, cx, x_in, b_xin, w_d, A, Bv, b_mod, u_out, ntiles=8, hmask=None, b_hmask=None, b_uout=None):
    nc = S.nc
    w = S.sb("w_pw1", [128, KC, 2 * D], BF16)
    b_w = Buf("w_pw1")
    for kc in range(KC):
        S.op("pool_dma", (lambda kc=kc: nc.gpsimd.dma_start(out=w[:, kc, :], in_=w_d[kc * 128:(kc + 1) * 128, :])), writes=[b_w])
    uo = [S.sb("uo%d" % i, [128, 512], BF16) for i in range(2)]
    b_uo = [Buf("uo%d" % i) for i in range(2)]
    xv_in = x_in.rearrange("(c p) t -> p c t", p=128)
    uv = u_out.rearrange("(c p) t -> p c t", p=128)
    bp1 = spc(cx, "b_pw1")
    for t in range(ntiles):
        xt, bx = load_x_tile(S, cx, xv_in, b_xin, t)
        emit_norm_mod(S, cx, xt, bx, A, Bv, b_mod)
        for oc in range(KC):
            k = oc % 2
            pa = 1 + k * 2
            pg = pa + 1

            def mma(oc=oc, pa=pa):
                for kc in range(KC):
                    m = nc.tensor.matmul(cx.psum[pa][:], lhsT=w[:, kc, oc * 128:(oc + 1) * 128], rhs=cx.h[:, kc, :], start=(kc == 0), stop=(kc == KC - 1))
                return m

            def mmg(oc=oc, pg=pg):
                for kc in range(KC):
                    m = nc.tensor.matmul(cx.psum[pg][:], lhsT=w[:, kc, D + oc * 128:D + (oc + 1) * 128], rhs=cx.h[:, kc, :], start=(kc == 0), stop=(kc == KC - 1))
                return m
            S.op("pe", mma, reads=[b_w, cx.b_h], writes=[cx.b_psum[pa]])
            S.op("pe", mmg, reads=[b_w, cx.b_h], writes=[cx.b_psum[pg]])
            sa = cx.sa[k]
            S.op("act", (lambda oc=oc, pg=pg, sa=sa: nc.scalar.activation(out=sa[:], in_=cx.psum[pg][:], func=AF.Sigmoid, bias=bp1[:, 8 + oc:9 + oc])),
                 reads=[cx.b_psum[pg], cx.b_sp], writes=[cx.b_sa[k]])
            S.op("dve", (lambda oc=oc, pa=pa, sa=sa, k=k: nc.vector.scalar_tensor_tensor(
                out=uo[k][:], in0=cx.psum[pa][:], scalar=bp1[:, oc:oc + 1], in1=sa[:], op0=ALU.add, op1=ALU.mult)),
                reads=[cx.b_psum[pa], cx.b_sa[k], cx.b_sp], writes=[b_uo[k]])
            if hmask is not None and t == 8:
                S.op("pool", (lambda k=k: nc.gpsimd.tensor_tensor(out=uo[k][:], in0=uo[k][:], in1=hmask, op=ALU.mult)),
                     reads=[b_uo[k], b_hmask], writes=[b_uo[k]])
            S.op("sp", (lambda oc=oc, k=k, t=t: nc.sync.dma_start(out=uv[:, oc, t * 512:(t + 1) * 512], in_=uo[k][:])), reads=[b_uo[k]],
                 writes=([b_uout[t]] if b_uout is not None else []))


def build_L2():
    nc = bass.Bass("TRN2", target_bir_lowering=False)
    din = lambda n, s, dt: nc.dram_tensor(n, s, dt, kind="ExternalInput").ap()
    sp_d = din("sp", [128, SP_N], F32)
    mod_d = din("modi", [128, 144], F32)
    x1 = din("x1", [1024, TOK], F32)
    d = {"KT": din("KTf", [NH, DH, SEQ], BF16), "ind": din("ind", [64, SEQ], BF16), "Vd": din("Vd", [NH, 128, 128, DH], BF16),
         "QT": din("QT", [NH, DH, TOK], BF16), "KTo": din("KTo", [NH, DH, TOK], BF16), "Vo": din("Vo", [NH, 128, 32, DH], BF16),
         "kbar": din("kbarf", [DH, NH, NB], F32), "cst": din("cst", [128, CST_N], F32)}
    w_o = din("w_o", [1024, 1024], F32)
    w_in1 = din("w_in1", [1024, 2 * DFF], F32)
    w_out1 = din("w_out1", [DFF, 1024], F32)
    w_in2 = din("w_in2", [1024, 2 * DFF], F32)
    w_out2 = din("w_out2", [DFF, 1024], F32)
    w_pw1 = din("w_pw1", [1024, 2048], F32)
    OT = nc.dram_tensor("OT", [NH, DH, TOK], BF16).ap()
    xa = nc.dram_tensor("xa", [1024, TOK], F32).ap()
    xb = nc.dram_tensor("xb", [1024, TOK], F32).ap()
    x4 = nc.dram_tensor("x4", [1024, TOK], F32, kind="ExternalOutput").ap()
    uT = nc.dram_tensor("uT", [1024, TOK], BF16, kind="ExternalOutput").ap()
    d["OT"] = OT
    d["b_OT"] = [Buf("OT%d" % t) for t in range(8)]
    S = Sched(nc)
    cx = Ctx()
    setup_common(S, cx)
    emit_load_small(S, cx, sp_d)
    modT = [S.sb("modT%d" % i, [128, 72], F32) for i in range(2)]
    b_modT = [Buf("modT%d" % i) for i in range(2)]
    for i in range(2):
        S.op("sp", (lambda i=i: nc.sync.dma_start(out=modT[i][:], in_=mod_d[:, i * 72:(i + 1) * 72])), writes=[b_modT[i]])
    m01 = emit_mod_vectors_i(S, cx, modT[0], b_modT[0], spc(cx, "ng01"), "l0s1", 1)
    m02 = emit_mod_vectors_i(S, cx, modT[0], b_modT[0], spc(cx, "ng02"), "l0s2", 2)
    m10 = emit_mod_vectors_i(S, cx, modT[1], b_modT[1], spc(cx, "ng10"), "l1s0", 0)
    m11 = emit_mod_vectors_i(S, cx, modT[1], b_modT[1], spc(cx, "ng11"), "l1s1", 1)
    n0 = len(S._stack)
    emit_attn_phase(S, cx, d)
    S.barrier_free(len(S._stack) - n0)
    alloc_tile_bufs(S, cx)
    b_x1 = [Buf("x1_%d" % t) for t in range(8)]
    b_xa = [Buf("xa_%d" % t) for t in range(8)]
    b_xb = [Buf("xb_%d" % t) for t in range(8)]
    b_x4 = [Buf("x4_%d" % t) for t in range(8)]
    n1 = len(S._stack)
    emit_proj_res_phase(S, cx, x1, b_x1, xa, b_xa, OT, d["b_OT"], w_o, m01[2], None, m01[3], "wo", True)
    S.barrier_free(len(S._stack) - n1)
    alloc_ffn_bufs(S, cx)
    emit_ffn_phase(S, cx, xa, b_xa, xb, b_xb, w_in1, w_out1, m02[0], m02[1], m02[2], 0.5, m02[3], 8, "f01")
    emit_ffn_phase(S, cx, xb, b_xb, x4, b_x4, w_in2, w_out2, m10[0], m10[1], m10[2], 0.5, m10[3], 8, "f10")
    S.barrier_free(len(S._stack) - n1)
    emit_glu_phase(S, cx, x4, b_x4, w_pw1, m11[0], m11[1], m11[3], uT)
    S.emit()
    return nc


def emit_conv_phase(S, cx, uext_d, x_in, b_xin, x_out, b_xout, w2_d, ident_d, G, b_mod, u_scr=None, b_u=None):
    nc = S.nc
    w2 = S.sb("w_pw2", [128, KC, D], BF16)
    b_w2 = Buf("w_pw2")
    for kc in range(KC):
        S.op("pool_dma", (lambda kc=kc: nc.gpsimd.dma_start(out=w2[:, kc, :], in_=w2_d[kc * 128:(kc + 1) * 128, :])), writes=[b_w2])
    ident = S.sb("identc", [128, 128], F32)
    b_id = Buf("identc")
    S.op("sp", lambda: nc.sync.dma_start(out=ident[:], in_=ident_d[:, :]), writes=[b_id])
    dg = S.sb("dg", [128, KC * CW, 128], BF16)
    b_dg = Buf("dg")
    wdw = spc(cx, "w_dw")
    for ck in range(KC * CW):
        S.op("dve", (lambda ck=ck: nc.vector.tensor_scalar(out=dg[:, ck, :], in0=ident[:], scalar1=wdw[:, ck:ck + 1], scalar2=None, op0=ALU.mult)),
             reads=[b_id, cx.b_sp], writes=[b_dg])
    ue = [S.sb("ue%d" % i, [128, KC, 288], BF16) for i in range(2)]
    b_ue = [Buf("ue%d" % i) for i in range(2)]
    vs = S.sb("vs", [128, KC, 256], F32)
    b_vs = [Buf("vs%d" % c) for c in range(KC)]
    vb = [S.sb("vb%d" % i, [128, 256], BF16) for i in range(2)]
    b_vb = [Buf("vb%d" % i) for i in range(2)]
    vq = [S.sb("vq%d" % i, [128, 256], BF16) for i in range(2)]
    b_vq = [Buf("vq%d" % i) for i in range(2)]
    mu = S.sb("mu", [128, 256], F32)
    rs = S.sb("rs", [128, 256], F32)
    b_st = Buf("lnstat")
    uv = uext_d.rearrange("(c p) l e -> p c l e", p=128) if uext_d is not None else None
    usv = u_scr.rearrange("(c p) t -> p c t", p=128) if u_scr is not None else None
    xv_in = x_in.rearrange("(c p) t -> p c t", p=128)
    xv_out = x_out.rearrange("(c p) t -> p c t", p=128)
    for l in range(NLB):
        u_ = ue[l % 2]
        if usv is None:
            S.op("sp", (lambda u_=u_, l=l: nc.sync.dma_start(out=u_[:], in_=uv[:, :, l, :])), writes=[b_ue[l % 2]])
        else:
            S.op("sp", (lambda u_=u_, l=l: nc.sync.dma_start(out=u_[:, :, 32:288], in_=usv[:, :, l * 256:(l + 1) * 256])),
                 reads=[b_u[l // 2]], writes=[b_ue[l % 2]])
            S.op("sp", (lambda u_=u_, l=l: nc.sync.dma_start(out=u_[:, :, 0:32], in_=usv[:, :, TOK + l * 32:TOK + (l + 1) * 32])),
                 reads=[b_u[8]], writes=[b_ue[l % 2]])
        xt, bx = load_x_tile(S, cx, xv_in, b_xin, l, n=256)
        for c in range(KC):
            k2 = c % 2
            pc = cx.psum[1 + k2]

            def mmc(c=c, pc=pc, u_=u_):
                for k in range(CW):
                    m = nc.tensor.matmul(pc[:, 0:256], lhsT=dg[:, c * CW + k, :], rhs=u_[:, c, 2 + k:2 + k + 256], start=(k == 0), stop=(k == CW - 1))
                return m
            S.op("pe", mmc, reads=[b_dg, b_ue[l % 2]], writes=[cx.b_psum[1 + k2]])
            S.op("act", (lambda c=c, pc=pc: nc.scalar.activation(out=vs[:, c, :], in_=pc[:, 0:256], func=AF.Identity, bias=spc(cx, "b_dw")[:, c:c + 1])),
                 reads=[cx.b_psum[1 + k2], cx.b_sp], writes=[b_vs[c]])
            S.op("pool", (lambda c=c, k2=k2: nc.gpsimd.tensor_copy(out=vb[k2][:], in_=vs[:, c, :])), reads=[b_vs[c]], writes=[b_vb[k2]])
            S.op("act", (lambda c=c, k2=k2: nc.scalar.activation(out=vq[k2][:], in_=vs[:, c, :], func=AF.Square)), reads=[b_vs[c]], writes=[b_vq[k2]])
            S.op("pe", (lambda c=c, k2=k2: nc.tensor.matmul(cx.psum[3][:, 0:256], lhsT=cx.ones_bf[:], rhs=vb[k2][:], start=(c == 0), stop=(c == KC - 1))),
                 reads=[b_vb[k2], cx.b_ones], writes=[cx.b_psum[3]])
            S.op("pe", (lambda c=c, k2=k2: nc.tensor.matmul(cx.psum[4][:, 0:256], lhsT=cx.ones_bf[:], rhs=vq[k2][:], start=(c == 0), stop=(c == KC - 1))),
                 reads=[b_vq[k2], cx.b_ones], writes=[cx.b_psum[4]])
        S.op("dve", lambda: nc.vector.tensor_scalar(out=mu[:], in0=cx.psum[3][:, 0:256], scalar1=1.0 / D, scalar2=None, op0=ALU.mult),
             reads=[cx.b_psum[3]], writes=[b_st])
        S.op("dve", lambda: nc.vector.tensor_tensor(out=rs[:], in0=mu[:], in1=mu[:], op=ALU.mult), reads=[b_st], writes=[b_st])
        S.op("dve", lambda: nc.vector.scalar_tensor_tensor(out=rs[:], in0=cx.psum[4][:, 0:256], scalar=1.0 / D, in1=rs[:], op0=ALU.mult, op1=ALU.subtract),
             reads=[cx.b_psum[4], b_st], writes=[b_st])
        S.op("dve", lambda: nc.vector.tensor_scalar(out=rs[:], in0=rs[:], scalar1=EPS, scalar2=None, op0=ALU.add), reads=[b_st], writes=[b_st])
        S.op("act", lambda: nc.scalar.activation(out=rs[:], in_=rs[:], func=AF.Sqrt), reads=[b_st], writes=[b_st])
        S.op("dve", lambda: nc.vector.reciprocal(out=rs[:], in_=rs[:]), reads=[b_st], writes=[b_st])
        for c in range(KC):
            tmp = cx.tmp[c % 2]
            S.op("dve", (lambda c=c, tmp=tmp: nc.vector.tensor_tensor(out=tmp[:, 0:256], in0=vs[:, c, :], in1=mu[:], op=ALU.subtract)),
                 reads=[b_vs[c], b_st], writes=[cx.b_tmp[c % 2]])
            S.op("dve", (lambda c=c, tmp=tmp: nc.vector.tensor_tensor(out=tmp[:, 0:256], in0=tmp[:, 0:256], in1=rs[:], op=ALU.mult)),
                 reads=[b_st, cx.b_tmp[c % 2]], writes=[cx.b_tmp[c % 2]])
            S.op("act", (lambda c=c, tmp=tmp: nc.scalar.activation(out=cx.h[:, c, 0:256], in_=tmp[:, 0:256], func=AF.Silu,
                                                                   bias=spc(cx, "ln_b")[:, c:c + 1], scale=spc(cx, "ln_g")[:, c:c + 1])),
                 reads=[cx.b_tmp[c % 2], cx.b_sp], writes=[cx.b_h])
        for oc in range(KC):
            py = 5 + (oc % 2)

            def mm(oc=oc, py=py):
                for kc in range(KC):
                    m = nc.tensor.matmul(cx.psum[py][:, 0:256], lhsT=w2[:, kc, oc * 128:(oc + 1) * 128], rhs=cx.h[:, kc, 0:256], start=(kc == 0), stop=(kc == KC - 1))
                return m
            S.op("pe", mm, reads=[b_w2, cx.b_h], writes=[cx.b_psum[py]])
            sa = cx.sa[oc % 2]
            S.op("act", (lambda oc=oc, py=py, sa=sa: nc.scalar.activation(out=sa[:, 0:256], in_=cx.psum[py][:, 0:256], func=AF.Identity,
                                                                        bias=spc(cx, "b_pw2")[:, oc:oc + 1])),
                 reads=[cx.b_psum[py], cx.b_sp], writes=[cx.b_sa[oc % 2]])
            S.op("dve", (lambda oc=oc, sa=sa, xt=xt: nc.vector.scalar_tensor_tensor(
                out=xt[:, oc, 0:256], in0=sa[:, 0:256], scalar=G[:, oc:oc + 1], in1=xt[:, oc, 0:256], op0=ALU.mult, op1=ALU.add)),
                reads=[cx.b_sa[oc % 2], b_mod], writes=[bx])
        S.op("sp", (lambda xt=xt, l=l: nc.sync.dma_start(out=xv_out[:, :, l * 256:(l + 1) * 256], in_=xt[:, :, 0:256])),
             reads=[bx], writes=[b_xout[l // 2]])


def build_L3():
    nc = bass.Bass("TRN2", target_bir_lowering=False)
    din = lambda n, s, dt: nc.dram_tensor(n, s, dt, kind="ExternalInput").ap()
    sp_d = din("sp", [128, SP_N], F32)
    mod_d = din("modi", [128, 144], F32)
    x4 = din("x4", [1024, TOK], F32)
    uext = din("uext", [1024, NLB, 288], BF16)
    ident_d = din("ident", [128, 128], F32)
    w_pw2 = din("w_pw2", [1024, 1024], F32)
    w_in3 = din("w_in3", [1024, 2 * DFF], F32)
    w_out3 = din("w_out3", [DFF, 1024], F32)
    x5 = nc.dram_tensor("x5", [1024, TOK], F32).ap()
    xo = nc.dram_tensor("xo", [1024, TOK], F32, kind="ExternalOutput").ap()
    S = Sched(nc)
    cx = Ctx()
    setup_common(S, cx)
    emit_load_small(S, cx, sp_d)
    modT = [S.sb("modT%d" % i, [128, 72], F32) for i in range(2)]
    b_modT = [Buf("modT%d" % i) for i in range(2)]
    for i in range(2):
        S.op("sp", (lambda i=i: nc.sync.dma_start(out=modT[i][:], in_=mod_d[:, i * 72:(i + 1) * 72])), writes=[b_modT[i]])
    m11 = emit_mod_vectors_i(S, cx, modT[1], b_modT[1], spc(cx, "ng11"), "l1s1", 1)
    m12 = emit_mod_vectors_i(S, cx, modT[1], b_modT[1], spc(cx, "ng12"), "l1s2", 2)
    alloc_tile_bufs(S, cx)
    b_x4 = [Buf("x4_%d" % t) for t in range(16)]
    b_x5 = [Buf("x5_%d" % t) for t in range(8)]
    b_xo = [Buf("xo_%d" % t) for t in range(8)]
    n1 = len(S._stack)
    emit_conv_phase(S, cx, uext, x4, b_x4, x5, b_x5, w_pw2, ident_d, m11[2], m11[3])
    S.barrier_free(len(S._stack) - n1)
    alloc_ffn_bufs(S, cx)
    emit_ffn_phase(S, cx, x5, b_x5, xo, b_xo, w_in3, w_out3, m12[0], m12[1], m12[2], 0.5, m12[3], 8, "f11")
    S.emit()
    return nc


_NC = {}


def _get(name, fn):
    if name not in _NC:
        _NC[name] = fn()
    return _NC[name]


def _deint(a):
    sh = a.shape
    return np.ascontiguousarray(a.reshape(sh[:-1] + (sh[-1] // 256, 128, 2)).swapaxes(-1, -2)).reshape(sh)


def kernel_unfused(**inp):
    import ml_dtypes
    bf = ml_dtypes.bfloat16
    inp = {k: np.asarray(v) for k, v in inp.items()}
    cores = list(range(8))
    toks = []
    for core in cores:
        blocks = snake_blocks(core % 4)
        toks.append(np.concatenate([np.arange(g * 256, (g + 1) * 256) for g in blocks]))
    sps = [pack_small(inp, c // 4) for c in cores]
    nc1 = _get("L1", build_L1)
    maps = [{"sp": sps[c], "ada_w": inp["ada_w"], "xin": np.ascontiguousarray(inp["x"][c // 4][toks[c]].T),
             "w_in": inp["ffn_w_in"][0, 0], "w_out": inp["ffn_w_out"][0, 0], "wqkv": inp["attn_w_qkv"][0]} for c in cores]
    r1 = run_bass_kernel_spmd(nc1, maps, core_ids=cores).results
    ind = np.zeros((64, SEQ), bf)
    for m in range(64):
        ind[m, m * 256:(m + 1) * 256] = 1
    ident = np.eye(128, dtype=np.float32)
    maps2 = []
    per_batch = {}
    for b in range(2):
        KTf = np.zeros((NH, DH, SEQ), bf)
        Vf = np.zeros((SEQ, D), bf)
        kbf = np.zeros((DH, NH, NB), np.float32)
        for j in range(4):
            c = 4 * b + j
            blocks = snake_blocks(j)
            KT = np.asarray(r1[c]["KT"])
            V = np.asarray(r1[c]["V"])
            kb = np.asarray(r1[c]["kbar"])
            for l, g in enumerate(blocks):
                KTf[:, :, g * 256:(g + 1) * 256] = KT[:, :, l * 256:(l + 1) * 256]
                Vf[g * 256:(g + 1) * 256] = V[l * 256:(l + 1) * 256]
                kbf[:, :, g] = kb[:, :, l].T
        KTf = _deint(KTf)
        Vd = np.ascontiguousarray(Vf.reshape(NB, 128, 2, NH, DH).transpose(3, 1, 0, 2, 4)).reshape(NH, 128, 128, DH)
        per_batch[b] = (KTf, Vd, kbf)
    for c in cores:
        b = c // 4
        KTf, Vd, kbf = per_batch[b]
        KTo = _deint(np.asarray(r1[c]["KT"]))
        V = np.asarray(r1[c]["V"])
        Vo = np.ascontiguousarray(V.reshape(NLB, 128, 2, NH, DH).transpose(3, 1, 0, 2, 4)).reshape(NH, 128, 32, DH)
        maps2.append({"sp": sps[c], "modi": r1[c]["modo"], "x1": r1[c]["x1"], "KTf": KTf, "ind": ind, "Vd": Vd,
                      "QT": r1[c]["QT"], "KTo": KTo, "Vo": Vo, "kbarf": kbf, "cst": make_cst(c % 4),
                      "w_o": inp["attn_w_o"][0], "w_in1": inp["ffn_w_in"][0, 1], "w_out1": inp["ffn_w_out"][0, 1],
                      "w_in2": inp["ffn_w_in"][1, 0], "w_out2": inp["ffn_w_out"][1, 0], "w_pw1": inp["conv_w_pw1"][0]})
    nc2 = _get("L2", build_L2)
    r2 = run_bass_kernel_spmd(nc2, maps2, core_ids=cores).results
    maps3 = []
    for b in range(2):
        Uf = np.zeros((D, SEQ), bf)
        for j in range(4):
            c = 4 * b + j
            Uf[:, toks[c]] = np.asarray(r2[c]["uT"])
        for j in range(4):
            c = 4 * b + j
            ue = np.zeros((D, NLB, 288), bf)
            for l, g in enumerate(snake_blocks(j)):
                ue[:, l, 32:] = Uf[:, g * 256:(g + 1) * 256]
                if g > 0:
                    ue[:, l, :32] = Uf[:, g * 256 - 32:g * 256]
            maps3.append({"sp": sps[c], "modi": r1[c]["modo"], "x4": r2[c]["x4"], "uext": ue, "ident": ident,
                          "w_pw2": inp["conv_w_pw2"][0], "w_in3": inp["ffn_w_in"][1, 1], "w_out3": inp["ffn_w_out"][1, 1]})
    nc3 = _get("L3", build_L3)
    r3 = run_bass_kernel_spmd(nc3, maps3, core_ids=cores).results
    out = np.zeros((BATCH, SEQ, D), np.float32)
    for c in cores:
        out[c // 4][toks[c]] = np.asarray(r3[c]["xo"]).T
    return out


NPOS = 80
NT_ALL = 40
TOKX = TOK + 512


def pos_order(j):
    real = []
    own = snake_blocks(j)
    for i in range(8):
        A, B = 8 * i + j, 8 * i + 7 - j
        real += [A, B] + [g for g in range(8 * i, 8 * i + 8) if g not in (A, B)]
    dup = [(g - 1 if g > 0 else None) for g in own]
    return real, dup


def emit_kvq_phase(S, cx, x_d, b_x, wqkv_d, A, Bv, b_mod, QT_d, KT_d, kbar_d, V_d, x1o_d, b_x1o, b_kv):
    nc = S.nc
    wq = S.sb("wqkv_sb", [128, KC, 3072], BF16)
    b_wq = [Buf("wqkv%d" % k) for k in range(KC)]
    for kc in range(KC):
        S.op("pool_dma", (lambda kc=kc: nc.gpsimd.dma_start(out=wq[:, kc, :], in_=wqkv_d[kc * 128:(kc + 1) * 128, :])), writes=[b_wq[kc]])
    bo = S.sb("blkones", [128, 128], BF16)
    b_bo = Buf("blkones")
    S.op("dve", lambda: nc.vector.memset(bo[:], 0.0), writes=[b_bo])
    S.op("dve", lambda: nc.vector.memset(bo[0:64, 0:64], 1.0), writes=[b_bo])
    S.op("dve", lambda: nc.vector.memset(bo[64:128, 64:128], 1.0), writes=[b_bo])
    g8 = S.sb("g8", [128, 2], F32)
    b_g8 = Buf("g8")
    S.op("dve", lambda: nc.vector.tensor_scalar(out=g8[:, 0:1], in0=spc(cx, "gq"), scalar1=0.125, scalar2=None, op0=ALU.mult),
         reads=[cx.b_sp], writes=[b_g8])
    S.op("dve", lambda: nc.vector.tensor_copy(out=g8[:, 1:2], in_=spc(cx, "gk")), reads=[cx.b_sp], writes=[b_g8])
    qo = [S.sb("qo%d" % i, [128, 512], BF16) for i in range(2)]
    b_qo = [Buf("qo%d" % i) for i in range(2)]
    kb = [S.sb("kb%d" % i, [128, 2], F32) for i in range(2)]
    b_kb = [Buf("kb%d" % i) for i in range(2)]
    vt = [S.sb("vt%d" % i, [128, 1024], BF16) for i in range(2)]
    b_vt = [Buf("vt%d" % i) for i in range(2)]
    xv = x_d.rearrange("(c p) t -> p c t", p=128)
    xov = x1o_d.rearrange("(c p) t -> p c t", p=128)
    QTv = QT_d.rearrange("h d t -> (h d) t")
    KTv = KT_d.rearrange("h d t -> (h d) t")
    cnt = 0
    for t in range(NT_ALL):
        is_own = (t < 32 and t % 4 == 0)
        is_dup = t >= 32
        xt, bx = load_x_tile(S, cx, xv, b_x, t)
        if is_own:
            S.op("sp", (lambda xt=xt, t=t: nc.sync.dma_start(out=xov[:, :, (t // 4) * 512:(t // 4 + 1) * 512], in_=xt[:])),
                 reads=[bx], writes=[b_x1o[t // 4]])
        if is_dup:
            i2 = t - 32
            for k2 in range(2):
                S.op("sp", (lambda xt=xt, i2=i2, k2=k2: nc.sync.dma_start(
                    out=xov[:, :, TOK + (2 * i2 + k2) * 32:TOK + (2 * i2 + k2 + 1) * 32], in_=xt[:, :, k2 * 256 + 224:k2 * 256 + 256])),
                    reads=[bx], writes=[b_x1o[8]])
        emit_norm_mod(S, cx, xt, bx, A, Bv, b_mod)
        items = [(w_, hp) for w_ in range(2) for hp in range(8) if not (w_ == 0 and not (is_own or is_dup))]
        base_cnt = cnt
        cnt += len(items)

        def st_A(ii, base_cnt=base_cnt, items=items):
            which, hp = items[ii]
            k = (base_cnt + ii) % 2
            goff = which * 1024
            pq = cx.psum[1 + k]

            def mm():
                for kc in range(KC):
                    m = nc.tensor.matmul(pq[:], lhsT=wq[:, kc, goff + hp * 128:goff + (hp + 1) * 128], rhs=cx.h[:, kc, :],
                                         start=(kc == 0), stop=(kc == KC - 1))
                return m
            S.op("pe", mm, reads=b_wq + [cx.b_h], writes=[cx.b_psum[1 + k]])
            S.op("act", (lambda: nc.scalar.activation(out=cx.sq[k][:], in_=pq[:], func=AF.Square)),
                 reads=[cx.b_psum[1 + k]], writes=[cx.b_sq[k]])

        def st_B(ii, base_cnt=base_cnt, items=items, t=t, is_own=is_own):
            which, hp = items[ii]
            k = (base_cnt + ii) % 2
            pq = cx.psum[1 + k]
            pss = cx.psum[3 + k]
            S.op("pe", (lambda: nc.tensor.matmul(pss[:], lhsT=bo[:], rhs=cx.sq[k][:], start=True, stop=True)),
                 reads=[cx.b_sq[k], b_bo], writes=[cx.b_psum[3 + k]])
            r1 = cx.tmp[k]
            S.op("dve", (lambda: nc.vector.tensor_scalar(out=r1[:], in0=pss[:], scalar1=1.0 / DH, scalar2=EPS, op0=ALU.mult, op1=ALU.add)),
                 reads=[cx.b_psum[3 + k]], writes=[cx.b_tmp[k]])
            S.op("act", (lambda: nc.scalar.activation(out=r1[:], in_=r1[:], func=AF.Ln)), reads=[cx.b_tmp[k]], writes=[cx.b_tmp[k]])
            S.op("act", (lambda: nc.scalar.activation(out=r1[:], in_=r1[:], func=AF.Exp, scale=-0.5)), reads=[cx.b_tmp[k]], writes=[cx.b_tmp[k]])
            S.op("dve", (lambda: nc.vector.tensor_tensor(out=r1[:], in0=pq[:], in1=r1[:], op=ALU.mult)),
                 reads=[cx.b_tmp[k], cx.b_psum[1 + k]], writes=[cx.b_tmp[k]])
            sa = cx.sa[k]
            S.op("act", (lambda: nc.scalar.activation(out=sa[:], in_=r1[:], func=AF.Copy, scale=g8[:, which:which + 1])),
                 reads=[cx.b_tmp[k], b_g8], writes=[cx.b_sa[k]])
            if which == 0:
                S.op("pool", (lambda: nc.gpsimd.tensor_copy(out=qo[k][:], in_=sa[:])), reads=[cx.b_sa[k]], writes=[b_qo[k]])
                if is_own:
                    for k2 in range(2):
                        lb = 2 * (t // 4) + k2
                        S.op("sp", (lambda k2=k2, lb=lb: nc.sync.dma_start(out=QTv[hp * 128:(hp + 1) * 128, lb * 288:lb * 288 + 256],
                                                                         in_=qo[k][:, k2 * 256:(k2 + 1) * 256])),
                             reads=[b_qo[k]], writes=[b_kv])
                else:
                    i2 = t - 32
                    for k2 in range(2):
                        lb = 2 * i2 + k2
                        S.op("sp", (lambda k2=k2, lb=lb: nc.sync.dma_start(
                            out=QTv[hp * 128:(hp + 1) * 128, lb * 288 + 256:lb * 288 + 288],
                            in_=qo[k][:, k2 * 256 + 224:k2 * 256 + 256])), reads=[b_qo[k]], writes=[b_kv])
            else:
                S.op("pool", (lambda: nc.gpsimd.tensor_copy(
                    out=qo[k][:].rearrange("q (b c p) -> q b c p", b=2, c=2),
                    in_=sa[:].rearrange("q (b p c) -> q b c p", b=2, c=2))), reads=[cx.b_sa[k]], writes=[b_qo[k]])
                S.op("sp", (lambda: nc.sync.dma_start(out=KTv[hp * 128:(hp + 1) * 128, t * 512:(t + 1) * 512], in_=qo[k][:])),
                     reads=[b_qo[k]], writes=[b_kv])
                S.op("dve", (lambda: nc.vector.tensor_reduce(out=kb[k][:], in_=sa[:].rearrange("p (b t) -> p b t", b=2), axis=AX.X, op=ALU.add)),
                     reads=[cx.b_sa[k]], writes=[b_kb[k]])
                S.op("dve", (lambda: nc.vector.tensor_scalar(out=kb[k][:], in0=kb[k][:], scalar1=1.0 / BLK, scalar2=None, op0=ALU.mult)),
                     reads=[b_kb[k]], writes=[b_kb[k]])
                S.op("sp", (lambda: nc.sync.dma_start(out=kbar_d[hp * 128:(hp + 1) * 128, 2 * t:2 * t + 2], in_=kb[k][:])),
                     reads=[b_kb[k]], writes=[b_kv])
        st_A(0)
        for ii in range(len(items)):
            if ii + 1 < len(items):
                st_A(ii + 1)
            st_B(ii)
        for tg in range(4):
            v = vt[tg % 2]
            for ns in range(2):
                pv = cx.psum[5 + ns]

                def mmv(tg=tg, ns=ns, pv=pv):
                    for kc in range(KC):
                        m = nc.tensor.matmul(pv[:], lhsT=cx.h[:, kc, tg * 128:(tg + 1) * 128], rhs=wq[:, kc, 2048 + ns * 512:2048 + (ns + 1) * 512],
                                             start=(kc == 0), stop=(kc == KC - 1))
                    return m
                S.op("pe", mmv, reads=b_wq + [cx.b_h], writes=[cx.b_psum[5 + ns]])
                if ns == 0:
                    S.op("act", (lambda v=v, pv=pv: nc.scalar.copy(out=v[:, 0:512], in_=pv[:])), reads=[cx.b_psum[5]], writes=[b_vt[tg % 2]])
                else:
                    S.op("dve", (lambda v=v, pv=pv: nc.vector.tensor_copy(out=v[:, 512:1024], in_=pv[:])), reads=[cx.b_psum[6]], writes=[b_vt[tg % 2]])
            blk = 2 * t + tg // 2
            hf = tg % 2
            for c2 in range(2):
                dst = V_d[:, hf * 64:(hf + 1) * 64, 2 * blk + c2, :].rearrange("h pp d -> pp h d")
                S.op("sp", (lambda v=v, dst=dst, c2=c2: nc.sync.dma_start(out=dst, in_=v[c2:128:2, :].rearrange("p (h d) -> p h d", h=NH))),
                     reads=[b_vt[tg % 2]], writes=[b_kv])


CST2_OFF = {}


def _cst2_layout():
    off = 0
    for name, w in (("past", 2048), ("base", 1024), ("irowX", 288), ("Dm", 512), ("causal", 512),
                    ("DmH", 64), ("causalH", 64), ("ident", 128), ("sel65", 64)):
        CST2_OFF[name] = (off, w)
        off += w
    return off


CST2_N = _cst2_layout()


def make_cst2(j):
    real, dup = pos_order(j)
    own = snake_blocks(j)
    c = np.zeros((128, CST2_N), np.float32)
    p = np.arange(128, dtype=np.float32)[:, None]
    gpos = np.array(real)
    past = np.zeros((32, 64), np.float32)
    base = np.zeros((128, 16, 64), np.float32)
    for row in range(32):
        o = own[row] if row < 16 else own[row - 16] - 1
        valid = gpos < o
        past[row] = np.where(valid, 0.0, NEG)
        if row < 16:
            dist = np.where(valid, o - gpos, 1).astype(np.float32)
            base[:, row, :] = 2.0 * p - 256.0 * dist[None, :]

    def put(name, arr):
        o_, w = CST2_OFF[name]
        c[:, o_:o_ + w] = arr.reshape(arr.shape[0], -1)
    put("past", np.tile(past.reshape(1, 2048), (128, 1)))
    put("base", base.reshape(128, 1024))
    irx = np.concatenate([np.arange(256, dtype=np.float32), -(32.0 - np.arange(32, dtype=np.float32))])
    put("irowX", np.tile(irx[None, :], (128, 1)))
    pp = np.arange(128, dtype=np.float32)[:, None, None]
    cc = np.arange(2, dtype=np.float32)[None, :, None]
    i = np.arange(256, dtype=np.float32)[None, None, :]
    put("Dm", np.broadcast_to(i - 2 * pp, (128, 2, 256)).copy())
    put("causal", np.where(i >= 2 * pp + cc, 0.0, NEG).astype(np.float32))
    ih = 224 + np.arange(32, dtype=np.float32)[None, None, :]
    put("DmH", np.broadcast_to(ih - 2 * pp, (128, 2, 32)).copy())
    put("causalH", np.where(ih >= 2 * pp + cc, 0.0, NEG).astype(np.float32))
    put("ident", np.eye(128, dtype=np.float32))
    s65 = np.zeros((128, 64), np.float32)
    s65[64] = 1.0
    put("sel65", s65)
    hmask = np.ones((128, 512), np.float32)
    for l in range(16):
        if dup[l] is None:
            hmask[:, l * 32:(l + 1) * 32] = 0.0
    return c, hmask


def emit_attn2(S, cx, d):
    nc = S.nc
    cst = S.sb("cst2", [128, CST2_N], F32)
    b_cst = Buf("cst2")
    S.op("sp", lambda: nc.sync.dma_start(out=cst[:], in_=d["cst"][:, :]), writes=[b_cst])

    def C(name):
        o, w = CST2_OFF[name]
        return cst[:, o:o + w]
    kbh = S.sb("kbh", [64, NH * NB], BF16)
    kbl = S.sb("kbl", [64, NH * NB], BF16)
    b_kb = Buf("kbhl")
    n0 = len(S._stack)
    kb32 = S.sb("kb32", [64, NH * NB], F32)
    kbt = S.sb("kbt", [64, NH * NB], F32)
    S.op("sp", lambda: nc.sync.dma_start(out=kb32[:].rearrange("d (h m) -> d h m", h=NH),
                                         in_=d["kbar"].rearrange("(h d) m -> d h m", h=NH)[:, :, 0:NB]), reads=[d["b_kv"]], writes=[b_kb])
    S.op("dve", lambda: nc.vector.tensor_copy(out=kbh[:], in_=kb32[:]), reads=[b_kb], writes=[b_kb])
    S.op("dve", lambda: nc.vector.tensor_copy(out=kbt[:], in_=kbh[:]), reads=[b_kb], writes=[b_kb])
    S.op("dve", lambda: nc.vector.tensor_tensor(out=kbt[:], in0=kb32[:], in1=kbt[:], op=ALU.subtract), reads=[b_kb], writes=[b_kb])
    S.op("dve", lambda: nc.vector.tensor_copy(out=kbl[:], in_=kbt[:]), reads=[b_kb], writes=[b_kb])
    S.barrier_free(len(S._stack) - n0)
    KTa = [S.sb("KTa%d" % i, [128, SEQ], BF16) for i in range(2)]
    Va = [S.sb("Va%d" % i, [128, 128, 65], BF16) for i in range(2)]
    Qa = [S.sb("Qa%d" % i, [128, TOKX], BF16) for i in range(2)]
    Kd = [S.sb("Kd%d" % i, [128, TOK], BF16) for i in range(2)]
    Vdd = [S.sb("Vdd%d" % i, [128, 32, 65], BF16) for i in range(2)]
    bT = [S.sb("bT%d" % i, [128, 1024], F32) for i in range(2)]
    dgb = [S.sb("dgb%d" % i, [128, 512], F32) for i in range(2)]
    dgbH = [S.sb("dgbH%d" % i, [128, 64], F32) for i in range(2)]
    cq = [S.sb("cq%d" % i, [128, 288], F32) for i in range(2)]
    b_KT = [Buf("KTa%d" % i) for i in range(2)]
    b_Va = [Buf("Va%d" % i) for i in range(2)]
    b_Qa = [Buf("Qa%d" % i) for i in range(2)]
    b_Qm = [[Buf("Qm%d_%d" % (i, l)) for l in range(16)] for i in range(2)]
    b_Kd = [Buf("Kd%d" % i) for i in range(2)]
    b_Vdd = [Buf("Vdd%d" % i) for i in range(2)]
    b_hd = [Buf("hd%d" % i) for i in range(2)]
    mbp = [S.sb("mbp%d" % i, [128, 128], F32) for i in range(2)]
    b_mbp = [Buf("mbp%d" % i) for i in range(2)]
    gmt = [S.sb("gmt%d" % i, [128, 64], F32) for i in range(2)]
    b_gmt = [Buf("gmt%d" % i) for i in range(2)]
    t8 = [S.sb("t8_%d" % i, [128, 8], F32) for i in range(2)]
    pt = [S.sb("pt%d" % i, [128, 2, 288], BF16) for i in range(2)]
    b_pt = [Buf("pt%d" % i) for i in range(2)]
    pt2 = S.sb("pt2", [128, 512], BF16)
    b_pt2 = Buf("pt2")
    dtmp = S.sb("dtmp", [128, 512], F32)
    b_dtmp = Buf("dtmp")
    o1 = S.sb("o1", [128, 288], F32)
    o2 = S.sb("o2", [128, 288], F32)
    rd = S.sb("rd", [128, 288], F32)
    ob = [S.sb("ob%d" % i, [128, 288], BF16) for i in range(2)]
    b_o = Buf("o12")
    b_ob = [Buf("ob%d" % i) for i in range(2)]
    for i in range(2):
        S.op("sp", (lambda i=i: nc.sync.dma_start(out=KTa[i][64:128, :], in_=d["ind"][:, :])), writes=[b_KT[i]])
        S.op("dve", (lambda i=i: nc.vector.memset(Va[i][:, :, 64:65], 1.0)), writes=[b_Va[i]])
        S.op("dve", (lambda i=i: nc.vector.memset(Vdd[i][:, :, 64:65], 1.0)), writes=[b_Vdd[i]])
        S.op("dve", (lambda i=i: nc.vector.memset(mbp[i][:], 0.0)), writes=[b_mbp[i]])
    ps = cx.psum
    bp = cx.b_psum
    SX = [cx.psall[:, 0:1024], cx.psall[:, 1024:2048]]
    b_SX = [[bp[0], bp[1]], [bp[2], bp[3]]]
    scnt = 0
    ocnt = 0
    NQ = 288
    for h in range(NH):
        cur = h % 2
        sl = SLOPES[h]
        es = math.exp(sl)
        S.op("sp", (lambda h=h, cur=cur: nc.sync.dma_start(out=KTa[cur][0:64, :], in_=d["KT"][h, :, 0:SEQ])), reads=[d["b_kv"]], writes=[b_KT[cur]])
        for q8 in range(8):
            S.op("sp", (lambda h=h, cur=cur, q8=q8: nc.sync.dma_start(out=Va[cur][:, q8 * 16:(q8 + 1) * 16, 0:64], in_=d["Vd"][h, :, q8 * 16:(q8 + 1) * 16, :])),
                 reads=[d["b_kv"]], writes=[b_Va[cur]])
        S.op("sp", (lambda h=h, cur=cur: nc.sync.dma_start(out=Qa[cur][0:64, :], in_=d["QT"][h])), reads=[d["b_kv"]], writes=[b_Qa[cur]] + b_Qm[cur])
        S.op("sp", (lambda h=h, cur=cur: nc.sync.dma_start(out=Kd[cur][0:64, :], in_=d["KT"][h, :, SEQ:SEQ + TOK])), reads=[d["b_kv"]], writes=[b_Kd[cur]])
        for q2 in range(2):
            S.op("sp", (lambda h=h, cur=cur, q2=q2: nc.sync.dma_start(out=Vdd[cur][:, q2 * 16:(q2 + 1) * 16, 0:64],
                                                                     in_=d["Vd"][h, :, 128 + q2 * 16:128 + (q2 + 1) * 16, :])),
                 reads=[d["b_kv"]], writes=[b_Vdd[cur]])
        for (VV, bV) in ((Va[cur], b_Va[cur]), (Vdd[cur], b_Vdd[cur])):
            Vodd = VV[:].rearrange("p (n two) e -> p n two e", two=2)[:, :, 1, :]
            S.op("dve", (lambda Vodd=Vodd, es=es: nc.vector.tensor_scalar(out=Vodd[:, :, 0:64], in0=Vodd[:, :, 0:64], scalar1=es, scalar2=None, op0=ALU.mult)),
                 reads=[bV], writes=[bV])
            S.op("dve", (lambda Vodd=Vodd, es=es: nc.vector.memset(Vodd[:, :, 64:65], es)), reads=[bV], writes=[bV])
        S.op("dve", (lambda cur=cur, sl=sl: nc.vector.tensor_scalar(out=bT[cur][:], in0=C("base"), scalar1=sl, scalar2=None, op0=ALU.mult)),
             reads=[b_cst], writes=[b_hd[cur]])
        S.op("dve", (lambda cur=cur, sl=sl: nc.vector.scalar_tensor_tensor(out=dgb[cur][:], in0=C("Dm"), scalar=-sl, in1=C("causal"),
                                                                         op0=ALU.mult, op1=ALU.add)), reads=[b_cst], writes=[b_hd[cur]])
        S.op("dve", (lambda cur=cur, sl=sl: nc.vector.scalar_tensor_tensor(out=dgbH[cur][:], in0=C("DmH"), scalar=-sl, in1=C("causalH"),
                                                                         op0=ALU.mult, op1=ALU.add)), reads=[b_cst], writes=[b_hd[cur]])
        S.op("act", (lambda cur=cur, sl=sl: nc.scalar.activation(out=cq[cur][:], in_=C("irowX"), func=AF.Exp, scale=-sl)),
             reads=[b_cst], writes=[b_hd[cur]])
        groups = []
        for l in range(NLB):
            groups.append((l * NQ, 128, l, l))
            groups.append((l * NQ + 128, 128, l, l))
            groups.append((l * NQ + 256, 32, 16 + l, l))

        def g_s1(gi, cur=cur, h=h, groups=groups):
            q0, nr, row, qmi = groups[gi]

            def mmg():
                nc.tensor.matmul(ps[6][0:nr, 0:64], lhsT=Qa[cur][0:64, q0:q0 + nr], rhs=kbh[:, h * 64:(h + 1) * 64], start=True, stop=False)
                return nc.tensor.matmul(ps[6][0:nr, 0:64], lhsT=Qa[cur][0:64, q0:q0 + nr], rhs=kbl[:, h * 64:(h + 1) * 64], start=False, stop=True)
            S.op("pe", mmg, reads=[b_Qa[cur], b_kb], writes=[bp[6]])

        def g_s2(gi, groups=groups):
            q0, nr, row, qmi = groups[gi]
            g = gi % 2
            pb = C("past")[0:nr, row * 64:(row + 1) * 64]
            S.op("dve", (lambda: nc.vector.tensor_tensor(out=gmt[g][0:nr, :], in0=ps[6][0:nr, 0:64], in1=pb, op=ALU.add)),
                 reads=[bp[6], b_cst], writes=[b_gmt[g]])
            S.op("dve", (lambda: nc.vector.max(out=t8[g][0:nr, :], in_=gmt[g][0:nr, :])), reads=[b_gmt[g]], writes=[b_gmt[g]])
            S.op("dve", (lambda: nc.vector.tensor_scalar(out=gmt[g][0:nr, :], in0=gmt[g][0:nr, :], scalar1=t8[g][0:nr, 2:3], scalar2=-NEG,
                                                         op0=ALU.is_ge, op1=ALU.mult)), reads=[b_gmt[g]], writes=[b_gmt[g]])
            S.op("dve", (lambda: nc.vector.scalar_tensor_tensor(out=mbp[g][0:nr, 64:128], in0=gmt[g][0:nr, :], scalar=NEG, in1=pb,
                                                                op0=ALU.add, op1=ALU.add)),
                 reads=[b_gmt[g], b_cst], writes=[b_mbp[g]])

        def g_s3(gi, cur=cur, groups=groups):
            q0, nr, row, qmi = groups[gi]
            g = gi % 2
            S.op("pe", (lambda: nc.tensor.transpose(ps[7][:, 0:nr], mbp[g][0:nr, :], C("ident")[0:nr, 0:nr])),
                 reads=[b_mbp[g], b_cst], writes=[bp[7]])
            S.op("act", (lambda: nc.scalar.copy(out=Qa[cur][64:128, q0:q0 + nr], in_=ps[7][64:128, 0:nr])),
                 reads=[bp[7]], writes=[b_Qm[cur][qmi]])
        g_s1(0)
        for gi in range(len(groups)):
            g_s2(gi)
            if gi + 1 < len(groups):
                g_s1(gi + 1)
            g_s3(gi)
        for i in range(8):
            nk = 8 * i + 8
            for k in range(2):
                l = 2 * i + k
                q0 = l * NQ
                po = 4 + k
                sbase = scnt
                scnt += nk

                def emit_S(n, cur=cur, q0=q0, l=l, sbase=sbase):
                    sb_ = (sbase + n) % 2

                    def mms():
                        for c in range(2):
                            m = nc.tensor.matmul(SX[sb_][:, c * 512:c * 512 + NQ], lhsT=KTa[cur][:, n * 256 + c * 128:n * 256 + (c + 1) * 128],
                                                 rhs=Qa[cur][:, q0:q0 + NQ], start=True, stop=True)
                        return m
                    S.op("pe", mms, reads=[b_KT[cur], b_Qa[cur], b_Qm[cur][l]], writes=b_SX[sb_])

                def emit_E(n, cur=cur, l=l, sbase=sbase):
                    sb_ = (sbase + n) % 2
                    S.op("act", (lambda: nc.scalar.activation(out=pt[sb_][:], in_=SX[sb_].rearrange("p (c x) -> p c x", c=2)[:, :, 0:NQ], func=AF.Exp,
                                                              bias=bT[cur][:, l * 64 + n:l * 64 + n + 1])),
                         reads=b_SX[sb_] + [b_hd[cur]], writes=[b_pt[sb_]])

                def emit_PV(n, cur=cur, po=po, nk=nk, sbase=sbase):
                    sb_ = (sbase + n) % 2

                    def mmpv():
                        for c in range(2):
                            m = nc.tensor.matmul(ps[po][0:65, 0:NQ], lhsT=Va[cur][:, 2 * n + c, :], rhs=pt[sb_][:, c, :],
                                                 start=(n == 0 and c == 0), stop=(n == nk - 1 and c == 1))
                        return m
                    S.op("pe", mmpv, reads=[b_Va[cur], b_pt[sb_]], writes=[bp[po]])
                emit_S(0)
                for n in range(nk):
                    emit_E(n)
                    if n + 1 < nk:
                        emit_S(n + 1)
                    emit_PV(n)
                for (Kt, koff, Vt, ch0, bK, bV, dg, qoff, nq, ocol) in (
                        (KTa[cur], (8 * i + k) * 256, Va[cur], 2 * (8 * i + k), b_KT[cur], b_Va[cur], dgb[cur], q0, 256, 0),
                        (Kd[cur], l * 256, Vdd[cur], 2 * l, b_Kd[cur], b_Vdd[cur], dgbH[cur], q0 + 256, 32, 256)):
                    def mmd(Kt=Kt, koff=koff, qoff=qoff, nq=nq, cur=cur):
                        for c in range(2):
                            m = nc.tensor.matmul(ps[6][:, c * nq:(c + 1) * nq], lhsT=Kt[0:64, koff + c * 128:koff + (c + 1) * 128],
                                                 rhs=Qa[cur][0:64, qoff:qoff + nq], start=True, stop=True)
                        return m
                    S.op("pe", mmd, reads=[bK, b_Qa[cur]], writes=[bp[6]])
                    S.op("dve", (lambda dg=dg, nq=nq: nc.vector.tensor_tensor(out=dtmp[:, 0:2 * nq], in0=ps[6][:, 0:2 * nq], in1=dg[:, 0:2 * nq], op=ALU.add)),
                         reads=[bp[6], b_hd[cur]], writes=[b_dtmp])
                    S.op("act", (lambda nq=nq: nc.scalar.activation(out=pt2[:, 0:2 * nq], in_=dtmp[:, 0:2 * nq], func=AF.Exp)), reads=[b_dtmp], writes=[b_pt2])

                    def mmpo(Vt=Vt, ch0=ch0, nq=nq, ocol=ocol):
                        for c in range(2):
                            m = nc.tensor.matmul(ps[7][0:65, ocol:ocol + nq], lhsT=Vt[:, ch0 + c, :], rhs=pt2[:, c * nq:(c + 1) * nq],
                                                 start=(c == 0), stop=(c == 1))
                        return m
                    S.op("pe", mmpo, reads=[bV, b_pt2], writes=[bp[7]])
                S.op("dve", (lambda cur=cur, po=po: nc.vector.tensor_tensor(out=o1[0:65, :], in0=ps[po][0:65, 0:NQ], in1=cq[cur][0:65, :], op=ALU.mult)),
                     reads=[bp[po], b_hd[cur]], writes=[b_o])
                S.op("dve", (lambda: nc.vector.tensor_tensor(out=o2[0:65, :], in0=o1[0:65, :], in1=ps[7][0:65, 0:NQ], op=ALU.add)),
                     reads=[bp[7], b_o], writes=[b_o])
                S.op("pe", (lambda: nc.tensor.matmul(ps[6][0:64, 0:NQ], lhsT=C("sel65")[0:65, :], rhs=o2[0:65, :], start=True, stop=True)),
                     reads=[b_o, b_cst], writes=[bp[6]])
                S.op("dve", (lambda: nc.vector.reciprocal(out=rd[0:64, :], in_=ps[6][0:64, 0:NQ])), reads=[bp[6]], writes=[b_o])
                kk = ocnt % 2
                ocnt += 1
                S.op("dve", (lambda kk=kk: nc.vector.tensor_tensor(out=ob[kk][0:64, :], in0=o2[0:64, :], in1=rd[0:64, :], op=ALU.mult)),
                     reads=[b_o], writes=[b_ob[kk]])
                S.op("sp", (lambda kk=kk, h=h, l=l: nc.sync.dma_start(out=d["OT"][h, :, l * 256:(l + 1) * 256], in_=ob[kk][0:64, 0:256])),
                     reads=[b_ob[kk]], writes=[d["b_OT"][l // 2]])
                S.op("sp", (lambda kk=kk, h=h, l=l: nc.sync.dma_start(out=d["OT"][h, :, TOK + l * 32:TOK + (l + 1) * 32], in_=ob[kk][0:64, 256:288])),
                     reads=[b_ob[kk]], writes=[d["b_OT"][8]])


def build_fused():
    nc = bass.Bass("TRN2", target_bir_lowering=False)
    din = lambda n, s, dt: nc.dram_tensor(n, s, dt, kind="ExternalInput").ap()
    scr = lambda n, s, dt: nc.dram_tensor(n, s, dt).ap()
    sp_d = din("sp", [128, SP_N], F32)
    ada_w = din("ada_w", [2, 1024, 9216], F32)
    xall = din("xall", [1024, NT_ALL * 512], F32)
    w_in = [din("w_in%d" % i, [1024, 2 * DFF], F32) for i in range(4)]
    w_out = [din("w_out%d" % i, [DFF, 1024], F32) for i in range(4)]
    wqkv = din("wqkv", [1024, 3072], F32)
    w_o = din("w_o", [1024, 1024], F32)
    w_pw1 = din("w_pw1", [1024, 2048], F32)
    w_pw2 = din("w_pw2", [1024, 1024], F32)
    ind_d = din("ind", [64, SEQ], BF16)
    cst_d = din("cst", [128, CST2_N], F32)
    hm_d = din("hmask", [128, 512], F32)
    ident_d = din("ident", [128, 128], F32)
    xo = nc.dram_tensor("xo", [1024, TOK], F32, kind="ExternalOutput").ap()
    x1all = scr("x1all", [1024, NT_ALL * 512], F32)
    x1o = scr("x1o", [1024, TOKX], F32)
    xa = scr("xa", [1024, TOKX], F32)
    xb = scr("xb", [1024, TOKX], F32)
    x4 = scr("x4", [1024, TOKX], F32)
    x5 = scr("x5", [1024, TOK], F32)
    uT = scr("uT", [1024, TOKX], BF16)
    KT = scr("KTs", [NH, DH, NPOS * 256], BF16)
    Vd = scr("Vds", [NH, 128, 2 * NPOS, DH], BF16)
    kbar = scr("kbars", [NH * DH, NPOS], F32)
    QT = scr("QTs", [NH, DH, TOKX], BF16)
    OT = scr("OTs", [NH, DH, TOKX], BF16)
    S = Sched(nc)
    cx = Ctx()
    setup_common(S, cx)
    emit_load_small(S, cx, sp_d)
    hmask = S.sb("hmask", [128, 512], F32)
    b_hm = Buf("hmask")
    S.op("sp", lambda: nc.sync.dma_start(out=hmask[:], in_=hm_d[:, :]), writes=[b_hm])
    modT = [S.sb("modT%d" % i, [128, 72], F32) for i in range(2)]
    b_modT = [Buf("modT%d" % i) for i in range(2)]
    emit_adaln_scoped(S, cx, ada_w, modT, b_modT)
    M = {}
    for i in range(2):
        for j in range(3):
            M[(i, j)] = emit_mod_vectors_i(S, cx, modT[i], b_modT[i], spc(cx, "ng%d%d" % (i, j)), "l%ds%d" % (i, j), j)
    nbase = len(S._stack)
    mk = lambda name, n: [Buf("%s_%d" % (name, t)) for t in range(n)]
    b_xall, b_x1all, b_x1o = mk("xall", NT_ALL), mk("x1all", NT_ALL), mk("x1o", 9)
    b_xa, b_xb, b_x4, b_u, b_x5, b_xo = mk("xa", 9), mk("xb", 9), mk("x4", 16), mk("u", 9), mk("x5", 8), mk("xo", 8)
    b_x4t = b_x4[:9]
    alloc_tile_bufs(S, cx)
    ntile = len(S._stack)
    alloc_ffn_bufs(S, cx)
    m = M[(0, 0)]
    emit_ffn_phase(S, cx, xall, b_xall, x1all, b_x1all, w_in[0], w_out[0], m[0], m[1], m[2], 0.5, m[3], NT_ALL, "f00")
    S.barrier_free(len(S._stack) - ntile)
    m = M[(0, 1)]
    b_kv = Buf("kvq")
    emit_kvq_phase(S, cx, x1all, b_x1all, wqkv, m[0], m[1], m[3], QT, KT, kbar, Vd, x1o, b_x1o, b_kv)
    S.barrier_free(len(S._stack) - nbase)
    b_OT = mk("OT", 9)
    emit_attn2(S, cx, {"KT": KT, "ind": ind_d, "Vd": Vd, "QT": QT, "kbar": kbar, "cst": cst_d, "OT": OT, "b_OT": b_OT, "b_kv": b_kv})
    S.barrier_free(len(S._stack) - nbase)
    alloc_tile_bufs(S, cx)
    emit_proj_res_phase(S, cx, x1o, b_x1o, xa, b_xa, OT, b_OT, w_o, m[2], None, m[3], "wo", True, ntiles=9)
    S.barrier_free(len(S._stack) - ntile)
    alloc_ffn_bufs(S, cx)
    m = M[(0, 2)]
    emit_ffn_phase(S, cx, xa, b_xa, xb, b_xb, w_in[1], w_out[1], m[0], m[1], m[2], 0.5, m[3], 9, "f01")
    m = M[(1, 0)]
    emit_ffn_phase(S, cx, xb, b_xb, x4, b_x4t, w_in[2], w_out[2], m[0], m[1], m[2], 0.5, m[3], 9, "f10")
    S.barrier_free(len(S._stack) - ntile)
    m = M[(1, 1)]
    emit_glu_phase(S, cx, x4, b_x4t, w_pw1, m[0], m[1], m[3], uT, ntiles=9, hmask=hmask[:], b_hmask=b_hm, b_uout=b_u)
    S.barrier_free(len(S._stack) - ntile)
    b_x4c = [Buf("x4c_%d" % t) for t in range(16)]
    emit_conv_phase(S, cx, None, x4, b_x4c, x5, b_x5, w_pw2, ident_d, m[2], m[3], u_scr=uT, b_u=b_u)
    S.barrier_free(len(S._stack) - ntile)
    alloc_ffn_bufs(S, cx)
    m = M[(1, 2)]
    emit_ffn_phase(S, cx, x5, b_x5, xo, b_xo, w_in[3], w_out[3], m[0], m[1], m[2], 0.5, m[3], 8, "f11")
    S.emit()
    return nc


def kernel(**inp):
    import ml_dtypes
    bf = ml_dtypes.bfloat16
    inp = {k: np.asarray(v) for k, v in inp.items()}
    cores = list(range(8))
    ind = np.zeros((64, SEQ), bf)
    for m in range(64):
        ind[m, m * 256:(m + 1) * 256] = 1
    ident = np.eye(128, dtype=np.float32)
    maps = []
    toks = []
    for c in cores:
        b, j = c // 4, c % 4
        real, dup = pos_order(j)
        xb_ = inp["x"][b]
        xall = np.zeros((NPOS * 256, D), np.float32)
        for pi, g in enumerate(real + dup):
            if g is not None:
                xall[pi * 256:(pi + 1) * 256] = xb_[g * 256:(g + 1) * 256]
        cst, hmask = make_cst2(j)
        toks.append(np.concatenate([np.arange(g * 256, (g + 1) * 256) for g in snake_blocks(j)]))
        mp = {"sp": pack_small(inp, b), "ada_w": inp["ada_w"], "xall": np.ascontiguousarray(xall.T),
              "wqkv": inp["attn_w_qkv"][0], "w_o": inp["attn_w_o"][0], "w_pw1": inp["conv_w_pw1"][0], "w_pw2": inp["conv_w_pw2"][0],
              "ind": ind, "cst": cst, "hmask": hmask, "ident": ident}
        for k, (i, w) in enumerate(((0, 0), (0, 1), (1, 0), (1, 1))):
            mp["w_in%d" % k] = inp["ffn_w_in"][i, w]
            mp["w_out%d" % k] = inp["ffn_w_out"][i, w]
        maps.append(mp)
    nc = _get("fused", build_fused)
    res = run_bass_kernel_spmd(nc, maps, core_ids=cores).results
    out = np.zeros((BATCH, SEQ, D), np.float32)
    for c in cores:
        out[c // 4][toks[c]] = np.asarray(res[c]["xo"]).T
    return out
```

```python
import math
import numpy as np
import concourse.bass as bass
import concourse.mybir as mybir
from concourse.bass_utils import run_bass_kernel_spmd

F32 = mybir.dt.float32
BF16 = mybir.dt.bfloat16
AF = mybir.ActivationFunctionType
ALU = mybir.AluOpType
AX = mybir.AxisListType

D = 1024
KC = 8
DFF = 2816
FC = 22
NH = 16
DH = 64
BLK = 256
NB = 64
SEQ = 16384
BATCH = 2
TOK = 4096
NLB = 16
EPS = 1e-6
NEG = -30000.0
CW = 31


class Buf:
    __slots__ = ("name", "last_w", "readers")

    def __init__(self, name):
        self.name = name
        self.last_w = None
        self.readers = []


class Sched:
    DMA_ENGS = ("sp", "pool_dma")

    def __init__(self, nc, n_dma_sems=12):
        self.nc = nc
        self.ops = []
        self.engs = {"pe": nc.tensor, "act": nc.scalar, "dve": nc.vector,
                     "pool": nc.gpsimd, "sp": nc.sync, "pool_dma": nc.gpsimd}
        self.stream = {"pe": "pe", "act": "act", "dve": "dve", "pool": "pool",
                       "sp": "sp", "pool_dma": "pool"}
        self.n_dma_sems = n_dma_sems
        self._stack = []
        self.bar_list = []
        cm = nc.sbuf_tensor("bar_t", [128, 8], F32)
        self.bar_t = cm.__enter__()

    def sb(self, name, shape, dt):
        self._uid = getattr(self, "_uid", 0) + 1
        cm = self.nc.sbuf_tensor("s%d_%s" % (self._uid, name), shape, dt)
        t = cm.__enter__()
        self._stack.append(cm)
        return t

    def ps(self, name, shape, dt=F32):
        cm = self.nc.psum_tensor("p_" + name, shape, dt)
        t = cm.__enter__()
        self._stack.append(cm)
        return t

    def barrier_free(self, n_free):
        nc = self.nc
        bb = Buf("barrier%d" % len(self.ops))
        prev = list(range(len(self.ops)))
        i0 = self.op("pool", lambda: nc.gpsimd.memset(self.bar_t[:, 0:1], 0.0), writes=[bb])
        self.ops[i0]["deps"] = set(prev)
        for e, fn in (("dve", lambda: nc.vector.memset(self.bar_t[:, 1:2], 0.0)),
                      ("act", lambda: nc.scalar.activation(out=self.bar_t[:, 2:3], in_=self.bar_t[:, 0:1], func=AF.Copy)),
                      ("pe", None),
                      ("sp", lambda: nc.sync.dma_start(out=self.bar_t[:, 4:5], in_=self.bar_t[:, 0:1]))):
            if fn is None:
                continue
            self.op(e, fn, reads=[bb])
        self.bar_after = i0
        self.bar_list.append(i0)
        for _ in range(n_free):
            cm = self._stack.pop()
            cm.__exit__(None, None, None)

    def op(self, eng, fn, reads=(), writes=()):
        deps = set()
        idx = len(self.ops)
        for b in reads:
            if b.last_w is not None:
                deps.add(b.last_w)
        for b in writes:
            if b.last_w is not None:
                deps.add(b.last_w)
            for r in b.readers:
                deps.add(r)
        for b in reads:
            b.readers.append(idx)
        for b in writes:
            b.last_w = idx
            b.readers = []
        deps.discard(idx)
        if self.bar_list:
            deps.add(self.bar_list[-1])
        self.ops.append({"eng": eng, "fn": fn, "deps": deps, "dma": eng in self.DMA_ENGS})
        return idx

    def emit(self):
        nc = self.nc
        ops = self.ops
        n = len(ops)
        need = [False] * n
        for i, o in enumerate(ops):
            if o["dma"]:
                need[i] = True
            for d in o["deps"]:
                if ops[d]["dma"]:
                    continue
                sd, si = self.stream[ops[d]["eng"]], self.stream[o["eng"]]
                if sd != si or sd != "pe":
                    need[d] = True
        sem_cm = []

        def mksem(name):
            cm = nc.semaphore(name)
            s = cm.__enter__()
            sem_cm.append(cm)
            return s

        eng_sem = {k: mksem("s_" + k) for k in ("pe", "act", "dve", "pool")}
        eng_cnt = {k: 0 for k in eng_sem}
        dma_sems = {q: [mksem("d_%s_%d" % (q, i)) for i in range(self.n_dma_sems)] for q in self.DMA_ENGS}
        dma_cnt = {q: [0] * self.n_dma_sems for q in self.DMA_ENGS}
        dma_last = {q: [None] * self.n_dma_sems for q in self.DMA_ENGS}
        dma_rr = {q: 0 for q in self.DMA_ENGS}
        ev = [None] * n
        waited = {s: {} for s in ("pe", "act", "dve", "pool", "sp")}
        for i, o in enumerate(ops):
            eng = o["eng"]
            st = self.stream[eng]
            e = self.engs[eng]
            waits = {}
            for d in o["deps"]:
                if ev[d] is None:
                    continue
                sem, val, key = ev[d]
                if key not in waits or waits[key][1] < val:
                    waits[key] = (sem, val)
            my = None
            if o["dma"]:
                k = dma_rr[eng]
                dma_rr[eng] = (k + 1) % self.n_dma_sems
                prev = dma_last[eng][k]
                if prev is not None:
                    sem, val, key = ev[prev]
                    if key not in waits or waits[key][1] < val:
                        waits[key] = (sem, val)
                dma_cnt[eng][k] += 16
                my = (dma_sems[eng][k], dma_cnt[eng][k], (eng, k))
                dma_last[eng][k] = i
            elif need[i]:
                eng_cnt[st] += 1
                my = (eng_sem[st], eng_cnt[st], st)
            for key, (sem, val) in waits.items():
                if waited[st].get(key, 0) >= val:
                    continue
                e.wait_ge(sem, val)
                waited[st][key] = val
            inst = o["fn"]()
            if my is not None:
                inst.then_inc(my[0], 16 if o["dma"] else 1)
                ev[i] = my
            o["fn"] = None
        sp = nc.sync
        for q in self.DMA_ENGS:
            for k in range(self.n_dma_sems):
                if dma_cnt[q][k] > 0:
                    sp.wait_ge(dma_sems[q][k], dma_cnt[q][k])
        for k in eng_sem:
            if eng_cnt[k] > 0:
                sp.wait_ge(eng_sem[k], eng_cnt[k])
        self.max_counts = dict(eng_cnt)


def snake_blocks(j):
    out = []
    for i in range(8):
        out.append(8 * i + j)
        out.append(8 * i + 7 - j)
    return out


class Ctx:
    pass


def setup_common(S, cx):
    nc = S.nc
    cx.ones_bf = S.sb("ones_bf", [128, 128], BF16)
    cx.b_ones = Buf("ones")
    S.op("dve", lambda: nc.vector.memset(cx.ones_bf[:], 1.0), writes=[cx.b_ones])
    cx.psall = S.ps("psall", [128, 8 * 512], F32)
    cx.psum = [cx.psall[:, i * 512:(i + 1) * 512] for i in range(8)]
    cx.b_psum = [Buf("psb%d" % i) for i in range(8)]


def emit_mod_vectors(S, cx, modT, normg, layer_slot, j):
    nc = S.nc
    A = S.sb("modA_%s" % layer_slot, [128, 8], F32)
    b = Buf("modA_%s" % layer_slot)
    base = j * 24
    S.op("dve", lambda: nc.vector.scalar_tensor_tensor(
        out=A[:], in0=modT[:, base + 8:base + 16], scalar=1.0, in1=normg,
        op0=ALU.add, op1=ALU.mult), reads=[cx.b_mod], writes=[b])
    return A, modT[:, base:base + 8], modT[:, base + 16:base + 24], b


def emit_ffn_phase(S, cx, x_in, b_xin, x_out, b_xout, w_in_d, w_out_d, A, Bv, G, gscale, b_mod, ntiles, tag):
    nc = S.nc
    win = cx.win
    wout = cx.wout
    HF = FC // 2
    for kc in range(KC):
        S.op("pool_dma", (lambda kc=kc: nc.gpsimd.dma_start(out=win[:, kc, :], in_=w_in_d[kc * 128:(kc + 1) * 128, :])),
             writes=[cx.b_win[kc]])
    for f in range(FC):
        S.op("pool_dma", (lambda f=f: nc.gpsimd.dma_start(out=wout[:, f, :], in_=w_out_d[f * 128:(f + 1) * 128, :])),
             writes=[cx.b_wout[f]])
    Gs = S.sb("Gs_" + tag, [128, 8], F32)
    b_Gs = Buf("Gs_" + tag)
    S.op("dve", lambda: nc.vector.tensor_scalar(out=Gs[:], in0=G, scalar1=float(gscale), scalar2=None, op0=ALU.mult),
         reads=[b_mod], writes=[b_Gs])
    xv_in = x_in.rearrange("(c p) t -> p c t", p=128)
    xv_out = x_out.rearrange("(c p) t -> p c t", p=128)
    def _ld(t):
        S.op("sp", (lambda: nc.sync.dma_start(out=cx.xt[t % 2][:], in_=xv_in[:, :, t * 512:(t + 1) * 512])),
             reads=[b_xin[t]], writes=[cx.b_xt[t % 2]])
    _ld(0)
    for t in range(ntiles):
        xt = cx.xt[t % 2]
        bx = cx.b_xt[t % 2]
        emit_norm_mod(S, cx, xt, bx, A, Bv, b_mod)
        if t + 1 < ntiles:
            _ld(t + 1)
        for hf in range(2):
            for fl in range(HF):
                f = hf * HF + fl
                pa = 1 + (f % 2) * 2
                pb = pa + 1

                def mm_a(f=f, pa=pa):
                    for kc in range(KC):
                        m = nc.tensor.matmul(cx.psum[pa][:], lhsT=win[:, kc, f * 128:(f + 1) * 128], rhs=cx.h[:, kc, :],
                                             start=(kc == 0), stop=(kc == KC - 1))
                    return m

                def mm_b(f=f, pb=pb):
                    for kc in range(KC):
                        m = nc.tensor.matmul(cx.psum[pb][:], lhsT=win[:, kc, DFF + f * 128:DFF + (f + 1) * 128],
                                             rhs=cx.h[:, kc, :], start=(kc == 0), stop=(kc == KC - 1))
                    return m
                S.op("pe", mm_a, reads=cx.b_win + [cx.b_h], writes=[cx.b_psum[pa]])
                S.op("pe", mm_b, reads=cx.b_win + [cx.b_h], writes=[cx.b_psum[pb]])
                sa = cx.sa[f % 2]
                S.op("act", (lambda sa=sa, pa=pa: nc.scalar.activation(out=sa[:], in_=cx.psum[pa][:], func=AF.Silu)),
                     reads=[cx.b_psum[pa]], writes=[cx.b_sa[f % 2]])
                S.op("dve", (lambda sa=sa, pb=pb, fl=fl: nc.vector.tensor_tensor(out=cx.u[:, fl, :], in0=sa[:], in1=cx.psum[pb][:], op=ALU.mult)),
                     reads=[cx.b_sa[f % 2], cx.b_psum[pb]], writes=[cx.b_u[fl]])
            for oc in range(KC):
                py = 5 + (oc % 2)

                def mm_y(oc=oc, py=py, hf=hf):
                    for fl in range(HF):
                        m = nc.tensor.matmul(cx.psum[py][:], lhsT=wout[:, hf * HF + fl, oc * 128:(oc + 1) * 128], rhs=cx.u[:, fl, :],
                                             start=(fl == 0), stop=(fl == HF - 1))
                    return m
                S.op("pe", mm_y, reads=cx.b_wout + cx.b_u, writes=[cx.b_psum[py]])
                S.op("dve", (lambda oc=oc, py=py, xt=xt: nc.vector.scalar_tensor_tensor(
                    out=xt[:, oc, :], in0=cx.psum[py][:], scalar=Gs[:, oc:oc + 1], in1=xt[:, oc, :],
                    op0=ALU.mult, op1=ALU.add)), reads=[cx.b_psum[py], b_Gs], writes=[bx])
        S.op("sp", (lambda xt=xt, t=t: nc.sync.dma_start(out=xv_out[:, :, t * 512:(t + 1) * 512], in_=xt[:])),
             reads=[bx], writes=[b_xout[t]])


def emit_norm_mod(S, cx, xt, bx, A, Bv, b_mod, n=512):
    nc = S.nc
    for c in range(KC):
        S.op("act", (lambda c=c: nc.scalar.activation(out=cx.sq[c % 2][:, :n], in_=xt[:, c, :n], func=AF.Square)),
             reads=[bx], writes=[cx.b_sq[c % 2]])
        S.op("pe", (lambda c=c: nc.tensor.matmul(cx.psum[0][:, :n], lhsT=cx.ones_bf[:], rhs=cx.sq[c % 2][:, :n],
                                                 start=(c == 0), stop=(c == KC - 1))),
             reads=[cx.b_sq[c % 2], cx.b_ones], writes=[cx.b_psum[0]])
    S.op("dve", lambda: nc.vector.tensor_scalar(out=cx.rstd[:, :n], in0=cx.psum[0][:, :n], scalar1=1.0 / D, scalar2=EPS,
                                                op0=ALU.mult, op1=ALU.add), reads=[cx.b_psum[0]], writes=[cx.b_rstd])
    S.op("act", lambda: nc.scalar.activation(out=cx.rstd[:, :n], in_=cx.rstd[:, :n], func=AF.Sqrt), reads=[cx.b_rstd], writes=[cx.b_rstd])
    S.op("dve", lambda: nc.vector.reciprocal(out=cx.rstd[:, :n], in_=cx.rstd[:, :n]), reads=[cx.b_rstd], writes=[cx.b_rstd])
    for c in range(KC):
        tmp = cx.tmp[c % 2]
        S.op("dve", (lambda c=c, tmp=tmp: nc.vector.tensor_tensor(out=tmp[:, :n], in0=xt[:, c, :n], in1=cx.rstd[:, :n], op=ALU.mult)),
             reads=[bx, cx.b_rstd], writes=[cx.b_tmp[c % 2]])
        S.op("act", (lambda c=c, tmp=tmp: nc.scalar.activation(out=cx.h[:, c, :n], in_=tmp[:, :n], func=AF.Identity,
                                                               bias=Bv[:, c:c + 1], scale=A[:, c:c + 1])),
             reads=[cx.b_tmp[c % 2], b_mod], writes=[cx.b_h])


def alloc_tile_bufs(S, cx):
    cx.xt = [S.sb("xt%d" % i, [128, KC, 512], F32) for i in range(2)]
    cx.b_xt = [Buf("xt%d" % i) for i in range(2)]
    cx.sq = [S.sb("sq%d" % i, [128, 512], BF16) for i in range(2)]
    cx.b_sq = [Buf("sq%d" % i) for i in range(2)]
    cx.h = S.sb("h", [128, KC, 512], BF16)
    cx.b_h = Buf("h")
    cx.rstd = S.sb("rstd", [128, 512], F32)
    cx.b_rstd = Buf("rstd")
    cx.tmp = [S.sb("tmp%d" % i, [128, 512], F32) for i in range(2)]
    cx.b_tmp = [Buf("tmp%d" % i) for i in range(2)]
    cx.sa = [S.sb("sa%d" % i, [128, 512], F32) for i in range(2)]
    cx.b_sa = [Buf("sa%d" % i) for i in range(2)]


def alloc_ffn_bufs(S, cx):
    cx.win = S.sb("win", [128, KC, 2 * DFF], BF16)
    cx.wout = S.sb("wout", [128, FC, D], BF16)
    cx.b_win = [Buf("win%d" % i) for i in range(KC)]
    cx.b_wout = [Buf("wout%d" % i) for i in range(FC)]
    cx.u = S.sb("u", [128, FC // 2, 512], BF16)
    cx.b_u = [Buf("u%d" % i) for i in range(FC // 2)]


def _fm(v, n=None):
    v = np.asarray(v, np.float32).reshape(-1, 128)
    return np.ascontiguousarray(v.T)


SP_OFF = {}


def _sp_layout():
    off = 0
    for name, w in (("c", 8), ("ada_b0", 72), ("ada_b1", 72),
                    ("ng00", 8), ("ng01", 8), ("ng02", 8), ("ng10", 8), ("ng11", 8), ("ng12", 8),
                    ("gq", 1), ("gk", 1), ("b_pw1", 16), ("b_dw", 8), ("ln_g", 8), ("ln_b", 8),
                    ("b_pw2", 8), ("w_dw", 8 * CW)):
        SP_OFF[name] = (off, w)
        off += w
    return off


SP_N = _sp_layout()


def pack_small(inp, b):
    sp = np.zeros((128, SP_N), np.float32)

    def put(name, arr):
        o, w = SP_OFF[name]
        assert arr.shape == (128, w), (name, arr.shape, w)
        sp[:, o:o + w] = arr
    put("c", _fm(inp["c"][b]))
    put("ada_b0", _fm(inp["ada_b"][0]))
    put("ada_b1", _fm(inp["ada_b"][1]))
    for i in range(2):
        for j in range(3):
            put("ng%d%d" % (i, j), _fm(inp["norm_g"][i, j]))
    put("gq", np.tile(np.asarray(inp["attn_g_q"][0], np.float32), 2).reshape(128, 1))
    put("gk", np.tile(np.asarray(inp["attn_g_k"][0], np.float32), 2).reshape(128, 1))
    put("b_pw1", _fm(inp["conv_b_pw1"][0]))
    put("b_dw", _fm(inp["conv_b_dw"][0]))
    put("ln_g", _fm(inp["conv_ln_g"][0]))
    put("ln_b", _fm(inp["conv_ln_b"][0]))
    put("b_pw2", _fm(inp["conv_b_pw2"][0]))
    wd = np.asarray(inp["conv_w_dw"][0], np.float32)
    put("w_dw", np.ascontiguousarray(wd.reshape(CW, 8, 128).transpose(2, 1, 0)).reshape(128, 8 * CW))
    return sp


def spc(cx, name):
    o, w = SP_OFF[name]
    return cx.spt[:, o:o + w]


def emit_load_small(S, cx, sp_d):
    nc = S.nc
    cx.spt = S.sb("spt", [128, SP_N], F32)
    cx.b_sp = Buf("spt")
    S.op("sp", lambda: nc.sync.dma_start(out=cx.spt[:], in_=sp_d[:, :]), writes=[cx.b_sp])


def emit_adaln(S, cx, ada_w_d, modT, b_modT):
    nc = S.nc
    NCOL = 1152
    stg = [S.sb("adastg%d" % i, [128, KC, NCOL], F32) for i in range(2)]
    b_stg = [Buf("adastg%d" % i) for i in range(2)]
    cact = S.sb("cact", [128, 8], F32)
    b_cact = Buf("cact")
    S.op("act", lambda: nc.scalar.activation(out=cact[:], in_=spc(cx, "c"), func=AF.Silu), reads=[cx.b_sp], writes=[b_cact])
    k = 0
    for i in range(2):
        wv = ada_w_d[i].rearrange("(kc p) n -> p kc n", p=128)
        for pc in range(9216 // NCOL):
            st = stg[k % 2]
            bs = b_stg[k % 2]
            k += 1
            S.op("sp", (lambda st=st, wv=wv, pc=pc: nc.sync.dma_start(out=st[:], in_=wv[:, :, pc * NCOL:(pc + 1) * NCOL])), writes=[bs])

            def mm(st=st, pc=pc, i=i):
                for m in range(NCOL // 128):
                    col = pc * (NCOL // 128) + m
                    for kc in range(KC):
                        r = nc.tensor.matmul(cx.psum[7][:, col:col + 1], lhsT=st[:, kc, m * 128:(m + 1) * 128], rhs=cact[:, kc:kc + 1],
                                             start=(kc == 0), stop=(kc == KC - 1))
                return r
            S.op("pe", mm, reads=[bs, b_cact], writes=[cx.b_psum[7]])
        S.op("dve", (lambda i=i: nc.vector.tensor_tensor(out=modT[i][:], in0=cx.psum[7][:, 0:72], in1=spc(cx, "ada_b%d" % i), op=ALU.add)),
             reads=[cx.b_psum[7], cx.b_sp], writes=[b_modT[i]])
    return stg


def joint_mod(S, cx, i, j, modT, b_modT):
    nc = S.nc
    A, Bv, G, bA = emit_mod_vectors_i(S, cx, modT[i], b_modT[i], spc(cx, "ng%d%d" % (i, j)), "l%ds%d" % (i, j), j)
    return A, Bv, G, bA


def emit_mod_vectors_i(S, cx, modT, b_modT, normg, tag, j):
    nc = S.nc
    AB = S.sb("modAB_" + tag, [128, 24], F32)
    b = Buf("modAB_" + tag)
    base = j * 24
    S.op("dve", lambda: nc.vector.tensor_copy(out=AB[:], in_=modT[:, base:base + 24]), reads=[b_modT, cx.b_sp], writes=[b])
    S.op("dve", lambda: nc.vector.scalar_tensor_tensor(
        out=AB[:, 8:16], in0=modT[:, base + 8:base + 16], scalar=1.0, in1=normg,
        op0=ALU.add, op1=ALU.mult), reads=[b_modT, cx.b_sp], writes=[b])
    return AB[:, 8:16], AB[:, 0:8], AB[:, 16:24], b


def load_x_tile(S, cx, xv, b_x, t, n=512):
    nc = S.nc
    xt = cx.xt[t % 2]
    bx = cx.b_xt[t % 2]
    S.op("sp", (lambda: nc.sync.dma_start(out=xt[:, :, :n], in_=xv[:, :, t * n:(t + 1) * n])), reads=[b_x[t]], writes=[bx])
    return xt, bx


def emit_qkv_phase(S, cx, x_d, b_x, wqkv_d, A, Bv, b_mod, QT_d, KT_d, kbar_d, V_d):
    nc = S.nc
    wq = S.sb("wqkv_sb", [128, KC, 3072], BF16)
    b_wq = [Buf("wqkv%d" % k) for k in range(KC)]
    for kc in range(KC):
        S.op("pool_dma", (lambda kc=kc: nc.gpsimd.dma_start(out=wq[:, kc, :], in_=wqkv_d[kc * 128:(kc + 1) * 128, :])), writes=[b_wq[kc]])
    bo = S.sb("blkones", [128, 128], BF16)
    b_bo = Buf("blkones")
    S.op("dve", lambda: nc.vector.memset(bo[:], 0.0), writes=[b_bo])
    S.op("dve", lambda: nc.vector.memset(bo[0:64, 0:64], 1.0), writes=[b_bo])
    S.op("dve", lambda: nc.vector.memset(bo[64:128, 64:128], 1.0), writes=[b_bo])
    g8 = S.sb("g8", [128, 2], F32)
    b_g8 = Buf("g8")
    S.op("dve", lambda: nc.vector.tensor_scalar(out=g8[:, 0:1], in0=spc(cx, "gq"), scalar1=0.125, scalar2=None, op0=ALU.mult),
         reads=[cx.b_sp], writes=[b_g8])
    S.op("dve", lambda: nc.vector.tensor_copy(out=g8[:, 1:2], in_=spc(cx, "gk")), reads=[cx.b_sp], writes=[b_g8])
    qo = [S.sb("qo%d" % i, [128, 512], BF16) for i in range(2)]
    b_qo = [Buf("qo%d" % i) for i in range(2)]
    kb = [S.sb("kb%d" % i, [128, 2], F32) for i in range(2)]
    b_kb = [Buf("kb%d" % i) for i in range(2)]
    vt = [S.sb("vt%d" % i, [128, 1024], BF16) for i in range(2)]
    b_vt = [Buf("vt%d" % i) for i in range(2)]
    xv = x_d.rearrange("(c p) t -> p c t", p=128)
    QTv = QT_d.rearrange("h d t -> (h d) t")
    KTv = KT_d.rearrange("h d t -> (h d) t")
    kbv = kbar_d.rearrange("h d l -> (h d) l")
    cnt = 0
    for t in range(8):
        xt, bx = load_x_tile(S, cx, xv, b_x, t)
        emit_norm_mod(S, cx, xt, bx, A, Bv, b_mod)
        for which in range(2):
            goff = which * 1024
            outv = QTv if which == 0 else KTv
            for hp in range(8):
                k = cnt % 2
                cnt += 1
                pq = cx.psum[1 + k]
                pss = cx.psum[3 + k]

                def mm(hp=hp, pq=pq, goff=goff):
                    for kc in range(KC):
                        m = nc.tensor.matmul(pq[:], lhsT=wq[:, kc, goff + hp * 128:goff + (hp + 1) * 128], rhs=cx.h[:, kc, :],
                                             start=(kc == 0), stop=(kc == KC - 1))
                    return m
                S.op("pe", mm, reads=b_wq + [cx.b_h], writes=[cx.b_psum[1 + k]])
                S.op("act", (lambda k=k, pq=pq: nc.scalar.activation(out=cx.sq[k][:], in_=pq[:], func=AF.Square)),
                     reads=[cx.b_psum[1 + k]], writes=[cx.b_sq[k]])
                S.op("pe", (lambda k=k, pss=pss: nc.tensor.matmul(pss[:], lhsT=bo[:], rhs=cx.sq[k][:], start=True, stop=True)),
                     reads=[cx.b_sq[k], b_bo], writes=[cx.b_psum[3 + k]])
                r1 = cx.tmp[k]
                S.op("dve", (lambda r1=r1, pss=pss: nc.vector.tensor_scalar(out=r1[:], in0=pss[:], scalar1=1.0 / DH, scalar2=EPS,
                                                                            op0=ALU.mult, op1=ALU.add)),
                     reads=[cx.b_psum[3 + k]], writes=[cx.b_tmp[k]])
                S.op("act", (lambda r1=r1: nc.scalar.activation(out=r1[:], in_=r1[:], func=AF.Sqrt)), reads=[cx.b_tmp[k]], writes=[cx.b_tmp[k]])
                S.op("dve", (lambda r1=r1: nc.vector.reciprocal(out=r1[:], in_=r1[:])), reads=[cx.b_tmp[k]], writes=[cx.b_tmp[k]])
                S.op("dve", (lambda r1=r1, pq=pq: nc.vector.tensor_tensor(out=r1[:], in0=pq[:], in1=r1[:], op=ALU.mult)),
                     reads=[cx.b_tmp[k], cx.b_psum[1 + k]], writes=[cx.b_tmp[k]])
                sa = cx.sa[k]
                S.op("act", (lambda r1=r1, sa=sa, which=which: nc.scalar.activation(out=sa[:], in_=r1[:], func=AF.Copy,
                                                                                  scale=g8[:, which:which + 1])),
                     reads=[cx.b_tmp[k], b_g8], writes=[cx.b_sa[k]])
                S.op("pool", (lambda k=k, sa=sa: nc.gpsimd.tensor_copy(out=qo[k][:], in_=sa[:])), reads=[cx.b_sa[k]], writes=[b_qo[k]])
                S.op("sp", (lambda k=k, hp=hp, t=t, outv=outv: nc.sync.dma_start(out=outv[hp * 128:(hp + 1) * 128, t * 512:(t + 1) * 512], in_=qo[k][:])),
                     reads=[b_qo[k]])
                if which == 1:
                    S.op("dve", (lambda k=k, sa=sa: nc.vector.tensor_reduce(out=kb[k][:], in_=sa[:].rearrange("p (b t) -> p b t", b=2),
                                                                          axis=AX.X, op=ALU.add)),
                         reads=[cx.b_sa[k]], writes=[b_kb[k]])
                    S.op("dve", (lambda k=k: nc.vector.tensor_scalar(out=kb[k][:], in0=kb[k][:], scalar1=1.0 / BLK, scalar2=None, op0=ALU.mult)),
                         reads=[b_kb[k]], writes=[b_kb[k]])
                    S.op("sp", (lambda k=k, hp=hp, t=t: nc.sync.dma_start(out=kbv[hp * 128:(hp + 1) * 128, 2 * t:2 * t + 2], in_=kb[k][:])),
                         reads=[b_kb[k]])
        for tg in range(4):
            v = vt[tg % 2]
            for ns in range(2):
                pv = cx.psum[5 + ns]

                def mmv(tg=tg, ns=ns, pv=pv):
                    for kc in range(KC):
                        m = nc.tensor.matmul(pv[:], lhsT=cx.h[:, kc, tg * 128:(tg + 1) * 128], rhs=wq[:, kc, 2048 + ns * 512:2048 + (ns + 1) * 512],
                                             start=(kc == 0), stop=(kc == KC - 1))
                    return m
                S.op("pe", mmv, reads=b_wq + [cx.b_h], writes=[cx.b_psum[5 + ns]])
                if ns == 0:
                    S.op("act", (lambda v=v, pv=pv: nc.scalar.copy(out=v[:, 0:512], in_=pv[:])), reads=[cx.b_psum[5]], writes=[b_vt[tg % 2]])
                else:
                    S.op("dve", (lambda v=v, pv=pv: nc.vector.tensor_copy(out=v[:, 512:1024], in_=pv[:])), reads=[cx.b_psum[6]], writes=[b_vt[tg % 2]])
            S.op("sp", (lambda v=v, tg=tg, t=t: nc.sync.dma_start(out=V_d[t * 512 + tg * 128:t * 512 + (tg + 1) * 128, :], in_=v[:])),
                 reads=[b_vt[tg % 2]])


def build_L1():
    nc = bass.Bass("TRN2", target_bir_lowering=False)
    sp_d = nc.dram_tensor("sp", [128, SP_N], F32, kind="ExternalInput").ap()
    ada_w = nc.dram_tensor("ada_w", [2, 1024, 9216], F32, kind="ExternalInput").ap()
    xin = nc.dram_tensor("xin", [1024, TOK], F32, kind="ExternalInput").ap()
    w_in = nc.dram_tensor("w_in", [1024, 2 * DFF], F32, kind="ExternalInput").ap()
    w_out = nc.dram_tensor("w_out", [DFF, 1024], F32, kind="ExternalInput").ap()
    wqkv = nc.dram_tensor("wqkv", [1024, 3072], F32, kind="ExternalInput").ap()
    x1 = nc.dram_tensor("x1", [1024, TOK], F32, kind="ExternalOutput").ap()
    modo = nc.dram_tensor("modo", [128, 144], F32, kind="ExternalOutput").ap()
    QT = nc.dram_tensor("QT", [NH, DH, TOK], BF16, kind="ExternalOutput").ap()
    KT = nc.dram_tensor("KT", [NH, DH, TOK], BF16, kind="ExternalOutput").ap()
    kbar = nc.dram_tensor("kbar", [NH, DH, NLB], F32, kind="ExternalOutput").ap()
    V = nc.dram_tensor("V", [TOK, 1024], BF16, kind="ExternalOutput").ap()
    S = Sched(nc)
    cx = Ctx()
    setup_common(S, cx)
    emit_load_small(S, cx, sp_d)
    modT = [S.sb("modT%d" % i, [128, 72], F32) for i in range(2)]
    b_modT = [Buf("modT%d" % i) for i in range(2)]
    A0, B0, G0, bm0 = None, None, None, None
    AB0 = joint_mod
    emit_adaln_scoped(S, cx, ada_w, modT, b_modT)
    for i in range(2):
        S.op("sp", (lambda i=i: nc.sync.dma_start(out=modo[:, i * 72:(i + 1) * 72], in_=modT[i][:])), reads=[b_modT[i]])
    A, Bv, G, bA = emit_mod_vectors_i(S, cx, modT[0], b_modT[0], spc(cx, "ng00"), "l0s0", 0)
    A1, Bv1, G1, bA1 = emit_mod_vectors_i(S, cx, modT[0], b_modT[0], spc(cx, "ng01"), "l0s1", 1)
    alloc_tile_bufs(S, cx)
    alloc_ffn_bufs(S, cx)
    b_xin = [Buf("xin%d" % t) for t in range(8)]
    b_x1 = [Buf("x1_%d" % t) for t in range(8)]
    emit_ffn_phase(S, cx, xin, b_xin, x1, b_x1, w_in, w_out, A, Bv, G, 0.5, bA, 8, "f00")
    S.barrier_free(3)
    emit_qkv_phase(S, cx, x1, b_x1, wqkv, A1, Bv1, bA1, QT, KT, kbar, V)
    S.emit()
    return nc


def emit_adaln_scoped(S, cx, ada_w, modT, b_modT):
    n0 = len(S._stack)
    emit_adaln(S, cx, ada_w, modT, b_modT)
    S.barrier_free(len(S._stack) - n0)


SLOPES = [2.0 ** (-8.0 * (h + 1) / NH) for h in range(NH)]


def emit_attn_phase(S, cx, d):
    nc = S.nc
    KTa = [S.sb("KTa%d" % i, [128, SEQ], BF16) for i in range(2)]
    Va = [S.sb("Va%d" % i, [128, 128, 65], BF16) for i in range(2)]
    Qa = [S.sb("Qa%d" % i, [128, TOK], BF16) for i in range(2)]
    Ko = [S.sb("Ko%d" % i, [128, TOK], BF16) for i in range(2)]
    Vo = [S.sb("Vo%d" % i, [128, 32, 65], BF16) for i in range(2)]
    bT = [S.sb("bT%d" % i, [128, 1024], F32) for i in range(2)]
    dgb = [S.sb("dgb%d" % i, [128, 512], F32) for i in range(2)]
    cq = [S.sb("cq%d" % i, [128, 256], F32) for i in range(2)]
    b_KT = [Buf("KTa%d" % i) for i in range(2)]
    b_Va = [Buf("Va%d" % i) for i in range(2)]
    b_Qa = [Buf("Qa%d" % i) for i in range(2)]
    b_Qm = [[Buf("Qm%d_%d" % (i, l)) for l in range(NLB)] for i in range(2)]
    b_Ko = [Buf("Ko%d" % i) for i in range(2)]
    b_Vo = [Buf("Vo%d" % i) for i in range(2)]
    b_hd = [Buf("hd%d" % i) for i in range(2)]
    cst = S.sb("cst", [128, CST_N], F32)
    b_cst = Buf("cst")
    S.op("sp", lambda: nc.sync.dma_start(out=cst[:], in_=d["cst"][:, :]), writes=[b_cst])

    def C(name):
        o, w = CST_OFF[name]
        return cst[:, o:o + w]
    kb32 = S.sb("kb32", [64, NH * NB], F32)
    kbh = S.sb("kbh", [64, NH * NB], BF16)
    kbl = S.sb("kbl", [64, NH * NB], BF16)
    kbt = S.sb("kbt", [64, NH * NB], F32)
    b_kb = Buf("kbhl")
    S.op("sp", lambda: nc.sync.dma_start(out=kb32[:], in_=d["kbar"].rearrange("d h m -> d (h m)")), writes=[b_kb])
    S.op("dve", lambda: nc.vector.tensor_copy(out=kbh[:], in_=kb32[:]), reads=[b_kb], writes=[b_kb])
    S.op("dve", lambda: nc.vector.tensor_copy(out=kbt[:], in_=kbh[:]), reads=[b_kb], writes=[b_kb])
    S.op("dve", lambda: nc.vector.tensor_tensor(out=kbt[:], in0=kb32[:], in1=kbt[:], op=ALU.subtract), reads=[b_kb], writes=[b_kb])
    S.op("dve", lambda: nc.vector.tensor_copy(out=kbl[:], in_=kbt[:]), reads=[b_kb], writes=[b_kb])
    mbp = [S.sb("mbp%d" % i, [128, 128], F32) for i in range(2)]
    b_mbp = [Buf("mbp%d" % i) for i in range(2)]
    gmt = [S.sb("gmt%d" % i, [128, 64], F32) for i in range(2)]
    b_gmt = [Buf("gmt%d" % i) for i in range(2)]
    t8 = [S.sb("t8_%d" % i, [128, 8], F32) for i in range(2)]
    pt = [S.sb("pt%d" % i, [128, 512], BF16) for i in range(2)]
    b_pt = [Buf("pt%d" % i) for i in range(2)]
    pt2 = S.sb("pt2", [128, 512], BF16)
    b_pt2 = Buf("pt2")
    dtmp = S.sb("dtmp", [128, 512], F32)
    b_dtmp = Buf("dtmp")
    o1 = S.sb("o1", [128, 256], F32)
    o2 = S.sb("o2", [128, 256], F32)
    rd = S.sb("rd", [128, 256], F32)
    ob = [S.sb("ob%d" % i, [128, 256], BF16) for i in range(2)]
    b_o = Buf("o12")
    b_ob = [Buf("ob%d" % i) for i in range(2)]
    for i in range(2):
        S.op("sp", (lambda i=i: nc.sync.dma_start(out=KTa[i][64:128, :], in_=d["ind"][:, :])), writes=[b_KT[i]])
        S.op("dve", (lambda i=i: nc.vector.memset(Va[i][:, :, 64:65], 1.0)), writes=[b_Va[i]])
        S.op("dve", (lambda i=i: nc.vector.memset(Vo[i][:, :, 64:65], 1.0)), writes=[b_Vo[i]])
        S.op("dve", (lambda i=i: nc.vector.memset(mbp[i][:], 0.0)), writes=[b_mbp[i]])
    ps = cx.psum
    bp = cx.b_psum
    scnt = 0
    gcnt = 0
    for h in range(NH):
        cur = h % 2
        sl = SLOPES[h]
        es = math.exp(sl)
        S.op("sp", (lambda h=h, cur=cur: nc.sync.dma_start(out=KTa[cur][0:64, :], in_=d["KT"][h])), writes=[b_KT[cur]])
        for q8 in range(8):
            S.op("sp", (lambda h=h, cur=cur, q8=q8: nc.sync.dma_start(out=Va[cur][:, q8 * 16:(q8 + 1) * 16, 0:64], in_=d["Vd"][h, :, q8 * 16:(q8 + 1) * 16, :])),
                 writes=[b_Va[cur]])
        S.op("sp", (lambda h=h, cur=cur: nc.sync.dma_start(out=Qa[cur][0:64, :], in_=d["QT"][h])), writes=[b_Qa[cur]] + b_Qm[cur])
        S.op("sp", (lambda h=h, cur=cur: nc.sync.dma_start(out=Ko[cur][0:64, :], in_=d["KTo"][h])), writes=[b_Ko[cur]])
        for q2 in range(2):
            S.op("sp", (lambda h=h, cur=cur, q2=q2: nc.sync.dma_start(out=Vo[cur][:, q2 * 16:(q2 + 1) * 16, 0:64], in_=d["Vo"][h, :, q2 * 16:(q2 + 1) * 16, :])),
                 writes=[b_Vo[cur]])
        for (VV, bV) in ((Va[cur], b_Va[cur]), (Vo[cur], b_Vo[cur])):
            Vodd = VV[:].rearrange("p (n two) e -> p n two e", two=2)[:, :, 1, :]
            S.op("dve", (lambda Vodd=Vodd, es=es: nc.vector.tensor_scalar(out=Vodd[:, :, 0:64], in0=Vodd[:, :, 0:64], scalar1=es, scalar2=None, op0=ALU.mult)),
                 reads=[bV], writes=[bV])
            S.op("dve", (lambda Vodd=Vodd, es=es: nc.vector.memset(Vodd[:, :, 64:65], es)), reads=[bV], writes=[bV])
        S.op("dve", (lambda cur=cur, sl=sl: nc.vector.tensor_scalar(out=bT[cur][:], in0=C("base"), scalar1=sl, scalar2=None, op0=ALU.mult)),
             reads=[b_cst], writes=[b_hd[cur]])
        S.op("dve", (lambda cur=cur, sl=sl: nc.vector.scalar_tensor_tensor(out=dgb[cur][:], in0=C("Dm"), scalar=-sl, in1=C("causal"),
                                                                         op0=ALU.mult, op1=ALU.add)), reads=[b_cst], writes=[b_hd[cur]])
        S.op("act", (lambda cur=cur, sl=sl: nc.scalar.activation(out=cq[cur][:], in_=C("irow"), func=AF.Exp, scale=-sl)),
             reads=[b_cst], writes=[b_hd[cur]])
        for l in range(NLB):
            for hq in range(2):
                g = gcnt % 2
                gcnt += 1
                q0 = l * 256 + hq * 128

                def mmg(cur=cur, q0=q0, h=h):
                    nc.tensor.matmul(ps[6][:, 0:64], lhsT=Qa[cur][0:64, q0:q0 + 128], rhs=kbh[:, h * 64:(h + 1) * 64], start=True, stop=False)
                    return nc.tensor.matmul(ps[6][:, 0:64], lhsT=Qa[cur][0:64, q0:q0 + 128], rhs=kbl[:, h * 64:(h + 1) * 64], start=False, stop=True)
                S.op("pe", mmg, reads=[b_Qa[cur], b_kb], writes=[bp[6]])
                pb = C("past")[:, l * 64:(l + 1) * 64]
                S.op("dve", (lambda g=g, pb=pb: nc.vector.tensor_tensor(out=gmt[g][:], in0=ps[6][:, 0:64], in1=pb, op=ALU.add)),
                     reads=[bp[6], b_cst], writes=[b_gmt[g]])
                S.op("dve", (lambda g=g: nc.vector.max(out=t8[g][:], in_=gmt[g][:])), reads=[b_gmt[g]], writes=[b_gmt[g]])
                S.op("dve", (lambda g=g: nc.vector.tensor_scalar(out=gmt[g][:], in0=gmt[g][:], scalar1=t8[g][:, 2:3], scalar2=-NEG,
                                                                 op0=ALU.is_ge, op1=ALU.mult)), reads=[b_gmt[g]], writes=[b_gmt[g]])
                S.op("dve", (lambda g=g, pb=pb: nc.vector.scalar_tensor_tensor(out=mbp[g][:, 64:128], in0=gmt[g][:], scalar=NEG, in1=pb,
                                                                             op0=ALU.add, op1=ALU.add)),
                     reads=[b_gmt[g], b_cst], writes=[b_mbp[g]])
                S.op("pe", (lambda g=g: nc.tensor.transpose(ps[7][:, 0:128], mbp[g][:], C("ident"))), reads=[b_mbp[g], b_cst], writes=[bp[7]])
                S.op("act", (lambda cur=cur, q0=q0: nc.scalar.copy(out=Qa[cur][64:128, q0:q0 + 128], in_=ps[7][64:128, 0:128])),
                     reads=[bp[7]], writes=[b_Qm[cur][l]])
        for i in range(8):
            nk = 8 * i + 7
            for X in (2 * i, 2 * i + 1):
                po = 2 + (X % 2)
                for n in range(nk):
                    sb_ = scnt % 2
                    scnt += 1

                    def mms(cur=cur, X=X, n=n, sb_=sb_):
                        for c in range(2):
                            m = nc.tensor.matmul(ps[sb_][:, c * 256:(c + 1) * 256], lhsT=KTa[cur][:, n * 256 + c * 128:n * 256 + (c + 1) * 128],
                                                 rhs=Qa[cur][:, X * 256:(X + 1) * 256], start=True, stop=True)
                        return m
                    S.op("pe", mms, reads=[b_KT[cur], b_Qa[cur], b_Qm[cur][X]], writes=[bp[sb_]])
                    S.op("act", (lambda cur=cur, X=X, n=n, sb_=sb_: nc.scalar.activation(out=pt[sb_][:], in_=ps[sb_][:], func=AF.Exp,
                                                                                        bias=bT[cur][:, X * 64 + n:X * 64 + n + 1])),
                         reads=[bp[sb_], b_hd[cur]], writes=[b_pt[sb_]])

                    def mmpv(cur=cur, n=n, sb_=sb_, po=po, nk=nk):
                        for c in range(2):
                            m = nc.tensor.matmul(ps[po][0:65, 0:256], lhsT=Va[cur][:, 2 * n + c, :], rhs=pt[sb_][:, c * 256:(c + 1) * 256],
                                                 start=(n == 0 and c == 0), stop=(n == nk - 1 and c == 1))
                        return m
                    S.op("pe", mmpv, reads=[b_Va[cur], b_pt[sb_]], writes=[bp[po]])

                def mmd(cur=cur, X=X):
                    for c in range(2):
                        m = nc.tensor.matmul(ps[4][:, c * 256:(c + 1) * 256], lhsT=Ko[cur][0:64, X * 256 + c * 128:X * 256 + (c + 1) * 128],
                                             rhs=Qa[cur][0:64, X * 256:(X + 1) * 256], start=True, stop=True)
                    return m
                S.op("pe", mmd, reads=[b_Ko[cur], b_Qa[cur]], writes=[bp[4]])
                S.op("dve", (lambda cur=cur: nc.vector.tensor_tensor(out=dtmp[:], in0=ps[4][:], in1=dgb[cur][:], op=ALU.add)),
                     reads=[bp[4], b_hd[cur]], writes=[b_dtmp])
                S.op("act", (lambda: nc.scalar.activation(out=pt2[:], in_=dtmp[:], func=AF.Exp)), reads=[b_dtmp], writes=[b_pt2])

                def mmpo(cur=cur, X=X):
                    for c in range(2):
                        m = nc.tensor.matmul(ps[5][0:65, 0:256], lhsT=Vo[cur][:, 2 * X + c, :], rhs=pt2[:, c * 256:(c + 1) * 256],
                                             start=(c == 0), stop=(c == 1))
                    return m
                S.op("pe", mmpo, reads=[b_Vo[cur], b_pt2], writes=[bp[5]])
                S.op("dve", (lambda cur=cur, po=po: nc.vector.tensor_tensor(out=o1[0:65, :], in0=ps[po][0:65, 0:256], in1=cq[cur][0:65, :], op=ALU.mult)),
                     reads=[bp[po], b_hd[cur]], writes=[b_o])
                S.op("dve", (lambda: nc.vector.tensor_tensor(out=o2[0:65, :], in0=o1[0:65, :], in1=ps[5][0:65, 0:256], op=ALU.add)),
                     reads=[bp[5], b_o], writes=[b_o])
                S.op("pe", (lambda: nc.tensor.matmul(ps[6][0:64, 0:256], lhsT=C("sel65")[0:65, :], rhs=o2[0:65, :], start=True, stop=True)),
                     reads=[b_o, b_cst], writes=[bp[6]])
                S.op("dve", (lambda: nc.vector.reciprocal(out=rd[0:64, :], in_=ps[6][0:64, 0:256])), reads=[bp[6]], writes=[b_o])
                k = X % 2
                S.op("dve", (lambda k=k: nc.vector.tensor_tensor(out=ob[k][0:64, :], in0=o2[0:64, :], in1=rd[0:64, :], op=ALU.mult)),
                     reads=[b_o], writes=[b_ob[k]])
                S.op("sp", (lambda k=k, h=h, X=X: nc.sync.dma_start(out=d["OT"][h, :, X * 256:(X + 1) * 256], in_=ob[k][0:64, :])),
                     reads=[b_ob[k]], writes=[d["b_OT"][X // 2]])


CST_OFF = {}


def _cst_layout():
    off = 0
    for name, w in (("past", 1024), ("base", 1024), ("irow", 256), ("Dm", 512), ("causal", 512), ("ident", 128), ("sel65", 64)):
        CST_OFF[name] = (off, w)
        off += w
    return off


CST_N = _cst_layout()


def make_cst(j):
    blocks = snake_blocks(j)
    c = np.zeros((128, CST_N), np.float32)
    p = np.arange(128, dtype=np.float32)[:, None]
    past = np.zeros((16, 64), np.float32)
    base = np.zeros((128, 16, 64), np.float32)
    for l, own in enumerate(blocks):
        m = np.arange(64)
        past[l] = np.where(m < own, 0.0, NEG)
        dist = np.where(m < own, own - m, 1).astype(np.float32)
        base[:, l, :] = 2.0 * p - 256.0 * dist[None, :]
    o, w = CST_OFF["past"]
    c[:, o:o + w] = np.tile(past.reshape(1, 1024), (128, 1))
    o, w = CST_OFF["base"]
    c[:, o:o + w] = base.reshape(128, 1024)
    o, w = CST_OFF["irow"]
    c[:, o:o + w] = np.arange(256, dtype=np.float32)[None, :]
    i = np.arange(256, dtype=np.float32)[None, None, :]
    pp = np.arange(128, dtype=np.float32)[:, None, None]
    cc = np.arange(2, dtype=np.float32)[None, :, None]
    Dm = np.broadcast_to(i - 2 * pp, (128, 2, 256))
    causal = np.where(i >= 2 * pp + cc, 0.0, NEG)
    o, w = CST_OFF["Dm"]
    c[:, o:o + w] = Dm.reshape(128, 512)
    o, w = CST_OFF["causal"]
    c[:, o:o + w] = causal.reshape(128, 512)
    o, w = CST_OFF["ident"]
    c[:, o:o + w] = np.eye(128, dtype=np.float32)
    o, w = CST_OFF["sel65"]
    c[64, o:o + w] = 1.0
    return c


def emit_proj_res_phase(S, cx, x_in, b_xin, x_out, b_xout, src_d, b_src, w_d, G, bias, b_mod, tag, src_is_heads, ntiles=8):
    nc = S.nc
    w = S.sb("w_" + tag, [128, KC, D], BF16)
    b_w = Buf("w_" + tag)
    for kc in range(KC):
        S.op("pool_dma", (lambda kc=kc: nc.gpsimd.dma_start(out=w[:, kc, :], in_=w_d[kc * 128:(kc + 1) * 128, :])), writes=[b_w])
    sv = src_d.rearrange("h d t -> (h d) t") if src_is_heads else src_d
    sv = sv.rearrange("(c p) t -> p c t", p=128)
    xv_in = x_in.rearrange("(c p) t -> p c t", p=128)
    xv_out = x_out.rearrange("(c p) t -> p c t", p=128)
    for t in range(ntiles):
        xt, bx = load_x_tile(S, cx, xv_in, b_xin, t)
        S.op("sp", (lambda t=t: nc.sync.dma_start(out=cx.h[:], in_=sv[:, :, t * 512:(t + 1) * 512])), reads=[b_src[t]], writes=[cx.b_h])
        for oc in range(KC):
            py = 5 + (oc % 2)

            def mm(oc=oc, py=py):
                for kc in range(KC):
                    m = nc.tensor.matmul(cx.psum[py][:], lhsT=w[:, kc, oc * 128:(oc + 1) * 128], rhs=cx.h[:, kc, :], start=(kc == 0), stop=(kc == KC - 1))
                return m
            S.op("pe", mm, reads=[b_w, cx.b_h], writes=[cx.b_psum[py]])
            if bias is None:
                S.op("dve", (lambda oc=oc, py=py, xt=xt: nc.vector.scalar_tensor_tensor(
                    out=xt[:, oc, :], in0=cx.psum[py][:], scalar=G[:, oc:oc + 1], in1=xt[:, oc, :], op0=ALU.mult, op1=ALU.add)),
                    reads=[cx.b_psum[py], b_mod], writes=[bx])
            else:
                tmp = cx.tmp[oc % 2]
                S.op("act", (lambda oc=oc, py=py, tmp=tmp: nc.scalar.activation(out=tmp[:], in_=cx.psum[py][:], func=AF.Identity, bias=bias[:, oc:oc + 1])),
                     reads=[cx.b_psum[py], cx.b_sp], writes=[cx.b_tmp[oc % 2]])
                S.op("dve", (lambda oc=oc, tmp=tmp, xt=xt: nc.vector.scalar_tensor_tensor(
                    out=xt[:, oc, :], in0=tmp[:], scalar=G[:, oc:oc + 1], in1=xt[:, oc, :], op0=ALU.mult, op1=ALU.add)),
                    reads=[cx.b_tmp[oc % 2], b_mod], writes=[bx])
        S.op("sp", (lambda xt=xt, t=t: nc.sync.dma_start(out=xv_out[:, :, t * 512:(t + 1) * 512], in_=xt[:])), reads=[bx], writes=[b_xout[t]])


def emit_glu_phase(S, cx, x_in, b_xin, w_d, A, Bv, b_mod, u_out, ntiles=8, hmask=None, b_hmask=None, b_uout=None):
    nc = S.nc
    w = S.sb("w_pw1", [128, KC, 2 * D], BF16)
    b_w = Buf("w_pw1")
    for kc in range(KC):
        S.op("pool_dma", (lambda kc=kc: nc.gpsimd.dma_start(out=w[:, kc, :], in_=w_d[kc * 128:(kc + 1) * 128, :])), writes=[b_w])
    uo = [S.sb("uo%d" % i, [128, 512], BF16) for i in range(2)]
    b_uo = [Buf("uo%d" % i) for i in range(2)]
    xv_in = x_in.rearrange("(c p) t -> p c t", p=128)
    uv = u_out.rearrange("(c p) t -> p c t", p=128)
    bp1 = spc(cx, "b_pw1")
    for t in range(ntiles):
        xt, bx = load_x_tile(S, cx, xv_in, b_xin, t)
        emit_norm_mod(S, cx, xt, bx, A, Bv, b_mod)
        for oc in range(KC):
            k = oc % 2
            pa = 1 + k * 2
            pg = pa + 1

            def mma(oc=oc, pa=pa):
                for kc in range(KC):
                    m = nc.tensor.matmul(cx.psum[pa][:], lhsT=w[:, kc, oc * 128:(oc + 1) * 128], rhs=cx.h[:, kc, :], start=(kc == 0), stop=(kc == KC - 1))
                return m

            def mmg(oc=oc, pg=pg):
                for kc in range(KC):
                    m = nc.tensor.matmul(cx.psum[pg][:], lhsT=w[:, kc, D + oc * 128:D + (oc + 1) * 128], rhs=cx.h[:, kc, :], start=(kc == 0), stop=(kc == KC - 1))
                return m
            S.op("pe", mma, reads=[b_w, cx.b_h], writes=[cx.b_psum[pa]])
            S.op("pe", mmg, reads=[b_w, cx.b_h], writes=[cx.b_psum[pg]])
            sa = cx.sa[k]
            S.op("act", (lambda oc=oc, pg=pg, sa=sa: nc.scalar.activation(out=sa[:], in_=cx.psum[pg][:], func=AF.Sigmoid, bias=bp1[:, 8 + oc:9 + oc])),
                 reads=[cx.b_psum[pg], cx.b_sp], writes=[cx.b_sa[k]])
            S.op("dve", (lambda oc=oc, pa=pa, sa=sa, k=k: nc.vector.scalar_tensor_tensor(
                out=uo[k][:], in0=cx.psum[pa][:], scalar=bp1[:, oc:oc + 1], in1=sa[:], op0=ALU.add, op1=ALU.mult)),
                reads=[cx.b_psum[pa], cx.b_sa[k], cx.b_sp], writes=[b_uo[k]])
            if hmask is not None and t == 8:
                S.op("pool", (lambda k=k: nc.gpsimd.tensor_tensor(out=uo[k][:], in0=uo[k][:], in1=hmask, op=ALU.mult)),
                     reads=[b_uo[k], b_hmask], writes=[b_uo[k]])
            S.op("sp", (lambda oc=oc, k=k, t=t: nc.sync.dma_start(out=uv[:, oc, t * 512:(t + 1) * 512], in_=uo[k][:])), reads=[b_uo[k]],
                 writes=([b_uout[t]] if b_uout is not None else []))


def build_L2():
    nc = bass.Bass("TRN2", target_bir_lowering=False)
    din = lambda n, s, dt: nc.dram_tensor(n, s, dt, kind="ExternalInput").ap()
    sp_d = din("sp", [128, SP_N], F32)
    mod_d = din("modi", [128, 144], F32)
    x1 = din("x1", [1024, TOK], F32)
    d = {"KT": din("KTf", [NH, DH, SEQ], BF16), "ind": din("ind", [64, SEQ], BF16), "Vd": din("Vd", [NH, 128, 128, DH], BF16),
         "QT": din("QT", [NH, DH, TOK], BF16), "KTo": din("KTo", [NH, DH, TOK], BF16), "Vo": din("Vo", [NH, 128, 32, DH], BF16),
         "kbar": din("kbarf", [DH, NH, NB], F32), "cst": din("cst", [128, CST_N], F32)}
    w_o = din("w_o", [1024, 1024], F32)
    w_in1 = din("w_in1", [1024, 2 * DFF], F32)
    w_out1 = din("w_out1", [DFF, 1024], F32)
    w_in2 = din("w_in2", [1024, 2 * DFF], F32)
    w_out2 = din("w_out2", [DFF, 1024], F32)
    w_pw1 = din("w_pw1", [1024, 2048], F32)
    OT = nc.dram_tensor("OT", [NH, DH, TOK], BF16).ap()
    xa = nc.dram_tensor("xa", [1024, TOK], F32).ap()
    xb = nc.dram_tensor("xb", [1024, TOK], F32).ap()
    x4 = nc.dram_tensor("x4", [1024, TOK], F32, kind="ExternalOutput").ap()
    uT = nc.dram_tensor("uT", [1024, TOK], BF16, kind="ExternalOutput").ap()
    d["OT"] = OT
    d["b_OT"] = [Buf("OT%d" % t) for t in range(8)]
    S = Sched(nc)
    cx = Ctx()
    setup_common(S, cx)
    emit_load_small(S, cx, sp_d)
    modT = [S.sb("modT%d" % i, [128, 72], F32) for i in range(2)]
    b_modT = [Buf("modT%d" % i) for i in range(2)]
    for i in range(2):
        S.op("sp", (lambda i=i: nc.sync.dma_start(out=modT[i][:], in_=mod_d[:, i * 72:(i + 1) * 72])), writes=[b_modT[i]])
    m01 = emit_mod_vectors_i(S, cx, modT[0], b_modT[0], spc(cx, "ng01"), "l0s1", 1)
    m02 = emit_mod_vectors_i(S, cx, modT[0], b_modT[0], spc(cx, "ng02"), "l0s2", 2)
    m10 = emit_mod_vectors_i(S, cx, modT[1], b_modT[1], spc(cx, "ng10"), "l1s0", 0)
    m11 = emit_mod_vectors_i(S, cx, modT[1], b_modT[1], spc(cx, "ng11"), "l1s1", 1)
    n0 = len(S._stack)
    emit_attn_phase(S, cx, d)
    S.barrier_free(len(S._stack) - n0)
    alloc_tile_bufs(S, cx)
    b_x1 = [Buf("x1_%d" % t) for t in range(8)]
    b_xa = [Buf("xa_%d" % t) for t in range(8)]
    b_xb = [Buf("xb_%d" % t) for t in range(8)]
    b_x4 = [Buf("x4_%d" % t) for t in range(8)]
    n1 = len(S._stack)
    emit_proj_res_phase(S, cx, x1, b_x1, xa, b_xa, OT, d["b_OT"], w_o, m01[2], None, m01[3], "wo", True)
    S.barrier_free(len(S._stack) - n1)
    alloc_ffn_bufs(S, cx)
    emit_ffn_phase(S, cx, xa, b_xa, xb, b_xb, w_in1, w_out1, m02[0], m02[1], m02[2], 0.5, m02[3], 8, "f01")
    emit_ffn_phase(S, cx, xb, b_xb, x4, b_x4, w_in2, w_out2, m10[0], m10[1], m10[2], 0.5, m10[3], 8, "f10")
    S.barrier_free(len(S._stack) - n1)
    emit_glu_phase(S, cx, x4, b_x4, w_pw1, m11[0], m11[1], m11[3], uT)
    S.emit()
    return nc


def emit_conv_phase(S, cx, uext_d, x_in, b_xin, x_out, b_xout, w2_d, ident_d, G, b_mod, u_scr=None, b_u=None):
    nc = S.nc
    w2 = S.sb("w_pw2", [128, KC, D], BF16)
    b_w2 = Buf("w_pw2")
    for kc in range(KC):
        S.op("pool_dma", (lambda kc=kc: nc.gpsimd.dma_start(out=w2[:, kc, :], in_=w2_d[kc * 128:(kc + 1) * 128, :])), writes=[b_w2])
    ident = S.sb("identc", [128, 128], F32)
    b_id = Buf("identc")
    S.op("sp", lambda: nc.sync.dma_start(out=ident[:], in_=ident_d[:, :]), writes=[b_id])
    dg = S.sb("dg", [128, KC * CW, 128], BF16)
    b_dg = Buf("dg")
    wdw = spc(cx, "w_dw")
    for ck in range(KC * CW):
        S.op("dve", (lambda ck=ck: nc.vector.tensor_scalar(out=dg[:, ck, :], in0=ident[:], scalar1=wdw[:, ck:ck + 1], scalar2=None, op0=ALU.mult)),
             reads=[b_id, cx.b_sp], writes=[b_dg])
    ue = [S.sb("ue%d" % i, [128, KC, 288], BF16) for i in range(2)]
    b_ue = [Buf("ue%d" % i) for i in range(2)]
    vs = S.sb("vs", [128, KC, 256], F32)
    b_vs = [Buf("vs%d" % c) for c in range(KC)]
    vb = [S.sb("vb%d" % i, [128, 256], BF16) for i in range(2)]
    b_vb = [Buf("vb%d" % i) for i in range(2)]
    vq = [S.sb("vq%d" % i, [128, 256], BF16) for i in range(2)]
    b_vq = [Buf("vq%d" % i) for i in range(2)]
    mu = S.sb("mu", [128, 256], F32)
    rs = S.sb("rs", [128, 256], F32)
    b_st = Buf("lnstat")
    uv = uext_d.rearrange("(c p) l e -> p c l e", p=128) if uext_d is not None else None
    usv = u_scr.rearrange("(c p) t -> p c t", p=128) if u_scr is not None else None
    xv_in = x_in.rearrange("(c p) t -> p c t", p=128)
    xv_out = x_out.rearrange("(c p) t -> p c t", p=128)
    for l in range(NLB):
        u_ = ue[l % 2]
        if usv is None:
            S.op("sp", (lambda u_=u_, l=l: nc.sync.dma_start(out=u_[:], in_=uv[:, :, l, :])), writes=[b_ue[l % 2]])
        else:
            S.op("sp", (lambda u_=u_, l=l: nc.sync.dma_start(out=u_[:, :, 32:288], in_=usv[:, :, l * 256:(l + 1) * 256])),
                 reads=[b_u[l // 2]], writes=[b_ue[l % 2]])
            S.op("sp", (lambda u_=u_, l=l: nc.sync.dma_start(out=u_[:, :, 0:32], in_=usv[:, :, TOK + l * 32:TOK + (l + 1) * 32])),
                 reads=[b_u[8]], writes=[b_ue[l % 2]])
        xt, bx = load_x_tile(S, cx, xv_in, b_xin, l, n=256)
        for c in range(KC):
            k2 = c % 2
            pc = cx.psum[1 + k2]

            def mmc(c=c, pc=pc, u_=u_):
                for k in range(CW):
                    m = nc.tensor.matmul(pc[:, 0:256], lhsT=dg[:, c * CW + k, :], rhs=u_[:, c, 2 + k:2 + k + 256], start=(k == 0), stop=(k == CW - 1))
                return m
            S.op("pe", mmc, reads=[b_dg, b_ue[l % 2]], writes=[cx.b_psum[1 + k2]])
            S.op("act", (lambda c=c, pc=pc: nc.scalar.activation(out=vs[:, c, :], in_=pc[:, 0:256], func=AF.Identity, bias=spc(cx, "b_dw")[:, c:c + 1])),
                 reads=[cx.b_psum[1 + k2], cx.b_sp], writes=[b_vs[c]])
            S.op("pool", (lambda c=c, k2=k2: nc.gpsimd.tensor_copy(out=vb[k2][:], in_=vs[:, c, :])), reads=[b_vs[c]], writes=[b_vb[k2]])
            S.op("act", (lambda c=c, k2=k2: nc.scalar.activation(out=vq[k2][:], in_=vs[:, c, :], func=AF.Square)), reads=[b_vs[c]], writes=[b_vq[k2]])
            S.op("pe", (lambda c=c, k2=k2: nc.tensor.matmul(cx.psum[3][:, 0:256], lhsT=cx.ones_bf[:], rhs=vb[k2][:], start=(c == 0), stop=(c == KC - 1))),
                 reads=[b_vb[k2], cx.b_ones], writes=[cx.b_psum[3]])
            S.op("pe", (lambda c=c, k2=k2: nc.tensor.matmul(cx.psum[4][:, 0:256], lhsT=cx.ones_bf[:], rhs=vq[k2][:], start=(c == 0), stop=(c == KC - 1))),
                 reads=[b_vq[k2], cx.b_ones], writes=[cx.b_psum[4]])
        S.op("dve", lambda: nc.vector.tensor_scalar(out=mu[:], in0=cx.psum[3][:, 0:256], scalar1=1.0 / D, scalar2=None, op0=ALU.mult),
             reads=[cx.b_psum[3]], writes=[b_st])
        S.op("dve", lambda: nc.vector.tensor_tensor(out=rs[:], in0=mu[:], in1=mu[:], op=ALU.mult), reads=[b_st], writes=[b_st])
        S.op("dve", lambda: nc.vector.scalar_tensor_tensor(out=rs[:], in0=cx.psum[4][:, 0:256], scalar=1.0 / D, in1=rs[:], op0=ALU.mult, op1=ALU.subtract),
             reads=[cx.b_psum[4], b_st], writes=[b_st])
        S.op("dve", lambda: nc.vector.tensor_scalar(out=rs[:], in0=rs[:], scalar1=EPS, scalar2=None, op0=ALU.add), reads=[b_st], writes=[b_st])
        S.op("act", lambda: nc.scalar.activation(out=rs[:], in_=rs[:], func=AF.Sqrt), reads=[b_st], writes=[b_st])
        S.op("dve", lambda: nc.vector.reciprocal(out=rs[:], in_=rs[:]), reads=[b_st], writes=[b_st])
        for c in range(KC):
            tmp = cx.tmp[c % 2]
            S.op("dve", (lambda c=c, tmp=tmp: nc.vector.tensor_tensor(out=tmp[:, 0:256], in0=vs[:, c, :], in1=mu[:], op=ALU.subtract)),
                 reads=[b_vs[c], b_st], writes=[cx.b_tmp[c % 2]])
            S.op("dve", (lambda c=c, tmp=tmp: nc.vector.tensor_tensor(out=tmp[:, 0:256], in0=tmp[:, 0:256], in1=rs[:], op=ALU.mult)),
                 reads=[b_st, cx.b_tmp[c % 2]], writes=[cx.b_tmp[c % 2]])
            S.op("act", (lambda c=c, tmp=tmp: nc.scalar.activation(out=cx.h[:, c, 0:256], in_=tmp[:, 0:256], func=AF.Silu,
                                                                   bias=spc(cx, "ln_b")[:, c:c + 1], scale=spc(cx, "ln_g")[:, c:c + 1])),
                 reads=[cx.b_tmp[c % 2], cx.b_sp], writes=[cx.b_h])
        for oc in range(KC):
            py = 5 + (oc % 2)

            def mm(oc=oc, py=py):
                for kc in range(KC):
                    m = nc.tensor.matmul(cx.psum[py][:, 0:256], lhsT=w2[:, kc, oc * 128:(oc + 1) * 128], rhs=cx.h[:, kc, 0:256], start=(kc == 0), stop=(kc == KC - 1))
                return m
            S.op("pe", mm, reads=[b_w2, cx.b_h], writes=[cx.b_psum[py]])
            sa = cx.sa[oc % 2]
            S.op("act", (lambda oc=oc, py=py, sa=sa: nc.scalar.activation(out=sa[:, 0:256], in_=cx.psum[py][:, 0:256], func=AF.Identity,
                                                                        bias=spc(cx, "b_pw2")[:, oc:oc + 1])),
                 reads=[cx.b_psum[py], cx.b_sp], writes=[cx.b_sa[oc % 2]])
            S.op("dve", (lambda oc=oc, sa=sa, xt=xt: nc.vector.scalar_tensor_tensor(
                out=xt[:, oc, 0:256], in0=sa[:, 0:256], scalar=G[:, oc:oc + 1], in1=xt[:, oc, 0:256], op0=ALU.mult, op1=ALU.add)),
                reads=[cx.b_sa[oc % 2], b_mod], writes=[bx])
        S.op("sp", (lambda xt=xt, l=l: nc.sync.dma_start(out=xv_out[:, :, l * 256:(l + 1) * 256], in_=xt[:, :, 0:256])),
             reads=[bx], writes=[b_xout[l // 2]])


def build_L3():
    nc = bass.Bass("TRN2", target_bir_lowering=False)
    din = lambda n, s, dt: nc.dram_tensor(n, s, dt, kind="ExternalInput").ap()
    sp_d = din("sp", [128, SP_N], F32)
    mod_d = din("modi", [128, 144], F32)
    x4 = din("x4", [1024, TOK], F32)
    uext = din("uext", [1024, NLB, 288], BF16)
    ident_d = din("ident", [128, 128], F32)
    w_pw2 = din("w_pw2", [1024, 1024], F32)
    w_in3 = din("w_in3", [1024, 2 * DFF], F32)
    w_out3 = din("w_out3", [DFF, 1024], F32)
    x5 = nc.dram_tensor("x5", [1024, TOK], F32).ap()
    xo = nc.dram_tensor("xo", [1024, TOK], F32, kind="ExternalOutput").ap()
    S = Sched(nc)
    cx = Ctx()
    setup_common(S, cx)
    emit_load_small(S, cx, sp_d)
    modT = [S.sb("modT%d" % i, [128, 72], F32) for i in range(2)]
    b_modT = [Buf("modT%d" % i) for i in range(2)]
    for i in range(2):
        S.op("sp", (lambda i=i: nc.sync.dma_start(out=modT[i][:], in_=mod_d[:, i * 72:(i + 1) * 72])), writes=[b_modT[i]])
    m11 = emit_mod_vectors_i(S, cx, modT[1], b_modT[1], spc(cx, "ng11"), "l1s1", 1)
    m12 = emit_mod_vectors_i(S, cx, modT[1], b_modT[1], spc(cx, "ng12"), "l1s2", 2)
    alloc_tile_bufs(S, cx)
    b_x4 = [Buf("x4_%d" % t) for t in range(16)]
    b_x5 = [Buf("x5_%d" % t) for t in range(8)]
    b_xo = [Buf("xo_%d" % t) for t in range(8)]
    n1 = len(S._stack)
    emit_conv_phase(S, cx, uext, x4, b_x4, x5, b_x5, w_pw2, ident_d, m11[2], m11[3])
    S.barrier_free(len(S._stack) - n1)
    alloc_ffn_bufs(S, cx)
    emit_ffn_phase(S, cx, x5, b_x5, xo, b_xo, w_in3, w_out3, m12[0], m12[1], m12[2], 0.5, m12[3], 8, "f11")
    S.emit()
    return nc


_NC = {}


def _get(name, fn):
    if name not in _NC:
        _NC[name] = fn()
    return _NC[name]


def _deint(a):
    sh = a.shape
    return np.ascontiguousarray(a.reshape(sh[:-1] + (sh[-1] // 256, 128, 2)).swapaxes(-1, -2)).reshape(sh)


def kernel_unfused(**inp):
    import ml_dtypes
    bf = ml_dtypes.bfloat16
    inp = {k: np.asarray(v) for k, v in inp.items()}
    cores = list(range(8))
    toks = []
    for core in cores:
        blocks = snake_blocks(core % 4)
        toks.append(np.concatenate([np.arange(g * 256, (g + 1) * 256) for g in blocks]))
    sps = [pack_small(inp, c // 4) for c in cores]
    nc1 = _get("L1", build_L1)
    maps = [{"sp": sps[c], "ada_w": inp["ada_w"], "xin": np.ascontiguousarray(inp["x"][c // 4][toks[c]].T),
             "w_in": inp["ffn_w_in"][0, 0], "w_out": inp["ffn_w_out"][0, 0], "wqkv": inp["attn_w_qkv"][0]} for c in cores]
    r1 = run_bass_kernel_spmd(nc1, maps, core_ids=cores).results
    ind = np.zeros((64, SEQ), bf)
    for m in range(64):
        ind[m, m * 256:(m + 1) * 256] = 1
    ident = np.eye(128, dtype=np.float32)
    maps2 = []
    per_batch = {}
    for b in range(2):
        KTf = np.zeros((NH, DH, SEQ), bf)
        Vf = np.zeros((SEQ, D), bf)
        kbf = np.zeros((DH, NH, NB), np.float32)
        for j in range(4):
            c = 4 * b + j
            blocks = snake_blocks(j)
            KT = np.asarray(r1[c]["KT"])
            V = np.asarray(r1[c]["V"])
            kb = np.asarray(r1[c]["kbar"])
            for l, g in enumerate(blocks):
                KTf[:, :, g * 256:(g + 1) * 256] = KT[:, :, l * 256:(l + 1) * 256]
                Vf[g * 256:(g + 1) * 256] = V[l * 256:(l + 1) * 256]
                kbf[:, :, g] = kb[:, :, l].T
        KTf = _deint(KTf)
        Vd = np.ascontiguousarray(Vf.reshape(NB, 128, 2, NH, DH).transpose(3, 1, 0, 2, 4)).reshape(NH, 128, 128, DH)
        per_batch[b] = (KTf, Vd, kbf)
    for c in cores:
        b = c // 4
        KTf, Vd, kbf = per_batch[b]
        KTo = _deint(np.asarray(r1[c]["KT"]))
        V = np.asarray(r1[c]["V"])
        Vo = np.ascontiguousarray(V.reshape(NLB, 128, 2, NH, DH).transpose(3, 1, 0, 2, 4)).reshape(NH, 128, 32, DH)
        maps2.append({"sp": sps[c], "modi": r1[c]["modo"], "x1": r1[c]["x1"], "KTf": KTf, "ind": ind, "Vd": Vd,
                      "QT": r1[c]["QT"], "KTo": KTo, "Vo": Vo, "kbarf": kbf, "cst": make_cst(c % 4),
                      "w_o": inp["attn_w_o"][0], "w_in1": inp["ffn_w_in"][0, 1], "w_out1": inp["ffn_w_out"][0, 1],
                      "w_in2": inp["ffn_w_in"][1, 0], "w_out2": inp["ffn_w_out"][1, 0], "w_pw1": inp["conv_w_pw1"][0]})
    nc2 = _get("L2", build_L2)
    r2 = run_bass_kernel_spmd(nc2, maps2, core_ids=cores).results
    maps3 = []
    for b in range(2):
        Uf = np.zeros((D, SEQ), bf)
        for j in range(4):
            c = 4 * b + j
            Uf[:, toks[c]] = np.asarray(r2[c]["uT"])
        for j in range(4):
            c = 4 * b + j
            ue = np.zeros((D, NLB, 288), bf)
            for l, g in enumerate(snake_blocks(j)):
                ue[:, l, 32:] = Uf[:, g * 256:(g + 1) * 256]
                if g > 0:
                    ue[:, l, :32] = Uf[:, g * 256 - 32:g * 256]
            maps3.append({"sp": sps[c], "modi": r1[c]["modo"], "x4": r2[c]["x4"], "uext": ue, "ident": ident,
                          "w_pw2": inp["conv_w_pw2"][0], "w_in3": inp["ffn_w_in"][1, 1], "w_out3": inp["ffn_w_out"][1, 1]})
    nc3 = _get("L3", build_L3)
    r3 = run_bass_kernel_spmd(nc3, maps3, core_ids=cores).results
    out = np.zeros((BATCH, SEQ, D), np.float32)
    for c in cores:
        out[c // 4][toks[c]] = np.asarray(r3[c]["xo"]).T
    return out


NPOS = 80
NT_ALL = 40
TOKX = TOK + 512


def pos_order(j):
    real = []
    own = snake_blocks(j)
    for i in range(8):
        A, B = 8 * i + j, 8 * i + 7 - j
        real += [A, B] + [g for g in range(8 * i, 8 * i + 8) if g not in (A, B)]
    dup = [(g - 1 if g > 0 else None) for g in own]
    return real, dup


def emit_kvq_phase(S, cx, x_d, b_x, wqkv_d, A, Bv, b_mod, QT_d, KT_d, kbar_d, V_d, x1o_d, b_x1o, b_kv):
    nc = S.nc
    wq = S.sb("wqkv_sb", [128, KC, 3072], BF16)
    b_wq = [Buf("wqkv%d" % k) for k in range(KC)]
    for kc in range(KC):
        S.op("pool_dma", (lambda kc=kc: nc.gpsimd.dma_start(out=wq[:, kc, :], in_=wqkv_d[kc * 128:(kc + 1) * 128, :])), writes=[b_wq[kc]])
    bo = S.sb("blkones", [128, 128], BF16)
    b_bo = Buf("blkones")
    S.op("dve", lambda: nc.vector.memset(bo[:], 0.0), writes=[b_bo])
    S.op("dve", lambda: nc.vector.memset(bo[0:64, 0:64], 1.0), writes=[b_bo])
    S.op("dve", lambda: nc.vector.memset(bo[64:128, 64:128], 1.0), writes=[b_bo])
    g8 = S.sb("g8", [128, 2], F32)
    b_g8 = Buf("g8")
    S.op("dve", lambda: nc.vector.tensor_scalar(out=g8[:, 0:1], in0=spc(cx, "gq"), scalar1=0.125, scalar2=None, op0=ALU.mult),
         reads=[cx.b_sp], writes=[b_g8])
    S.op("dve", lambda: nc.vector.tensor_copy(out=g8[:, 1:2], in_=spc(cx, "gk")), reads=[cx.b_sp], writes=[b_g8])
    epst = S.sb("epst", [128, 1], F32)
    b_eps = Buf("epst")
    S.op("dve", lambda: nc.vector.memset(epst[:], EPS), writes=[b_eps])
    qo = [S.sb("qo%d" % i, [128, 512], BF16) for i in range(2)]
    b_qo = [Buf("qo%d" % i) for i in range(2)]
    kb = [S.sb("kb%d" % i, [128, 2], F32) for i in range(2)]
    b_kb = [Buf("kb%d" % i) for i in range(2)]
    vt = [S.sb("vt%d" % i, [128, 1024], BF16) for i in range(2)]
    b_vt = [Buf("vt%d" % i) for i in range(2)]
    xv = x_d.rearrange("(c p) t -> p c t", p=128)
    xov = x1o_d.rearrange("(c p) t -> p c t", p=128)
    QTv = QT_d.rearrange("h d t -> (h d) t")
    KTv = KT_d.rearrange("h d t -> (h d) t")
    cnt = 0
    for t in range(NT_ALL):
        is_own = (t < 32 and t % 4 == 0)
        is_dup = t >= 32
        xt, bx = cx.xt[t % 2], cx.b_xt[t % 2]
        if t == 0:
            load_x_tile(S, cx, xv, b_x, 0)
        if is_own:
            S.op("sp", (lambda xt=xt, t=t: nc.sync.dma_start(out=xov[:, :, (t // 4) * 512:(t // 4 + 1) * 512], in_=xt[:])),
                 reads=[bx], writes=[b_x1o[t // 4]])
        if is_dup:
            i2 = t - 32
            for k2 in range(2):
                S.op("sp", (lambda xt=xt, i2=i2, k2=k2: nc.sync.dma_start(
                    out=xov[:, :, TOK + (2 * i2 + k2) * 32:TOK + (2 * i2 + k2 + 1) * 32], in_=xt[:, :, k2 * 256 + 224:k2 * 256 + 256])),
                    reads=[bx], writes=[b_x1o[8]])
        emit_norm_mod(S, cx, xt, bx, A, Bv, b_mod)
        if t + 1 < NT_ALL:
            load_x_tile(S, cx, xv, b_x, t + 1)
        items = [(w_, hp) for w_ in range(2) for hp in range(8) if not (w_ == 0 and not (is_own or is_dup))]
        base_cnt = cnt
        cnt += len(items)

        def st_A(ii, base_cnt=base_cnt, items=items):
            which, hp = items[ii]
            k = (base_cnt + ii) % 2
            goff = which * 1024
            pq = cx.psum[1 + k]

            def mm():
                for kc in range(KC):
                    m = nc.tensor.matmul(pq[:], lhsT=wq[:, kc, goff + hp * 128:goff + (hp + 1) * 128], rhs=cx.h[:, kc, :],
                                         start=(kc == 0), stop=(kc == KC - 1))
                return m
            S.op("pe", mm, reads=b_wq + [cx.b_h], writes=[cx.b_psum[1 + k]])
            S.op("act", (lambda: nc.scalar.activation(out=cx.sq[k][:], in_=pq[:], func=AF.Square)),
                 reads=[cx.b_psum[1 + k]], writes=[cx.b_sq[k]])

        def st_B(ii, base_cnt=base_cnt, items=items, t=t, is_own=is_own):
            which, hp = items[ii]
            k = (base_cnt + ii) % 2
            pq = cx.psum[1 + k]
            pss = cx.psum[3 + k]
            S.op("pe", (lambda: nc.tensor.matmul(pss[:], lhsT=bo[:], rhs=cx.sq[k][:], start=True, stop=True)),
                 reads=[cx.b_sq[k], b_bo], writes=[cx.b_psum[3 + k]])
            r1 = cx.tmp[k]
            S.op("act", (lambda: nc.scalar.activation(out=r1[:], in_=pss[:], func=AF.Ln, bias=epst[:, 0:1], scale=1.0 / DH)),
                 reads=[cx.b_psum[3 + k], b_eps], writes=[cx.b_tmp[k]])
            S.op("act", (lambda: nc.scalar.activation(out=r1[:], in_=r1[:], func=AF.Exp, scale=-0.5)), reads=[cx.b_tmp[k]], writes=[cx.b_tmp[k]])
            if which == 0:
                S.op("dve", (lambda: nc.vector.scalar_tensor_tensor(out=qo[k][:], in0=pq[:], scalar=g8[:, 0:1], in1=r1[:], op0=ALU.mult, op1=ALU.mult)),
                     reads=[cx.b_tmp[k], cx.b_psum[1 + k], b_g8], writes=[b_qo[k]])
                if is_own:
                    for k2 in range(2):
                        lb = 2 * (t // 4) + k2
                        S.op("sp", (lambda k2=k2, lb=lb: nc.sync.dma_start(out=QTv[hp * 128:(hp + 1) * 128, lb * 288:lb * 288 + 256],
                                                                         in_=qo[k][:, k2 * 256:(k2 + 1) * 256])),
                             reads=[b_qo[k]], writes=[b_kv])
                else:
                    i2 = t - 32
                    for k2 in range(2):
                        lb = 2 * i2 + k2
                        S.op("sp", (lambda k2=k2, lb=lb: nc.sync.dma_start(
                            out=QTv[hp * 128:(hp + 1) * 128, lb * 288 + 256:lb * 288 + 288],
                            in_=qo[k][:, k2 * 256 + 224:k2 * 256 + 256])), reads=[b_qo[k]], writes=[b_kv])
            else:
                for bb in range(2):
                    S.op("dve", (lambda bb=bb: nc.vector.scalar_tensor_tensor(
                        out=qo[k][:, bb * 256:(bb + 1) * 256].rearrange("q (c p) -> q c p", c=2),
                        in0=pq[:, bb * 256:(bb + 1) * 256].rearrange("q (p c) -> q c p", c=2), scalar=g8[:, 1:2],
                        in1=r1[:, bb * 256:(bb + 1) * 256].rearrange("q (p c) -> q c p", c=2), op0=ALU.mult, op1=ALU.mult)),
                        reads=[cx.b_tmp[k], cx.b_psum[1 + k], b_g8], writes=[b_qo[k]])
                S.op("sp", (lambda: nc.sync.dma_start(out=KTv[hp * 128:(hp + 1) * 128, t * 512:(t + 1) * 512], in_=qo[k][:])),
                     reads=[b_qo[k]], writes=[b_kv])
                S.op("dve", (lambda: nc.vector.tensor_reduce(out=kb[k][:], in_=qo[k][:].rearrange("p (b t) -> p b t", b=2), axis=AX.X, op=ALU.add)),
                     reads=[b_qo[k]], writes=[b_kb[k]])
                S.op("dve", (lambda: nc.vector.tensor_scalar(out=kb[k][:], in0=kb[k][:], scalar1=1.0 / BLK, scalar2=None, op0=ALU.mult)),
                     reads=[b_kb[k]], writes=[b_kb[k]])
                S.op("sp", (lambda: nc.sync.dma_start(out=kbar_d[hp * 128:(hp + 1) * 128, 2 * t:2 * t + 2], in_=kb[k][:])),
                     reads=[b_kb[k]], writes=[b_kv])
        st_A(0)
        for ii in range(len(items)):
            if ii + 1 < len(items):
                st_A(ii + 1)
            st_B(ii)
        for tg in range(4):
            v = vt[tg % 2]
            for ns in range(2):
                pv = cx.psum[5 + ns]

                def mmv(tg=tg, ns=ns, pv=pv):
                    for kc in range(KC):
                        m = nc.tensor.matmul(pv[:], lhsT=cx.h[:, kc, tg * 128:(tg + 1) * 128], rhs=wq[:, kc, 2048 + ns * 512:2048 + (ns + 1) * 512],
                                             start=(kc == 0), stop=(kc == KC - 1))
                    return m
                S.op("pe", mmv, reads=b_wq + [cx.b_h], writes=[cx.b_psum[5 + ns]])
                if ns == 0:
                    S.op("act", (lambda v=v, pv=pv: nc.scalar.copy(out=v[:, 0:512], in_=pv[:])), reads=[cx.b_psum[5]], writes=[b_vt[tg % 2]])
                else:
                    S.op("dve", (lambda v=v, pv=pv: nc.vector.tensor_copy(out=v[:, 512:1024], in_=pv[:])), reads=[cx.b_psum[6]], writes=[b_vt[tg % 2]])
            blk = 2 * t + tg // 2
            hf = tg % 2
            for c2 in range(2):
                dst = V_d[:, hf * 64:(hf + 1) * 64, 2 * blk + c2, :].rearrange("h pp d -> pp h d")
                S.op("sp", (lambda v=v, dst=dst, c2=c2: nc.sync.dma_start(out=dst, in_=v[c2:128:2, :].rearrange("p (h d) -> p h d", h=NH))),
                     reads=[b_vt[tg % 2]], writes=[b_kv])


CST2_OFF = {}


def _cst2_layout():
    off = 0
    for name, w in (("past", 2048), ("base", 1024), ("irowX", 288), ("Dm", 512), ("causal", 512),
                    ("DmH", 64), ("causalH", 64), ("ident", 128), ("sel65", 64)):
        CST2_OFF[name] = (off, w)
        off += w
    return off


CST2_N = _cst2_layout()


def make_cst2(j):
    real, dup = pos_order(j)
    own = snake_blocks(j)
    c = np.zeros((128, CST2_N), np.float32)
    p = np.arange(128, dtype=np.float32)[:, None]
    gpos = np.array(real)
    past = np.zeros((32, 64), np.float32)
    base = np.zeros((128, 16, 64), np.float32)
    for row in range(32):
        o = own[row] if row < 16 else own[row - 16] - 1
        valid = gpos < o
        past[row] = np.where(valid, 0.0, NEG)
        if row < 16:
            dist = np.where(valid, o - gpos, 1).astype(np.float32)
            base[:, row, :] = 2.0 * p - 256.0 * dist[None, :]

    def put(name, arr):
        o_, w = CST2_OFF[name]
        c[:, o_:o_ + w] = arr.reshape(arr.shape[0], -1)
    put("past", np.tile(past.reshape(1, 2048), (128, 1)))
    put("base", base.reshape(128, 1024))
    irx = np.concatenate([np.arange(256, dtype=np.float32), -(32.0 - np.arange(32, dtype=np.float32))])
    put("irowX", np.tile(irx[None, :], (128, 1)))
    pp = np.arange(128, dtype=np.float32)[:, None, None]
    cc = np.arange(2, dtype=np.float32)[None, :, None]
    i = np.arange(256, dtype=np.float32)[None, None, :]
    put("Dm", np.broadcast_to(i - 2 * pp, (128, 2, 256)).copy())
    put("causal", np.where(i >= 2 * pp + cc, 0.0, NEG).astype(np.float32))
    ih = 224 + np.arange(32, dtype=np.float32)[None, None, :]
    put("DmH", np.broadcast_to(ih - 2 * pp, (128, 2, 32)).copy())
    put("causalH", np.where(ih >= 2 * pp + cc, 0.0, NEG).astype(np.float32))
    put("ident", np.eye(128, dtype=np.float32))
    s65 = np.zeros((128, 64), np.float32)
    s65[64] = 1.0
    put("sel65", s65)
    hmask = np.ones((128, 512), np.float32)
    for l in range(16):
        if dup[l] is None:
            hmask[:, l * 32:(l + 1) * 32] = 0.0
    return c, hmask


def emit_attn2(S, cx, d):
    nc = S.nc
    cst = S.sb("cst2", [128, CST2_N], F32)
    b_cst = Buf("cst2")
    S.op("sp", lambda: nc.sync.dma_start(out=cst[:], in_=d["cst"][:, :]), writes=[b_cst])

    def C(name):
        o, w = CST2_OFF[name]
        return cst[:, o:o + w]
    kbh = S.sb("kbh", [64, NH * NB], BF16)
    kbl = S.sb("kbl", [64, NH * NB], BF16)
    b_kb = Buf("kbhl")
    n0 = len(S._stack)
    kb32 = S.sb("kb32", [64, NH * NB], F32)
    kbt = S.sb("kbt", [64, NH * NB], F32)
    S.op("sp", lambda: nc.sync.dma_start(out=kb32[:].rearrange("d (h m) -> d h m", h=NH),
                                         in_=d["kbar"].rearrange("(h d) m -> d h m", h=NH)[:, :, 0:NB]), reads=[d["b_kv"]], writes=[b_kb])
    S.op("dve", lambda: nc.vector.tensor_copy(out=kbh[:], in_=kb32[:]), reads=[b_kb], writes=[b_kb])
    S.op("dve", lambda: nc.vector.tensor_copy(out=kbt[:], in_=kbh[:]), reads=[b_kb], writes=[b_kb])
    S.op("dve", lambda: nc.vector.tensor_tensor(out=kbt[:], in0=kb32[:], in1=kbt[:], op=ALU.subtract), reads=[b_kb], writes=[b_kb])
    S.op("dve", lambda: nc.vector.tensor_copy(out=kbl[:], in_=kbt[:]), reads=[b_kb], writes=[b_kb])
    S.barrier_free(len(S._stack) - n0)
    KTa = [S.sb("KTa%d" % i, [128, SEQ], BF16) for i in range(2)]
    Va = [S.sb("Va%d" % i, [128, 128, 65], BF16) for i in range(2)]
    Qa = [S.sb("Qa%d" % i, [128, TOKX], BF16) for i in range(2)]
    Kd = [S.sb("Kd%d" % i, [128, TOK], BF16) for i in range(2)]
    Vdd = [S.sb("Vdd%d" % i, [128, 32, 65], BF16) for i in range(2)]
    bT = [S.sb("bT%d" % i, [128, 1024], F32) for i in range(2)]
    dgb = [S.sb("dgb%d" % i, [128, 512], F32) for i in range(2)]
    dgbH = [S.sb("dgbH%d" % i, [128, 64], F32) for i in range(2)]
    cq = [S.sb("cq%d" % i, [128, 288], F32) for i in range(2)]
    b_KT = [Buf("KTa%d" % i) for i in range(2)]
    b_Va = [Buf("Va%d" % i) for i in range(2)]
    b_Qa = [Buf("Qa%d" % i) for i in range(2)]
    b_Qm = [[Buf("Qm%d_%d" % (i, l)) for l in range(16)] for i in range(2)]
    b_Kd = [Buf("Kd%d" % i) for i in range(2)]
    b_Vdd = [Buf("Vdd%d" % i) for i in range(2)]
    b_hd = [Buf("hd%d" % i) for i in range(2)]
    mbp = [S.sb("mbp%d" % i, [128, 128], F32) for i in range(2)]
    b_mbp = [Buf("mbp%d" % i) for i in range(2)]
    gmt = [S.sb("gmt%d" % i, [128, 64], F32) for i in range(2)]
    b_gmt = [Buf("gmt%d" % i) for i in range(2)]
    t8 = [S.sb("t8_%d" % i, [128, 8], F32) for i in range(2)]
    pt = [S.sb("pt%d" % i, [128, 2, 288], BF16) for i in range(2)]
    b_pt = [Buf("pt%d" % i) for i in range(2)]
    pt2 = S.sb("pt2", [128, 512], BF16)
    b_pt2 = Buf("pt2")
    dtmp = S.sb("dtmp", [128, 512], F32)
    b_dtmp = Buf("dtmp")
    o1 = S.sb("o1", [128, 288], F32)
    o2 = S.sb("o2", [128, 288], F32)
    rd = S.sb("rd", [128, 288], F32)
    ob = [S.sb("ob%d" % i, [128, 288], BF16) for i in range(2)]
    b_o = Buf("o12")
    b_ob = [Buf("ob%d" % i) for i in range(2)]
    for i in range(2):
        S.op("sp", (lambda i=i: nc.sync.dma_start(out=KTa[i][64:128, :], in_=d["ind"][:, :])), writes=[b_KT[i]])
        S.op("dve", (lambda i=i: nc.vector.memset(Va[i][:, :, 64:65], 1.0)), writes=[b_Va[i]])
        S.op("dve", (lambda i=i: nc.vector.memset(Vdd[i][:, :, 64:65], 1.0)), writes=[b_Vdd[i]])
        S.op("dve", (lambda i=i: nc.vector.memset(mbp[i][:], 0.0)), writes=[b_mbp[i]])
    ps = cx.psum
    bp = cx.b_psum
    SX = [cx.psall[:, 0:1024], cx.psall[:, 1024:2048]]
    b_SX = [[bp[0], bp[1]], [bp[2], bp[3]]]
    scnt = 0
    ocnt = 0
    NQ = 288
    for h in range(NH):
        cur = h % 2
        sl = SLOPES[h]
        es = math.exp(sl)
        S.op("sp", (lambda h=h, cur=cur: nc.sync.dma_start(out=KTa[cur][0:64, :], in_=d["KT"][h, :, 0:SEQ])), reads=[d["b_kv"]], writes=[b_KT[cur]])
        for q8 in range(8):
            S.op("sp", (lambda h=h, cur=cur, q8=q8: nc.sync.dma_start(out=Va[cur][:, q8 * 16:(q8 + 1) * 16, 0:64], in_=d["Vd"][h, :, q8 * 16:(q8 + 1) * 16, :])),
                 reads=[d["b_kv"]], writes=[b_Va[cur]])
        S.op("sp", (lambda h=h, cur=cur: nc.sync.dma_start(out=Qa[cur][0:64, :], in_=d["QT"][h])), reads=[d["b_kv"]], writes=[b_Qa[cur]] + b_Qm[cur])
        S.op("sp", (lambda h=h, cur=cur: nc.sync.dma_start(out=Kd[cur][0:64, :], in_=d["KT"][h, :, SEQ:SEQ + TOK])), reads=[d["b_kv"]], writes=[b_Kd[cur]])
        for q2 in range(2):
            S.op("sp", (lambda h=h, cur=cur, q2=q2: nc.sync.dma_start(out=Vdd[cur][:, q2 * 16:(q2 + 1) * 16, 0:64],
                                                                     in_=d["Vd"][h, :, 128 + q2 * 16:128 + (q2 + 1) * 16, :])),
                 reads=[d["b_kv"]], writes=[b_Vdd[cur]])
        for (VV, bV) in ((Va[cur], b_Va[cur]), (Vdd[cur], b_Vdd[cur])):
            Vodd = VV[:].rearrange("p (n two) e -> p n two e", two=2)[:, :, 1, :]
            S.op("dve", (lambda Vodd=Vodd, es=es: nc.vector.tensor_scalar(out=Vodd[:, :, 0:64], in0=Vodd[:, :, 0:64], scalar1=es, scalar2=None, op0=ALU.mult)),
                 reads=[bV], writes=[bV])
            S.op("dve", (lambda Vodd=Vodd, es=es: nc.vector.memset(Vodd[:, :, 64:65], es)), reads=[bV], writes=[bV])
        S.op("dve", (lambda cur=cur, sl=sl: nc.vector.tensor_scalar(out=bT[cur][:], in0=C("base"), scalar1=sl, scalar2=None, op0=ALU.mult)),
             reads=[b_cst], writes=[b_hd[cur]])
        S.op("dve", (lambda cur=cur, sl=sl: nc.vector.scalar_tensor_tensor(out=dgb[cur][:], in0=C("Dm"), scalar=-sl, in1=C("causal"),
                                                                         op0=ALU.mult, op1=ALU.add)), reads=[b_cst], writes=[b_hd[cur]])
        S.op("dve", (lambda cur=cur, sl=sl: nc.vector.scalar_tensor_tensor(out=dgbH[cur][:], in0=C("DmH"), scalar=-sl, in1=C("causalH"),
                                                                         op0=ALU.mult, op1=ALU.add)), reads=[b_cst], writes=[b_hd[cur]])
        S.op("act", (lambda cur=cur, sl=sl: nc.scalar.activation(out=cq[cur][:], in_=C("irowX"), func=AF.Exp, scale=-sl)),
             reads=[b_cst], writes=[b_hd[cur]])
        groups = []
        for l in range(NLB):
            groups.append((l * NQ, 128, l, l))
            groups.append((l * NQ + 128, 128, l, l))
            groups.append((l * NQ + 256, 32, 16 + l, l))

        def g_s1(gi, cur=cur, h=h, groups=groups):
            q0, nr, row, qmi = groups[gi]

            def mmg():
                nc.tensor.matmul(ps[6][0:nr, 0:64], lhsT=Qa[cur][0:64, q0:q0 + nr], rhs=kbh[:, h * 64:(h + 1) * 64], start=True, stop=False)
                return nc.tensor.matmul(ps[6][0:nr, 0:64], lhsT=Qa[cur][0:64, q0:q0 + nr], rhs=kbl[:, h * 64:(h + 1) * 64], start=False, stop=True)
            S.op("pe", mmg, reads=[b_Qa[cur], b_kb], writes=[bp[6]])

        def g_s2(gi, groups=groups):
            q0, nr, row, qmi = groups[gi]
            g = gi % 2
            pb = C("past")[0:nr, row * 64:(row + 1) * 64]
            S.op("dve", (lambda: nc.vector.tensor_tensor(out=gmt[g][0:nr, :], in0=ps[6][0:nr, 0:64], in1=pb, op=ALU.add)),
                 reads=[bp[6], b_cst], writes=[b_gmt[g]])
            S.op("dve", (lambda: nc.vector.max(out=t8[g][0:nr, :], in_=gmt[g][0:nr, :])), reads=[b_gmt[g]], writes=[b_gmt[g]])
            S.op("dve", (lambda: nc.vector.tensor_scalar(out=gmt[g][0:nr, :], in0=gmt[g][0:nr, :], scalar1=t8[g][0:nr, 2:3], scalar2=-NEG,
                                                         op0=ALU.is_ge, op1=ALU.mult)), reads=[b_gmt[g]], writes=[b_gmt[g]])
            S.op("dve", (lambda: nc.vector.scalar_tensor_tensor(out=mbp[g][0:nr, 64:128], in0=gmt[g][0:nr, :], scalar=NEG, in1=pb,
                                                                op0=ALU.add, op1=ALU.add)),
                 reads=[b_gmt[g], b_cst], writes=[b_mbp[g]])

        def g_s3(gi, cur=cur, groups=groups):
            q0, nr, row, qmi = groups[gi]
            g = gi % 2
            S.op("pe", (lambda: nc.tensor.transpose(ps[7][:, 0:nr], mbp[g][0:nr, :], C("ident")[0:nr, 0:nr])),
                 reads=[b_mbp[g], b_cst], writes=[bp[7]])
            S.op("act", (lambda: nc.scalar.copy(out=Qa[cur][64:128, q0:q0 + nr], in_=ps[7][64:128, 0:nr])),
                 reads=[bp[7]], writes=[b_Qm[cur][qmi]])
        g_s1(0)
        for gi in range(len(groups)):
            g_s2(gi)
            if gi + 1 < len(groups):
                g_s1(gi + 1)
            g_s3(gi)
        for i in range(8):
            nk = 8 * i + 8
            for k in range(2):
                l = 2 * i + k
                q0 = l * NQ
                po = 4 + k
                sbase = scnt
                scnt += nk

                def emit_S(n, cur=cur, q0=q0, l=l, sbase=sbase):
                    sb_ = (sbase + n) % 2

                    def mms():
                        for c in range(2):
                            m = nc.tensor.matmul(SX[sb_][:, c * 512:c * 512 + NQ], lhsT=KTa[cur][:, n * 256 + c * 128:n * 256 + (c + 1) * 128],
                                                 rhs=Qa[cur][:, q0:q0 + NQ], start=True, stop=True)
                        return m
                    S.op("pe", mms, reads=[b_KT[cur], b_Qa[cur], b_Qm[cur][l]], writes=b_SX[sb_])

                def emit_E(n, cur=cur, l=l, sbase=sbase):
                    sb_ = (sbase + n) % 2
                    S.op("act", (lambda: nc.scalar.activation(out=pt[sb_][:], in_=SX[sb_].rearrange("p (c x) -> p c x", c=2)[:, :, 0:NQ], func=AF.Exp,
                                                              bias=bT[cur][:, l * 64 + n:l * 64 + n + 1])),
                         reads=b_SX[sb_] + [b_hd[cur]], writes=[b_pt[sb_]])

                def emit_PV(n, cur=cur, po=po, nk=nk, sbase=sbase):
                    sb_ = (sbase + n) % 2

                    def mmpv():
                        for c in range(2):
                            m = nc.tensor.matmul(ps[po][0:65, 0:NQ], lhsT=Va[cur][:, 2 * n + c, :], rhs=pt[sb_][:, c, :],
                                                 start=(n == 0 and c == 0), stop=(n == nk - 1 and c == 1))
                        return m
                    S.op("pe", mmpv, reads=[b_Va[cur], b_pt[sb_]], writes=[bp[po]])
                emit_S(0)
                for n in range(nk):
                    emit_E(n)
                    if n + 1 < nk:
                        emit_S(n + 1)
                    emit_PV(n)
                for (Kt, koff, Vt, ch0, bK, bV, dg, qoff, nq, ocol) in (
                        (KTa[cur], (8 * i + k) * 256, Va[cur], 2 * (8 * i + k), b_KT[cur], b_Va[cur], dgb[cur], q0, 256, 0),
                        (Kd[cur], l * 256, Vdd[cur], 2 * l, b_Kd[cur], b_Vdd[cur], dgbH[cur], q0 + 256, 32, 256)):
                    def mmd(Kt=Kt, koff=koff, qoff=qoff, nq=nq, cur=cur):
                        for c in range(2):
                            m = nc.tensor.matmul(ps[6][:, c * nq:(c + 1) * nq], lhsT=Kt[0:64, koff + c * 128:koff + (c + 1) * 128],
                                                 rhs=Qa[cur][0:64, qoff:qoff + nq], start=True, stop=True)
                        return m
                    S.op("pe", mmd, reads=[bK, b_Qa[cur]], writes=[bp[6]])
                    S.op("dve", (lambda dg=dg, nq=nq: nc.vector.tensor_tensor(out=dtmp[:, 0:2 * nq], in0=ps[6][:, 0:2 * nq], in1=dg[:, 0:2 * nq], op=ALU.add)),
                         reads=[bp[6], b_hd[cur]], writes=[b_dtmp])
                    S.op("act", (lambda nq=nq: nc.scalar.activation(out=pt2[:, 0:2 * nq], in_=dtmp[:, 0:2 * nq], func=AF.Exp)), reads=[b_dtmp], writes=[b_pt2])

                    def mmpo(Vt=Vt, ch0=ch0, nq=nq, ocol=ocol):
                        for c in range(2):
                            m = nc.tensor.matmul(ps[7][0:65, ocol:ocol + nq], lhsT=Vt[:, ch0 + c, :], rhs=pt2[:, c * nq:(c + 1) * nq],
                                                 start=(c == 0), stop=(c == 1))
                        return m
                    S.op("pe", mmpo, reads=[bV, b_pt2], writes=[bp[7]])
                S.op("dve", (lambda cur=cur, po=po: nc.vector.tensor_tensor(out=o1[0:65, :], in0=ps[po][0:65, 0:NQ], in1=cq[cur][0:65, :], op=ALU.mult)),
                     reads=[bp[po], b_hd[cur]], writes=[b_o])
                S.op("dve", (lambda: nc.vector.tensor_tensor(out=o2[0:65, :], in0=o1[0:65, :], in1=ps[7][0:65, 0:NQ], op=ALU.add)),
                     reads=[bp[7], b_o], writes=[b_o])
                S.op("pe", (lambda: nc.tensor.matmul(ps[6][0:64, 0:NQ], lhsT=C("sel65")[0:65, :], rhs=o2[0:65, :], start=True, stop=True)),
                     reads=[b_o, b_cst], writes=[bp[6]])
                S.op("dve", (lambda: nc.vector.reciprocal(out=rd[0:64, :], in_=ps[6][0:64, 0:NQ])), reads=[bp[6]], writes=[b_o])
                kk = ocnt % 2
                ocnt += 1
                S.op("dve", (lambda kk=kk: nc.vector.tensor_tensor(out=ob[kk][0:64, :], in0=o2[0:64, :], in1=rd[0:64, :], op=ALU.mult)),
                     reads=[b_o], writes=[b_ob[kk]])
                S.op("sp", (lambda kk=kk, h=h, l=l: nc.sync.dma_start(out=d["OT"][h, :, l * 256:(l + 1) * 256], in_=ob[kk][0:64, 0:256])),
                     reads=[b_ob[kk]], writes=[d["b_OT"][l // 2]])
                S.op("sp", (lambda kk=kk, h=h, l=l: nc.sync.dma_start(out=d["OT"][h, :, TOK + l * 32:TOK + (l + 1) * 32], in_=ob[kk][0:64, 256:288])),
                     reads=[b_ob[kk]], writes=[d["b_OT"][8]])


def build_fused():
    nc = bass.Bass("TRN2", target_bir_lowering=False)
    din = lambda n, s, dt: nc.dram_tensor(n, s, dt, kind="ExternalInput").ap()
    scr = lambda n, s, dt: nc.dram_tensor(n, s, dt).ap()
    sp_d = din("sp", [128, SP_N], F32)
    ada_w = din("ada_w", [2, 1024, 9216], F32)
    xall = din("xall", [1024, NT_ALL * 512], F32)
    w_in = [din("w_in%d" % i, [1024, 2 * DFF], F32) for i in range(4)]
    w_out = [din("w_out%d" % i, [DFF, 1024], F32) for i in range(4)]
    wqkv = din("wqkv", [1024, 3072], F32)
    w_o = din("w_o", [1024, 1024], F32)
    w_pw1 = din("w_pw1", [1024, 2048], F32)
    w_pw2 = din("w_pw2", [1024, 1024], F32)
    ind_d = din("ind", [64, SEQ], BF16)
    cst_d = din("cst", [128, CST2_N], F32)
    hm_d = din("hmask", [128, 512], F32)
    ident_d = din("ident", [128, 128], F32)
    xo = nc.dram_tensor("xo", [1024, TOK], F32, kind="ExternalOutput").ap()
    x1all = scr("x1all", [1024, NT_ALL * 512], F32)
    x1o = scr("x1o", [1024, TOKX], F32)
    xa = scr("xa", [1024, TOKX], F32)
    xb = scr("xb", [1024, TOKX], F32)
    x4 = scr("x4", [1024, TOKX], F32)
    x5 = scr("x5", [1024, TOK], F32)
    uT = scr("uT", [1024, TOKX], BF16)
    KT = scr("KTs", [NH, DH, NPOS * 256], BF16)
    Vd = scr("Vds", [NH, 128, 2 * NPOS, DH], BF16)
    kbar = scr("kbars", [NH * DH, NPOS], F32)
    QT = scr("QTs", [NH, DH, TOKX], BF16)
    OT = scr("OTs", [NH, DH, TOKX], BF16)
    S = Sched(nc)
    cx = Ctx()
    setup_common(S, cx)
    emit_load_small(S, cx, sp_d)
    hmask = S.sb("hmask", [128, 512], F32)
    b_hm = Buf("hmask")
    S.op("sp", lambda: nc.sync.dma_start(out=hmask[:], in_=hm_d[:, :]), writes=[b_hm])
    modT = [S.sb("modT%d" % i, [128, 72], F32) for i in range(2)]
    b_modT = [Buf("modT%d" % i) for i in range(2)]
    emit_adaln_scoped(S, cx, ada_w, modT, b_modT)
    M = {}
    for i in range(2):
        for j in range(3):
            M[(i, j)] = emit_mod_vectors_i(S, cx, modT[i], b_modT[i], spc(cx, "ng%d%d" % (i, j)), "l%ds%d" % (i, j), j)
    nbase = len(S._stack)
    mk = lambda name, n: [Buf("%s_%d" % (name, t)) for t in range(n)]
    b_xall, b_x1all, b_x1o = mk("xall", NT_ALL), mk("x1all", NT_ALL), mk("x1o", 9)
    b_xa, b_xb, b_x4, b_u, b_x5, b_xo = mk("xa", 9), mk("xb", 9), mk("x4", 16), mk("u", 9), mk("x5", 8), mk("xo", 8)
    b_x4t = b_x4[:9]
    alloc_tile_bufs(S, cx)
    ntile = len(S._stack)
    alloc_ffn_bufs(S, cx)
    m = M[(0, 0)]
    emit_ffn_phase(S, cx, xall, b_xall, x1all, b_x1all, w_in[0], w_out[0], m[0], m[1], m[2], 0.5, m[3], NT_ALL, "f00")
    S.barrier_free(len(S._stack) - ntile)
    m = M[(0, 1)]
    b_kv = Buf("kvq")
    emit_kvq_phase(S, cx, x1all, b_x1all, wqkv, m[0], m[1], m[3], QT, KT, kbar, Vd, x1o, b_x1o, b_kv)
    S.barrier_free(len(S._stack) - nbase)
    b_OT = mk("OT", 9)
    emit_attn2(S, cx, {"KT": KT, "ind": ind_d, "Vd": Vd, "QT": QT, "kbar": kbar, "cst": cst_d, "OT": OT, "b_OT": b_OT, "b_kv": b_kv})
    S.barrier_free(len(S._stack) - nbase)
    alloc_tile_bufs(S, cx)
    emit_proj_res_phase(S, cx, x1o, b_x1o, xa, b_xa, OT, b_OT, w_o, m[2], None, m[3], "wo", True, ntiles=9)
    S.barrier_free(len(S._stack) - ntile)
    alloc_ffn_bufs(S, cx)
    m = M[(0, 2)]
    emit_ffn_phase(S, cx, xa, b_xa, xb, b_xb, w_in[1], w_out[1], m[0], m[1], m[2], 0.5, m[3], 9, "f01")
    m = M[(1, 0)]
    emit_ffn_phase(S, cx, xb, b_xb, x4, b_x4t, w_in[2], w_out[2], m[0], m[1], m[2], 0.5, m[3], 9, "f10")
    S.barrier_free(len(S._stack) - ntile)
    m = M[(1, 1)]
    emit_glu_phase(S, cx, x4, b_x4t, w_pw1, m[0], m[1], m[3], uT, ntiles=9, hmask=hmask[:], b_hmask=b_hm, b_uout=b_u)
    S.barrier_free(len(S._stack) - ntile)
    b_x4c = [Buf("x4c_%d" % t) for t in range(16)]
    emit_conv_phase(S, cx, None, x4, b_x4c, x5, b_x5, w_pw2, ident_d, m[2], m[3], u_scr=uT, b_u=b_u)
    S.barrier_free(len(S._stack) - ntile)
    alloc_ffn_bufs(S, cx)
    m = M[(1, 2)]
    emit_ffn_phase(S, cx, x5, b_x5, xo, b_xo, w_in[3], w_out[3], m[0], m[1], m[2], 0.5, m[3], 8, "f11")
    S.emit()
    return nc


def kernel(**inp):
    import ml_dtypes
    bf = ml_dtypes.bfloat16
    inp = {k: np.asarray(v) for k, v in inp.items()}
    cores = list(range(8))
    ind = np.zeros((64, SEQ), bf)
    for m in range(64):
        ind[m, m * 256:(m + 1) * 256] = 1
    ident = np.eye(128, dtype=np.float32)
    maps = []
    toks = []
    for c in cores:
        b, j = c // 4, c % 4
        real, dup = pos_order(j)
        xb_ = inp["x"][b]
        xall = np.zeros((NPOS * 256, D), np.float32)
        for pi, g in enumerate(real + dup):
            if g is not None:
                xall[pi * 256:(pi + 1) * 256] = xb_[g * 256:(g + 1) * 256]
        cst, hmask = make_cst2(j)
        toks.append(np.concatenate([np.arange(g * 256, (g + 1) * 256) for g in snake_blocks(j)]))
        mp = {"sp": pack_small(inp, b), "ada_w": inp["ada_w"], "xall": np.ascontiguousarray(xall.T),
              "wqkv": inp["attn_w_qkv"][0], "w_o": inp["attn_w_o"][0], "w_pw1": inp["conv_w_pw1"][0], "w_pw2": inp["conv_w_pw2"][0],
              "ind": ind, "cst": cst, "hmask": hmask, "ident": ident}
        for k, (i, w) in enumerate(((0, 0), (0, 1), (1, 0), (1, 1))):
            mp["w_in%d" % k] = inp["ffn_w_in"][i, w]
            mp["w_out%d" % k] = inp["ffn_w_out"][i, w]
        maps.append(mp)
    nc = _get("fused", build_fused)
    res = run_bass_kernel_spmd(nc, maps, core_ids=cores).results
    out = np.zeros((BATCH, SEQ, D), np.float32)
    for c in cores:
        out[c // 4][toks[c]] = np.asarray(res[c]["xo"]).T
    return out
```

```python
import math
import numpy as np
import concourse.bass as bass
import concourse.mybir as mybir
from concourse.bass_utils import run_bass_kernel_spmd

F32 = mybir.dt.float32
BF16 = mybir.dt.bfloat16
AF = mybir.ActivationFunctionType
ALU = mybir.AluOpType
AX = mybir.AxisListType

D = 1024
KC = 8
DFF = 2816
FC = 22
NH = 16
DH = 64
BLK = 256
NB = 64
SEQ = 16384
BATCH = 2
TOK = 4096
NLB = 16
EPS = 1e-6
NEG = -30000.0
CW = 31


class Buf:
    __slots__ = ("name", "last_w", "readers")

    def __init__(self, name):
        self.name = name
        self.last_w = None
        self.readers = []


class Sched:
    DMA_ENGS = ("sp", "pool_dma")

    def __init__(self, nc, n_dma_sems=12):
        self.nc = nc
        self.ops = []
        self.engs = {"pe": nc.tensor, "act": nc.scalar, "dve": nc.vector,
                     "pool": nc.gpsimd, "sp": nc.sync, "pool_dma": nc.gpsimd}
        self.stream = {"pe": "pe", "act": "act", "dve": "dve", "pool": "pool",
                       "sp": "sp", "pool_dma": "pool"}
        self.n_dma_sems = n_dma_sems
        self._stack = []
        self.bar_list = []
        cm = nc.sbuf_tensor("bar_t", [128, 8], F32)
        self.bar_t = cm.__enter__()

    def sb(self, name, shape, dt):
        self._uid = getattr(self, "_uid", 0) + 1
        cm = self.nc.sbuf_tensor("s%d_%s" % (self._uid, name), shape, dt)
        t = cm.__enter__()
        self._stack.append(cm)
        return t

    def ps(self, name, shape, dt=F32):
        cm = self.nc.psum_tensor("p_" + name, shape, dt)
        t = cm.__enter__()
        self._stack.append(cm)
        return t

    def barrier_free(self, n_free):
        nc = self.nc
        bb = Buf("barrier%d" % len(self.ops))
        prev = list(range(len(self.ops)))
        i0 = self.op("pool", lambda: nc.gpsimd.memset(self.bar_t[:, 0:1], 0.0), writes=[bb])
        self.ops[i0]["deps"] = set(prev)
        for e, fn in (("dve", lambda: nc.vector.memset(self.bar_t[:, 1:2], 0.0)),
                      ("act", lambda: nc.scalar.activation(out=self.bar_t[:, 2:3], in_=self.bar_t[:, 0:1], func=AF.Copy)),
                      ("pe", None),
                      ("sp", lambda: nc.sync.dma_start(out=self.bar_t[:, 4:5], in_=self.bar_t[:, 0:1]))):
            if fn is None:
                continue
            self.op(e, fn, reads=[bb])
        self.bar_after = i0
        self.bar_list.append(i0)
        for _ in range(n_free):
            cm = self._stack.pop()
            cm.__exit__(None, None, None)

    def op(self, eng, fn, reads=(), writes=()):
        deps = set()
        idx = len(self.ops)
        for b in reads:
            if b.last_w is not None:
                deps.add(b.last_w)
        for b in writes:
            if b.last_w is not None:
                deps.add(b.last_w)
            for r in b.readers:
                deps.add(r)
        for b in reads:
            b.readers.append(idx)
        for b in writes:
            b.last_w = idx
            b.readers = []
        deps.discard(idx)
        if self.bar_list:
            deps.add(self.bar_list[-1])
        self.ops.append({"eng": eng, "fn": fn, "deps": deps, "dma": eng in self.DMA_ENGS})
        return idx

    def emit(self):
        nc = self.nc
        ops = self.ops
        n = len(ops)
        need = [False] * n
        for i, o in enumerate(ops):
            if o["dma"]:
                need[i] = True
            for d in o["deps"]:
                if ops[d]["dma"]:
                    continue
                sd, si = self.stream[ops[d]["eng"]], self.stream[o["eng"]]
                if sd != si or sd != "pe":
                    need[d] = True
        sem_cm = []

        def mksem(name):
            cm = nc.semaphore(name)
            s = cm.__enter__()
            sem_cm.append(cm)
            return s

        eng_sem = {k: mksem("s_" + k) for k in ("pe", "act", "dve", "pool")}
        eng_cnt = {k: 0 for k in eng_sem}
        dma_sems = {q: [mksem("d_%s_%d" % (q, i)) for i in range(self.n_dma_sems)] for q in self.DMA_ENGS}
        dma_cnt = {q: [0] * self.n_dma_sems for q in self.DMA_ENGS}
        dma_last = {q: [None] * self.n_dma_sems for q in self.DMA_ENGS}
        dma_rr = {q: 0 for q in self.DMA_ENGS}
        ev = [None] * n
        waited = {s: {} for s in ("pe", "act", "dve", "pool", "sp")}
        for i, o in enumerate(ops):
            eng = o["eng"]
            st = self.stream[eng]
            e = self.engs[eng]
            waits = {}
            for d in o["deps"]:
                if ev[d] is None:
                    continue
                sem, val, key = ev[d]
                if key not in waits or waits[key][1] < val:
                    waits[key] = (sem, val)
            my = None
            if o["dma"]:
                k = dma_rr[eng]
                dma_rr[eng] = (k + 1) % self.n_dma_sems
                prev = dma_last[eng][k]
                if prev is not None:
                    sem, val, key = ev[prev]
                    if key not in waits or waits[key][1] < val:
                        waits[key] = (sem, val)
                dma_cnt[eng][k] += 16
                my = (dma_sems[eng][k], dma_cnt[eng][k], (eng, k))
                dma_last[eng][k] = i
            elif need[i]:
                eng_cnt[st] += 1
                my = (eng_sem[st], eng_cnt[st], st)
            for key, (sem, val) in waits.items():
                if waited[st].get(key, 0) >= val:
                    continue
                e.wait_ge(sem, val)
                waited[st][key] = val
            inst = o["fn"]()
            if my is not None:
                inst.then_inc(my[0], 16 if o["dma"] else 1)
                ev[i] = my
            o["fn"] = None
        sp = nc.sync
        for q in self.DMA_ENGS:
            for k in range(self.n_dma_sems):
                if dma_cnt[q][k] > 0:
                    sp.wait_ge(dma_sems[q][k], dma_cnt[q][k])
        for k in eng_sem:
            if eng_cnt[k] > 0:
                sp.wait_ge(eng_sem[k], eng_cnt[k])
        self.max_counts = dict(eng_cnt)


def snake_blocks(j):
    out = []
    for i in range(8):
        out.append(8 * i + j)
        out.append(8 * i + 7 - j)
    return out


class Ctx:
    pass


def setup_common(S, cx):
    nc = S.nc
    cx.ones_bf = S.sb("ones_bf", [128, 128], BF16)
    cx.b_ones = Buf("ones")
    S.op("dve", lambda: nc.vector.memset(cx.ones_bf[:], 1.0), writes=[cx.b_ones])
    cx.psall = S.ps("psall", [128, 8 * 512], F32)
    cx.psum = [cx.psall[:, i * 512:(i + 1) * 512] for i in range(8)]
    cx.b_psum = [Buf("psb%d" % i) for i in range(8)]


def emit_mod_vectors(S, cx, modT, normg, layer_slot, j):
    nc = S.nc
    A = S.sb("modA_%s" % layer_slot, [128, 8], F32)
    b = Buf("modA_%s" % layer_slot)
    base = j * 24
    S.op("dve", lambda: nc.vector.scalar_tensor_tensor(
        out=A[:], in0=modT[:, base + 8:base + 16], scalar=1.0, in1=normg,
        op0=ALU.add, op1=ALU.mult), reads=[cx.b_mod], writes=[b])
    return A, modT[:, base:base + 8], modT[:, base + 16:base + 24], b


def emit_ffn_phase(S, cx, x_in, b_xin, x_out, b_xout, w_in_d, w_out_d, A, Bv, G, gscale, b_mod, ntiles, tag):
    nc = S.nc
    win = cx.win
    wout = cx.wout
    HF = FC // 2
    for kc in range(KC):
        S.op("pool_dma", (lambda kc=kc: nc.gpsimd.dma_start(out=win[:, kc, :], in_=w_in_d[kc * 128:(kc + 1) * 128, :])),
             writes=[cx.b_win[kc]])
    for f in range(FC):
        S.op("pool_dma", (lambda f=f: nc.gpsimd.dma_start(out=wout[:, f, :], in_=w_out_d[f * 128:(f + 1) * 128, :])),
             writes=[cx.b_wout[f]])
    Gs = S.sb("Gs_" + tag, [128, 8], F32)
    b_Gs = Buf("Gs_" + tag)
    S.op("dve", lambda: nc.vector.tensor_scalar(out=Gs[:], in0=G, scalar1=float(gscale), scalar2=None, op0=ALU.mult),
         reads=[b_mod], writes=[b_Gs])
    xv_in = x_in.rearrange("(c p) t -> p c t", p=128)
    xv_out = x_out.rearrange("(c p) t -> p c t", p=128)
    def _ld(t):
        S.op("sp", (lambda: nc.sync.dma_start(out=cx.xt[t % 2][:], in_=xv_in[:, :, t * 512:(t + 1) * 512])),
             reads=[b_xin[t]], writes=[cx.b_xt[t % 2]])
    _ld(0)
    for t in range(ntiles):
        xt = cx.xt[t % 2]
        bx = cx.b_xt[t % 2]
        emit_norm_mod(S, cx, xt, bx, A, Bv, b_mod)
        if t + 1 < ntiles:
            _ld(t + 1)
        for hf in range(2):
            for fl in range(HF):
                f = hf * HF + fl
                pa = 1 + (f % 2) * 2
                pb = pa + 1

                def mm_a(f=f, pa=pa):
                    for kc in range(KC):
                        m = nc.tensor.matmul(cx.psum[pa][:], lhsT=win[:, kc, f * 128:(f + 1) * 128], rhs=cx.h[:, kc, :],
                                             start=(kc == 0), stop=(kc == KC - 1))
                    return m

                def mm_b(f=f, pb=pb):
                    for kc in range(KC):
                        m = nc.tensor.matmul(cx.psum[pb][:], lhsT=win[:, kc, DFF + f * 128:DFF + (f + 1) * 128],
                                             rhs=cx.h[:, kc, :], start=(kc == 0), stop=(kc == KC - 1))
                    return m
                S.op("pe", mm_a, reads=cx.b_win + [cx.b_h], writes=[cx.b_psum[pa]])
                S.op("pe", mm_b, reads=cx.b_win + [cx.b_h], writes=[cx.b_psum[pb]])
                sa = cx.sa[f % 2]
                S.op("act", (lambda sa=sa, pa=pa: nc.scalar.activation(out=sa[:], in_=cx.psum[pa][:], func=AF.Silu)),
                     reads=[cx.b_psum[pa]], writes=[cx.b_sa[f % 2]])
                S.op("dve", (lambda sa=sa, pb=pb, fl=fl: nc.vector.tensor_tensor(out=cx.u[:, fl, :], in0=sa[:], in1=cx.psum[pb][:], op=ALU.mult)),
                     reads=[cx.b_sa[f % 2], cx.b_psum[pb]], writes=[cx.b_u[fl]])
            for oc in range(KC):
                py = 5 + (oc % 2)

                def mm_y(oc=oc, py=py, hf=hf):
                    for fl in range(HF):
                        m = nc.tensor.matmul(cx.psum[py][:], lhsT=wout[:, hf * HF + fl, oc * 128:(oc + 1) * 128], rhs=cx.u[:, fl, :],
                                             start=(fl == 0), stop=(fl == HF - 1))
                    return m
                S.op("pe", mm_y, reads=cx.b_wout + cx.b_u, writes=[cx.b_psum[py]])
                S.op("dve", (lambda oc=oc, py=py, xt=xt: nc.vector.scalar_tensor_tensor(
                    out=xt[:, oc, :], in0=cx.psum[py][:], scalar=Gs[:, oc:oc + 1], in1=xt[:, oc, :],
                    op0=ALU.mult, op1=ALU.add)), reads=[cx.b_psum[py], b_Gs], writes=[bx])
        S.op("sp", (lambda xt=xt, t=t: nc.sync.dma_start(out=xv_out[:, :, t * 512:(t + 1) * 512], in_=xt[:])),
             reads=[bx], writes=[b_xout[t]])


def emit_norm_mod(S, cx, xt, bx, A, Bv, b_mod, n=512):
    nc = S.nc
    for c in range(KC):
        S.op("act", (lambda c=c: nc.scalar.activation(out=cx.sq[c % 2][:, :n], in_=xt[:, c, :n], func=AF.Square)),
             reads=[bx], writes=[cx.b_sq[c % 2]])
        S.op("pe", (lambda c=c: nc.tensor.matmul(cx.psum[0][:, :n], lhsT=cx.ones_bf[:], rhs=cx.sq[c % 2][:, :n],
                                                 start=(c == 0), stop=(c == KC - 1))),
             reads=[cx.b_sq[c % 2], cx.b_ones], writes=[cx.b_psum[0]])
    S.op("dve", lambda: nc.vector.tensor_scalar(out=cx.rstd[:, :n], in0=cx.psum[0][:, :n], scalar1=1.0 / D, scalar2=EPS,
                                                op0=ALU.mult, op1=ALU.add), reads=[cx.b_psum[0]], writes=[cx.b_rstd])
    S.op("act", lambda: nc.scalar.activation(out=cx.rstd[:, :n], in_=cx.rstd[:, :n], func=AF.Sqrt), reads=[cx.b_rstd], writes=[cx.b_rstd])
    S.op("dve", lambda: nc.vector.reciprocal(out=cx.rstd[:, :n], in_=cx.rstd[:, :n]), reads=[cx.b_rstd], writes=[cx.b_rstd])
    for c in range(KC):
        tmp = cx.tmp[c % 2]
        S.op("dve", (lambda c=c, tmp=tmp: nc.vector.tensor_tensor(out=tmp[:, :n], in0=xt[:, c, :n], in1=cx.rstd[:, :n], op=ALU.mult)),
             reads=[bx, cx.b_rstd], writes=[cx.b_tmp[c % 2]])
        S.op("act", (lambda c=c, tmp=tmp: nc.scalar.activation(out=cx.h[:, c, :n], in_=tmp[:, :n], func=AF.Identity,
                                                               bias=Bv[:, c:c + 1], scale=A[:, c:c + 1])),
             reads=[cx.b_tmp[c % 2], b_mod], writes=[cx.b_h])


def alloc_tile_bufs(S, cx):
    cx.xt = [S.sb("xt%d" % i, [128, KC, 512], F32) for i in range(2)]
    cx.b_xt = [Buf("xt%d" % i) for i in range(2)]
    cx.sq = [S.sb("sq%d" % i, [128, 512], BF16) for i in range(2)]
    cx.b_sq = [Buf("sq%d" % i) for i in range(2)]
    cx.h = S.sb("h", [128, KC, 512], BF16)
    cx.b_h = Buf("h")
    cx.rstd = S.sb("rstd", [128, 512], F32)
    cx.b_rstd = Buf("rstd")
    cx.tmp = [S.sb("tmp%d" % i, [128, 512], F32) for i in range(2)]
    cx.b_tmp = [Buf("tmp%d" % i) for i in range(2)]
    cx.sa = [S.sb("sa%d" % i, [128, 512], F32) for i in range(2)]
    cx.b_sa = [Buf("sa%d" % i) for i in range(2)]


def alloc_ffn_bufs(S, cx):
    cx.win = S.sb("win", [128, KC, 2 * DFF], BF16)
    cx.wout = S.sb("wout", [128, FC, D], BF16)
    cx.b_win = [Buf("win%d" % i) for i in range(KC)]
    cx.b_wout = [Buf("wout%d" % i) for i in range(FC)]
    cx.u = S.sb("u", [128, FC // 2, 512], BF16)
    cx.b_u = [Buf("u%d" % i) for i in range(FC // 2)]


def _fm(v, n=None):
    v = np.asarray(v, np.float32).reshape(-1, 128)
    return np.ascontiguousarray(v.T)


SP_OFF = {}


def _sp_layout():
    off = 0
    for name, w in (("c", 8), ("ada_b0", 72), ("ada_b1", 72),
                    ("ng00", 8), ("ng01", 8), ("ng02", 8), ("ng10", 8), ("ng11", 8), ("ng12", 8),
                    ("gq", 1), ("gk", 1), ("b_pw1", 16), ("b_dw", 8), ("ln_g", 8), ("ln_b", 8),
                    ("b_pw2", 8), ("w_dw", 8 * CW)):
        SP_OFF[name] = (off, w)
        off += w
    return off


SP_N = _sp_layout()


def pack_small(inp, b):
    sp = np.zeros((128, SP_N), np.float32)

    def put(name, arr):
        o, w = SP_OFF[name]
        assert arr.shape == (128, w), (name, arr.shape, w)
        sp[:, o:o + w] = arr
    put("c", _fm(inp["c"][b]))
    put("ada_b0", _fm(inp["ada_b"][0]))
    put("ada_b1", _fm(inp["ada_b"][1]))
    for i in range(2):
        for j in range(3):
            put("ng%d%d" % (i, j), _fm(inp["norm_g"][i, j]))
    put("gq", np.tile(np.asarray(inp["attn_g_q"][0], np.float32), 2).reshape(128, 1))
    put("gk", np.tile(np.asarray(inp["attn_g_k"][0], np.float32), 2).reshape(128, 1))
    put("b_pw1", _fm(inp["conv_b_pw1"][0]))
    put("b_dw", _fm(inp["conv_b_dw"][0]))
    put("ln_g", _fm(inp["conv_ln_g"][0]))
    put("ln_b", _fm(inp["conv_ln_b"][0]))
    put("b_pw2", _fm(inp["conv_b_pw2"][0]))
    wd = np.asarray(inp["conv_w_dw"][0], np.float32)
    put("w_dw", np.ascontiguousarray(wd.reshape(CW, 8, 128).transpose(2, 1, 0)).reshape(128, 8 * CW))
    return sp


def spc(cx, name):
    o, w = SP_OFF[name]
    return cx.spt[:, o:o + w]


def emit_load_small(S, cx, sp_d):
    nc = S.nc
    cx.spt = S.sb("spt", [128, SP_N], F32)
    cx.b_sp = Buf("spt")
    S.op("sp", lambda: nc.sync.dma_start(out=cx.spt[:], in_=sp_d[:, :]), writes=[cx.b_sp])


def emit_adaln(S, cx, ada_w_d, modT, b_modT):
    nc = S.nc
    NCOL = 1152
    stg = [S.sb("adastg%d" % i, [128, KC, NCOL], F32) for i in range(2)]
    b_stg = [Buf("adastg%d" % i) for i in range(2)]
    cact = S.sb("cact", [128, 8], F32)
    b_cact = Buf("cact")
    S.op("act", lambda: nc.scalar.activation(out=cact[:], in_=spc(cx, "c"), func=AF.Silu), reads=[cx.b_sp], writes=[b_cact])
    k = 0
    for i in range(2):
        wv = ada_w_d[i].rearrange("(kc p) n -> p kc n", p=128)
        for pc in range(9216 // NCOL):
            st = stg[k % 2]
            bs = b_stg[k % 2]
            k += 1
            S.op("sp", (lambda st=st, wv=wv, pc=pc: nc.sync.dma_start(out=st[:], in_=wv[:, :, pc * NCOL:(pc + 1) * NCOL])), writes=[bs])

            def mm(st=st, pc=pc, i=i):
                for m in range(NCOL // 128):
                    col = pc * (NCOL // 128) + m
                    for kc in range(KC):
                        r = nc.tensor.matmul(cx.psum[7][:, col:col + 1], lhsT=st[:, kc, m * 128:(m + 1) * 128], rhs=cact[:, kc:kc + 1],
                                             start=(kc == 0), stop=(kc == KC - 1))
                return r
            S.op("pe", mm, reads=[bs, b_cact], writes=[cx.b_psum[7]])
        S.op("dve", (lambda i=i: nc.vector.tensor_tensor(out=modT[i][:], in0=cx.psum[7][:, 0:72], in1=spc(cx, "ada_b%d" % i), op=ALU.add)),
             reads=[cx.b_psum[7], cx.b_sp], writes=[b_modT[i]])
    return stg


def joint_mod(S, cx, i, j, modT, b_modT):
    nc = S.nc
    A, Bv, G, bA = emit_mod_vectors_i(S, cx, modT[i], b_modT[i], spc(cx, "ng%d%d" % (i, j)), "l%ds%d" % (i, j), j)
    return A, Bv, G, bA


def emit_mod_vectors_i(S, cx, modT, b_modT, normg, tag, j):
    nc = S.nc
    AB = S.sb("modAB_" + tag, [128, 24], F32)
    b = Buf("modAB_" + tag)
    base = j * 24
    S.op("dve", lambda: nc.vector.tensor_copy(out=AB[:], in_=modT[:, base:base + 24]), reads=[b_modT, cx.b_sp], writes=[b])
    S.op("dve", lambda: nc.vector.scalar_tensor_tensor(
        out=AB[:, 8:16], in0=modT[:, base + 8:base + 16], scalar=1.0, in1=normg,
        op0=ALU.add, op1=ALU.mult), reads=[b_modT, cx.b_sp], writes=[b])
    return AB[:, 8:16], AB[:, 0:8], AB[:, 16:24], b


def load_x_tile(S, cx, xv, b_x, t, n=512):
    nc = S.nc
    xt = cx.xt[t % 2]
    bx = cx.b_xt[t % 2]
    S.op("sp", (lambda: nc.sync.dma_start(out=xt[:, :, :n], in_=xv[:, :, t * n:(t + 1) * n])), reads=[b_x[t]], writes=[bx])
    return xt, bx


def emit_qkv_phase(S, cx, x_d, b_x, wqkv_d, A, Bv, b_mod, QT_d, KT_d, kbar_d, V_d):
    nc = S.nc
    wq = S.sb("wqkv_sb", [128, KC, 3072], BF16)
    b_wq = [Buf("wqkv%d" % k) for k in range(KC)]
    for kc in range(KC):
        S.op("pool_dma", (lambda kc=kc: nc.gpsimd.dma_start(out=wq[:, kc, :], in_=wqkv_d[kc * 128:(kc + 1) * 128, :])), writes=[b_wq[kc]])
    bo = S.sb("blkones", [128, 128], BF16)
    b_bo = Buf("blkones")
    S.op("dve", lambda: nc.vector.memset(bo[:], 0.0), writes=[b_bo])
    S.op("dve", lambda: nc.vector.memset(bo[0:64, 0:64], 1.0), writes=[b_bo])
    S.op("dve", lambda: nc.vector.memset(bo[64:128, 64:128], 1.0), writes=[b_bo])
    g8 = S.sb("g8", [128, 2], F32)
    b_g8 = Buf("g8")
    S.op("dve", lambda: nc.vector.tensor_scalar(out=g8[:, 0:1], in0=spc(cx, "gq"), scalar1=0.125, scalar2=None, op0=ALU.mult),
         reads=[cx.b_sp], writes=[b_g8])
    S.op("dve", lambda: nc.vector.tensor_copy(out=g8[:, 1:2], in_=spc(cx, "gk")), reads=[cx.b_sp], writes=[b_g8])
    qo = [S.sb("qo%d" % i, [128, 512], BF16) for i in range(2)]
    b_qo = [Buf("qo%d" % i) for i in range(2)]
    kb = [S.sb("kb%d" % i, [128, 2], F32) for i in range(2)]
    b_kb = [Buf("kb%d" % i) for i in range(2)]
    vt = [S.sb("vt%d" % i, [128, 1024], BF16) for i in range(2)]
    b_vt = [Buf("vt%d" % i) for i in range(2)]
    xv = x_d.rearrange("(c p) t -> p c t", p=128)
    QTv = QT_d.rearrange("h d t -> (h d) t")
    KTv = KT_d.rearrange("h d t -> (h d) t")
    kbv = kbar_d.rearrange("h d l -> (h d) l")
    cnt = 0
    for t in range(8):
        xt, bx = load_x_tile(S, cx, xv, b_x, t)
        emit_norm_mod(S, cx, xt, bx, A, Bv, b_mod)
        for which in range(2):
            goff = which * 1024
            outv = QTv if which == 0 else KTv
            for hp in range(8):
                k = cnt % 2
                cnt += 1
                pq = cx.psum[1 + k]
                pss = cx.psum[3 + k]

                def mm(hp=hp, pq=pq, goff=goff):
                    for kc in range(KC):
                        m = nc.tensor.matmul(pq[:], lhsT=wq[:, kc, goff + hp * 128:goff + (hp + 1) * 128], rhs=cx.h[:, kc, :],
                                             start=(kc == 0), stop=(kc == KC - 1))
                    return m
                S.op("pe", mm, reads=b_wq + [cx.b_h], writes=[cx.b_psum[1 + k]])
                S.op("act", (lambda k=k, pq=pq: nc.scalar.activation(out=cx.sq[k][:], in_=pq[:], func=AF.Square)),
                     reads=[cx.b_psum[1 + k]], writes=[cx.b_sq[k]])
                S.op("pe", (lambda k=k, pss=pss: nc.tensor.matmul(pss[:], lhsT=bo[:], rhs=cx.sq[k][:], start=True, stop=True)),
                     reads=[cx.b_sq[k], b_bo], writes=[cx.b_psum[3 + k]])
                r1 = cx.tmp[k]
                S.op("dve", (lambda r1=r1, pss=pss: nc.vector.tensor_scalar(out=r1[:], in0=pss[:], scalar1=1.0 / DH, scalar2=EPS,
                                                                            op0=ALU.mult, op1=ALU.add)),
                     reads=[cx.b_psum[3 + k]], writes=[cx.b_tmp[k]])
                S.op("act", (lambda r1=r1: nc.scalar.activation(out=r1[:], in_=r1[:], func=AF.Sqrt)), reads=[cx.b_tmp[k]], writes=[cx.b_tmp[k]])
                S.op("dve", (lambda r1=r1: nc.vector.reciprocal(out=r1[:], in_=r1[:])), reads=[cx.b_tmp[k]], writes=[cx.b_tmp[k]])
                S.op("dve", (lambda r1=r1, pq=pq: nc.vector.tensor_tensor(out=r1[:], in0=pq[:], in1=r1[:], op=ALU.mult)),
                     reads=[cx.b_tmp[k], cx.b_psum[1 + k]], writes=[cx.b_tmp[k]])
                sa = cx.sa[k]
                S.op("act", (lambda r1=r1, sa=sa, which=which: nc.scalar.activation(out=sa[:], in_=r1[:], func=AF.Copy,
                                                                                  scale=g8[:, which:which + 1])),
                     reads=[cx.b_tmp[k], b_g8], writes=[cx.b_sa[k]])
                S.op("pool", (lambda k=k, sa=sa: nc.gpsimd.tensor_copy(out=qo[k][:], in_=sa[:])), reads=[cx.b_sa[k]], writes=[b_qo[k]])
                S.op("sp", (lambda k=k, hp=hp, t=t, outv=outv: nc.sync.dma_start(out=outv[hp * 128:(hp + 1) * 128, t * 512:(t + 1) * 512], in_=qo[k][:])),
                     reads=[b_qo[k]])
                if which == 1:
                    S.op("dve", (lambda k=k, sa=sa: nc.vector.tensor_reduce(out=kb[k][:], in_=sa[:].rearrange("p (b t) -> p b t", b=2),
                                                                          axis=AX.X, op=ALU.add)),
                         reads=[cx.b_sa[k]], writes=[b_kb[k]])
                    S.op("dve", (lambda k=k: nc.vector.tensor_scalar(out=kb[k][:], in0=kb[k][:], scalar1=1.0 / BLK, scalar2=None, op0=ALU.mult)),
                         reads=[b_kb[k]], writes=[b_kb[k]])
                    S.op("sp", (lambda k=k, hp=hp, t=t: nc.sync.dma_start(out=kbv[hp * 128:(hp + 1) * 128, 2 * t:2 * t + 2], in_=kb[k][:])),
                         reads=[b_kb[k]])
        for tg in range(4):
            v = vt[tg % 2]
            for ns in range(2):
                pv = cx.psum[5 + ns]

                def mmv(tg=tg, ns=ns, pv=pv):
                    for kc in range(KC):
                        m = nc.tensor.matmul(pv[:], lhsT=cx.h[:, kc, tg * 128:(tg + 1) * 128], rhs=wq[:, kc, 2048 + ns * 512:2048 + (ns + 1) * 512],
                                             start=(kc == 0), stop=(kc == KC - 1))
                    return m
                S.op("pe", mmv, reads=b_wq + [cx.b_h], writes=[cx.b_psum[5 + ns]])
                if ns == 0:
                    S.op("act", (lambda v=v, pv=pv: nc.scalar.copy(out=v[:, 0:512], in_=pv[:])), reads=[cx.b_psum[5]], writes=[b_vt[tg % 2]])
                else:
                    S.op("dve", (lambda v=v, pv=pv: nc.vector.tensor_copy(out=v[:, 512:1024], in_=pv[:])), reads=[cx.b_psum[6]], writes=[b_vt[tg % 2]])
            S.op("sp", (lambda v=v, tg=tg, t=t: nc.sync.dma_start(out=V_d[t * 512 + tg * 128:t * 512 + (tg + 1) * 128, :], in_=v[:])),
                 reads=[b_vt[tg % 2]])


def build_L1():
    nc = bass.Bass("TRN2", target_bir_lowering=False)
    sp_d = nc.dram_tensor("sp", [128, SP_N], F32, kind="ExternalInput").ap()
    ada_w = nc.dram_tensor("ada_w", [2, 1024, 9216], F32, kind="ExternalInput").ap()
    xin = nc.dram_tensor("xin", [1024, TOK], F32, kind="ExternalInput").ap()
    w_in = nc.dram_tensor("w_in", [1024, 2 * DFF], F32, kind="ExternalInput").ap()
    w_out = nc.dram_tensor("w_out", [DFF, 1024], F32, kind="ExternalInput").ap()
    wqkv = nc.dram_tensor("wqkv", [1024, 3072], F32, kind="ExternalInput").ap()
    x1 = nc.dram_tensor("x1", [1024, TOK], F32, kind="ExternalOutput").ap()
    modo = nc.dram_tensor("modo", [128, 144], F32, kind="ExternalOutput").ap()
    QT = nc.dram_tensor("QT", [NH, DH, TOK], BF16, kind="ExternalOutput").ap()
    KT = nc.dram_tensor("KT", [NH, DH, TOK], BF16, kind="ExternalOutput").ap()
    kbar = nc.dram_tensor("kbar", [NH, DH, NLB], F32, kind="ExternalOutput").ap()
    V = nc.dram_tensor("V", [TOK, 1024], BF16, kind="ExternalOutput").ap()
    S = Sched(nc)
    cx = Ctx()
    setup_common(S, cx)
    emit_load_small(S, cx, sp_d)
    modT = [S.sb("modT%d" % i, [128, 72], F32) for i in range(2)]
    b_modT = [Buf("modT%d" % i) for i in range(2)]
    A0, B0, G0, bm0 = None, None, None, None
    AB0 = joint_mod
    emit_adaln_scoped(S, cx, ada_w, modT, b_modT)
    for i in range(2):
        S.op("sp", (lambda i=i: nc.sync.dma_start(out=modo[:, i * 72:(i + 1) * 72], in_=modT[i][:])), reads=[b_modT[i]])
    A, Bv, G, bA = emit_mod_vectors_i(S, cx, modT[0], b_modT[0], spc(cx, "ng00"), "l0s0", 0)
    A1, Bv1, G1, bA1 = emit_mod_vectors_i(S, cx, modT[0], b_modT[0], spc(cx, "ng01"), "l0s1", 1)
    alloc_tile_bufs(S, cx)
    alloc_ffn_bufs(S, cx)
    b_xin = [Buf("xin%d" % t) for t in range(8)]
    b_x1 = [Buf("x1_%d" % t) for t in range(8)]
    emit_ffn_phase(S, cx, xin, b_xin, x1, b_x1, w_in, w_out, A, Bv, G, 0.5, bA, 8, "f00")
    S.barrier_free(3)
    emit_qkv_phase(S, cx, x1, b_x1, wqkv, A1, Bv1, bA1, QT, KT, kbar, V)
    S.emit()
    return nc


def emit_adaln_scoped(S, cx, ada_w, modT, b_modT):
    n0 = len(S._stack)
    emit_adaln(S, cx, ada_w, modT, b_modT)
    S.barrier_free(len(S._stack) - n0)


SLOPES = [2.0 ** (-8.0 * (h + 1) / NH) for h in range(NH)]


def emit_attn_phase(S, cx, d):
    nc = S.nc
    KTa = [S.sb("KTa%d" % i, [128, SEQ], BF16) for i in range(2)]
    Va = [S.sb("Va%d" % i, [128, 128, 65], BF16) for i in range(2)]
    Qa = [S.sb("Qa%d" % i, [128, TOK], BF16) for i in range(2)]
    Ko = [S.sb("Ko%d" % i, [128, TOK], BF16) for i in range(2)]
    Vo = [S.sb("Vo%d" % i, [128, 32, 65], BF16) for i in range(2)]
    bT = [S.sb("bT%d" % i, [128, 1024], F32) for i in range(2)]
    dgb = [S.sb("dgb%d" % i, [128, 512], F32) for i in range(2)]
    cq = [S.sb("cq%d" % i, [128, 256], F32) for i in range(2)]
    b_KT = [Buf("KTa%d" % i) for i in range(2)]
    b_Va = [Buf("Va%d" % i) for i in range(2)]
    b_Qa = [Buf("Qa%d" % i) for i in range(2)]
    b_Qm = [[Buf("Qm%d_%d" % (i, l)) for l in range(NLB)] for i in range(2)]
    b_Ko = [Buf("Ko%d" % i) for i in range(2)]
    b_Vo = [Buf("Vo%d" % i) for i in range(2)]
    b_hd = [Buf("hd%d" % i) for i in range(2)]
    cst = S.sb("cst", [128, CST_N], F32)
    b_cst = Buf("cst")
    S.op("sp", lambda: nc.sync.dma_start(out=cst[:], in_=d["cst"][:, :]), writes=[b_cst])

    def C(name):
        o, w = CST_OFF[name]
        return cst[:, o:o + w]
    kb32 = S.sb("kb32", [64, NH * NB], F32)
    kbh = S.sb("kbh", [64, NH * NB], BF16)
    kbl = S.sb("kbl", [64, NH * NB], BF16)
    kbt = S.sb("kbt", [64, NH * NB], F32)
    b_kb = Buf("kbhl")
    S.op("sp", lambda: nc.sync.dma_start(out=kb32[:], in_=d["kbar"].rearrange("d h m -> d (h m)")), writes=[b_kb])
    S.op("dve", lambda: nc.vector.tensor_copy(out=kbh[:], in_=kb32[:]), reads=[b_kb], writes=[b_kb])
    S.op("dve", lambda: nc.vector.tensor_copy(out=kbt[:], in_=kbh[:]), reads=[b_kb], writes=[b_kb])
    S.op("dve", lambda: nc.vector.tensor_tensor(out=kbt[:], in0=kb32[:], in1=kbt[:], op=ALU.subtract), reads=[b_kb], writes=[b_kb])
    S.op("dve", lambda: nc.vector.tensor_copy(out=kbl[:], in_=kbt[:]), reads=[b_kb], writes=[b_kb])
    mbp = [S.sb("mbp%d" % i, [128, 128], F32) for i in range(2)]
    b_mbp = [Buf("mbp%d" % i) for i in range(2)]
    gmt = [S.sb("gmt%d" % i, [128, 64], F32) for i in range(2)]
    b_gmt = [Buf("gmt%d" % i) for i in range(2)]
    t8 = [S.sb("t8_%d" % i, [128, 8], F32) for i in range(2)]
    pt = [S.sb("pt%d" % i, [128, 512], BF16) for i in range(2)]
    b_pt = [Buf("pt%d" % i) for i in range(2)]
    pt2 = S.sb("pt2", [128, 512], BF16)
    b_pt2 = Buf("pt2")
    dtmp = S.sb("dtmp", [128, 512], F32)
    b_dtmp = Buf("dtmp")
    o1 = S.sb("o1", [128, 256], F32)
    o2 = S.sb("o2", [128, 256], F32)
    rd = S.sb("rd", [128, 256], F32)
    ob = [S.sb("ob%d" % i, [128, 256], BF16) for i in range(2)]
    b_o = Buf("o12")
    b_ob = [Buf("ob%d" % i) for i in range(2)]
    for i in range(2):
        S.op("sp", (lambda i=i: nc.sync.dma_start(out=KTa[i][64:128, :], in_=d["ind"][:, :])), writes=[b_KT[i]])
        S.op("dve", (lambda i=i: nc.vector.memset(Va[i][:, :, 64:65], 1.0)), writes=[b_Va[i]])
        S.op("dve", (lambda i=i: nc.vector.memset(Vo[i][:, :, 64:65], 1.0)), writes=[b_Vo[i]])
        S.op("dve", (lambda i=i: nc.vector.memset(mbp[i][:], 0.0)), writes=[b_mbp[i]])
    ps = cx.psum
    bp = cx.b_psum
    scnt = 0
    gcnt = 0
    for h in range(NH):
        cur = h % 2
        sl = SLOPES[h]
        es = math.exp(sl)
        S.op("sp", (lambda h=h, cur=cur: nc.sync.dma_start(out=KTa[cur][0:64, :], in_=d["KT"][h])), writes=[b_KT[cur]])
        for q8 in range(8):
            S.op("sp", (lambda h=h, cur=cur, q8=q8: nc.sync.dma_start(out=Va[cur][:, q8 * 16:(q8 + 1) * 16, 0:64], in_=d["Vd"][h, :, q8 * 16:(q8 + 1) * 16, :])),
                 writes=[b_Va[cur]])
        S.op("sp", (lambda h=h, cur=cur: nc.sync.dma_start(out=Qa[cur][0:64, :], in_=d["QT"][h])), writes=[b_Qa[cur]] + b_Qm[cur])
        S.op("sp", (lambda h=h, cur=cur: nc.sync.dma_start(out=Ko[cur][0:64, :], in_=d["KTo"][h])), writes=[b_Ko[cur]])
        for q2 in range(2):
            S.op("sp", (lambda h=h, cur=cur, q2=q2: nc.sync.dma_start(out=Vo[cur][:, q2 * 16:(q2 + 1) * 16, 0:64], in_=d["Vo"][h, :, q2 * 16:(q2 + 1) * 16, :])),
                 writes=[b_Vo[cur]])
        for (VV, bV) in ((Va[cur], b_Va[cur]), (Vo[cur], b_Vo[cur])):
            Vodd = VV[:].rearrange("p (n two) e -> p n two e", two=2)[:, :, 1, :]
            S.op("dve", (lambda Vodd=Vodd, es=es: nc.vector.tensor_scalar(out=Vodd[:, :, 0:64], in0=Vodd[:, :, 0:64], scalar1=es, scalar2=None, op0=ALU.mult)),
                 reads=[bV], writes=[bV])
            S.op("dve", (lambda Vodd=Vodd, es=es: nc.vector.memset(Vodd[:, :, 64:65], es)), reads=[bV], writes=[bV])
        S.op("dve", (lambda cur=cur, sl=sl: nc.vector.tensor_scalar(out=bT[cur][:], in0=C("base"), scalar1=sl, scalar2=None, op0=ALU.mult)),
             reads=[b_cst], writes=[b_hd[cur]])
        S.op("dve", (lambda cur=cur, sl=sl: nc.vector.scalar_tensor_tensor(out=dgb[cur][:], in0=C("Dm"), scalar=-sl, in1=C("causal"),
                                                                         op0=ALU.mult, op1=ALU.add)), reads=[b_cst], writes=[b_hd[cur]])
        S.op("act", (lambda cur=cur, sl=sl: nc.scalar.activation(out=cq[cur][:], in_=C("irow"), func=AF.Exp, scale=-sl)),
             reads=[b_cst], writes=[b_hd[cur]])
        for l in range(NLB):
            for hq in range(2):
                g = gcnt % 2
                gcnt += 1
                q0 = l * 256 + hq * 128

                def mmg(cur=cur, q0=q0, h=h):
                    nc.tensor.matmul(ps[6][:, 0:64], lhsT=Qa[cur][0:64, q0:q0 + 128], rhs=kbh[:, h * 64:(h + 1) * 64], start=True, stop=False)
                    return nc.tensor.matmul(ps[6][:, 0:64], lhsT=Qa[cur][0:64, q0:q0 + 128], rhs=kbl[:, h * 64:(h + 1) * 64], start=False, stop=True)
                S.op("pe", mmg, reads=[b_Qa[cur], b_kb], writes=[bp[6]])
                pb = C("past")[:, l * 64:(l + 1) * 64]
                S.op("dve", (lambda g=g, pb=pb: nc.vector.tensor_tensor(out=gmt[g][:], in0=ps[6][:, 0:64], in1=pb, op=ALU.add)),
                     reads=[bp[6], b_cst], writes=[b_gmt[g]])
                S.op("dve", (lambda g=g: nc.vector.max(out=t8[g][:], in_=gmt[g][:])), reads=[b_gmt[g]], writes=[b_gmt[g]])
                S.op("dve", (lambda g=g: nc.vector.tensor_scalar(out=gmt[g][:], in0=gmt[g][:], scalar1=t8[g][:, 2:3], scalar2=-NEG,
                                                                 op0=ALU.is_ge, op1=ALU.mult)), reads=[b_gmt[g]], writes=[b_gmt[g]])
                S.op("dve", (lambda g=g, pb=pb: nc.vector.scalar_tensor_tensor(out=mbp[g][:, 64:128], in0=gmt[g][:], scalar=NEG, in1=pb,
                                                                             op0=ALU.add, op1=ALU.add)),
                     reads=[b_gmt[g], b_cst], writes=[b_mbp[g]])
                S.op("pe", (lambda g=g: nc.tensor.transpose(ps[7][:, 0:128], mbp[g][:], C("ident"))), reads=[b_mbp[g], b_cst], writes=[bp[7]])
                S.op("act", (lambda cur=cur, q0=q0: nc.scalar.copy(out=Qa[cur][64:128, q0:q0 + 128], in_=ps[7][64:128, 0:128])),
                     reads=[bp[7]], writes=[b_Qm[cur][l]])
        for i in range(8):
            nk = 8 * i + 7
            for X in (2 * i, 2 * i + 1):
                po = 2 + (X % 2)
                for n in range(nk):
                    sb_ = scnt % 2
                    scnt += 1

                    def mms(cur=cur, X=X, n=n, sb_=sb_):
                        for c in range(2):
                            m = nc.tensor.matmul(ps[sb_][:, c * 256:(c + 1) * 256], lhsT=KTa[cur][:, n * 256 + c * 128:n * 256 + (c + 1) * 128],
                                                 rhs=Qa[cur][:, X * 256:(X + 1) * 256], start=True, stop=True)
                        return m
                    S.op("pe", mms, reads=[b_KT[cur], b_Qa[cur], b_Qm[cur][X]], writes=[bp[sb_]])
                    S.op("act", (lambda cur=cur, X=X, n=n, sb_=sb_: nc.scalar.activation(out=pt[sb_][:], in_=ps[sb_][:], func=AF.Exp,
                                                                                        bias=bT[cur][:, X * 64 + n:X * 64 + n + 1])),
                         reads=[bp[sb_], b_hd[cur]], writes=[b_pt[sb_]])

                    def mmpv(cur=cur, n=n, sb_=sb_, po=po, nk=nk):
                        for c in range(2):
                            m = nc.tensor.matmul(ps[po][0:65, 0:256], lhsT=Va[cur][:, 2 * n + c, :], rhs=pt[sb_][:, c * 256:(c + 1) * 256],
                                                 start=(n == 0 and c == 0), stop=(n == nk - 1 and c == 1))
                        return m
                    S.op("pe", mmpv, reads=[b_Va[cur], b_pt[sb_]], writes=[bp[po]])

                def mmd(cur=cur, X=X):
                    for c in range(2):
                        m = nc.tensor.matmul(ps[4][:, c * 256:(c + 1) * 256], lhsT=Ko[cur][0:64, X * 256 + c * 128:X * 256 + (c + 1) * 128],
                                             rhs=Qa[cur][0:64, X * 256:(X + 1) * 256], start=True, stop=True)
                    return m
                S.op("pe", mmd, reads=[b_Ko[cur], b_Qa[cur]], writes=[bp[4]])
                S.op("dve", (lambda cur=cur: nc.vector.tensor_tensor(out=dtmp[:], in0=ps[4][:], in1=dgb[cur][:], op=ALU.add)),
                     reads=[bp[4], b_hd[cur]], writes=[b_dtmp])
                S.op("act", (lambda: nc.scalar.activation(out=pt2[:], in_=dtmp[:], func=AF.Exp)), reads=[b_dtmp], writes=[b_pt2])

                def mmpo(cur=cur, X=X):
                    for c in range(2):
                        m = nc.tensor.matmul(ps[5][0:65, 0:256], lhsT=Vo[cur][:, 2 * X + c, :], rhs=pt2[:, c * 256:(c + 1) * 256],
                                             start=(c == 0), stop=(c == 1))
                    return m
                S.op("pe", mmpo, reads=[b_Vo[cur], b_pt2], writes=[bp[5]])
                S.op("dve", (lambda cur=cur, po=po: nc.vector.tensor_tensor(out=o1[0:65, :], in0=ps[po][0:65, 0:256], in1=cq[cur][0:65, :], op=ALU.mult)),
                     reads=[bp[po], b_hd[cur]], writes=[b_o])
                S.op("dve", (lambda: nc.vector.tensor_tensor(out=o2[0:65, :], in0=o1[0:65, :], in1=ps[5][0:65, 0:256], op=ALU.add)),
                     reads=[bp[5], b_o], writes=[b_o])
                S.op("pe", (lambda: nc.tensor.matmul(ps[6][0:64, 0:256], lhsT=C("sel65")[0:65, :], rhs=o2[0:65, :], start=True, stop=True)),
                     reads=[b_o, b_cst], writes=[bp[6]])
                S.op("dve", (lambda: nc.vector.reciprocal(out=rd[0:64, :], in_=ps[6][0:64, 0:256])), reads=[bp[6]], writes=[b_o])
                k = X % 2
                S.op("dve", (lambda k=k: nc.vector.tensor_tensor(out=ob[k][0:64, :], in0=o2[0:64, :], in1=rd[0:64, :], op=ALU.mult)),
                     reads=[b_o], writes=[b_ob[k]])
                S.op("sp", (lambda k=k, h=h, X=X: nc.sync.dma_start(out=d["OT"][h, :, X * 256:(X + 1) * 256], in_=ob[k][0:64, :])),
                     reads=[b_ob[k]], writes=[d["b_OT"][X // 2]])


CST_OFF = {}


def _cst_layout():
    off = 0
    for name, w in (("past", 1024), ("base", 1024), ("irow", 256), ("Dm", 512), ("causal", 512), ("ident", 128), ("sel65", 64)):
        CST_OFF[name] = (off, w)
        off += w
    return off


CST_N = _cst_layout()


def make_cst(j):
    blocks = snake_blocks(j)
    c = np.zeros((128, CST_N), np.float32)
    p = np.arange(128, dtype=np.float32)[:, None]
    past = np.zeros((16, 64), np.float32)
    base = np.zeros((128, 16, 64), np.float32)
    for l, own in enumerate(blocks):
        m = np.arange(64)
        past[l] = np.where(m < own, 0.0, NEG)
        dist = np.where(m < own, own - m, 1).astype(np.float32)
        base[:, l, :] = 2.0 * p - 256.0 * dist[None, :]
    o, w = CST_OFF["past"]
    c[:, o:o + w] = np.tile(past.reshape(1, 1024), (128, 1))
    o, w = CST_OFF["base"]
    c[:, o:o + w] = base.reshape(128, 1024)
    o, w = CST_OFF["irow"]
    c[:, o:o + w] = np.arange(256, dtype=np.float32)[None, :]
    i = np.arange(256, dtype=np.float32)[None, None, :]
    pp = np.arange(128, dtype=np.float32)[:, None, None]
    cc = np.arange(2, dtype=np.float32)[None, :, None]
    Dm = np.broadcast_to(i - 2 * pp, (128, 2, 256))
    causal = np.where(i >= 2 * pp + cc, 0.0, NEG)
    o, w = CST_OFF["Dm"]
    c[:, o:o + w] = Dm.reshape(128, 512)
    o, w = CST_OFF["causal"]
    c[:, o:o + w] = causal.reshape(128, 512)
    o, w = CST_OFF["ident"]
    c[:, o:o + w] = np.eye(128, dtype=np.float32)
    o, w = CST_OFF["sel65"]
    c[64, o:o + w] = 1.0
    return c


def emit_proj_res_phase(S, cx, x_in, b_xin, x_out, b_xout, src_d, b_src, w_d, G, bias, b_mod, tag, src_is_heads, ntiles=8):
    nc = S.nc
    w = S.sb("w_" + tag, [128, KC, D], BF16)
    b_w = Buf("w_" + tag)
    for kc in range(KC):
        S.op("pool_dma", (lambda kc=kc: nc.gpsimd.dma_start(out=w[:, kc, :], in_=w_d[kc * 128:(kc + 1) * 128, :])), writes=[b_w])
    sv = src_d.rearrange("h d t -> (h d) t") if src_is_heads else src_d
    sv = sv.rearrange("(c p) t -> p c t", p=128)
    xv_in = x_in.rearrange("(c p) t -> p c t", p=128)
    xv_out = x_out.rearrange("(c p) t -> p c t", p=128)
    for t in range(ntiles):
        xt, bx = load_x_tile(S, cx, xv_in, b_xin, t)
        S.op("sp", (lambda t=t: nc.sync.dma_start(out=cx.h[:], in_=sv[:, :, t * 512:(t + 1) * 512])), reads=[b_src[t]], writes=[cx.b_h])
        for oc in range(KC):
            py = 5 + (oc % 2)

            def mm(oc=oc, py=py):
                for kc in range(KC):
                    m = nc.tensor.matmul(cx.psum[py][:], lhsT=w[:, kc, oc * 128:(oc + 1) * 128], rhs=cx.h[:, kc, :], start=(kc == 0), stop=(kc == KC - 1))
                return m
            S.op("pe", mm, reads=[b_w, cx.b_h], writes=[cx.b_psum[py]])
            if bias is None:
                S.op("dve", (lambda oc=oc, py=py, xt=xt: nc.vector.scalar_tensor_tensor(
                    out=xt[:, oc, :], in0=cx.psum[py][:], scalar=G[:, oc:oc + 1], in1=xt[:, oc, :], op0=ALU.mult, op1=ALU.add)),
                    reads=[cx.b_psum[py], b_mod], writes=[bx])
            else:
                tmp = cx.tmp[oc % 2]
                S.op("act", (lambda oc=oc, py=py, tmp=tmp: nc.scalar.activation(out=tmp[:], in_=cx.psum[py][:], func=AF.Identity, bias=bias[:, oc:oc + 1])),
                     reads=[cx.b_psum[py], cx.b_sp], writes=[cx.b_tmp[oc % 2]])
                S.op("dve", (lambda oc=oc, tmp=tmp, xt=xt: nc.vector.scalar_tensor_tensor(
                    out=xt[:, oc, :], in0=tmp[:], scalar=G[:, oc:oc + 1], in1=xt[:, oc, :], op0=ALU.mult, op1=ALU.add)),
                    reads=[cx.b_tmp[oc % 2], b_mod], writes=[bx])
        S.op("sp", (lambda xt=xt, t=t: nc.sync.dma_start(out=xv_out[:, :, t * 512:(t + 1) * 512], in_=xt[:])), reads=[bx], writes=[b_xout[t]])


def emit_glu_phase(S, cx, x_in, b_xin, w_d, A, Bv, b_mod, u_out, ntiles=8, hmask=None, b_hmask=None, b_uout=None):
    nc = S.nc
    w = S.sb("w_pw1", [128, KC, 2 * D], BF16)
    b_w = Buf("w_pw1")
    for kc in range(KC):
        S.op("pool_dma", (lambda kc=kc: nc.gpsimd.dma_start(out=w[:, kc, :], in_=w_d[kc * 128:(kc + 1) * 128, :])), writes=[b_w])
    uo = [S.sb("uo%d" % i, [128, 512], BF16) for i in range(2)]
    b_uo = [Buf("uo%d" % i) for i in range(2)]
    xv_in = x_in.rearrange("(c p) t -> p c t", p=128)
    uv = u_out.rearrange("(c p) t -> p c t", p=128)
    bp1 = spc(cx, "b_pw1")
    for t in range(ntiles):
        xt, bx = load_x_tile(S, cx, xv_in, b_xin, t)
        emit_norm_mod(S, cx, xt, bx, A, Bv, b_mod)
        for oc in range(KC):
            k = oc % 2
            pa = 1 + k * 2
            pg = pa + 1

            def mma(oc=oc, pa=pa):
                for kc in range(KC):
                    m = nc.tensor.matmul(cx.psum[pa][:], lhsT=w[:, kc, oc * 128:(oc + 1) * 128], rhs=cx.h[:, kc, :], start=(kc == 0), stop=(kc == KC - 1))
                return m

            def mmg(oc=oc, pg=pg):
                for kc in range(KC):
                    m = nc.tensor.matmul(cx.psum[pg][:], lhsT=w[:, kc, D + oc * 128:D + (oc + 1) * 128], rhs=cx.h[:, kc, :], start=(kc == 0), stop=(kc == KC - 1))
                return m
            S.op("pe", mma, reads=[b_w, cx.b_h], writes=[cx.b_psum[pa]])
            S.op("pe", mmg, reads=[b_w, cx.b_h], writes=[cx.b_psum[pg]])
            sa = cx.sa[k]
            S.op("act", (lambda oc=oc, pg=pg, sa=sa: nc.scalar.activation(out=sa[:], in_=cx.psum[pg][:], func=AF.Sigmoid, bias=bp1[:, 8 + oc:9 + oc])),
                 reads=[cx.b_psum[pg], cx.b_sp], writes=[cx.b_sa[k]])
            S.op("dve", (lambda oc=oc, pa=pa, sa=sa, k=k: nc.vector.scalar_tensor_tensor(
                out=uo[k][:], in0=cx.psum[pa][:], scalar=bp1[:, oc:oc + 1], in1=sa[:], op0=ALU.add, op1=ALU.mult)),
                reads=[cx.b_psum[pa], cx.b_sa[k], cx.b_sp], writes=[b_uo[k]])
            if hmask is not None and t == 8:
                S.op("pool", (lambda k=k: nc.gpsimd.tensor_tensor(out=uo[k][:], in0=uo[k][:], in1=hmask, op=ALU.mult)),
                     reads=[b_uo[k], b_hmask], writes=[b_uo[k]])
            S.op("sp", (lambda oc=oc, k=k, t=t: nc.sync.dma_start(out=uv[:, oc, t * 512:(t + 1) * 512], in_=uo[k][:])), reads=[b_uo[k]],
                 writes=([b_uout[t]] if b_uout is not None else []))


def build_L2():
    nc = bass.Bass("TRN2", target_bir_lowering=False)
    din = lambda n, s, dt: nc.dram_tensor(n, s, dt, kind="ExternalInput").ap()
    sp_d = din("sp", [128, SP_N], F32)
    mod_d = din("modi", [128, 144], F32)
    x1 = din("x1", [1024, TOK], F32)
    d = {"KT": din("KTf", [NH, DH, SEQ], BF16), "ind": din("ind", [64, SEQ], BF16), "Vd": din("Vd", [NH, 128, 128, DH], BF16),
         "QT": din("QT", [NH, DH, TOK], BF16), "KTo": din("KTo", [NH, DH, TOK], BF16), "Vo": din("Vo", [NH, 128, 32, DH], BF16),
         "kbar": din("kbarf", [DH, NH, NB], F32), "cst": din("cst", [128, CST_N], F32)}
    w_o = din("w_o", [1024, 1024], F32)
    w_in1 = din("w_in1", [1024, 2 * DFF], F32)
    w_out1 = din("w_out1", [DFF, 1024], F32)
    w_in2 = din("w_in2", [1024, 2 * DFF], F32)
    w_out2 = din("w_out2", [DFF, 1024], F32)
    w_pw1 = din("w_pw1", [1024, 2048], F32)
    OT = nc.dram_tensor("OT", [NH, DH, TOK], BF16).ap()
    xa = nc.dram_tensor("xa", [1024, TOK], F32).ap()
    xb = nc.dram_tensor("xb", [1024, TOK], F32).ap()
    x4 = nc.dram_tensor("x4", [1024, TOK], F32, kind="ExternalOutput").ap()
    uT = nc.dram_tensor("uT", [1024, TOK], BF16, kind="ExternalOutput").ap()
    d["OT"] = OT
    d["b_OT"] = [Buf("OT%d" % t) for t in range(8)]
    S = Sched(nc)
    cx = Ctx()
    setup_common(S, cx)
    emit_load_small(S, cx, sp_d)
    modT = [S.sb("modT%d" % i, [128, 72], F32) for i in range(2)]
    b_modT = [Buf("modT%d" % i) for i in range(2)]
    for i in range(2):
        S.op("sp", (lambda i=i: nc.sync.dma_start(out=modT[i][:], in_=mod_d[:, i * 72:(i + 1) * 72])), writes=[b_modT[i]])
    m01 = emit_mod_vectors_i(S, cx, modT[0], b_modT[0], spc(cx, "ng01"), "l0s1", 1)
    m02 = emit_mod_vectors_i(S, cx, modT[0], b_modT[0], spc(cx, "ng02"), "l0s2", 2)
    m10 = emit_mod_vectors_i(S, cx, modT[1], b_modT[1], spc(cx, "ng10"), "l1s0", 0)
    m11 = emit_mod_vectors_i(S, cx, modT[1], b_modT[1], spc(cx, "ng11"), "l1s1", 1)
    n0 = len(S._stack)
    emit_attn_phase(S, cx, d)
    S.barrier_free(len(S._stack) - n0)
    alloc_tile_bufs(S, cx)
    b_x1 = [Buf("x1_%d" % t) for t in range(8)]
    b_xa = [Buf("xa_%d" % t) for t in range(8)]
    b_xb = [Buf("xb_%d" % t) for t in range(8)]
    b_x4 = [Buf("x4_%d" % t) for t in range(8)]
    n1 = len(S._stack)
    emit_proj_res_phase(S, cx, x1, b_x1, xa, b_xa, OT, d["b_OT"], w_o, m01[2], None, m01[3], "wo", True)
    S.barrier_free(len(S._stack) - n1)
    alloc_ffn_bufs(S, cx)
    emit_ffn_phase(S, cx, xa, b_xa, xb, b_xb, w_in1, w_out1, m02[0], m02[1], m02[2], 0.5, m02[3], 8, "f01")
    emit_ffn_phase(S, cx, xb, b_xb, x4, b_x4, w_in2, w_out2, m10[0], m10[1], m10[2], 0.5, m10[3], 8, "f10")
    S.barrier_free(len(S._stack) - n1)
    emit_glu_phase(S, cx, x4, b_x4, w_pw1, m11[0], m11[1], m11[3], uT)
    S.emit()
    return nc


def emit_conv_phase(S, cx, uext_d, x_in, b_xin, x_out, b_xout, w2_d, ident_d, G, b_mod, u_scr=None, b_u=None):
    nc = S.nc
    w2 = S.sb("w_pw2", [128, KC, D], BF16)
    b_w2 = Buf("w_pw2")
    for kc in range(KC):
        S.op("pool_dma", (lambda kc=kc: nc.gpsimd.dma_start(out=w2[:, kc, :], in_=w2_d[kc * 128:(kc + 1) * 128, :])), writes=[b_w2])
    ident = S.sb("identc", [128, 128], F32)
    b_id = Buf("identc")
    S.op("sp", lambda: nc.sync.dma_start(out=ident[:], in_=ident_d[:, :]), writes=[b_id])
    dg = S.sb("dg", [128, KC * CW, 128], BF16)
    b_dg = Buf("dg")
    wdw = spc(cx, "w_dw")
    for ck in range(KC * CW):
        S.op("dve", (lambda ck=ck: nc.vector.tensor_scalar(out=dg[:, ck, :], in0=ident[:], scalar1=wdw[:, ck:ck + 1], scalar2=None, op0=ALU.mult)),
             reads=[b_id, cx.b_sp], writes=[b_dg])
    ue = [S.sb("ue%d" % i, [128, KC, 288], BF16) for i in range(2)]
    b_ue = [Buf("ue%d" % i) for i in range(2)]
    vs = S.sb("vs", [128, KC, 256], F32)
    b_vs = [Buf("vs%d" % c) for c in range(KC)]
    vb = [S.sb("vb%d" % i, [128, 256], BF16) for i in range(2)]
    b_vb = [Buf("vb%d" % i) for i in range(2)]
    vq = [S.sb("vq%d" % i, [128, 256], BF16) for i in range(2)]
    b_vq = [Buf("vq%d" % i) for i in range(2)]
    mu = S.sb("mu", [128, 256], F32)
    rs = S.sb("rs", [128, 256], F32)
    b_st = Buf("lnstat")
    uv = uext_d.rearrange("(c p) l e -> p c l e", p=128) if uext_d is not None else None
    usv = u_scr.rearrange("(c p) t -> p c t", p=128) if u_scr is not None else None
    xv_in = x_in.rearrange("(c p) t -> p c t", p=128)
    xv_out = x_out.rearrange("(c p) t -> p c t", p=128)
    for l in range(NLB):
        u_ = ue[l % 2]
        if usv is None:
            S.op("sp", (lambda u_=u_, l=l: nc.sync.dma_start(out=u_[:], in_=uv[:, :, l, :])), writes=[b_ue[l % 2]])
        else:
            S.op("sp", (lambda u_=u_, l=l: nc.sync.dma_start(out=u_[:, :, 32:288], in_=usv[:, :, l * 256:(l + 1) * 256])),
                 reads=[b_u[l // 2]], writes=[b_ue[l % 2]])
            S.op("sp", (lambda u_=u_, l=l: nc.sync.dma_start(out=u_[:, :, 0:32], in_=usv[:, :, TOK + l * 32:TOK + (l + 1) * 32])),
                 reads=[b_u[8]], writes=[b_ue[l % 2]])
        xt, bx = load_x_tile(S, cx, xv_in, b_xin, l, n=256)
        for c in range(KC):
            k2 = c % 2
            pc = cx.psum[1 + k2]

            def mmc(c=c, pc=pc, u_=u_):
                for k in range(CW):
                    m = nc.tensor.matmul(pc[:, 0:256], lhsT=dg[:, c * CW + k, :], rhs=u_[:, c, 2 + k:2 + k + 256], start=(k == 0), stop=(k == CW - 1))
                return m
            S.op("pe", mmc, reads=[b_dg, b_ue[l % 2]], writes=[cx.b_psum[1 + k2]])
            S.op("act", (lambda c=c, pc=pc: nc.scalar.activation(out=vs[:, c, :], in_=pc[:, 0:256], func=AF.Identity, bias=spc(cx, "b_dw")[:, c:c + 1])),
                 reads=[cx.b_psum[1 + k2], cx.b_sp], writes=[b_vs[c]])
            S.op("pool", (lambda c=c, k2=k2: nc.gpsimd.tensor_copy(out=vb[k2][:], in_=vs[:, c, :])), reads=[b_vs[c]], writes=[b_vb[k2]])
            S.op("act", (lambda c=c, k2=k2: nc.scalar.activation(out=vq[k2][:], in_=vs[:, c, :], func=AF.Square)), reads=[b_vs[c]], writes=[b_vq[k2]])
            S.op("pe", (lambda c=c, k2=k2: nc.tensor.matmul(cx.psum[3][:, 0:256], lhsT=cx.ones_bf[:], rhs=vb[k2][:], start=(c == 0), stop=(c == KC - 1))),
                 reads=[b_vb[k2], cx.b_ones], writes=[cx.b_psum[3]])
            S.op("pe", (lambda c=c, k2=k2: nc.tensor.matmul(cx.psum[4][:, 0:256], lhsT=cx.ones_bf[:], rhs=vq[k2][:], start=(c == 0), stop=(c == KC - 1))),
                 reads=[b_vq[k2], cx.b_ones], writes=[cx.b_psum[4]])
        S.op("dve", lambda: nc.vector.tensor_scalar(out=mu[:], in0=cx.psum[3][:, 0:256], scalar1=1.0 / D, scalar2=None, op0=ALU.mult),
             reads=[cx.b_psum[3]], writes=[b_st])
        S.op("dve", lambda: nc.vector.tensor_tensor(out=rs[:], in0=mu[:], in1=mu[:], op=ALU.mult), reads=[b_st], writes=[b_st])
        S.op("dve", lambda: nc.vector.scalar_tensor_tensor(out=rs[:], in0=cx.psum[4][:, 0:256], scalar=1.0 / D, in1=rs[:], op0=ALU.mult, op1=ALU.subtract),
             reads=[cx.b_psum[4], b_st], writes=[b_st])
        S.op("dve", lambda: nc.vector.tensor_scalar(out=rs[:], in0=rs[:], scalar1=EPS, scalar2=None, op0=ALU.add), reads=[b_st], writes=[b_st])
        S.op("act", lambda: nc.scalar.activation(out=rs[:], in_=rs[:], func=AF.Sqrt), reads=[b_st], writes=[b_st])
        S.op("dve", lambda: nc.vector.reciprocal(out=rs[:], in_=rs[:]), reads=[b_st], writes=[b_st])
        for c in range(KC):
            tmp = cx.tmp[c % 2]
            S.op("dve", (lambda c=c, tmp=tmp: nc.vector.tensor_tensor(out=tmp[:, 0:256], in0=vs[:, c, :], in1=mu[:], op=ALU.subtract)),
                 reads=[b_vs[c], b_st], writes=[cx.b_tmp[c % 2]])
            S.op("dve", (lambda c=c, tmp=tmp: nc.vector.tensor_tensor(out=tmp[:, 0:256], in0=tmp[:, 0:256], in1=rs[:], op=ALU.mult)),
                 reads=[b_st, cx.b_tmp[c % 2]], writes=[cx.b_tmp[c % 2]])
            S.op("act", (lambda c=c, tmp=tmp: nc.scalar.activation(out=cx.h[:, c, 0:256], in_=tmp[:, 0:256], func=AF.Silu,
                                                                   bias=spc(cx, "ln_b")[:, c:c + 1], scale=spc(cx, "ln_g")[:, c:c + 1])),
                 reads=[cx.b_tmp[c % 2], cx.b_sp], writes=[cx.b_h])
        for oc in range(KC):
            py = 5 + (oc % 2)

            def mm(oc=oc, py=py):
                for kc in range(KC):
                    m = nc.tensor.matmul(cx.psum[py][:, 0:256], lhsT=w2[:, kc, oc * 128:(oc + 1) * 128], rhs=cx.h[:, kc, 0:256], start=(kc == 0), stop=(kc == KC - 1))
                return m
            S.op("pe", mm, reads=[b_w2, cx.b_h], writes=[cx.b_psum[py]])
            sa = cx.sa[oc % 2]
            S.op("act", (lambda oc=oc, py=py, sa=sa: nc.scalar.activation(out=sa[:, 0:256], in_=cx.psum[py][:, 0:256], func=AF.Identity,
                                                                        bias=spc(cx, "b_pw2")[:, oc:oc + 1])),
                 reads=[cx.b_psum[py], cx.b_sp], writes=[cx.b_sa[oc % 2]])
            S.op("dve", (lambda oc=oc, sa=sa, xt=xt: nc.vector.scalar_tensor_tensor(
                out=xt[:, oc, 0:256], in0=sa[:, 0:256], scalar=G[:, oc:oc + 1], in1=xt[:, oc, 0:256], op0=ALU.mult, op1=ALU.add)),
                reads=[cx.b_sa[oc % 2], b_mod], writes=[bx])
        S.op("sp", (lambda xt=xt, l=l: nc.sync.dma_start(out=xv_out[:, :, l * 256:(l + 1) * 256], in_=xt[:, :, 0:256])),
             reads=[bx], writes=[b_xout[l // 2]])


def build_L3():
    nc = bass.Bass("TRN2", target_bir_lowering=False)
    din = lambda n, s, dt: nc.dram_tensor(n, s, dt, kind="ExternalInput").ap()
    sp_d = din("sp", [128, SP_N], F32)
    mod_d = din("modi", [128, 144], F32)
    x4 = din("x4", [1024, TOK], F32)
    uext = din("uext", [1024, NLB, 288], BF16)
    ident_d = din("ident", [128, 128], F32)
    w_pw2 = din("w_pw2", [1024, 1024], F32)
    w_in3 = din("w_in3", [1024, 2 * DFF], F32)
    w_out3 = din("w_out3", [DFF, 1024], F32)
    x5 = nc.dram_tensor("x5", [1024, TOK], F32).ap()
    xo = nc.dram_tensor("xo", [1024, TOK], F32, kind="ExternalOutput").ap()
    S = Sched(nc)
    cx = Ctx()
    setup_common(S, cx)
    emit_load_small(S, cx, sp_d)
    modT = [S.sb("modT%d" % i, [128, 72], F32) for i in range(2)]
    b_modT = [Buf("modT%d" % i) for i in range(2)]
    for i in range(2):
        S.op("sp", (lambda i=i: nc.sync.dma_start(out=modT[i][:], in_=mod_d[:, i * 72:(i + 1) * 72])), writes=[b_modT[i]])
    m11 = emit_mod_vectors_i(S, cx, modT[1], b_modT[1], spc(cx, "ng11"), "l1s1", 1)
    m12 = emit_mod_vectors_i(S, cx, modT[1], b_modT[1], spc(cx, "ng12"), "l1s2", 2)
    alloc_tile_bufs(S, cx)
    b_x4 = [Buf("x4_%d" % t) for t in range(16)]
    b_x5 = [Buf("x5_%d" % t) for t in range(8)]
    b_xo = [Buf("xo_%d" % t) for t in range(8)]
    n1 = len(S._stack)
    emit_conv_phase(S, cx, uext, x4, b_x4, x5, b_x5, w_pw2, ident_d, m11[2], m11[3])
    S.barrier_free(len(S._stack) - n1)
    alloc_ffn_bufs(S, cx)
    emit_ffn_phase(S, cx, x5, b_x5, xo, b_xo, w_in3, w_out3, m12[0], m12[1], m12[2], 0.5, m12[3], 8, "f11")
    S.emit()
    return nc


_NC = {}


def _get(name, fn):
    if name not in _NC:
        _NC[name] = fn()
    return _NC[name]


def _deint(a):
    sh = a.shape
    return np.ascontiguousarray(a.reshape(sh[:-1] + (sh[-1] // 256, 128, 2)).swapaxes(-1, -2)).reshape(sh)


def kernel_unfused(**inp):
    import ml_dtypes
    bf = ml_dtypes.bfloat16
    inp = {k: np.asarray(v) for k, v in inp.items()}
    cores = list(range(8))
    toks = []
    for core in cores:
        blocks = snake_blocks(core % 4)
        toks.append(np.concatenate([np.arange(g * 256, (g + 1) * 256) for g in blocks]))
    sps = [pack_small(inp, c // 4) for c in cores]
    nc1 = _get("L1", build_L1)
    maps = [{"sp": sps[c], "ada_w": inp["ada_w"], "xin": np.ascontiguousarray(inp["x"][c // 4][toks[c]].T),
             "w_in": inp["ffn_w_in"][0, 0], "w_out": inp["ffn_w_out"][0, 0], "wqkv": inp["attn_w_qkv"][0]} for c in cores]
    r1 = run_bass_kernel_spmd(nc1, maps, core_ids=cores).results
    ind = np.zeros((64, SEQ), bf)
    for m in range(64):
        ind[m, m * 256:(m + 1) * 256] = 1
    ident = np.eye(128, dtype=np.float32)
    maps2 = []
    per_batch = {}
    for b in range(2):
        KTf = np.zeros((NH, DH, SEQ), bf)
        Vf = np.zeros((SEQ, D), bf)
        kbf = np.zeros((DH, NH, NB), np.float32)
        for j in range(4):
            c = 4 * b + j
            blocks = snake_blocks(j)
            KT = np.asarray(r1[c]["KT"])
            V = np.asarray(r1[c]["V"])
            kb = np.asarray(r1[c]["kbar"])
            for l, g in enumerate(blocks):
                KTf[:, :, g * 256:(g + 1) * 256] = KT[:, :, l * 256:(l + 1) * 256]
                Vf[g * 256:(g + 1) * 256] = V[l * 256:(l + 1) * 256]
                kbf[:, :, g] = kb[:, :, l].T
        KTf = _deint(KTf)
        Vd = np.ascontiguousarray(Vf.reshape(NB, 128, 2, NH, DH).transpose(3, 1, 0, 2, 4)).reshape(NH, 128, 128, DH)
        per_batch[b] = (KTf, Vd, kbf)
    for c in cores:
        b = c // 4
        KTf, Vd, kbf = per_batch[b]
        KTo = _deint(np.asarray(r1[c]["KT"]))
        V = np.asarray(r1[c]["V"])
        Vo = np.ascontiguousarray(V.reshape(NLB, 128, 2, NH, DH).transpose(3, 1, 0, 2, 4)).reshape(NH, 128, 32, DH)
        maps2.append({"sp": sps[c], "modi": r1[c]["modo"], "x1": r1[c]["x1"], "KTf": KTf, "ind": ind, "Vd": Vd,
                      "QT": r1[c]["QT"], "KTo": KTo, "Vo": Vo, "kbarf": kbf, "cst": make_cst(c % 4),
                      "w_o": inp["attn_w_o"][0], "w_in1": inp["ffn_w_in"][0, 1], "w_out1": inp["ffn_w_out"][0, 1],
                      "w_in2": inp["ffn_w_in"][1, 0], "w_out2": inp["ffn_w_out"][1, 0], "w_pw1": inp["conv_w_pw1"][0]})
    nc2 = _get("L2", build_L2)
    r2 = run_bass_kernel_spmd(nc2, maps2, core_ids=cores).results
    maps3 = []
    for b in range(2):
        Uf = np.zeros((D, SEQ), bf)
        for j in range(4):
            c = 4 * b + j
            Uf[:, toks[c]] = np.asarray(r2[c]["uT"])
        for j in range(4):
            c = 4 * b + j
            ue = np.zeros((D, NLB, 288), bf)
            for l, g in enumerate(snake_blocks(j)):
                ue[:, l, 32:] = Uf[:, g * 256:(g + 1) * 256]
                if g > 0:
                    ue[:, l, :32] = Uf[:, g * 256 - 32:g * 256]
            maps3.append({"sp": sps[c], "modi": r1[c]["modo"], "x4": r2[c]["x4"], "uext": ue, "ident": ident,
                          "w_pw2": inp["conv_w_pw2"][0], "w_in3": inp["ffn_w_in"][1, 1], "w_out3": inp["ffn_w_out"][1, 1]})
    nc3 = _get("L3", build_L3)
    r3 = run_bass_kernel_spmd(nc3, maps3, core_ids=cores).results
    out = np.zeros((BATCH, SEQ, D), np.float32)
    for c in cores:
        out[c // 4][toks[c]] = np.asarray(r3[c]["xo"]).T
    return out


NPOS = 80
NT_ALL = 40
TOKX = TOK + 512


def pos_order(j):
    real = []
    own = snake_blocks(j)
    for i in range(8):
        A, B = 8 * i + j, 8 * i + 7 - j
        real += [A, B] + [g for g in range(8 * i, 8 * i + 8) if g not in (A, B)]
    dup = [(g - 1 if g > 0 else None) for g in own]
    return real, dup


def emit_kvq_phase(S, cx, x_d, b_x, wqkv_d, A, Bv, b_mod, QT_d, KT_d, kbar_d, V_d, x1o_d, b_x1o, b_kv):
    nc = S.nc
    wq = S.sb("wqkv_sb", [128, KC, 3072], BF16)
    b_wq = [Buf("wqkv%d" % k) for k in range(KC)]
    for kc in range(KC):
        S.op("pool_dma", (lambda kc=kc: nc.gpsimd.dma_start(out=wq[:, kc, :], in_=wqkv_d[kc * 128:(kc + 1) * 128, :])), writes=[b_wq[kc]])
    bo = S.sb("blkones", [128, 128], BF16)
    b_bo = Buf("blkones")
    S.op("dve", lambda: nc.vector.memset(bo[:], 0.0), writes=[b_bo])
    S.op("dve", lambda: nc.vector.memset(bo[0:64, 0:64], 1.0), writes=[b_bo])
    S.op("dve", lambda: nc.vector.memset(bo[64:128, 64:128], 1.0), writes=[b_bo])
    g8 = S.sb("g8", [128, 2], F32)
    b_g8 = Buf("g8")
    S.op("dve", lambda: nc.vector.tensor_scalar(out=g8[:, 0:1], in0=spc(cx, "gq"), scalar1=0.125, scalar2=None, op0=ALU.mult),
         reads=[cx.b_sp], writes=[b_g8])
    S.op("dve", lambda: nc.vector.tensor_copy(out=g8[:, 1:2], in_=spc(cx, "gk")), reads=[cx.b_sp], writes=[b_g8])
    epst = S.sb("epst", [128, 1], F32)
    b_eps = Buf("epst")
    S.op("dve", lambda: nc.vector.memset(epst[:], EPS), writes=[b_eps])
    qo = [S.sb("qo%d" % i, [128, 512], BF16) for i in range(2)]
    b_qo = [Buf("qo%d" % i) for i in range(2)]
    kb = [S.sb("kb%d" % i, [128, 2], F32) for i in range(2)]
    b_kb = [Buf("kb%d" % i) for i in range(2)]
    vt = [S.sb("vt%d" % i, [128, 1024], BF16) for i in range(2)]
    b_vt = [Buf("vt%d" % i) for i in range(2)]
    xv = x_d.rearrange("(c p) t -> p c t", p=128)
    xov = x1o_d.rearrange("(c p) t -> p c t", p=128)
    QTv = QT_d.rearrange("h d t -> (h d) t")
    KTv = KT_d.rearrange("h d t -> (h d) t")
    cnt = 0
    PQB = (1, 2, 4, 7)
    for t in range(NT_ALL):
        is_own = (t < 32 and t % 4 == 0)
        is_dup = t >= 32
        xt, bx = cx.xt[t % 2], cx.b_xt[t % 2]
        if t == 0:
            load_x_tile(S, cx, xv, b_x, 0)
        if is_own:
            S.op("sp", (lambda xt=xt, t=t: nc.sync.dma_start(out=xov[:, :, (t // 4) * 512:(t // 4 + 1) * 512], in_=xt[:])),
                 reads=[bx], writes=[b_x1o[t // 4]])
        if is_dup:
            i2 = t - 32
            for k2 in range(2):
                S.op("sp", (lambda xt=xt, i2=i2, k2=k2: nc.sync.dma_start(
                    out=xov[:, :, TOK + (2 * i2 + k2) * 32:TOK + (2 * i2 + k2 + 1) * 32], in_=xt[:, :, k2 * 256 + 224:k2 * 256 + 256])),
                    reads=[bx], writes=[b_x1o[8]])
        emit_norm_mod(S, cx, xt, bx, A, Bv, b_mod)
        if t + 1 < NT_ALL:
            load_x_tile(S, cx, xv, b_x, t + 1)
        items = [(w_, hp) for w_ in range(2) for hp in range(8) if not (w_ == 0 and not (is_own or is_dup))]
        base_cnt = cnt
        cnt += len(items)

        def st_A(ii, base_cnt=base_cnt, items=items):
            which, hp = items[ii]
            k = (base_cnt + ii) % 2
            goff = which * 1024
            pqi = PQB[(base_cnt + ii) % 4]
            pq = cx.psum[pqi]

            def mm():
                for kc in range(KC):
                    m = nc.tensor.matmul(pq[:], lhsT=wq[:, kc, goff + hp * 128:goff + (hp + 1) * 128], rhs=cx.h[:, kc, :],
                                         start=(kc == 0), stop=(kc == KC - 1))
                return m
            S.op("pe", mm, reads=b_wq + [cx.b_h], writes=[cx.b_psum[pqi]])
            S.op("act", (lambda: nc.scalar.activation(out=cx.sq[k][:], in_=pq[:], func=AF.Square)),
                 reads=[cx.b_psum[pqi]], writes=[cx.b_sq[k]])

        def st_B(ii, base_cnt=base_cnt, items=items, t=t, is_own=is_own):
            which, hp = items[ii]
            k = (base_cnt + ii) % 2
            pqi = PQB[(base_cnt + ii) % 4]
            pq = cx.psum[pqi]
            pss = cx.psum[3]
            S.op("pe", (lambda: nc.tensor.matmul(pss[:], lhsT=bo[:], rhs=cx.sq[k][:], start=True, stop=True)),
                 reads=[cx.b_sq[k], b_bo], writes=[cx.b_psum[3]])
            r1 = cx.tmp[k]
            S.op("act", (lambda: nc.scalar.activation(out=r1[:], in_=pss[:], func=AF.Ln, bias=epst[:, 0:1], scale=1.0 / DH)),
                 reads=[cx.b_psum[3], b_eps], writes=[cx.b_tmp[k]])
            S.op("act", (lambda: nc.scalar.activation(out=r1[:], in_=r1[:], func=AF.Exp, scale=-0.5)), reads=[cx.b_tmp[k]], writes=[cx.b_tmp[k]])
            if which == 0:
                S.op("dve", (lambda: nc.vector.scalar_tensor_tensor(out=qo[k][:], in0=pq[:], scalar=g8[:, 0:1], in1=r1[:], op0=ALU.mult, op1=ALU.mult)),
                     reads=[cx.b_tmp[k], cx.b_psum[pqi], b_g8], writes=[b_qo[k]])
                if is_own:
                    for k2 in range(2):
                        lb = 2 * (t // 4) + k2
                        S.op("sp", (lambda k2=k2, lb=lb: nc.sync.dma_start(out=QTv[hp * 128:(hp + 1) * 128, lb * 288:lb * 288 + 256],
                                                                         in_=qo[k][:, k2 * 256:(k2 + 1) * 256])),
                             reads=[b_qo[k]], writes=[b_kv])
                else:
                    i2 = t - 32
                    for k2 in range(2):
                        lb = 2 * i2 + k2
                        S.op("sp", (lambda k2=k2, lb=lb: nc.sync.dma_start(
                            out=QTv[hp * 128:(hp + 1) * 128, lb * 288 + 256:lb * 288 + 288],
                            in_=qo[k][:, k2 * 256 + 224:k2 * 256 + 256])), reads=[b_qo[k]], writes=[b_kv])
            else:
                for bb in range(2):
                    S.op("dve", (lambda bb=bb: nc.vector.scalar_tensor_tensor(
                        out=qo[k][:, bb * 256:(bb + 1) * 256].rearrange("q (c p) -> q c p", c=2),
                        in0=pq[:, bb * 256:(bb + 1) * 256].rearrange("q (p c) -> q c p", c=2), scalar=g8[:, 1:2],
                        in1=r1[:, bb * 256:(bb + 1) * 256].rearrange("q (p c) -> q c p", c=2), op0=ALU.mult, op1=ALU.mult)),
                        reads=[cx.b_tmp[k], cx.b_psum[pqi], b_g8], writes=[b_qo[k]])
                S.op("sp", (lambda: nc.sync.dma_start(out=KTv[hp * 128:(hp + 1) * 128, t * 512:(t + 1) * 512], in_=qo[k][:])),
                     reads=[b_qo[k]], writes=[b_kv])
                S.op("dve", (lambda: nc.vector.tensor_reduce(out=kb[k][:], in_=qo[k][:].rearrange("p (b t) -> p b t", b=2), axis=AX.X, op=ALU.add)),
                     reads=[b_qo[k]], writes=[b_kb[k]])
                S.op("dve", (lambda: nc.vector.tensor_scalar(out=kb[k][:], in0=kb[k][:], scalar1=1.0 / BLK, scalar2=None, op0=ALU.mult)),
                     reads=[b_kb[k]], writes=[b_kb[k]])
                S.op("sp", (lambda: nc.sync.dma_start(out=kbar_d[hp * 128:(hp + 1) * 128, 2 * t:2 * t + 2], in_=kb[k][:])),
                     reads=[b_kb[k]], writes=[b_kv])
        vgroups = [(tg, ns) for tg in range(4) for ns in range(2)]

        def st_V(gi, t=t):
            tg, ns = vgroups[gi]
            v = vt[tg % 2]
            pv = cx.psum[5 + ns]

            def mmv():
                for kc in range(KC):
                    m = nc.tensor.matmul(pv[:], lhsT=cx.h[:, kc, tg * 128:(tg + 1) * 128], rhs=wq[:, kc, 2048 + ns * 512:2048 + (ns + 1) * 512],
                                         start=(kc == 0), stop=(kc == KC - 1))
                return m
            S.op("pe", mmv, reads=b_wq + [cx.b_h], writes=[cx.b_psum[5 + ns]])
            if ns == 0:
                S.op("act", (lambda: nc.scalar.copy(out=v[:, 0:512], in_=pv[:])), reads=[cx.b_psum[5]], writes=[b_vt[tg % 2]])
            else:
                S.op("dve", (lambda: nc.vector.tensor_copy(out=v[:, 512:1024], in_=pv[:])), reads=[cx.b_psum[6]], writes=[b_vt[tg % 2]])
                blk = 2 * t + tg // 2
                hf = tg % 2
                for c2 in range(2):
                    dst = V_d[:, hf * 64:(hf + 1) * 64, 2 * blk + c2, :].rearrange("h pp d -> pp h d")
                    S.op("sp", (lambda dst=dst, c2=c2: nc.sync.dma_start(out=dst, in_=v[c2:128:2, :].rearrange("p (h d) -> p h d", h=NH))),
                         reads=[b_vt[tg % 2]], writes=[b_kv])
        vper = len(items) // 8
        st_A(0)
        for ii in range(len(items)):
            if ii + 1 < len(items):
                st_A(ii + 1)
            st_B(ii)
            if (ii + 1) % vper == 0:
                st_V((ii + 1) // vper - 1)


CST2_OFF = {}


def _cst2_layout():
    off = 0
    for name, w in (("past", 2048), ("base", 1024), ("irowX", 288), ("Dm", 512), ("causal", 512),
                    ("DmH", 64), ("causalH", 64), ("ident", 128), ("sel65", 64)):
        CST2_OFF[name] = (off, w)
        off += w
    return off


CST2_N = _cst2_layout()


def make_cst2(j):
    real, dup = pos_order(j)
    own = snake_blocks(j)
    c = np.zeros((128, CST2_N), np.float32)
    p = np.arange(128, dtype=np.float32)[:, None]
    gpos = np.array(real)
    past = np.zeros((32, 64), np.float32)
    base = np.zeros((128, 16, 64), np.float32)
    for row in range(32):
        o = own[row] if row < 16 else own[row - 16] - 1
        valid = gpos < o
        past[row] = np.where(valid, 0.0, NEG)
        if row < 16:
            dist = np.where(valid, o - gpos, 1).astype(np.float32)
            base[:, row, :] = 2.0 * p - 256.0 * dist[None, :]

    def put(name, arr):
        o_, w = CST2_OFF[name]
        c[:, o_:o_ + w] = arr.reshape(arr.shape[0], -1)
    put("past", np.tile(past.reshape(1, 2048), (128, 1)))
    put("base", base.reshape(128, 1024))
    irx = np.concatenate([np.arange(256, dtype=np.float32), -(32.0 - np.arange(32, dtype=np.float32))])
    put("irowX", np.tile(irx[None, :], (128, 1)))
    pp = np.arange(128, dtype=np.float32)[:, None, None]
    cc = np.arange(2, dtype=np.float32)[None, :, None]
    i = np.arange(256, dtype=np.float32)[None, None, :]
    put("Dm", np.broadcast_to(i - 2 * pp, (128, 2, 256)).copy())
    put("causal", np.where(i >= 2 * pp + cc, 0.0, NEG).astype(np.float32))
    ih = 224 + np.arange(32, dtype=np.float32)[None, None, :]
    put("DmH", np.broadcast_to(ih - 2 * pp, (128, 2, 32)).copy())
    put("causalH", np.where(ih >= 2 * pp + cc, 0.0, NEG).astype(np.float32))
    put("ident", np.eye(128, dtype=np.float32))
    s65 = np.zeros((128, 64), np.float32)
    s65[64] = 1.0
    put("sel65", s65)
    hmask = np.ones((128, 512), np.float32)
    for l in range(16):
        if dup[l] is None:
            hmask[:, l * 32:(l + 1) * 32] = 0.0
    return c, hmask


def emit_attn2(S, cx, d):
    nc = S.nc
    cst = S.sb("cst2", [128, CST2_N], F32)
    b_cst = Buf("cst2")
    S.op("sp", lambda: nc.sync.dma_start(out=cst[:], in_=d["cst"][:, :]), writes=[b_cst])

    def C(name):
        o, w = CST2_OFF[name]
        return cst[:, o:o + w]
    kbh = S.sb("kbh", [64, NH * NB], BF16)
    kbl = S.sb("kbl", [64, NH * NB], BF16)
    b_kb = Buf("kbhl")
    n0 = len(S._stack)
    kb32 = S.sb("kb32", [64, NH * NB], F32)
    kbt = S.sb("kbt", [64, NH * NB], F32)
    S.op("sp", lambda: nc.sync.dma_start(out=kb32[:].rearrange("d (h m) -> d h m", h=NH),
                                         in_=d["kbar"].rearrange("(h d) m -> d h m", h=NH)[:, :, 0:NB]), reads=[d["b_kv"]], writes=[b_kb])
    S.op("dve", lambda: nc.vector.tensor_copy(out=kbh[:], in_=kb32[:]), reads=[b_kb], writes=[b_kb])
    S.op("dve", lambda: nc.vector.tensor_copy(out=kbt[:], in_=kbh[:]), reads=[b_kb], writes=[b_kb])
    S.op("dve", lambda: nc.vector.tensor_tensor(out=kbt[:], in0=kb32[:], in1=kbt[:], op=ALU.subtract), reads=[b_kb], writes=[b_kb])
    S.op("dve", lambda: nc.vector.tensor_copy(out=kbl[:], in_=kbt[:]), reads=[b_kb], writes=[b_kb])
    S.barrier_free(len(S._stack) - n0)
    KTa = [S.sb("KTa%d" % i, [128, SEQ], BF16) for i in range(2)]
    Va = [S.sb("Va%d" % i, [128, 128, 128], BF16) for i in range(2)]
    Qa = [S.sb("Qa%d" % i, [128, TOKX], BF16) for i in range(2)]
    Kd = [S.sb("Kd0", [128, TOK], BF16)] * 2
    Vdd = [S.sb("Vdd0", [128, 32, 65], BF16)] * 2
    bT = [S.sb("bT0", [128, 1024], F32)] * 2
    dgb = [S.sb("dgb0", [128, 512], F32)] * 2
    dgbH = [S.sb("dgbH%d" % i, [128, 64], F32) for i in range(2)]
    cq = [S.sb("cq%d" % i, [128, 288], F32) for i in range(2)]
    b_KT = [Buf("KTa%d" % i) for i in range(2)]
    b_Va = [Buf("Va%d" % i) for i in range(2)]
    b_Qa = [Buf("Qa%d" % i) for i in range(2)]
    b_Qm = [[Buf("Qm%d_%d" % (i, l)) for l in range(16)] for i in range(2)]
    b_Kd = [Buf("Kd0")] * 2
    b_Vdd = [Buf("Vdd0")] * 2
    b_hd = [Buf("hd0")] * 2
    mbp = [S.sb("mbp%d" % i, [128, 128], F32) for i in range(2)]
    b_mbp = [Buf("mbp%d" % i) for i in range(2)]
    gmt = [S.sb("gmt%d" % i, [128, 64], F32) for i in range(2)]
    b_gmt = [Buf("gmt%d" % i) for i in range(2)]
    t8 = [S.sb("t8_%d" % i, [128, 8], F32) for i in range(2)]
    pt = [S.sb("pt%d" % i, [128, 2, 288], BF16) for i in range(2)]
    b_pt = [Buf("pt%d" % i) for i in range(2)]
    pt2 = S.sb("pt2", [128, 512], BF16)
    b_pt2 = Buf("pt2")
    dtmp = S.sb("dtmp", [128, 512], F32)
    b_dtmp = Buf("dtmp")
    o1 = S.sb("o1", [128, 288], F32)
    o2 = S.sb("o2", [128, 288], F32)
    rd = S.sb("rd", [128, 288], F32)
    ob = [S.sb("ob%d" % i, [128, 288], BF16) for i in range(2)]
    b_o = Buf("o12")
    b_ob = [Buf("ob%d" % i) for i in range(2)]
    for i in range(2):
        S.op("sp", (lambda i=i: nc.sync.dma_start(out=KTa[i][64:128, :], in_=d["ind"][:, :])), writes=[b_KT[i]])
        S.op("pool", (lambda i=i: nc.gpsimd.memset(Va[i][:, :, 64:128], 0.0)), writes=[b_Va[i]])
        S.op("dve", (lambda i=i: nc.vector.memset(Va[i][:, :, 64:65], 1.0)), writes=[b_Va[i]])
        S.op("dve", (lambda i=i: nc.vector.memset(Vdd[i][:, :, 64:65], 1.0)), writes=[b_Vdd[i]])
        S.op("dve", (lambda i=i: nc.vector.memset(mbp[i][:], 0.0)), writes=[b_mbp[i]])
    ps = cx.psum
    bp = cx.b_psum
    SX = [cx.psall[:, 0:1024], cx.psall[:, 1024:2048]]
    b_SX = [[bp[0], bp[1]], [bp[2], bp[3]]]
    scnt = 0
    ocnt = 0
    NQ = 288
    for h in range(NH):
        cur = h % 2
        sl = SLOPES[h]
        es = math.exp(sl)
        S.op("sp", (lambda h=h, cur=cur: nc.sync.dma_start(out=KTa[cur][0:64, :], in_=d["KT"][h, :, 0:SEQ])), reads=[d["b_kv"]], writes=[b_KT[cur]])
        for q8 in range(8):
            S.op("sp", (lambda h=h, cur=cur, q8=q8: nc.sync.dma_start(out=Va[cur][:, q8 * 16:(q8 + 1) * 16, 0:64], in_=d["Vd"][h, :, q8 * 16:(q8 + 1) * 16, :])),
                 reads=[d["b_kv"]], writes=[b_Va[cur]])
        S.op("sp", (lambda h=h, cur=cur: nc.sync.dma_start(out=Qa[cur][0:64, :], in_=d["QT"][h])), reads=[d["b_kv"]], writes=[b_Qa[cur]] + b_Qm[cur])
        S.op("sp", (lambda h=h, cur=cur: nc.sync.dma_start(out=Kd[cur][0:64, :], in_=d["KT"][h, :, SEQ:SEQ + TOK])), reads=[d["b_kv"]], writes=[b_Kd[cur]])
        for q2 in range(2):
            S.op("sp", (lambda h=h, cur=cur, q2=q2: nc.sync.dma_start(out=Vdd[cur][:, q2 * 16:(q2 + 1) * 16, 0:64],
                                                                     in_=d["Vd"][h, :, 128 + q2 * 16:128 + (q2 + 1) * 16, :])),
                 reads=[d["b_kv"]], writes=[b_Vdd[cur]])
        for (VV, bV) in ((Va[cur], b_Va[cur]), (Vdd[cur], b_Vdd[cur])):
            Vodd = VV[:].rearrange("p (n two) e -> p n two e", two=2)[:, :, 1, :]
            S.op("dve", (lambda Vodd=Vodd, es=es: nc.vector.tensor_scalar(out=Vodd[:, :, 0:64], in0=Vodd[:, :, 0:64], scalar1=es, scalar2=None, op0=ALU.mult)),
                 reads=[bV], writes=[bV])
            S.op("dve", (lambda Vodd=Vodd, es=es: nc.vector.memset(Vodd[:, :, 64:65], es)), reads=[bV], writes=[bV])
        S.op("dve", (lambda cur=cur, sl=sl: nc.vector.tensor_scalar(out=bT[cur][:], in0=C("base"), scalar1=sl, scalar2=None, op0=ALU.mult)),
             reads=[b_cst], writes=[b_hd[cur]])
        S.op("dve", (lambda cur=cur, sl=sl: nc.vector.scalar_tensor_tensor(out=dgb[cur][:], in0=C("Dm"), scalar=-sl, in1=C("causal"),
                                                                         op0=ALU.mult, op1=ALU.add)), reads=[b_cst], writes=[b_hd[cur]])
        S.op("dve", (lambda cur=cur, sl=sl: nc.vector.scalar_tensor_tensor(out=dgbH[cur][:], in0=C("DmH"), scalar=-sl, in1=C("causalH"),
                                                                         op0=ALU.mult, op1=ALU.add)), reads=[b_cst], writes=[b_hd[cur]])
        S.op("act", (lambda cur=cur, sl=sl: nc.scalar.activation(out=cq[cur][:], in_=C("irowX"), func=AF.Exp, scale=-sl)),
             reads=[b_cst], writes=[b_hd[cur]])
        groups = []
        for l in range(NLB):
            groups.append((l * NQ, 128, l, l))
            groups.append((l * NQ + 128, 128, l, l))
            groups.append((l * NQ + 256, 32, 16 + l, l))

        def g_s1(gi, cur=cur, h=h, groups=groups):
            q0, nr, row, qmi = groups[gi]

            def mmg():
                nc.tensor.matmul(ps[6][0:nr, 0:64], lhsT=Qa[cur][0:64, q0:q0 + nr], rhs=kbh[:, h * 64:(h + 1) * 64], start=True, stop=False)
                return nc.tensor.matmul(ps[6][0:nr, 0:64], lhsT=Qa[cur][0:64, q0:q0 + nr], rhs=kbl[:, h * 64:(h + 1) * 64], start=False, stop=True)
            S.op("pe", mmg, reads=[b_Qa[cur], b_kb], writes=[bp[6]])

        def g_s2(gi, groups=groups):
            q0, nr, row, qmi = groups[gi]
            g = gi % 2
            pb = C("past")[0:nr, row * 64:(row + 1) * 64]
            S.op("dve", (lambda: nc.vector.tensor_tensor(out=gmt[g][0:nr, :], in0=ps[6][0:nr, 0:64], in1=pb, op=ALU.add)),
                 reads=[bp[6], b_cst], writes=[b_gmt[g]])
            S.op("dve", (lambda: nc.vector.max(out=t8[g][0:nr, :], in_=gmt[g][0:nr, :])), reads=[b_gmt[g]], writes=[b_gmt[g]])
            S.op("dve", (lambda: nc.vector.tensor_scalar(out=gmt[g][0:nr, :], in0=gmt[g][0:nr, :], scalar1=t8[g][0:nr, 2:3], scalar2=-NEG,
                                                         op0=ALU.is_ge, op1=ALU.mult)), reads=[b_gmt[g]], writes=[b_gmt[g]])
            S.op("dve", (lambda: nc.vector.scalar_tensor_tensor(out=mbp[g][0:nr, 64:128], in0=gmt[g][0:nr, :], scalar=NEG, in1=pb,
                                                                op0=ALU.add, op1=ALU.add)),
                 reads=[b_gmt[g], b_cst], writes=[b_mbp[g]])

        def g_s3(gi, cur=cur, groups=groups):
            q0, nr, row, qmi = groups[gi]
            g = gi % 2
            S.op("pe", (lambda: nc.tensor.transpose(ps[7][:, 0:nr], mbp[g][0:nr, :], C("ident")[0:nr, 0:nr])),
                 reads=[b_mbp[g], b_cst], writes=[bp[7]])
            S.op("act", (lambda: nc.scalar.copy(out=Qa[cur][64:128, q0:q0 + nr], in_=ps[7][64:128, 0:nr])),
                 reads=[bp[7]], writes=[b_Qm[cur][qmi]])
        g_s1(0)
        for gi in range(len(groups)):
            g_s2(gi)
            if gi + 1 < len(groups):
                g_s1(gi + 1)
            g_s3(gi)
        for i in range(8):
            nk = 8 * i + 8
            for k in range(2):
                l = 2 * i + k
                q0 = l * NQ
                po = 4 + k
                sbase = scnt
                scnt += nk

                def emit_S(n, cur=cur, q0=q0, l=l, sbase=sbase):
                    sb_ = (sbase + n) % 2

                    def mms():
                        for c in range(2):
                            m = nc.tensor.matmul(SX[sb_][:, c * 512:c * 512 + NQ], lhsT=KTa[cur][:, n * 256 + c * 128:n * 256 + (c + 1) * 128],
                                                 rhs=Qa[cur][:, q0:q0 + NQ], start=True, stop=True)
                        return m
                    S.op("pe", mms, reads=[b_KT[cur], b_Qa[cur], b_Qm[cur][l]], writes=b_SX[sb_])

                def emit_E(n, cur=cur, l=l, sbase=sbase):
                    sb_ = (sbase + n) % 2
                    S.op("act", (lambda: nc.scalar.activation(out=pt[sb_][:], in_=SX[sb_].rearrange("p (c x) -> p c x", c=2)[:, :, 0:NQ], func=AF.Exp,
                                                              bias=bT[cur][:, l * 64 + n:l * 64 + n + 1])),
                         reads=b_SX[sb_] + [b_hd[cur]], writes=[b_pt[sb_]])

                def emit_PV(n, cur=cur, po=po, nk=nk, sbase=sbase):
                    sb_ = (sbase + n) % 2

                    def mmpv():
                        for c in range(2):
                            m = nc.tensor.matmul(ps[po][:, 0:NQ], lhsT=Va[cur][:, 2 * n + c, :], rhs=pt[sb_][:, c, :],
                                                 start=(n == 0 and c == 0), stop=(n == nk - 1 and c == 1))
                        return m
                    S.op("pe", mmpv, reads=[b_Va[cur], b_pt[sb_]], writes=[bp[po]])
                emit_S(0)
                for n in range(nk):
                    emit_E(n)
                    if n + 1 < nk:
                        emit_S(n + 1)
                    emit_PV(n)
                for (Kt, koff, Vt, ch0, bK, bV, dg, qoff, nq, ocol) in (
                        (KTa[cur], (8 * i + k) * 256, Va[cur], 2 * (8 * i + k), b_KT[cur], b_Va[cur], dgb[cur], q0, 256, 0),
                        (Kd[cur], l * 256, Vdd[cur], 2 * l, b_Kd[cur], b_Vdd[cur], dgbH[cur], q0 + 256, 32, 256)):
                    def mmd(Kt=Kt, koff=koff, qoff=qoff, nq=nq, cur=cur):
                        for c in range(2):
                            m = nc.tensor.matmul(ps[6][:, c * nq:(c + 1) * nq], lhsT=Kt[0:64, koff + c * 128:koff + (c + 1) * 128],
                                                 rhs=Qa[cur][0:64, qoff:qoff + nq], start=True, stop=True)
                        return m
                    S.op("pe", mmd, reads=[bK, b_Qa[cur]], writes=[bp[6]])
                    S.op("dve", (lambda dg=dg, nq=nq: nc.vector.tensor_tensor(out=dtmp[:, 0:2 * nq], in0=ps[6][:, 0:2 * nq], in1=dg[:, 0:2 * nq], op=ALU.add)),
                         reads=[bp[6], b_hd[cur]], writes=[b_dtmp])
                    S.op("act", (lambda nq=nq: nc.scalar.activation(out=pt2[:, 0:2 * nq], in_=dtmp[:, 0:2 * nq], func=AF.Exp)), reads=[b_dtmp], writes=[b_pt2])

                    def mmpo(Vt=Vt, ch0=ch0, nq=nq, ocol=ocol):
                        for c in range(2):
                            m = nc.tensor.matmul(ps[7][0:Vt.shape[2], ocol:ocol + nq], lhsT=Vt[:, ch0 + c, :], rhs=pt2[:, c * nq:(c + 1) * nq],
                                                 start=(c == 0), stop=(c == 1))
                        return m
                    S.op("pe", mmpo, reads=[bV, b_pt2], writes=[bp[7]])
                S.op("dve", (lambda cur=cur, po=po: nc.vector.tensor_tensor(out=o1[0:65, :], in0=ps[po][0:65, 0:NQ], in1=cq[cur][0:65, :], op=ALU.mult)),
                     reads=[bp[po], b_hd[cur]], writes=[b_o])
                S.op("dve", (lambda: nc.vector.tensor_tensor(out=o2[0:65, :], in0=o1[0:65, :], in1=ps[7][0:65, 0:NQ], op=ALU.add)),
                     reads=[bp[7], b_o], writes=[b_o])
                S.op("pe", (lambda: nc.tensor.matmul(ps[6][0:64, 0:NQ], lhsT=C("sel65")[0:65, :], rhs=o2[0:65, :], start=True, stop=True)),
                     reads=[b_o, b_cst], writes=[bp[6]])
                S.op("dve", (lambda: nc.vector.reciprocal(out=rd[0:64, :], in_=ps[6][0:64, 0:NQ])), reads=[bp[6]], writes=[b_o])
                kk = ocnt % 2
                ocnt += 1
                S.op("dve", (lambda kk=kk: nc.vector.tensor_tensor(out=ob[kk][0:64, :], in0=o2[0:64, :], in1=rd[0:64, :], op=ALU.mult)),
                     reads=[b_o], writes=[b_ob[kk]])
                S.op("sp", (lambda kk=kk, h=h, l=l: nc.sync.dma_start(out=d["OT"][h, :, l * 256:(l + 1) * 256], in_=ob[kk][0:64, 0:256])),
                     reads=[b_ob[kk]], writes=[d["b_OT"][l // 2]])
                S.op("sp", (lambda kk=kk, h=h, l=l: nc.sync.dma_start(out=d["OT"][h, :, TOK + l * 32:TOK + (l + 1) * 32], in_=ob[kk][0:64, 256:288])),
                     reads=[b_ob[kk]], writes=[d["b_OT"][8]])


def build_fused():
    nc = bass.Bass("TRN2", target_bir_lowering=False)
    din = lambda n, s, dt: nc.dram_tensor(n, s, dt, kind="ExternalInput").ap()
    scr = lambda n, s, dt: nc.dram_tensor(n, s, dt).ap()
    sp_d = din("sp", [128, SP_N], F32)
    ada_w = din("ada_w", [2, 1024, 9216], F32)
    xall = din("xall", [1024, NT_ALL * 512], F32)
    w_in = [din("w_in%d" % i, [1024, 2 * DFF], F32) for i in range(4)]
    w_out = [din("w_out%d" % i, [DFF, 1024], F32) for i in range(4)]
    wqkv = din("wqkv", [1024, 3072], F32)
    w_o = din("w_o", [1024, 1024], F32)
    w_pw1 = din("w_pw1", [1024, 2048], F32)
    w_pw2 = din("w_pw2", [1024, 1024], F32)
    ind_d = din("ind", [64, SEQ], BF16)
    cst_d = din("cst", [128, CST2_N], F32)
    hm_d = din("hmask", [128, 512], F32)
    ident_d = din("ident", [128, 128], F32)
    xo = nc.dram_tensor("xo", [1024, TOK], F32, kind="ExternalOutput").ap()
    x1all = scr("x1all", [1024, NT_ALL * 512], F32)
    x1o = scr("x1o", [1024, TOKX], F32)
    xa = scr("xa", [1024, TOKX], F32)
    xb = scr("xb", [1024, TOKX], F32)
    x4 = scr("x4", [1024, TOKX], F32)
    x5 = scr("x5", [1024, TOK], F32)
    uT = scr("uT", [1024, TOKX], BF16)
    KT = scr("KTs", [NH, DH, NPOS * 256], BF16)
    Vd = scr("Vds", [NH, 128, 2 * NPOS, DH], BF16)
    kbar = scr("kbars", [NH * DH, NPOS], F32)
    QT = scr("QTs", [NH, DH, TOKX], BF16)
    OT = scr("OTs", [NH, DH, TOKX], BF16)
    S = Sched(nc)
    cx = Ctx()
    setup_common(S, cx)
    emit_load_small(S, cx, sp_d)
    hmask = S.sb("hmask", [128, 512], F32)
    b_hm = Buf("hmask")
    S.op("sp", lambda: nc.sync.dma_start(out=hmask[:], in_=hm_d[:, :]), writes=[b_hm])
    modT = [S.sb("modT%d" % i, [128, 72], F32) for i in range(2)]
    b_modT = [Buf("modT%d" % i) for i in range(2)]
    emit_adaln_scoped(S, cx, ada_w, modT, b_modT)
    M = {}
    for i in range(2):
        for j in range(3):
            M[(i, j)] = emit_mod_vectors_i(S, cx, modT[i], b_modT[i], spc(cx, "ng%d%d" % (i, j)), "l%ds%d" % (i, j), j)
    nbase = len(S._stack)
    mk = lambda name, n: [Buf("%s_%d" % (name, t)) for t in range(n)]
    b_xall, b_x1all, b_x1o = mk("xall", NT_ALL), mk("x1all", NT_ALL), mk("x1o", 9)
    b_xa, b_xb, b_x4, b_u, b_x5, b_xo = mk("xa", 9), mk("xb", 9), mk("x4", 16), mk("u", 9), mk("x5", 8), mk("xo", 8)
    b_x4t = b_x4[:9]
    alloc_tile_bufs(S, cx)
    ntile = len(S._stack)
    alloc_ffn_bufs(S, cx)
    m = M[(0, 0)]
    emit_ffn_phase(S, cx, xall, b_xall, x1all, b_x1all, w_in[0], w_out[0], m[0], m[1], m[2], 0.5, m[3], NT_ALL, "f00")
    S.barrier_free(len(S._stack) - ntile)
    m = M[(0, 1)]
    b_kv = Buf("kvq")
    emit_kvq_phase(S, cx, x1all, b_x1all, wqkv, m[0], m[1], m[3], QT, KT, kbar, Vd, x1o, b_x1o, b_kv)
    S.barrier_free(len(S._stack) - nbase)
    b_OT = mk("OT", 9)
    emit_attn2(S, cx, {"KT": KT, "ind": ind_d, "Vd": Vd, "QT": QT, "kbar": kbar, "cst": cst_d, "OT": OT, "b_OT": b_OT, "b_kv": b_kv})
    S.barrier_free(len(S._stack) - nbase)
    alloc_tile_bufs(S, cx)
    emit_proj_res_phase(S, cx, x1o, b_x1o, xa, b_xa, OT, b_OT, w_o, m[2], None, m[3], "wo", True, ntiles=9)
    S.barrier_free(len(S._stack) - ntile)
    alloc_ffn_bufs(S, cx)
    m = M[(0, 2)]
    emit_ffn_phase(S, cx, xa, b_xa, xb, b_xb, w_in[1], w_out[1], m[0], m[1], m[2], 0.5, m[3], 9, "f01")
    m = M[(1, 0)]
    emit_ffn_phase(S, cx, xb, b_xb, x4, b_x4t, w_in[2], w_out[2], m[0], m[1], m[2], 0.5, m[3], 9, "f10")
    S.barrier_free(len(S._stack) - ntile)
    m = M[(1, 1)]
    emit_glu_phase(S, cx, x4, b_x4t, w_pw1, m[0], m[1], m[3], uT, ntiles=9, hmask=hmask[:], b_hmask=b_hm, b_uout=b_u)
    S.barrier_free(len(S._stack) - ntile)
    b_x4c = [Buf("x4c_%d" % t) for t in range(16)]
    emit_conv_phase(S, cx, None, x4, b_x4c, x5, b_x5, w_pw2, ident_d, m[2], m[3], u_scr=uT, b_u=b_u)
    S.barrier_free(len(S._stack) - ntile)
    alloc_ffn_bufs(S, cx)
    m = M[(1, 2)]
    emit_ffn_phase(S, cx, x5, b_x5, xo, b_xo, w_in[3], w_out[3], m[0], m[1], m[2], 0.5, m[3], 8, "f11")
    S.emit()
    return nc


def kernel(**inp):
    import ml_dtypes
    bf = ml_dtypes.bfloat16
    inp = {k: np.asarray(v) for k, v in inp.items()}
    cores = list(range(8))
    ind = np.zeros((64, SEQ), bf)
    for m in range(64):
        ind[m, m * 256:(m + 1) * 256] = 1
    ident = np.eye(128, dtype=np.float32)
    maps = []
    toks = []
    for c in cores:
        b, j = c // 4, c % 4
        real, dup = pos_order(j)
        xb_ = inp["x"][b]
        xall = np.zeros((NPOS * 256, D), np.float32)
        for pi, g in enumerate(real + dup):
            if g is not None:
                xall[pi * 256:(pi + 1) * 256] = xb_[g * 256:(g + 1) * 256]
        cst, hmask = make_cst2(j)
        toks.append(np.concatenate([np.arange(g * 256, (g + 1) * 256) for g in snake_blocks(j)]))
        mp = {"sp": pack_small(inp, b), "ada_w": inp["ada_w"], "xall": np.ascontiguousarray(xall.T),
              "wqkv": inp["attn_w_qkv"][0], "w_o": inp["attn_w_o"][0], "w_pw1": inp["conv_w_pw1"][0], "w_pw2": inp["conv_w_pw2"][0],
              "ind": ind, "cst": cst, "hmask": hmask, "ident": ident}
        for k, (i, w) in enumerate(((0, 0), (0, 1), (1, 0), (1, 1))):
            mp["w_in%d" % k] = inp["ffn_w_in"][i, w]
            mp["w_out%d" % k] = inp["ffn_w_out"][i, w]
        maps.append(mp)
    nc = _get("fused", build_fused)
    res = run_bass_kernel_spmd(nc, maps, core_ids=cores).results
    out = np.zeros((BATCH, SEQ, D), np.float32)
    for c in cores:
        out[c // 4][toks[c]] = np.asarray(res[c]["xo"]).T
    return out
```

```python
import math
import numpy as np
import concourse.bass as bass
import concourse.mybir as mybir
from concourse.bass_utils import run_bass_kernel_spmd

F32 = mybir.dt.float32
BF16 = mybir.dt.bfloat16
AF = mybir.ActivationFunctionType
ALU = mybir.AluOpType
AX = mybir.AxisListType

D = 1024
KC = 8
DFF = 2816
FC = 22
NH = 16
DH = 64
BLK = 256
NB = 64
SEQ = 16384
BATCH = 2
TOK = 4096
NLB = 16
EPS = 1e-6
NEG = -30000.0
CW = 31


class Buf:
    __slots__ = ("name", "last_w", "readers")

    def __init__(self, name):
        self.name = name
        self.last_w = None
        self.readers = []


class Sched:
    DMA_ENGS = ("sp", "pool_dma")

    def __init__(self, nc, n_dma_sems=12):
        self.nc = nc
        self.ops = []
        self.engs = {"pe": nc.tensor, "act": nc.scalar, "dve": nc.vector,
                     "pool": nc.gpsimd, "sp": nc.sync, "pool_dma": nc.gpsimd}
        self.stream = {"pe": "pe", "act": "act", "dve": "dve", "pool": "pool",
                       "sp": "sp", "pool_dma": "pool"}
        self.n_dma_sems = n_dma_sems
        self._stack = []
        self.bar_list = []
        cm = nc.sbuf_tensor("bar_t", [128, 8], F32)
        self.bar_t = cm.__enter__()

    def sb(self, name, shape, dt):
        self._uid = getattr(self, "_uid", 0) + 1
        cm = self.nc.sbuf_tensor("s%d_%s" % (self._uid, name), shape, dt)
        t = cm.__enter__()
        self._stack.append(cm)
        return t

    def ps(self, name, shape, dt=F32):
        cm = self.nc.psum_tensor("p_" + name, shape, dt)
        t = cm.__enter__()
        self._stack.append(cm)
        return t

    def barrier_free(self, n_free):
        nc = self.nc
        bb = Buf("barrier%d" % len(self.ops))
        prev = list(range(len(self.ops)))
        i0 = self.op("pool", lambda: nc.gpsimd.memset(self.bar_t[:, 0:1], 0.0), writes=[bb])
        self.ops[i0]["deps"] = set(prev)
        for e, fn in (("dve", lambda: nc.vector.memset(self.bar_t[:, 1:2], 0.0)),
                      ("act", lambda: nc.scalar.activation(out=self.bar_t[:, 2:3], in_=self.bar_t[:, 0:1], func=AF.Copy)),
                      ("pe", None),
                      ("sp", lambda: nc.sync.dma_start(out=self.bar_t[:, 4:5], in_=self.bar_t[:, 0:1]))):
            if fn is None:
                continue
            self.op(e, fn, reads=[bb])
        self.bar_after = i0
        self.bar_list.append(i0)
        for _ in range(n_free):
            cm = self._stack.pop()
            cm.__exit__(None, None, None)

    def op(self, eng, fn, reads=(), writes=()):
        deps = set()
        idx = len(self.ops)
        for b in reads:
            if b.last_w is not None:
                deps.add(b.last_w)
        for b in writes:
            if b.last_w is not None:
                deps.add(b.last_w)
            for r in b.readers:
                deps.add(r)
        for b in reads:
            b.readers.append(idx)
        for b in writes:
            b.last_w = idx
            b.readers = []
        deps.discard(idx)
        if self.bar_list:
            deps.add(self.bar_list[-1])
        self.ops.append({"eng": eng, "fn": fn, "deps": deps, "dma": eng in self.DMA_ENGS})
        return idx

    def emit(self):
        nc = self.nc
        ops = self.ops
        n = len(ops)
        need = [False] * n
        for i, o in enumerate(ops):
            if o["dma"]:
                need[i] = True
            for d in o["deps"]:
                if ops[d]["dma"]:
                    continue
                sd, si = self.stream[ops[d]["eng"]], self.stream[o["eng"]]
                if sd != si or sd != "pe":
                    need[d] = True
        sem_cm = []

        def mksem(name):
            cm = nc.semaphore(name)
            s = cm.__enter__()
            sem_cm.append(cm)
            return s

        eng_sem = {k: mksem("s_" + k) for k in ("pe", "act", "dve", "pool")}
        eng_cnt = {k: 0 for k in eng_sem}
        dma_sems = {q: [mksem("d_%s_%d" % (q, i)) for i in range(self.n_dma_sems)] for q in self.DMA_ENGS}
        dma_cnt = {q: [0] * self.n_dma_sems for q in self.DMA_ENGS}
        dma_last = {q: [None] * self.n_dma_sems for q in self.DMA_ENGS}
        dma_rr = {q: 0 for q in self.DMA_ENGS}
        ev = [None] * n
        waited = {s: {} for s in ("pe", "act", "dve", "pool", "sp")}
        for i, o in enumerate(ops):
            eng = o["eng"]
            st = self.stream[eng]
            e = self.engs[eng]
            waits = {}
            for d in o["deps"]:
                if ev[d] is None:
                    continue
                sem, val, key = ev[d]
                if key not in waits or waits[key][1] < val:
                    waits[key] = (sem, val)
            my = None
            if o["dma"]:
                k = dma_rr[eng]
                dma_rr[eng] = (k + 1) % self.n_dma_sems
                prev = dma_last[eng][k]
                if prev is not None:
                    sem, val, key = ev[prev]
                    if key not in waits or waits[key][1] < val:
                        waits[key] = (sem, val)
                dma_cnt[eng][k] += 16
                my = (dma_sems[eng][k], dma_cnt[eng][k], (eng, k))
                dma_last[eng][k] = i
            elif need[i]:
                eng_cnt[st] += 1
                my = (eng_sem[st], eng_cnt[st], st)
            for key, (sem, val) in waits.items():
                if waited[st].get(key, 0) >= val:
                    continue
                e.wait_ge(sem, val)
                waited[st][key] = val
            inst = o["fn"]()
            if my is not None:
                inst.then_inc(my[0], 16 if o["dma"] else 1)
                ev[i] = my
            o["fn"] = None
        sp = nc.sync
        for q in self.DMA_ENGS:
            for k in range(self.n_dma_sems):
                if dma_cnt[q][k] > 0:
                    sp.wait_ge(dma_sems[q][k], dma_cnt[q][k])
        for k in eng_sem:
            if eng_cnt[k] > 0:
                sp.wait_ge(eng_sem[k], eng_cnt[k])
        self.max_counts = dict(eng_cnt)


def snake_blocks(j):
    out = []
    for i in range(8):
        out.append(8 * i + j)
        out.append(8 * i + 7 - j)
    return out


class Ctx:
    pass


def setup_common(S, cx):
    nc = S.nc
    cx.ones_bf = S.sb("ones_bf", [128, 128], BF16)
    cx.b_ones = Buf("ones")
    S.op("dve", lambda: nc.vector.memset(cx.ones_bf[:], 1.0), writes=[cx.b_ones])
    cx.psall = S.ps("psall", [128, 8 * 512], F32)
    cx.psum = [cx.psall[:, i * 512:(i + 1) * 512] for i in range(8)]
    cx.b_psum = [Buf("psb%d" % i) for i in range(8)]


def emit_mod_vectors(S, cx, modT, normg, layer_slot, j):
    nc = S.nc
    A = S.sb("modA_%s" % layer_slot, [128, 8], F32)
    b = Buf("modA_%s" % layer_slot)
    base = j * 24
    S.op("dve", lambda: nc.vector.scalar_tensor_tensor(
        out=A[:], in0=modT[:, base + 8:base + 16], scalar=1.0, in1=normg,
        op0=ALU.add, op1=ALU.mult), reads=[cx.b_mod], writes=[b])
    return A, modT[:, base:base + 8], modT[:, base + 16:base + 24], b


def emit_ffn_phase(S, cx, x_in, b_xin, x_out, b_xout, w_in_d, w_out_d, A, Bv, G, gscale, b_mod, ntiles, tag):
    nc = S.nc
    win = cx.win
    wout = cx.wout
    HF = FC // 2
    for kc in range(KC):
        S.op("pool_dma", (lambda kc=kc: nc.gpsimd.dma_start(out=win[:, kc, :], in_=w_in_d[kc * 128:(kc + 1) * 128, :])),
             writes=[cx.b_win[kc]])
    for f in range(FC):
        S.op("pool_dma", (lambda f=f: nc.gpsimd.dma_start(out=wout[:, f, :], in_=w_out_d[f * 128:(f + 1) * 128, :])),
             writes=[cx.b_wout[f]])
    Gs = S.sb("Gs_" + tag, [128, 8], F32)
    b_Gs = Buf("Gs_" + tag)
    S.op("dve", lambda: nc.vector.tensor_scalar(out=Gs[:], in0=G, scalar1=float(gscale), scalar2=None, op0=ALU.mult),
         reads=[b_mod], writes=[b_Gs])
    xv_in = x_in.rearrange("(c p) t -> p c t", p=128)
    xv_out = x_out.rearrange("(c p) t -> p c t", p=128)
    def _ld(t):
        S.op("sp", (lambda: nc.sync.dma_start(out=cx.xt[t % 2][:], in_=xv_in[:, :, t * 512:(t + 1) * 512])),
             reads=[b_xin[t]], writes=[cx.b_xt[t % 2]])
    _ld(0)
    for t in range(ntiles):
        xt = cx.xt[t % 2]
        bx = cx.b_xt[t % 2]
        emit_norm_mod(S, cx, xt, bx, A, Bv, b_mod)
        if t + 1 < ntiles:
            _ld(t + 1)
        for hf in range(2):
            for fl in range(HF):
                f = hf * HF + fl
                pa = 1 + (f % 2) * 2
                pb = pa + 1

                def mm_a(f=f, pa=pa):
                    for kc in range(KC):
                        m = nc.tensor.matmul(cx.psum[pa][:], lhsT=win[:, kc, f * 128:(f + 1) * 128], rhs=cx.h[:, kc, :],
                                             start=(kc == 0), stop=(kc == KC - 1))
                    return m

                def mm_b(f=f, pb=pb):
                    for kc in range(KC):
                        m = nc.tensor.matmul(cx.psum[pb][:], lhsT=win[:, kc, DFF + f * 128:DFF + (f + 1) * 128],
                                             rhs=cx.h[:, kc, :], start=(kc == 0), stop=(kc == KC - 1))
                    return m
                S.op("pe", mm_a, reads=cx.b_win + [cx.b_h], writes=[cx.b_psum[pa]])
                S.op("pe", mm_b, reads=cx.b_win + [cx.b_h], writes=[cx.b_psum[pb]])
                sa = cx.sa[f % 2]
                S.op("act", (lambda sa=sa, pa=pa: nc.scalar.activation(out=sa[:], in_=cx.psum[pa][:], func=AF.Silu)),
                     reads=[cx.b_psum[pa]], writes=[cx.b_sa[f % 2]])
                S.op("dve", (lambda sa=sa, pb=pb, fl=fl: nc.vector.tensor_tensor(out=cx.u[:, fl, :], in0=sa[:], in1=cx.psum[pb][:], op=ALU.mult)),
                     reads=[cx.b_sa[f % 2], cx.b_psum[pb]], writes=[cx.b_u[fl]])
            for oc in range(KC):
                py = 5 + (oc % 2)

                def mm_y(oc=oc, py=py, hf=hf):
                    for fl in range(HF):
                        m = nc.tensor.matmul(cx.psum[py][:], lhsT=wout[:, hf * HF + fl, oc * 128:(oc + 1) * 128], rhs=cx.u[:, fl, :],
                                             start=(fl == 0), stop=(fl == HF - 1))
                    return m
                S.op("pe", mm_y, reads=cx.b_wout + cx.b_u, writes=[cx.b_psum[py]])
                S.op("dve", (lambda oc=oc, py=py, xt=xt: nc.vector.scalar_tensor_tensor(
                    out=xt[:, oc, :], in0=cx.psum[py][:], scalar=Gs[:, oc:oc + 1], in1=xt[:, oc, :],
                    op0=ALU.mult, op1=ALU.add)), reads=[cx.b_psum[py], b_Gs], writes=[bx])
        S.op("sp", (lambda xt=xt, t=t: nc.sync.dma_start(out=xv_out[:, :, t * 512:(t + 1) * 512], in_=xt[:])),
             reads=[bx], writes=[b_xout[t]])


def emit_norm_mod(S, cx, xt, bx, A, Bv, b_mod, n=512):
    nc = S.nc
    for c in range(KC):
        S.op("act", (lambda c=c: nc.scalar.activation(out=cx.sq[c % 2][:, :n], in_=xt[:, c, :n], func=AF.Square)),
             reads=[bx], writes=[cx.b_sq[c % 2]])
        S.op("pe", (lambda c=c: nc.tensor.matmul(cx.psum[0][:, :n], lhsT=cx.ones_bf[:], rhs=cx.sq[c % 2][:, :n],
                                                 start=(c == 0), stop=(c == KC - 1))),
             reads=[cx.b_sq[c % 2], cx.b_ones], writes=[cx.b_psum[0]])
    S.op("dve", lambda: nc.vector.tensor_scalar(out=cx.rstd[:, :n], in0=cx.psum[0][:, :n], scalar1=1.0 / D, scalar2=EPS,
                                                op0=ALU.mult, op1=ALU.add), reads=[cx.b_psum[0]], writes=[cx.b_rstd])
    S.op("act", lambda: nc.scalar.activation(out=cx.rstd[:, :n], in_=cx.rstd[:, :n], func=AF.Sqrt), reads=[cx.b_rstd], writes=[cx.b_rstd])
    S.op("dve", lambda: nc.vector.reciprocal(out=cx.rstd[:, :n], in_=cx.rstd[:, :n]), reads=[cx.b_rstd], writes=[cx.b_rstd])
    for c in range(KC):
        tmp = cx.tmp[c % 2]
        S.op("dve", (lambda c=c, tmp=tmp: nc.vector.tensor_tensor(out=tmp[:, :n], in0=xt[:, c, :n], in1=cx.rstd[:, :n], op=ALU.mult)),
             reads=[bx, cx.b_rstd], writes=[cx.b_tmp[c % 2]])
        S.op("act", (lambda c=c, tmp=tmp: nc.scalar.activation(out=cx.h[:, c, :n], in_=tmp[:, :n], func=AF.Identity,
                                                               bias=Bv[:, c:c + 1], scale=A[:, c:c + 1])),
             reads=[cx.b_tmp[c % 2], b_mod], writes=[cx.b_h])


def alloc_tile_bufs(S, cx):
    cx.xt = [S.sb("xt%d" % i, [128, KC, 512], F32) for i in range(2)]
    cx.b_xt = [Buf("xt%d" % i) for i in range(2)]
    cx.sq = [S.sb("sq%d" % i, [128, 512], BF16) for i in range(2)]
    cx.b_sq = [Buf("sq%d" % i) for i in range(2)]
    cx.h = S.sb("h", [128, KC, 512], BF16)
    cx.b_h = Buf("h")
    cx.rstd = S.sb("rstd", [128, 512], F32)
    cx.b_rstd = Buf("rstd")
    cx.tmp = [S.sb("tmp%d" % i, [128, 512], F32) for i in range(2)]
    cx.b_tmp = [Buf("tmp%d" % i) for i in range(2)]
    cx.sa = [S.sb("sa%d" % i, [128, 512], F32) for i in range(2)]
    cx.b_sa = [Buf("sa%d" % i) for i in range(2)]


def alloc_ffn_bufs(S, cx):
    cx.win = S.sb("win", [128, KC, 2 * DFF], BF16)
    cx.wout = S.sb("wout", [128, FC, D], BF16)
    cx.b_win = [Buf("win%d" % i) for i in range(KC)]
    cx.b_wout = [Buf("wout%d" % i) for i in range(FC)]
    cx.u = S.sb("u", [128, FC // 2, 512], BF16)
    cx.b_u = [Buf("u%d" % i) for i in range(FC // 2)]


def _fm(v, n=None):
    v = np.asarray(v, np.float32).reshape(-1, 128)
    return np.ascontiguousarray(v.T)


SP_OFF = {}


def _sp_layout():
    off = 0
    for name, w in (("c", 8), ("ada_b0", 72), ("ada_b1", 72),
                    ("ng00", 8), ("ng01", 8), ("ng02", 8), ("ng10", 8), ("ng11", 8), ("ng12", 8),
                    ("gq", 1), ("gk", 1), ("b_pw1", 16), ("b_dw", 8), ("ln_g", 8), ("ln_b", 8),
                    ("b_pw2", 8), ("w_dw", 8 * CW)):
        SP_OFF[name] = (off, w)
        off += w
    return off


SP_N = _sp_layout()


def pack_small(inp, b):
    sp = np.zeros((128, SP_N), np.float32)

    def put(name, arr):
        o, w = SP_OFF[name]
        assert arr.shape == (128, w), (name, arr.shape, w)
        sp[:, o:o + w] = arr
    put("c", _fm(inp["c"][b]))
    put("ada_b0", _fm(inp["ada_b"][0]))
    put("ada_b1", _fm(inp["ada_b"][1]))
    for i in range(2):
        for j in range(3):
            put("ng%d%d" % (i, j), _fm(inp["norm_g"][i, j]))
    put("gq", np.tile(np.asarray(inp["attn_g_q"][0], np.float32), 2).reshape(128, 1))
    put("gk", np.tile(np.asarray(inp["attn_g_k"][0], np.float32), 2).reshape(128, 1))
    put("b_pw1", _fm(inp["conv_b_pw1"][0]))
    put("b_dw", _fm(inp["conv_b_dw"][0]))
    put("ln_g", _fm(inp["conv_ln_g"][0]))
    put("ln_b", _fm(inp["conv_ln_b"][0]))
    put("b_pw2", _fm(inp["conv_b_pw2"][0]))
    wd = np.asarray(inp["conv_w_dw"][0], np.float32)
    put("w_dw", np.ascontiguousarray(wd.reshape(CW, 8, 128).transpose(2, 1, 0)).reshape(128, 8 * CW))
    return sp


def spc(cx, name):
    o, w = SP_OFF[name]
    return cx.spt[:, o:o + w]


def emit_load_small(S, cx, sp_d):
    nc = S.nc
    cx.spt = S.sb("spt", [128, SP_N], F32)
    cx.b_sp = Buf("spt")
    S.op("sp", lambda: nc.sync.dma_start(out=cx.spt[:], in_=sp_d[:, :]), writes=[cx.b_sp])


def emit_adaln(S, cx, ada_w_d, modT, b_modT):
    nc = S.nc
    NCOL = 1152
    stg = [S.sb("adastg%d" % i, [128, KC, NCOL], F32) for i in range(2)]
    b_stg = [Buf("adastg%d" % i) for i in range(2)]
    cact = S.sb("cact", [128, 8], F32)
    b_cact = Buf("cact")
    S.op("act", lambda: nc.scalar.activation(out=cact[:], in_=spc(cx, "c"), func=AF.Silu), reads=[cx.b_sp], writes=[b_cact])
    k = 0
    for i in range(2):
        wv = ada_w_d[i].rearrange("(kc p) n -> p kc n", p=128)
        for pc in range(9216 // NCOL):
            st = stg[k % 2]
            bs = b_stg[k % 2]
            k += 1
            S.op("sp", (lambda st=st, wv=wv, pc=pc: nc.sync.dma_start(out=st[:], in_=wv[:, :, pc * NCOL:(pc + 1) * NCOL])), writes=[bs])

            def mm(st=st, pc=pc, i=i):
                for m in range(NCOL // 128):
                    col = pc * (NCOL // 128) + m
                    for kc in range(KC):
                        r = nc.tensor.matmul(cx.psum[7][:, col:col + 1], lhsT=st[:, kc, m * 128:(m + 1) * 128], rhs=cact[:, kc:kc + 1],
                                             start=(kc == 0), stop=(kc == KC - 1))
                return r
            S.op("pe", mm, reads=[bs, b_cact], writes=[cx.b_psum[7]])
        S.op("dve", (lambda i=i: nc.vector.tensor_tensor(out=modT[i][:], in0=cx.psum[7][:, 0:72], in1=spc(cx, "ada_b%d" % i), op=ALU.add)),
             reads=[cx.b_psum[7], cx.b_sp], writes=[b_modT[i]])
    return stg


def joint_mod(S, cx, i, j, modT, b_modT):
    nc = S.nc
    A, Bv, G, bA = emit_mod_vectors_i(S, cx, modT[i], b_modT[i], spc(cx, "ng%d%d" % (i, j)), "l%ds%d" % (i, j), j)
    return A, Bv, G, bA


def emit_mod_vectors_i(S, cx, modT, b_modT, normg, tag, j):
    nc = S.nc
    AB = S.sb("modAB_" + tag, [128, 24], F32)
    b = Buf("modAB_" + tag)
    base = j * 24
    S.op("dve", lambda: nc.vector.tensor_copy(out=AB[:], in_=modT[:, base:base + 24]), reads=[b_modT, cx.b_sp], writes=[b])
    S.op("dve", lambda: nc.vector.scalar_tensor_tensor(
        out=AB[:, 8:16], in0=modT[:, base + 8:base + 16], scalar=1.0, in1=normg,
        op0=ALU.add, op1=ALU.mult), reads=[b_modT, cx.b_sp], writes=[b])
    return AB[:, 8:16], AB[:, 0:8], AB[:, 16:24], b


def load_x_tile(S, cx, xv, b_x, t, n=512):
    nc = S.nc
    xt = cx.xt[t % 2]
    bx = cx.b_xt[t % 2]
    S.op("sp", (lambda: nc.sync.dma_start(out=xt[:, :, :n], in_=xv[:, :, t * n:(t + 1) * n])), reads=[b_x[t]], writes=[bx])
    return xt, bx


def emit_qkv_phase(S, cx, x_d, b_x, wqkv_d, A, Bv, b_mod, QT_d, KT_d, kbar_d, V_d):
    nc = S.nc
    wq = S.sb("wqkv_sb", [128, KC, 3072], BF16)
    b_wq = [Buf("wqkv%d" % k) for k in range(KC)]
    for kc in range(KC):
        S.op("pool_dma", (lambda kc=kc: nc.gpsimd.dma_start(out=wq[:, kc, :], in_=wqkv_d[kc * 128:(kc + 1) * 128, :])), writes=[b_wq[kc]])
    bo = S.sb("blkones", [128, 128], BF16)
    b_bo = Buf("blkones")
    S.op("dve", lambda: nc.vector.memset(bo[:], 0.0), writes=[b_bo])
    S.op("dve", lambda: nc.vector.memset(bo[0:64, 0:64], 1.0), writes=[b_bo])
    S.op("dve", lambda: nc.vector.memset(bo[64:128, 64:128], 1.0), writes=[b_bo])
    g8 = S.sb("g8", [128, 2], F32)
    b_g8 = Buf("g8")
    S.op("dve", lambda: nc.vector.tensor_scalar(out=g8[:, 0:1], in0=spc(cx, "gq"), scalar1=0.125, scalar2=None, op0=ALU.mult),
         reads=[cx.b_sp], writes=[b_g8])
    S.op("dve", lambda: nc.vector.tensor_copy(out=g8[:, 1:2], in_=spc(cx, "gk")), reads=[cx.b_sp], writes=[b_g8])
    qo = [S.sb("qo%d" % i, [128, 512], BF16) for i in range(2)]
    b_qo = [Buf("qo%d" % i) for i in range(2)]
    kb = [S.sb("kb%d" % i, [128, 2], F32) for i in range(2)]
    b_kb = [Buf("kb%d" % i) for i in range(2)]
    vt = [S.sb("vt%d" % i, [128, 1024], BF16) for i in range(2)]
    b_vt = [Buf("vt%d" % i) for i in range(2)]
    xv = x_d.rearrange("(c p) t -> p c t", p=128)
    QTv = QT_d.rearrange("h d t -> (h d) t")
    KTv = KT_d.rearrange("h d t -> (h d) t")
    kbv = kbar_d.rearrange("h d l -> (h d) l")
    cnt = 0
    for t in range(8):
        xt, bx = load_x_tile(S, cx, xv, b_x, t)
        emit_norm_mod(S, cx, xt, bx, A, Bv, b_mod)
        for which in range(2):
            goff = which * 1024
            outv = QTv if which == 0 else KTv
            for hp in range(8):
                k = cnt % 2
                cnt += 1
                pq = cx.psum[1 + k]
                pss = cx.psum[3 + k]

                def mm(hp=hp, pq=pq, goff=goff):
                    for kc in range(KC):
                        m = nc.tensor.matmul(pq[:], lhsT=wq[:, kc, goff + hp * 128:goff + (hp + 1) * 128], rhs=cx.h[:, kc, :],
                                             start=(kc == 0), stop=(kc == KC - 1))
                    return m
                S.op("pe", mm, reads=b_wq + [cx.b_h], writes=[cx.b_psum[1 + k]])
                S.op("act", (lambda k=k, pq=pq: nc.scalar.activation(out=cx.sq[k][:], in_=pq[:], func=AF.Square)),
                     reads=[cx.b_psum[1 + k]], writes=[cx.b_sq[k]])
                S.op("pe", (lambda k=k, pss=pss: nc.tensor.matmul(pss[:], lhsT=bo[:], rhs=cx.sq[k][:], start=True, stop=True)),
                     reads=[cx.b_sq[k], b_bo], writes=[cx.b_psum[3 + k]])
                r1 = cx.tmp[k]
                S.op("dve", (lambda r1=r1, pss=pss: nc.vector.tensor_scalar(out=r1[:], in0=pss[:], scalar1=1.0 / DH, scalar2=EPS,
                                                                            op0=ALU.mult, op1=ALU.add)),
                     reads=[cx.b_psum[3 + k]], writes=[cx.b_tmp[k]])
                S.op("act", (lambda r1=r1: nc.scalar.activation(out=r1[:], in_=r1[:], func=AF.Sqrt)), reads=[cx.b_tmp[k]], writes=[cx.b_tmp[k]])
                S.op("dve", (lambda r1=r1: nc.vector.reciprocal(out=r1[:], in_=r1[:])), reads=[cx.b_tmp[k]], writes=[cx.b_tmp[k]])
                S.op("dve", (lambda r1=r1, pq=pq: nc.vector.tensor_tensor(out=r1[:], in0=pq[:], in1=r1[:], op=ALU.mult)),
                     reads=[cx.b_tmp[k], cx.b_psum[1 + k]], writes=[cx.b_tmp[k]])
                sa = cx.sa[k]
                S.op("act", (lambda r1=r1, sa=sa, which=which: nc.scalar.activation(out=sa[:], in_=r1[:], func=AF.Copy,
                                                                                  scale=g8[:, which:which + 1])),
                     reads=[cx.b_tmp[k], b_g8], writes=[cx.b_sa[k]])
                S.op("pool", (lambda k=k, sa=sa: nc.gpsimd.tensor_copy(out=qo[k][:], in_=sa[:])), reads=[cx.b_sa[k]], writes=[b_qo[k]])
                S.op("sp", (lambda k=k, hp=hp, t=t, outv=outv: nc.sync.dma_start(out=outv[hp * 128:(hp + 1) * 128, t * 512:(t + 1) * 512], in_=qo[k][:])),
                     reads=[b_qo[k]])
                if which == 1:
                    S.op("dve", (lambda k=k, sa=sa: nc.vector.tensor_reduce(out=kb[k][:], in_=sa[:].rearrange("p (b t) -> p b t", b=2),
                                                                          axis=AX.X, op=ALU.add)),
                         reads=[cx.b_sa[k]], writes=[b_kb[k]])
                    S.op("dve", (lambda k=k: nc.vector.tensor_scalar(out=kb[k][:], in0=kb[k][:], scalar1=1.0 / BLK, scalar2=None, op0=ALU.mult)),
                         reads=[b_kb[k]], writes=[b_kb[k]])
                    S.op("sp", (lambda k=k, hp=hp, t=t: nc.sync.dma_start(out=kbv[hp * 128:(hp + 1) * 128, 2 * t:2 * t + 2], in_=kb[k][:])),
                         reads=[b_kb[k]])
        for tg in range(4):
            v = vt[tg % 2]
            for ns in range(2):
                pv = cx.psum[5 + ns]

                def mmv(tg=tg, ns=ns, pv=pv):
                    for kc in range(KC):
                        m = nc.tensor.matmul(pv[:], lhsT=cx.h[:, kc, tg * 128:(tg + 1) * 128], rhs=wq[:, kc, 2048 + ns * 512:2048 + (ns + 1) * 512],
                                             start=(kc == 0), stop=(kc == KC - 1))
                    return m
                S.op("pe", mmv, reads=b_wq + [cx.b_h], writes=[cx.b_psum[5 + ns]])
                if ns == 0:
                    S.op("act", (lambda v=v, pv=pv: nc.scalar.copy(out=v[:, 0:512], in_=pv[:])), reads=[cx.b_psum[5]], writes=[b_vt[tg % 2]])
                else:
                    S.op("dve", (lambda v=v, pv=pv: nc.vector.tensor_copy(out=v[:, 512:1024], in_=pv[:])), reads=[cx.b_psum[6]], writes=[b_vt[tg % 2]])
            S.op("sp", (lambda v=v, tg=tg, t=t: nc.sync.dma_start(out=V_d[t * 512 + tg * 128:t * 512 + (tg + 1) * 128, :], in_=v[:])),
                 reads=[b_vt[tg % 2]])


def build_L1():
    nc = bass.Bass("TRN2", target_bir_lowering=False)
    sp_d = nc.dram_tensor("sp", [128, SP_N], F32, kind="ExternalInput").ap()
    ada_w = nc.dram_tensor("ada_w", [2, 1024, 9216], F32, kind="ExternalInput").ap()
    xin = nc.dram_tensor("xin", [1024, TOK], F32, kind="ExternalInput").ap()
    w_in = nc.dram_tensor("w_in", [1024, 2 * DFF], F32, kind="ExternalInput").ap()
    w_out = nc.dram_tensor("w_out", [DFF, 1024], F32, kind="ExternalInput").ap()
    wqkv = nc.dram_tensor("wqkv", [1024, 3072], F32, kind="ExternalInput").ap()
    x1 = nc.dram_tensor("x1", [1024, TOK], F32, kind="ExternalOutput").ap()
    modo = nc.dram_tensor("modo", [128, 144], F32, kind="ExternalOutput").ap()
    QT = nc.dram_tensor("QT", [NH, DH, TOK], BF16, kind="ExternalOutput").ap()
    KT = nc.dram_tensor("KT", [NH, DH, TOK], BF16, kind="ExternalOutput").ap()
    kbar = nc.dram_tensor("kbar", [NH, DH, NLB], F32, kind="ExternalOutput").ap()
    V = nc.dram_tensor("V", [TOK, 1024], BF16, kind="ExternalOutput").ap()
    S = Sched(nc)
    cx = Ctx()
    setup_common(S, cx)
    emit_load_small(S, cx, sp_d)
    modT = [S.sb("modT%d" % i, [128, 72], F32) for i in range(2)]
    b_modT = [Buf("modT%d" % i) for i in range(2)]
    A0, B0, G0, bm0 = None, None, None, None
    AB0 = joint_mod
    emit_adaln_scoped(S, cx, ada_w, modT, b_modT)
    for i in range(2):
        S.op("sp", (lambda i=i: nc.sync.dma_start(out=modo[:, i * 72:(i + 1) * 72], in_=modT[i][:])), reads=[b_modT[i]])
    A, Bv, G, bA = emit_mod_vectors_i(S, cx, modT[0], b_modT[0], spc(cx, "ng00"), "l0s0", 0)
    A1, Bv1, G1, bA1 = emit_mod_vectors_i(S, cx, modT[0], b_modT[0], spc(cx, "ng01"), "l0s1", 1)
    alloc_tile_bufs(S, cx)
    alloc_ffn_bufs(S, cx)
    b_xin = [Buf("xin%d" % t) for t in range(8)]
    b_x1 = [Buf("x1_%d" % t) for t in range(8)]
    emit_ffn_phase(S, cx, xin, b_xin, x1, b_x1, w_in, w_out, A, Bv, G, 0.5, bA, 8, "f00")
    S.barrier_free(3)
    emit_qkv_phase(S, cx, x1, b_x1, wqkv, A1, Bv1, bA1, QT, KT, kbar, V)
    S.emit()
    return nc


def emit_adaln_scoped(S, cx, ada_w, modT, b_modT):
    n0 = len(S._stack)
    emit_adaln(S, cx, ada_w, modT, b_modT)
    S.barrier_free(len(S._stack) - n0)


SLOPES = [2.0 ** (-8.0 * (h + 1) / NH) for h in range(NH)]


def emit_attn_phase(S, cx, d):
    nc = S.nc
    KTa = [S.sb("KTa%d" % i, [128, SEQ], BF16) for i in range(2)]
    Va = [S.sb("Va%d" % i, [128, 128, 65], BF16) for i in range(2)]
    Qa = [S.sb("Qa%d" % i, [128, TOK], BF16) for i in range(2)]
    Ko = [S.sb("Ko%d" % i, [128, TOK], BF16) for i in range(2)]
    Vo = [S.sb("Vo%d" % i, [128, 32, 65], BF16) for i in range(2)]
    bT = [S.sb("bT%d" % i, [128, 1024], F32) for i in range(2)]
    dgb = [S.sb("dgb%d" % i, [128, 512], F32) for i in range(2)]
    cq = [S.sb("cq%d" % i, [128, 256], F32) for i in range(2)]
    b_KT = [Buf("KTa%d" % i) for i in range(2)]
    b_Va = [Buf("Va%d" % i) for i in range(2)]
    b_Qa = [Buf("Qa%d" % i) for i in range(2)]
    b_Qm = [[Buf("Qm%d_%d" % (i, l)) for l in range(NLB)] for i in range(2)]
    b_Ko = [Buf("Ko%d" % i) for i in range(2)]
    b_Vo = [Buf("Vo%d" % i) for i in range(2)]
    b_hd = [Buf("hd%d" % i) for i in range(2)]
    cst = S.sb("cst", [128, CST_N], F32)
    b_cst = Buf("cst")
    S.op("sp", lambda: nc.sync.dma_start(out=cst[:], in_=d["cst"][:, :]), writes=[b_cst])

    def C(name):
        o, w = CST_OFF[name]
        return cst[:, o:o + w]
    kb32 = S.sb("kb32", [64, NH * NB], F32)
    kbh = S.sb("kbh", [64, NH * NB], BF16)
    kbl = S.sb("kbl", [64, NH * NB], BF16)
    kbt = S.sb("kbt", [64, NH * NB], F32)
    b_kb = Buf("kbhl")
    S.op("sp", lambda: nc.sync.dma_start(out=kb32[:], in_=d["kbar"].rearrange("d h m -> d (h m)")), writes=[b_kb])
    S.op("dve", lambda: nc.vector.tensor_copy(out=kbh[:], in_=kb32[:]), reads=[b_kb], writes=[b_kb])
    S.op("dve", lambda: nc.vector.tensor_copy(out=kbt[:], in_=kbh[:]), reads=[b_kb], writes=[b_kb])
    S.op("dve", lambda: nc.vector.tensor_tensor(out=kbt[:], in0=kb32[:], in1=kbt[:], op=ALU.subtract), reads=[b_kb], writes=[b_kb])
    S.op("dve", lambda: nc.vector.tensor_copy(out=kbl[:], in_=kbt[:]), reads=[b_kb], writes=[b_kb])
    mbp = [S.sb("mbp%d" % i, [128, 128], F32) for i in range(2)]
    b_mbp = [Buf("mbp%d" % i) for i in range(2)]
    gmt = [S.sb("gmt%d" % i, [128, 64], F32) for i in range(2)]
    b_gmt = [Buf("gmt%d" % i) for i in range(2)]
    t8 = [S.sb("t8_%d" % i, [128, 8], F32) for i in range(2)]
    pt = [S.sb("pt%d" % i, [128, 512], BF16) for i in range(2)]
    b_pt = [Buf("pt%d" % i) for i in range(2)]
    pt2 = S.sb("pt2", [128, 512], BF16)
    b_pt2 = Buf("pt2")
    dtmp = S.sb("dtmp", [128, 512], F32)
    b_dtmp = Buf("dtmp")
    o1 = S.sb("o1", [128, 256], F32)
    o2 = S.sb("o2", [128, 256], F32)
    rd = S.sb("rd", [128, 256], F32)
    ob = [S.sb("ob%d" % i, [128, 256], BF16) for i in range(2)]
    b_o = Buf("o12")
    b_ob = [Buf("ob%d" % i) for i in range(2)]
    for i in range(2):
        S.op("sp", (lambda i=i: nc.sync.dma_start(out=KTa[i][64:128, :], in_=d["ind"][:, :])), writes=[b_KT[i]])
        S.op("dve", (lambda i=i: nc.vector.memset(Va[i][:, :, 64:65], 1.0)), writes=[b_Va[i]])
        S.op("dve", (lambda i=i: nc.vector.memset(Vo[i][:, :, 64:65], 1.0)), writes=[b_Vo[i]])
        S.op("dve", (lambda i=i: nc.vector.memset(mbp[i][:], 0.0)), writes=[b_mbp[i]])
    ps = cx.psum
    bp = cx.b_psum
    scnt = 0
    gcnt = 0
    for h in range(NH):
        cur = h % 2
        sl = SLOPES[h]
        es = math.exp(sl)
        S.op("sp", (lambda h=h, cur=cur: nc.sync.dma_start(out=KTa[cur][0:64, :], in_=d["KT"][h])), writes=[b_KT[cur]])
        for q8 in range(8):
            S.op("sp", (lambda h=h, cur=cur, q8=q8: nc.sync.dma_start(out=Va[cur][:, q8 * 16:(q8 + 1) * 16, 0:64], in_=d["Vd"][h, :, q8 * 16:(q8 + 1) * 16, :])),
                 writes=[b_Va[cur]])
        S.op("sp", (lambda h=h, cur=cur: nc.sync.dma_start(out=Qa[cur][0:64, :], in_=d["QT"][h])), writes=[b_Qa[cur]] + b_Qm[cur])
        S.op("sp", (lambda h=h, cur=cur: nc.sync.dma_start(out=Ko[cur][0:64, :], in_=d["KTo"][h])), writes=[b_Ko[cur]])
        for q2 in range(2):
            S.op("sp", (lambda h=h, cur=cur, q2=q2: nc.sync.dma_start(out=Vo[cur][:, q2 * 16:(q2 + 1) * 16, 0:64], in_=d["Vo"][h, :, q2 * 16:(q2 + 1) * 16, :])),
                 writes=[b_Vo[cur]])
        for (VV, bV) in ((Va[cur], b_Va[cur]), (Vo[cur], b_Vo[cur])):
            Vodd = VV[:].rearrange("p (n two) e -> p n two e", two=2)[:, :, 1, :]
            S.op("dve", (lambda Vodd=Vodd, es=es: nc.vector.tensor_scalar(out=Vodd[:, :, 0:64], in0=Vodd[:, :, 0:64], scalar1=es, scalar2=None, op0=ALU.mult)),
                 reads=[bV], writes=[bV])
            S.op("dve", (lambda Vodd=Vodd, es=es: nc.vector.memset(Vodd[:, :, 64:65], es)), reads=[bV], writes=[bV])
        S.op("dve", (lambda cur=cur, sl=sl: nc.vector.tensor_scalar(out=bT[cur][:], in0=C("base"), scalar1=sl, scalar2=None, op0=ALU.mult)),
             reads=[b_cst], writes=[b_hd[cur]])
        S.op("dve", (lambda cur=cur, sl=sl: nc.vector.scalar_tensor_tensor(out=dgb[cur][:], in0=C("Dm"), scalar=-sl, in1=C("causal"),
                                                                         op0=ALU.mult, op1=ALU.add)), reads=[b_cst], writes=[b_hd[cur]])
        S.op("act", (lambda cur=cur, sl=sl: nc.scalar.activation(out=cq[cur][:], in_=C("irow"), func=AF.Exp, scale=-sl)),
             reads=[b_cst], writes=[b_hd[cur]])
        for l in range(NLB):
            for hq in range(2):
                g = gcnt % 2
                gcnt += 1
                q0 = l * 256 + hq * 128

                def mmg(cur=cur, q0=q0, h=h):
                    nc.tensor.matmul(ps[6][:, 0:64], lhsT=Qa[cur][0:64, q0:q0 + 128], rhs=kbh[:, h * 64:(h + 1) * 64], start=True, stop=False)
                    return nc.tensor.matmul(ps[6][:, 0:64], lhsT=Qa[cur][0:64, q0:q0 + 128], rhs=kbl[:, h * 64:(h + 1) * 64], start=False, stop=True)
                S.op("pe", mmg, reads=[b_Qa[cur], b_kb], writes=[bp[6]])
                pb = C("past")[:, l * 64:(l + 1) * 64]
                S.op("dve", (lambda g=g, pb=pb: nc.vector.tensor_tensor(out=gmt[g][:], in0=ps[6][:, 0:64], in1=pb, op=ALU.add)),
                     reads=[bp[6], b_cst], writes=[b_gmt[g]])
                S.op("dve", (lambda g=g: nc.vector.max(out=t8[g][:], in_=gmt[g][:])), reads=[b_gmt[g]], writes=[b_gmt[g]])
                S.op("dve", (lambda g=g: nc.vector.tensor_scalar(out=gmt[g][:], in0=gmt[g][:], scalar1=t8[g][:, 2:3], scalar2=-NEG,
                                                                 op0=ALU.is_ge, op1=ALU.mult)), reads=[b_gmt[g]], writes=[b_gmt[g]])
                S.op("dve", (lambda g=g, pb=pb: nc.vector.scalar_tensor_tensor(out=mbp[g][:, 64:128], in0=gmt[g][:], scalar=NEG, in1=pb,
                                                                             op0=ALU.add, op1=ALU.add)),
                     reads=[b_gmt[g], b_cst], writes=[b_mbp[g]])
                S.op("pe", (lambda g=g: nc.tensor.transpose(ps[7][:, 0:128], mbp[g][:], C("ident"))), reads=[b_mbp[g], b_cst], writes=[bp[7]])
                S.op("act", (lambda cur=cur, q0=q0: nc.scalar.copy(out=Qa[cur][64:128, q0:q0 + 128], in_=ps[7][64:128, 0:128])),
                     reads=[bp[7]], writes=[b_Qm[cur][l]])
        for i in range(8):
            nk = 8 * i + 7
            for X in (2 * i, 2 * i + 1):
                po = 2 + (X % 2)
                for n in range(nk):
                    sb_ = scnt % 2
                    scnt += 1

                    def mms(cur=cur, X=X, n=n, sb_=sb_):
                        for c in range(2):
                            m = nc.tensor.matmul(ps[sb_][:, c * 256:(c + 1) * 256], lhsT=KTa[cur][:, n * 256 + c * 128:n * 256 + (c + 1) * 128],
                                                 rhs=Qa[cur][:, X * 256:(X + 1) * 256], start=True, stop=True)
                        return m
                    S.op("pe", mms, reads=[b_KT[cur], b_Qa[cur], b_Qm[cur][X]], writes=[bp[sb_]])
                    S.op("act", (lambda cur=cur, X=X, n=n, sb_=sb_: nc.scalar.activation(out=pt[sb_][:], in_=ps[sb_][:], func=AF.Exp,
                                                                                        bias=bT[cur][:, X * 64 + n:X * 64 + n + 1])),
                         reads=[bp[sb_], b_hd[cur]], writes=[b_pt[sb_]])

                    def mmpv(cur=cur, n=n, sb_=sb_, po=po, nk=nk):
                        for c in range(2):
                            m = nc.tensor.matmul(ps[po][0:65, 0:256], lhsT=Va[cur][:, 2 * n + c, :], rhs=pt[sb_][:, c * 256:(c + 1) * 256],
                                                 start=(n == 0 and c == 0), stop=(n == nk - 1 and c == 1))
                        return m
                    S.op("pe", mmpv, reads=[b_Va[cur], b_pt[sb_]], writes=[bp[po]])

                def mmd(cur=cur, X=X):
                    for c in range(2):
                        m = nc.tensor.matmul(ps[4][:, c * 256:(c + 1) * 256], lhsT=Ko[cur][0:64, X * 256 + c * 128:X * 256 + (c + 1) * 128],
                                             rhs=Qa[cur][0:64, X * 256:(X + 1) * 256], start=True, stop=True)
                    return m
                S.op("pe", mmd, reads=[b_Ko[cur], b_Qa[cur]], writes=[bp[4]])
                S.op("dve", (lambda cur=cur: nc.vector.tensor_tensor(out=dtmp[:], in0=ps[4][:], in1=dgb[cur][:], op=ALU.add)),
                     reads=[bp[4], b_hd[cur]], writes=[b_dtmp])
                S.op("act", (lambda: nc.scalar.activation(out=pt2[:], in_=dtmp[:], func=AF.Exp)), reads=[b_dtmp], writes=[b_pt2])

                def mmpo(cur=cur, X=X):
                    for c in range(2):
                        m = nc.tensor.matmul(ps[5][0:65, 0:256], lhsT=Vo[cur][:, 2 * X + c, :], rhs=pt2[:, c * 256:(c + 1) * 256],
                                             start=(c == 0), stop=(c == 1))
                    return m
                S.op("pe", mmpo, reads=[b_Vo[cur], b_pt2], writes=[bp[5]])
                S.op("dve", (lambda cur=cur, po=po: nc.vector.tensor_tensor(out=o1[0:65, :], in0=ps[po][0:65, 0:256], in1=cq[cur][0:65, :], op=ALU.mult)),
                     reads=[bp[po], b_hd[cur]], writes=[b_o])
                S.op("dve", (lambda: nc.vector.tensor_tensor(out=o2[0:65, :], in0=o1[0:65, :], in1=ps[5][0:65, 0:256], op=ALU.add)),
                     reads=[bp[5], b_o], writes=[b_o])
                S.op("pe", (lambda: nc.tensor.matmul(ps[6][0:64, 0:256], lhsT=C("sel65")[0:65, :], rhs=o2[0:65, :], start=True, stop=True)),
                     reads=[b_o, b_cst], writes=[bp[6]])
                S.op("dve", (lambda: nc.vector.reciprocal(out=rd[0:64, :], in_=ps[6][0:64, 0:256])), reads=[bp[6]], writes=[b_o])
                k = X % 2
                S.op("dve", (lambda k=k: nc.vector.tensor_tensor(out=ob[k][0:64, :], in0=o2[0:64, :], in1=rd[0:64, :], op=ALU.mult)),
                     reads=[b_o], writes=[b_ob[k]])
                S.op("sp", (lambda k=k, h=h, X=X: nc.sync.dma_start(out=d["OT"][h, :, X * 256:(X + 1) * 256], in_=ob[k][0:64, :])),
                     reads=[b_ob[k]], writes=[d["b_OT"][X // 2]])


CST_OFF = {}


def _cst_layout():
    off = 0
    for name, w in (("past", 1024), ("base", 1024), ("irow", 256), ("Dm", 512), ("causal", 512), ("ident", 128), ("sel65", 64)):
        CST_OFF[name] = (off, w)
        off += w
    return off


CST_N = _cst_layout()


def make_cst(j):
    blocks = snake_blocks(j)
    c = np.zeros((128, CST_N), np.float32)
    p = np.arange(128, dtype=np.float32)[:, None]
    past = np.zeros((16, 64), np.float32)
    base = np.zeros((128, 16, 64), np.float32)
    for l, own in enumerate(blocks):
        m = np.arange(64)
        past[l] = np.where(m < own, 0.0, NEG)
        dist = np.where(m < own, own - m, 1).astype(np.float32)
        base[:, l, :] = 2.0 * p - 256.0 * dist[None, :]
    o, w = CST_OFF["past"]
    c[:, o:o + w] = np.tile(past.reshape(1, 1024), (128, 1))
    o, w = CST_OFF["base"]
    c[:, o:o + w] = base.reshape(128, 1024)
    o, w = CST_OFF["irow"]
    c[:, o:o + w] = np.arange(256, dtype=np.float32)[None, :]
    i = np.arange(256, dtype=np.float32)[None, None, :]
    pp = np.arange(128, dtype=np.float32)[:, None, None]
    cc = np.arange(2, dtype=np.float32)[None, :, None]
    Dm = np.broadcast_to(i - 2 * pp, (128, 2, 256))
    causal = np.where(i >= 2 * pp + cc, 0.0, NEG)
    o, w = CST_OFF["Dm"]
    c[:, o:o + w] = Dm.reshape(128, 512)
    o, w = CST_OFF["causal"]
    c[:, o:o + w] = causal.reshape(128, 512)
    o, w = CST_OFF["ident"]
    c[:, o:o + w] = np.eye(128, dtype=np.float32)
    o, w = CST_OFF["sel65"]
    c[64, o:o + w] = 1.0
    return c


def emit_proj_res_phase(S, cx, x_in, b_xin, x_out, b_xout, src_d, b_src, w_d, G, bias, b_mod, tag, src_is_heads, ntiles=8):
    nc = S.nc
    w = S.sb("w_" + tag, [128, KC, D], BF16)
    b_w = Buf("w_" + tag)
    for kc in range(KC):
        S.op("pool_dma", (lambda kc=kc: nc.gpsimd.dma_start(out=w[:, kc, :], in_=w_d[kc * 128:(kc + 1) * 128, :])), writes=[b_w])
    sv = src_d.rearrange("h d t -> (h d) t") if src_is_heads else src_d
    sv = sv.rearrange("(c p) t -> p c t", p=128)
    xv_in = x_in.rearrange("(c p) t -> p c t", p=128)
    xv_out = x_out.rearrange("(c p) t -> p c t", p=128)
    for t in range(ntiles):
        xt, bx = load_x_tile(S, cx, xv_in, b_xin, t)
        S.op("sp", (lambda t=t: nc.sync.dma_start(out=cx.h[:], in_=sv[:, :, t * 512:(t + 1) * 512])), reads=[b_src[t]], writes=[cx.b_h])
        for oc in range(KC):
            py = 5 + (oc % 2)

            def mm(oc=oc, py=py):
                for kc in range(KC):
                    m = nc.tensor.matmul(cx.psum[py][:], lhsT=w[:, kc, oc * 128:(oc + 1) * 128], rhs=cx.h[:, kc, :], start=(kc == 0), stop=(kc == KC - 1))
                return m
            S.op("pe", mm, reads=[b_w, cx.b_h], writes=[cx.b_psum[py]])
            if bias is None:
                S.op("dve", (lambda oc=oc, py=py, xt=xt: nc.vector.scalar_tensor_tensor(
                    out=xt[:, oc, :], in0=cx.psum[py][:], scalar=G[:, oc:oc + 1], in1=xt[:, oc, :], op0=ALU.mult, op1=ALU.add)),
                    reads=[cx.b_psum[py], b_mod], writes=[bx])
            else:
                tmp = cx.tmp[oc % 2]
                S.op("act", (lambda oc=oc, py=py, tmp=tmp: nc.scalar.activation(out=tmp[:], in_=cx.psum[py][:], func=AF.Identity, bias=bias[:, oc:oc + 1])),
                     reads=[cx.b_psum[py], cx.b_sp], writes=[cx.b_tmp[oc % 2]])
                S.op("dve", (lambda oc=oc, tmp=tmp, xt=xt: nc.vector.scalar_tensor_tensor(
                    out=xt[:, oc, :], in0=tmp[:], scalar=G[:, oc:oc + 1], in1=xt[:, oc, :], op0=ALU.mult, op1=ALU.add)),
                    reads=[cx.b_tmp[oc % 2], b_mod], writes=[bx])
        S.op("sp", (lambda xt=xt, t=t: nc.sync.dma_start(out=xv_out[:, :, t * 512:(t + 1) * 512], in_=xt[:])), reads=[bx], writes=[b_xout[t]])


def emit_glu_phase(S, cx, x_in, b_xin, w_d, A, Bv, b_mod, u_out, ntiles=8, hmask=None, b_hmask=None, b_uout=None):
    nc = S.nc
    w = S.sb("w_pw1", [128, KC, 2 * D], BF16)
    b_w = Buf("w_pw1")
    for kc in range(KC):
        S.op("pool_dma", (lambda kc=kc: nc.gpsimd.dma_start(out=w[:, kc, :], in_=w_d[kc * 128:(kc + 1) * 128, :])), writes=[b_w])
    uo = [S.sb("uo%d" % i, [128, 512], BF16) for i in range(2)]
    b_uo = [Buf("uo%d" % i) for i in range(2)]
    xv_in = x_in.rearrange("(c p) t -> p c t", p=128)
    uv = u_out.rearrange("(c p) t -> p c t", p=128)
    bp1 = spc(cx, "b_pw1")
    for t in range(ntiles):
        xt, bx = load_x_tile(S, cx, xv_in, b_xin, t)
        emit_norm_mod(S, cx, xt, bx, A, Bv, b_mod)
        for oc in range(KC):
            k = oc % 2
            pa = 1 + k * 2
            pg = pa + 1

            def mma(oc=oc, pa=pa):
                for kc in range(KC):
                    m = nc.tensor.matmul(cx.psum[pa][:], lhsT=w[:, kc, oc * 128:(oc + 1) * 128], rhs=cx.h[:, kc, :], start=(kc == 0), stop=(kc == KC - 1))
                return m

            def mmg(oc=oc, pg=pg):
                for kc in range(KC):
                    m = nc.tensor.matmul(cx.psum[pg][:], lhsT=w[:, kc, D + oc * 128:D + (oc + 1) * 128], rhs=cx.h[:, kc, :], start=(kc == 0), stop=(kc == KC - 1))
                return m
            S.op("pe", mma, reads=[b_w, cx.b_h], writes=[cx.b_psum[pa]])
            S.op("pe", mmg, reads=[b_w, cx.b_h], writes=[cx.b_psum[pg]])
            sa = cx.sa[k]
            S.op("act", (lambda oc=oc, pg=pg, sa=sa: nc.scalar.activation(out=sa[:], in_=cx.psum[pg][:], func=AF.Sigmoid, bias=bp1[:, 8 + oc:9 + oc])),
                 reads=[cx.b_psum[pg], cx.b_sp], writes=[cx.b_sa[k]])
            S.op("dve", (lambda oc=oc, pa=pa, sa=sa, k=k: nc.vector.scalar_tensor_tensor(
                out=uo[k][:], in0=cx.psum[pa][:], scalar=bp1[:, oc:oc + 1], in1=sa[:], op0=ALU.add, op1=ALU.mult)),
                reads=[cx.b_psum[pa], cx.b_sa[k], cx.b_sp], writes=[b_uo[k]])
            if hmask is not None and t == 8:
                S.op("pool", (lambda k=k: nc.gpsimd.tensor_tensor(out=uo[k][:], in0=uo[k][:], in1=hmask, op=ALU.mult)),
                     reads=[b_uo[k], b_hmask], writes=[b_uo[k]])
            S.op("sp", (lambda oc=oc, k=k, t=t: nc.sync.dma_start(out=uv[:, oc, t * 512:(t + 1) * 512], in_=uo[k][:])), reads=[b_uo[k]],
                 writes=([b_uout[t]] if b_uout is not None else []))


def build_L2():
    nc = bass.Bass("TRN2", target_bir_lowering=False)
    din = lambda n, s, dt: nc.dram_tensor(n, s, dt, kind="ExternalInput").ap()
    sp_d = din("sp", [128, SP_N], F32)
    mod_d = din("modi", [128, 144], F32)
    x1 = din("x1", [1024, TOK], F32)
    d = {"KT": din("KTf", [NH, DH, SEQ], BF16), "ind": din("ind", [64, SEQ], BF16), "Vd": din("Vd", [NH, 128, 128, DH], BF16),
         "QT": din("QT", [NH, DH, TOK], BF16), "KTo": din("KTo", [NH, DH, TOK], BF16), "Vo": din("Vo", [NH, 128, 32, DH], BF16),
         "kbar": din("kbarf", [DH, NH, NB], F32), "cst": din("cst", [128, CST_N], F32)}
    w_o = din("w_o", [1024, 1024], F32)
    w_in1 = din("w_in1", [1024, 2 * DFF], F32)
    w_out1 = din("w_out1", [DFF, 1024], F32)
    w_in2 = din("w_in2", [1024, 2 * DFF], F32)
    w_out2 = din("w_out2", [DFF, 1024], F32)
    w_pw1 = din("w_pw1", [1024, 2048], F32)
    OT = nc.dram_tensor("OT", [NH, DH, TOK], BF16).ap()
    xa = nc.dram_tensor("xa", [1024, TOK], F32).ap()
    xb = nc.dram_tensor("xb", [1024, TOK], F32).ap()
    x4 = nc.dram_tensor("x4", [1024, TOK], F32, kind="ExternalOutput").ap()
    uT = nc.dram_tensor("uT", [1024, TOK], BF16, kind="ExternalOutput").ap()
    d["OT"] = OT
    d["b_OT"] = [Buf("OT%d" % t) for t in range(8)]
    S = Sched(nc)
    cx = Ctx()
    setup_common(S, cx)
    emit_load_small(S, cx, sp_d)
    modT = [S.sb("modT%d" % i, [128, 72], F32) for i in range(2)]
    b_modT = [Buf("modT%d" % i) for i in range(2)]
    for i in range(2):
        S.op("sp", (lambda i=i: nc.sync.dma_start(out=modT[i][:], in_=mod_d[:, i * 72:(i + 1) * 72])), writes=[b_modT[i]])
    m01 = emit_mod_vectors_i(S, cx, modT[0], b_modT[0], spc(cx, "ng01"), "l0s1", 1)
    m02 = emit_mod_vectors_i(S, cx, modT[0], b_modT[0], spc(cx, "ng02"), "l0s2", 2)
    m10 = emit_mod_vectors_i(S, cx, modT[1], b_modT[1], spc(cx, "ng10"), "l1s0", 0)
    m11 = emit_mod_vectors_i(S, cx, modT[1], b_modT[1], spc(cx, "ng11"), "l1s1", 1)
    n0 = len(S._stack)
    emit_attn_phase(S, cx, d)
    S.barrier_free(len(S._stack) - n0)
    alloc_tile_bufs(S, cx)
    b_x1 = [Buf("x1_%d" % t) for t in range(8)]
    b_xa = [Buf("xa_%d" % t) for t in range(8)]
    b_xb = [Buf("xb_%d" % t) for t in range(8)]
    b_x4 = [Buf("x4_%d" % t) for t in range(8)]
    n1 = len(S._stack)
    emit_proj_res_phase(S, cx, x1, b_x1, xa, b_xa, OT, d["b_OT"], w_o, m01[2], None, m01[3], "wo", True)
    S.barrier_free(len(S._stack) - n1)
    alloc_ffn_bufs(S, cx)
    emit_ffn_phase(S, cx, xa, b_xa, xb, b_xb, w_in1, w_out1, m02[0], m02[1], m02[2], 0.5, m02[3], 8, "f01")
    emit_ffn_phase(S, cx, xb, b_xb, x4, b_x4, w_in2, w_out2, m10[0], m10[1], m10[2], 0.5, m10[3], 8, "f10")
    S.barrier_free(len(S._stack) - n1)
    emit_glu_phase(S, cx, x4, b_x4, w_pw1, m11[0], m11[1], m11[3], uT)
    S.emit()
    return nc


def emit_conv_phase(S, cx, uext_d, x_in, b_xin, x_out, b_xout, w2_d, ident_d, G, b_mod, u_scr=None, b_u=None):
    nc = S.nc
    w2 = S.sb("w_pw2", [128, KC, D], BF16)
    b_w2 = Buf("w_pw2")
    for kc in range(KC):
        S.op("pool_dma", (lambda kc=kc: nc.gpsimd.dma_start(out=w2[:, kc, :], in_=w2_d[kc * 128:(kc + 1) * 128, :])), writes=[b_w2])
    ident = S.sb("identc", [128, 128], F32)
    b_id = Buf("identc")
    S.op("sp", lambda: nc.sync.dma_start(out=ident[:], in_=ident_d[:, :]), writes=[b_id])
    dg = S.sb("dg", [128, KC * CW, 128], BF16)
    b_dg = Buf("dg")
    wdw = spc(cx, "w_dw")
    for ck in range(KC * CW):
        S.op("dve", (lambda ck=ck: nc.vector.tensor_scalar(out=dg[:, ck, :], in0=ident[:], scalar1=wdw[:, ck:ck + 1], scalar2=None, op0=ALU.mult)),
             reads=[b_id, cx.b_sp], writes=[b_dg])
    ue = [S.sb("ue%d" % i, [128, KC, 288], BF16) for i in range(2)]
    b_ue = [Buf("ue%d" % i) for i in range(2)]
    vs = S.sb("vs", [128, KC, 256], F32)
    b_vs = [Buf("vs%d" % c) for c in range(KC)]
    vb = [S.sb("vb%d" % i, [128, 256], BF16) for i in range(2)]
    b_vb = [Buf("vb%d" % i) for i in range(2)]
    vq = [S.sb("vq%d" % i, [128, 256], BF16) for i in range(2)]
    b_vq = [Buf("vq%d" % i) for i in range(2)]
    mu = S.sb("mu", [128, 256], F32)
    rs = S.sb("rs", [128, 256], F32)
    b_st = Buf("lnstat")
    uv = uext_d.rearrange("(c p) l e -> p c l e", p=128) if uext_d is not None else None
    usv = u_scr.rearrange("(c p) t -> p c t", p=128) if u_scr is not None else None
    xv_in = x_in.rearrange("(c p) t -> p c t", p=128)
    xv_out = x_out.rearrange("(c p) t -> p c t", p=128)
    for l in range(NLB):
        u_ = ue[l % 2]
        if usv is None:
            S.op("sp", (lambda u_=u_, l=l: nc.sync.dma_start(out=u_[:], in_=uv[:, :, l, :])), writes=[b_ue[l % 2]])
        else:
            S.op("sp", (lambda u_=u_, l=l: nc.sync.dma_start(out=u_[:, :, 32:288], in_=usv[:, :, l * 256:(l + 1) * 256])),
                 reads=[b_u[l // 2]], writes=[b_ue[l % 2]])
            S.op("sp", (lambda u_=u_, l=l: nc.sync.dma_start(out=u_[:, :, 0:32], in_=usv[:, :, TOK + l * 32:TOK + (l + 1) * 32])),
                 reads=[b_u[8]], writes=[b_ue[l % 2]])
        xt, bx = load_x_tile(S, cx, xv_in, b_xin, l, n=256)
        for c in range(KC):
            k2 = c % 2
            pc = cx.psum[1 + k2]

            def mmc(c=c, pc=pc, u_=u_):
                for k in range(CW):
                    m = nc.tensor.matmul(pc[:, 0:256], lhsT=dg[:, c * CW + k, :], rhs=u_[:, c, 2 + k:2 + k + 256], start=(k == 0), stop=(k == CW - 1))
                return m
            S.op("pe", mmc, reads=[b_dg, b_ue[l % 2]], writes=[cx.b_psum[1 + k2]])
            S.op("act", (lambda c=c, pc=pc: nc.scalar.activation(out=vs[:, c, :], in_=pc[:, 0:256], func=AF.Identity, bias=spc(cx, "b_dw")[:, c:c + 1])),
                 reads=[cx.b_psum[1 + k2], cx.b_sp], writes=[b_vs[c]])
            S.op("pool", (lambda c=c, k2=k2: nc.gpsimd.tensor_copy(out=vb[k2][:], in_=vs[:, c, :])), reads=[b_vs[c]], writes=[b_vb[k2]])
            S.op("act", (lambda c=c, k2=k2: nc.scalar.activation(out=vq[k2][:], in_=vs[:, c, :], func=AF.Square)), reads=[b_vs[c]], writes=[b_vq[k2]])
            S.op("pe", (lambda c=c, k2=k2: nc.tensor.matmul(cx.psum[3][:, 0:256], lhsT=cx.ones_bf[:], rhs=vb[k2][:], start=(c == 0), stop=(c == KC - 1))),
                 reads=[b_vb[k2], cx.b_ones], writes=[cx.b_psum[3]])
            S.op("pe", (lambda c=c, k2=k2: nc.tensor.matmul(cx.psum[4][:, 0:256], lhsT=cx.ones_bf[:], rhs=vq[k2][:], start=(c == 0), stop=(c == KC - 1))),
                 reads=[b_vq[k2], cx.b_ones], writes=[cx.b_psum[4]])
        S.op("dve", lambda: nc.vector.tensor_scalar(out=mu[:], in0=cx.psum[3][:, 0:256], scalar1=1.0 / D, scalar2=None, op0=ALU.mult),
             reads=[cx.b_psum[3]], writes=[b_st])
        S.op("dve", lambda: nc.vector.tensor_tensor(out=rs[:], in0=mu[:], in1=mu[:], op=ALU.mult), reads=[b_st], writes=[b_st])
        S.op("dve", lambda: nc.vector.scalar_tensor_tensor(out=rs[:], in0=cx.psum[4][:, 0:256], scalar=1.0 / D, in1=rs[:], op0=ALU.mult, op1=ALU.subtract),
             reads=[cx.b_psum[4], b_st], writes=[b_st])
        S.op("dve", lambda: nc.vector.tensor_scalar(out=rs[:], in0=rs[:], scalar1=EPS, scalar2=None, op0=ALU.add), reads=[b_st], writes=[b_st])
        S.op("act", lambda: nc.scalar.activation(out=rs[:], in_=rs[:], func=AF.Sqrt), reads=[b_st], writes=[b_st])
        S.op("dve", lambda: nc.vector.reciprocal(out=rs[:], in_=rs[:]), reads=[b_st], writes=[b_st])
        for c in range(KC):
            tmp = cx.tmp[c % 2]
            S.op("dve", (lambda c=c, tmp=tmp: nc.vector.tensor_tensor(out=tmp[:, 0:256], in0=vs[:, c, :], in1=mu[:], op=ALU.subtract)),
                 reads=[b_vs[c], b_st], writes=[cx.b_tmp[c % 2]])
            S.op("dve", (lambda c=c, tmp=tmp: nc.vector.tensor_tensor(out=tmp[:, 0:256], in0=tmp[:, 0:256], in1=rs[:], op=ALU.mult)),
                 reads=[b_st, cx.b_tmp[c % 2]], writes=[cx.b_tmp[c % 2]])
            S.op("act", (lambda c=c, tmp=tmp: nc.scalar.activation(out=cx.h[:, c, 0:256], in_=tmp[:, 0:256], func=AF.Silu,
                                                                   bias=spc(cx, "ln_b")[:, c:c + 1], scale=spc(cx, "ln_g")[:, c:c + 1])),
                 reads=[cx.b_tmp[c % 2], cx.b_sp], writes=[cx.b_h])
        for oc in range(KC):
            py = 5 + (oc % 2)

            def mm(oc=oc, py=py):
                for kc in range(KC):
                    m = nc.tensor.matmul(cx.psum[py][:, 0:256], lhsT=w2[:, kc, oc * 128:(oc + 1) * 128], rhs=cx.h[:, kc, 0:256], start=(kc == 0), stop=(kc == KC - 1))
                return m
            S.op("pe", mm, reads=[b_w2, cx.b_h], writes=[cx.b_psum[py]])
            sa = cx.sa[oc % 2]
            S.op("act", (lambda oc=oc, py=py, sa=sa: nc.scalar.activation(out=sa[:, 0:256], in_=cx.psum[py][:, 0:256], func=AF.Identity,
                                                                        bias=spc(cx, "b_pw2")[:, oc:oc + 1])),
                 reads=[cx.b_psum[py], cx.b_sp], writes=[cx.b_sa[oc % 2]])
            S.op("dve", (lambda oc=oc, sa=sa, xt=xt: nc.vector.scalar_tensor_tensor(
                out=xt[:, oc, 0:256], in0=sa[:, 0:256], scalar=G[:, oc:oc + 1], in1=xt[:, oc, 0:256], op0=ALU.mult, op1=ALU.add)),
                reads=[cx.b_sa[oc % 2], b_mod], writes=[bx])
        S.op("sp", (lambda xt=xt, l=l: nc.sync.dma_start(out=xv_out[:, :, l * 256:(l + 1) * 256], in_=xt[:, :, 0:256])),
             reads=[bx], writes=[b_xout[l // 2]])


def build_L3():
    nc = bass.Bass("TRN2", target_bir_lowering=False)
    din = lambda n, s, dt: nc.dram_tensor(n, s, dt, kind="ExternalInput").ap()
    sp_d = din("sp", [128, SP_N], F32)
    mod_d = din("modi", [128, 144], F32)
    x4 = din("x4", [1024, TOK], F32)
    uext = din("uext", [1024, NLB, 288], BF16)
    ident_d = din("ident", [128, 128], F32)
    w_pw2 = din("w_pw2", [1024, 1024], F32)
    w_in3 = din("w_in3", [1024, 2 * DFF], F32)
    w_out3 = din("w_out3", [DFF, 1024], F32)
    x5 = nc.dram_tensor("x5", [1024, TOK], F32).ap()
    xo = nc.dram_tensor("xo", [1024, TOK], F32, kind="ExternalOutput").ap()
    S = Sched(nc)
    cx = Ctx()
    setup_common(S, cx)
    emit_load_small(S, cx, sp_d)
    modT = [S.sb("modT%d" % i, [128, 72], F32) for i in range(2)]
    b_modT = [Buf("modT%d" % i) for i in range(2)]
    for i in range(2):
        S.op("sp", (lambda i=i: nc.sync.dma_start(out=modT[i][:], in_=mod_d[:, i * 72:(i + 1) * 72])), writes=[b_modT[i]])
    m11 = emit_mod_vectors_i(S, cx, modT[1], b_modT[1], spc(cx, "ng11"), "l1s1", 1)
    m12 = emit_mod_vectors_i(S, cx, modT[1], b_modT[1], spc(cx, "ng12"), "l1s2", 2)
    alloc_tile_bufs(S, cx)
    b_x4 = [Buf("x4_%d" % t) for t in range(16)]
    b_x5 = [Buf("x5_%d" % t) for t in range(8)]
    b_xo = [Buf("xo_%d" % t) for t in range(8)]
    n1 = len(S._stack)
    emit_conv_phase(S, cx, uext, x4, b_x4, x5, b_x5, w_pw2, ident_d, m11[2], m11[3])
    S.barrier_free(len(S._stack) - n1)
    alloc_ffn_bufs(S, cx)
    emit_ffn_phase(S, cx, x5, b_x5, xo, b_xo, w_in3, w_out3, m12[0], m12[1], m12[2], 0.5, m12[3], 8, "f11")
    S.emit()
    return nc


_NC = {}


def _get(name, fn):
    if name not in _NC:
        _NC[name] = fn()
    return _NC[name]


def _deint(a):
    sh = a.shape
    return np.ascontiguousarray(a.reshape(sh[:-1] + (sh[-1] // 256, 128, 2)).swapaxes(-1, -2)).reshape(sh)


def kernel_unfused(**inp):
    import ml_dtypes
    bf = ml_dtypes.bfloat16
    inp = {k: np.asarray(v) for k, v in inp.items()}
    cores = list(range(8))
    toks = []
    for core in cores:
        blocks = snake_blocks(core % 4)
        toks.append(np.concatenate([np.arange(g * 256, (g + 1) * 256) for g in blocks]))
    sps = [pack_small(inp, c // 4) for c in cores]
    nc1 = _get("L1", build_L1)
    maps = [{"sp": sps[c], "ada_w": inp["ada_w"], "xin": np.ascontiguousarray(inp["x"][c // 4][toks[c]].T),
             "w_in": inp["ffn_w_in"][0, 0], "w_out": inp["ffn_w_out"][0, 0], "wqkv": inp["attn_w_qkv"][0]} for c in cores]
    r1 = run_bass_kernel_spmd(nc1, maps, core_ids=cores).results
    ind = np.zeros((64, SEQ), bf)
    for m in range(64):
        ind[m, m * 256:(m + 1) * 256] = 1
    ident = np.eye(128, dtype=np.float32)
    maps2 = []
    per_batch = {}
    for b in range(2):
        KTf = np.zeros((NH, DH, SEQ), bf)
        Vf = np.zeros((SEQ, D), bf)
        kbf = np.zeros((DH, NH, NB), np.float32)
        for j in range(4):
            c = 4 * b + j
            blocks = snake_blocks(j)
            KT = np.asarray(r1[c]["KT"])
            V = np.asarray(r1[c]["V"])
            kb = np.asarray(r1[c]["kbar"])
            for l, g in enumerate(blocks):
                KTf[:, :, g * 256:(g + 1) * 256] = KT[:, :, l * 256:(l + 1) * 256]
                Vf[g * 256:(g + 1) * 256] = V[l * 256:(l + 1) * 256]
                kbf[:, :, g] = kb[:, :, l].T
        KTf = _deint(KTf)
        Vd = np.ascontiguousarray(Vf.reshape(NB, 128, 2, NH, DH).transpose(3, 1, 0, 2, 4)).reshape(NH, 128, 128, DH)
        per_batch[b] = (KTf, Vd, kbf)
    for c in cores:
        b = c // 4
        KTf, Vd, kbf = per_batch[b]
        KTo = _deint(np.asarray(r1[c]["KT"]))
        V = np.asarray(r1[c]["V"])
        Vo = np.ascontiguousarray(V.reshape(NLB, 128, 2, NH, DH).transpose(3, 1, 0, 2, 4)).reshape(NH, 128, 32, DH)
        maps2.append({"sp": sps[c], "modi": r1[c]["modo"], "x1": r1[c]["x1"], "KTf": KTf, "ind": ind, "Vd": Vd,
                      "QT": r1[c]["QT"], "KTo": KTo, "Vo": Vo, "kbarf": kbf, "cst": make_cst(c % 4),
                      "w_o": inp["attn_w_o"][0], "w_in1": inp["ffn_w_in"][0, 1], "w_out1": inp["ffn_w_out"][0, 1],
                      "w_in2": inp["ffn_w_in"][1, 0], "w_out2": inp["ffn_w_out"][1, 0], "w_pw1": inp["conv_w_pw1"][0]})
    nc2 = _get("L2", build_L2)
    r2 = run_bass_kernel_spmd(nc2, maps2, core_ids=cores).results
    maps3 = []
    for b in range(2):
        Uf = np.zeros((D, SEQ), bf)
        for j in range(4):
            c = 4 * b + j
            Uf[:, toks[c]] = np.asarray(r2[c]["uT"])
        for j in range(4):
            c = 4 * b + j
            ue = np.zeros((D, NLB, 288), bf)
            for l, g in enumerate(snake_blocks(j)):
                ue[:, l, 32:] = Uf[:, g * 256:(g + 1) * 256]
                if g > 0:
                    ue[:, l, :32] = Uf[:, g * 256 - 32:g * 256]
            maps3.append({"sp": sps[c], "modi": r1[c]["modo"], "x4": r2[c]["x4"], "uext": ue, "ident": ident,
                          "w_pw2": inp["conv_w_pw2"][0], "w_in3": inp["ffn_w_in"][1, 1], "w_out3": inp["ffn_w_out"][1, 1]})
    nc3 = _get("L3", build_L3)
    r3 = run_bass_kernel_spmd(nc3, maps3, core_ids=cores).results
    out = np.zeros((BATCH, SEQ, D), np.float32)
    for c in cores:
        out[c // 4][toks[c]] = np.asarray(r3[c]["xo"]).T
    return out


NPOS = 80
NT_ALL = 40
TOKX = TOK + 512


def pos_order(j):
    real = []
    own = snake_blocks(j)
    for i in range(8):
        A, B = 8 * i + j, 8 * i + 7 - j
        real += [A, B] + [g for g in range(8 * i, 8 * i + 8) if g not in (A, B)]
    dup = [(g - 1 if g > 0 else None) for g in own]
    return real, dup


def emit_kvq_phase(S, cx, x_d, b_x, wqkv_d, A, Bv, b_mod, QT_d, KT_d, kbar_d, V_d, x1o_d, b_x1o, b_kv):
    nc = S.nc
    wq = S.sb("wqkv_sb", [128, KC, 3072], BF16)
    b_wq = [Buf("wqkv%d" % k) for k in range(KC)]
    for kc in range(KC):
        S.op("pool_dma", (lambda kc=kc: nc.gpsimd.dma_start(out=wq[:, kc, :], in_=wqkv_d[kc * 128:(kc + 1) * 128, :])), writes=[b_wq[kc]])
    bo = S.sb("blkones", [128, 128], BF16)
    b_bo = Buf("blkones")
    S.op("dve", lambda: nc.vector.memset(bo[:], 0.0), writes=[b_bo])
    S.op("dve", lambda: nc.vector.memset(bo[0:64, 0:64], 1.0), writes=[b_bo])
    S.op("dve", lambda: nc.vector.memset(bo[64:128, 64:128], 1.0), writes=[b_bo])
    g8 = S.sb("g8", [128, 2], F32)
    b_g8 = Buf("g8")
    S.op("dve", lambda: nc.vector.tensor_scalar(out=g8[:, 0:1], in0=spc(cx, "gq"), scalar1=0.125, scalar2=None, op0=ALU.mult),
         reads=[cx.b_sp], writes=[b_g8])
    S.op("dve", lambda: nc.vector.tensor_copy(out=g8[:, 1:2], in_=spc(cx, "gk")), reads=[cx.b_sp], writes=[b_g8])
    epst = S.sb("epst", [128, 1], F32)
    b_eps = Buf("epst")
    S.op("dve", lambda: nc.vector.memset(epst[:], EPS), writes=[b_eps])
    qo = [S.sb("qo%d" % i, [128, 512], BF16) for i in range(2)]
    b_qo = [Buf("qo%d" % i) for i in range(2)]
    kb = [S.sb("kb%d" % i, [128, 2], F32) for i in range(2)]
    b_kb = [Buf("kb%d" % i) for i in range(2)]
    vt = [S.sb("vt%d" % i, [128, 1024], BF16) for i in range(2)]
    b_vt = [Buf("vt%d" % i) for i in range(2)]
    xv = x_d.rearrange("(c p) t -> p c t", p=128)
    xov = x1o_d.rearrange("(c p) t -> p c t", p=128)
    QTv = QT_d.rearrange("h d t -> (h d) t")
    KTv = KT_d.rearrange("h d t -> (h d) t")
    cnt = 0
    PQB = (1, 2, 4, 7)
    for t in range(NT_ALL):
        is_own = (t < 32 and t % 4 == 0)
        is_dup = t >= 32
        xt, bx = cx.xt[t % 2], cx.b_xt[t % 2]
        if t == 0:
            load_x_tile(S, cx, xv, b_x, 0)
        if is_own:
            S.op("sp", (lambda xt=xt, t=t: nc.sync.dma_start(out=xov[:, :, (t // 4) * 512:(t // 4 + 1) * 512], in_=xt[:])),
                 reads=[bx], writes=[b_x1o[t // 4]])
        if is_dup:
            i2 = t - 32
            for k2 in range(2):
                S.op("sp", (lambda xt=xt, i2=i2, k2=k2: nc.sync.dma_start(
                    out=xov[:, :, TOK + (2 * i2 + k2) * 32:TOK + (2 * i2 + k2 + 1) * 32], in_=xt[:, :, k2 * 256 + 224:k2 * 256 + 256])),
                    reads=[bx], writes=[b_x1o[8]])
        emit_norm_mod(S, cx, xt, bx, A, Bv, b_mod)
        if t + 1 < NT_ALL:
            load_x_tile(S, cx, xv, b_x, t + 1)
        items = [(w_, hp) for w_ in range(2) for hp in range(8) if not (w_ == 0 and not (is_own or is_dup))]
        base_cnt = cnt
        cnt += len(items)

        def st_A(ii, base_cnt=base_cnt, items=items):
            which, hp = items[ii]
            k = (base_cnt + ii) % 2
            goff = which * 1024
            pqi = PQB[(base_cnt + ii) % 4]
            pq = cx.psum[pqi]

            def mm():
                for kc in range(KC):
                    m = nc.tensor.matmul(pq[:], lhsT=wq[:, kc, goff + hp * 128:goff + (hp + 1) * 128], rhs=cx.h[:, kc, :],
                                         start=(kc == 0), stop=(kc == KC - 1))
                return m
            S.op("pe", mm, reads=b_wq + [cx.b_h], writes=[cx.b_psum[pqi]])
            S.op("act", (lambda: nc.scalar.activation(out=cx.sq[k][:], in_=pq[:], func=AF.Square)),
                 reads=[cx.b_psum[pqi]], writes=[cx.b_sq[k]])

        def st_B(ii, base_cnt=base_cnt, items=items, t=t, is_own=is_own):
            which, hp = items[ii]
            k = (base_cnt + ii) % 2
            pqi = PQB[(base_cnt + ii) % 4]
            pq = cx.psum[pqi]
            pss = cx.psum[3]
            S.op("pe", (lambda: nc.tensor.matmul(pss[:], lhsT=bo[:], rhs=cx.sq[k][:], start=True, stop=True)),
                 reads=[cx.b_sq[k], b_bo], writes=[cx.b_psum[3]])
            r1 = cx.tmp[k]
            S.op("act", (lambda: nc.scalar.activation(out=r1[:], in_=pss[:], func=AF.Ln, bias=epst[:, 0:1], scale=1.0 / DH)),
                 reads=[cx.b_psum[3], b_eps], writes=[cx.b_tmp[k]])
            S.op("act", (lambda: nc.scalar.activation(out=r1[:], in_=r1[:], func=AF.Exp, scale=-0.5)), reads=[cx.b_tmp[k]], writes=[cx.b_tmp[k]])
            if which == 0:
                S.op("dve", (lambda: nc.vector.scalar_tensor_tensor(out=qo[k][:], in0=pq[:], scalar=g8[:, 0:1], in1=r1[:], op0=ALU.mult, op1=ALU.mult)),
                     reads=[cx.b_tmp[k], cx.b_psum[pqi], b_g8], writes=[b_qo[k]])
                if is_own:
                    for k2 in range(2):
                        lb = 2 * (t // 4) + k2
                        S.op("sp", (lambda k2=k2, lb=lb: nc.sync.dma_start(out=QTv[hp * 128:(hp + 1) * 128, lb * 288:lb * 288 + 256],
                                                                         in_=qo[k][:, k2 * 256:(k2 + 1) * 256])),
                             reads=[b_qo[k]], writes=[b_kv])
                else:
                    i2 = t - 32
                    for k2 in range(2):
                        lb = 2 * i2 + k2
                        S.op("sp", (lambda k2=k2, lb=lb: nc.sync.dma_start(
                            out=QTv[hp * 128:(hp + 1) * 128, lb * 288 + 256:lb * 288 + 288],
                            in_=qo[k][:, k2 * 256 + 224:k2 * 256 + 256])), reads=[b_qo[k]], writes=[b_kv])
            else:
                for bb in range(2):
                    S.op("dve", (lambda bb=bb: nc.vector.scalar_tensor_tensor(
                        out=qo[k][:, bb * 256:(bb + 1) * 256].rearrange("q (c p) -> q c p", c=2),
                        in0=pq[:, bb * 256:(bb + 1) * 256].rearrange("q (p c) -> q c p", c=2), scalar=g8[:, 1:2],
                        in1=r1[:, bb * 256:(bb + 1) * 256].rearrange("q (p c) -> q c p", c=2), op0=ALU.mult, op1=ALU.mult)),
                        reads=[cx.b_tmp[k], cx.b_psum[pqi], b_g8], writes=[b_qo[k]])
                S.op("sp", (lambda: nc.sync.dma_start(out=KTv[hp * 128:(hp + 1) * 128, t * 512:(t + 1) * 512], in_=qo[k][:])),
                     reads=[b_qo[k]], writes=[b_kv])
                S.op("dve", (lambda: nc.vector.tensor_reduce(out=kb[k][:], in_=qo[k][:].rearrange("p (b t) -> p b t", b=2), axis=AX.X, op=ALU.add)),
                     reads=[b_qo[k]], writes=[b_kb[k]])
                S.op("dve", (lambda: nc.vector.tensor_scalar(out=kb[k][:], in0=kb[k][:], scalar1=1.0 / BLK, scalar2=None, op0=ALU.mult)),
                     reads=[b_kb[k]], writes=[b_kb[k]])
                S.op("sp", (lambda: nc.sync.dma_start(out=kbar_d[hp * 128:(hp + 1) * 128, 2 * t:2 * t + 2], in_=kb[k][:])),
                     reads=[b_kb[k]], writes=[b_kv])
        vgroups = [(tg, ns) for tg in range(4) for ns in range(2)]

        def st_V(gi, t=t):
            tg, ns = vgroups[gi]
            v = vt[tg % 2]
            pv = cx.psum[5 + ns]

            def mmv():
                for kc in range(KC):
                    m = nc.tensor.matmul(pv[:], lhsT=cx.h[:, kc, tg * 128:(tg + 1) * 128], rhs=wq[:, kc, 2048 + ns * 512:2048 + (ns + 1) * 512],
                                         start=(kc == 0), stop=(kc == KC - 1))
                return m
            S.op("pe", mmv, reads=b_wq + [cx.b_h], writes=[cx.b_psum[5 + ns]])
            if ns == 0:
                S.op("act", (lambda: nc.scalar.copy(out=v[:, 0:512], in_=pv[:])), reads=[cx.b_psum[5]], writes=[b_vt[tg % 2]])
            else:
                S.op("dve", (lambda: nc.vector.tensor_copy(out=v[:, 512:1024], in_=pv[:])), reads=[cx.b_psum[6]], writes=[b_vt[tg % 2]])
                blk = 2 * t + tg // 2
                hf = tg % 2
                for c2 in range(2):
                    dst = V_d[:, hf * 64:(hf + 1) * 64, 2 * blk + c2, :].rearrange("h pp d -> pp h d")
                    S.op("sp", (lambda dst=dst, c2=c2: nc.sync.dma_start(out=dst, in_=v[c2:128:2, :].rearrange("p (h d) -> p h d", h=NH))),
                         reads=[b_vt[tg % 2]], writes=[b_kv])
        vper = len(items) // 8
        st_A(0)
        for ii in range(len(items)):
            if ii + 1 < len(items):
                st_A(ii + 1)
            st_B(ii)
            if (ii + 1) % vper == 0:
                st_V((ii + 1) // vper - 1)


CST2_OFF = {}


def _cst2_layout():
    off = 0
    for name, w in (("past", 2048), ("base", 1024), ("irowX", 288), ("Dm", 512), ("causal", 512),
                    ("DmH", 64), ("causalH", 64), ("ident", 128), ("sel65", 64)):
        CST2_OFF[name] = (off, w)
        off += w
    return off


CST2_N = _cst2_layout()


def make_cst2(j):
    real, dup = pos_order(j)
    own = snake_blocks(j)
    c = np.zeros((128, CST2_N), np.float32)
    p = np.arange(128, dtype=np.float32)[:, None]
    gpos = np.array(real)
    past = np.zeros((32, 64), np.float32)
    base = np.zeros((128, 16, 64), np.float32)
    for row in range(32):
        o = own[row] if row < 16 else own[row - 16] - 1
        valid = gpos < o
        past[row] = np.where(valid, 0.0, NEG)
        if row < 16:
            dist = np.where(valid, o - gpos, 1).astype(np.float32)
            base[:, row, :] = 2.0 * p - 256.0 * dist[None, :]

    def put(name, arr):
        o_, w = CST2_OFF[name]
        c[:, o_:o_ + w] = arr.reshape(arr.shape[0], -1)
    put("past", np.tile(past.reshape(1, 2048), (128, 1)))
    put("base", base.reshape(128, 1024))
    irx = np.concatenate([np.arange(256, dtype=np.float32), -(32.0 - np.arange(32, dtype=np.float32))])
    put("irowX", np.tile(irx[None, :], (128, 1)))
    pp = np.arange(128, dtype=np.float32)[:, None, None]
    cc = np.arange(2, dtype=np.float32)[None, :, None]
    i = np.arange(256, dtype=np.float32)[None, None, :]
    put("Dm", np.broadcast_to(i - 2 * pp, (128, 2, 256)).copy())
    put("causal", np.where(i >= 2 * pp + cc, 0.0, NEG).astype(np.float32))
    ih = 224 + np.arange(32, dtype=np.float32)[None, None, :]
    put("DmH", np.broadcast_to(ih - 2 * pp, (128, 2, 32)).copy())
    put("causalH", np.where(ih >= 2 * pp + cc, 0.0, NEG).astype(np.float32))
    put("ident", np.eye(128, dtype=np.float32))
    s65 = np.zeros((128, 64), np.float32)
    s65[64] = 1.0
    put("sel65", s65)
    hmask = np.ones((128, 512), np.float32)
    for l in range(16):
        if dup[l] is None:
            hmask[:, l * 32:(l + 1) * 32] = 0.0
    return c, hmask


def emit_attn2(S, cx, d):
    nc = S.nc
    cst = S.sb("cst2", [128, CST2_N], F32)
    b_cst = Buf("cst2")
    S.op("sp", lambda: nc.sync.dma_start(out=cst[:], in_=d["cst"][:, :]), writes=[b_cst])

    def C(name):
        o, w = CST2_OFF[name]
        return cst[:, o:o + w]
    kbh = S.sb("kbh", [64, NH * NB], BF16)
    kbl = S.sb("kbl", [64, NH * NB], BF16)
    b_kb = Buf("kbhl")
    n0 = len(S._stack)
    kb32 = S.sb("kb32", [64, NH * NB], F32)
    kbt = S.sb("kbt", [64, NH * NB], F32)
    S.op("sp", lambda: nc.sync.dma_start(out=kb32[:].rearrange("d (h m) -> d h m", h=NH),
                                         in_=d["kbar"].rearrange("(h d) m -> d h m", h=NH)[:, :, 0:NB]), reads=[d["b_kv"]], writes=[b_kb])
    S.op("dve", lambda: nc.vector.tensor_copy(out=kbh[:], in_=kb32[:]), reads=[b_kb], writes=[b_kb])
    S.op("dve", lambda: nc.vector.tensor_copy(out=kbt[:], in_=kbh[:]), reads=[b_kb], writes=[b_kb])
    S.op("dve", lambda: nc.vector.tensor_tensor(out=kbt[:], in0=kb32[:], in1=kbt[:], op=ALU.subtract), reads=[b_kb], writes=[b_kb])
    S.op("dve", lambda: nc.vector.tensor_copy(out=kbl[:], in_=kbt[:]), reads=[b_kb], writes=[b_kb])
    S.barrier_free(len(S._stack) - n0)
    KTa = [S.sb("KTa%d" % i, [128, SEQ], BF16) for i in range(2)]
    Va = [S.sb("Va%d" % i, [128, 128, 128], BF16) for i in range(2)]
    Qa = [S.sb("Qa%d" % i, [128, TOKX], BF16) for i in range(2)]
    Kd = [S.sb("Kd0", [128, TOK], BF16)] * 2
    Vdd = [S.sb("Vdd0", [128, 32, 65], BF16)] * 2
    bT = [S.sb("bT0", [128, 1024], F32)] * 2
    dgb = [S.sb("dgb0", [128, 512], F32)] * 2
    dgbH = [S.sb("dgbH%d" % i, [128, 64], F32) for i in range(2)]
    cq = [S.sb("cq%d" % i, [128, 288], F32) for i in range(2)]
    b_KT = [Buf("KTa%d" % i) for i in range(2)]
    b_Va = [Buf("Va%d" % i) for i in range(2)]
    b_Qa = [Buf("Qa%d" % i) for i in range(2)]
    b_Qm = [[Buf("Qm%d_%d" % (i, l)) for l in range(16)] for i in range(2)]
    b_Kd = [Buf("Kd0")] * 2
    b_Vdd = [Buf("Vdd0")] * 2
    b_hd = [Buf("hd0")] * 2
    mbp = [S.sb("mbp%d" % i, [128, 128], F32) for i in range(2)]
    b_mbp = [Buf("mbp%d" % i) for i in range(2)]
    gmt = [S.sb("gmt%d" % i, [128, 64], F32) for i in range(2)]
    b_gmt = [Buf("gmt%d" % i) for i in range(2)]
    t8 = [S.sb("t8_%d" % i, [128, 8], F32) for i in range(2)]
    pt = [S.sb("pt%d" % i, [128, 2, 288], BF16) for i in range(2)]
    b_pt = [Buf("pt%d" % i) for i in range(2)]
    pt2 = S.sb("pt2", [128, 512], BF16)
    b_pt2 = Buf("pt2")
    dtmp = S.sb("dtmp", [128, 512], F32)
    b_dtmp = Buf("dtmp")
    o1 = S.sb("o1", [128, 288], F32)
    o2 = S.sb("o2", [128, 288], F32)
    rd = S.sb("rd", [128, 288], F32)
    ob = [S.sb("ob%d" % i, [128, 288], BF16) for i in range(2)]
    b_o = Buf("o12")
    b_ob = [Buf("ob%d" % i) for i in range(2)]
    for i in range(2):
        S.op("sp", (lambda i=i: nc.sync.dma_start(out=KTa[i][64:128, :], in_=d["ind"][:, :])), writes=[b_KT[i]])
        S.op("pool", (lambda i=i: nc.gpsimd.memset(Va[i][:, :, 64:128], 0.0)), writes=[b_Va[i]])
        S.op("dve", (lambda i=i: nc.vector.memset(Va[i][:, :, 64:65], 1.0)), writes=[b_Va[i]])
        S.op("dve", (lambda i=i: nc.vector.memset(Vdd[i][:, :, 64:65], 1.0)), writes=[b_Vdd[i]])
        S.op("dve", (lambda i=i: nc.vector.memset(mbp[i][:], 0.0)), writes=[b_mbp[i]])
    ps = cx.psum
    bp = cx.b_psum
    SX = [cx.psall[:, 0:1024], cx.psall[:, 1024:2048]]
    b_SX = [[bp[0], bp[1]], [bp[2], bp[3]]]
    scnt = 0
    ocnt = 0
    NQ = 288
    for h in range(NH):
        cur = h % 2
        sl = SLOPES[h]
        es = math.exp(sl)
        S.op("sp", (lambda h=h, cur=cur: nc.sync.dma_start(out=KTa[cur][0:64, :], in_=d["KT"][h, :, 0:SEQ])), reads=[d["b_kv"]], writes=[b_KT[cur]])
        for q8 in range(8):
            S.op("sp", (lambda h=h, cur=cur, q8=q8: nc.sync.dma_start(out=Va[cur][:, q8 * 16:(q8 + 1) * 16, 0:64], in_=d["Vd"][h, :, q8 * 16:(q8 + 1) * 16, :])),
                 reads=[d["b_kv"]], writes=[b_Va[cur]])
        S.op("sp", (lambda h=h, cur=cur: nc.sync.dma_start(out=Qa[cur][0:64, :], in_=d["QT"][h])), reads=[d["b_kv"]], writes=[b_Qa[cur]] + b_Qm[cur])
        S.op("sp", (lambda h=h, cur=cur: nc.sync.dma_start(out=Kd[cur][0:64, :], in_=d["KT"][h, :, SEQ:SEQ + TOK])), reads=[d["b_kv"]], writes=[b_Kd[cur]])
        for q2 in range(2):
            S.op("sp", (lambda h=h, cur=cur, q2=q2: nc.sync.dma_start(out=Vdd[cur][:, q2 * 16:(q2 + 1) * 16, 0:64],
                                                                     in_=d["Vd"][h, :, 128 + q2 * 16:128 + (q2 + 1) * 16, :])),
                 reads=[d["b_kv"]], writes=[b_Vdd[cur]])
        for (VV, bV) in ((Va[cur], b_Va[cur]), (Vdd[cur], b_Vdd[cur])):
            Vodd = VV[:].rearrange("p (n two) e -> p n two e", two=2)[:, :, 1, :]
            S.op("dve", (lambda Vodd=Vodd, es=es: nc.vector.tensor_scalar(out=Vodd[:, :, 0:64], in0=Vodd[:, :, 0:64], scalar1=es, scalar2=None, op0=ALU.mult)),
                 reads=[bV], writes=[bV])
            S.op("dve", (lambda Vodd=Vodd, es=es: nc.vector.memset(Vodd[:, :, 64:65], es)), reads=[bV], writes=[bV])
        S.op("dve", (lambda cur=cur, sl=sl: nc.vector.tensor_scalar(out=bT[cur][:], in0=C("base"), scalar1=sl, scalar2=None, op0=ALU.mult)),
             reads=[b_cst], writes=[b_hd[cur]])
        S.op("dve", (lambda cur=cur, sl=sl: nc.vector.scalar_tensor_tensor(out=dgb[cur][:], in0=C("Dm"), scalar=-sl, in1=C("causal"),
                                                                         op0=ALU.mult, op1=ALU.add)), reads=[b_cst], writes=[b_hd[cur]])
        S.op("dve", (lambda cur=cur, sl=sl: nc.vector.scalar_tensor_tensor(out=dgbH[cur][:], in0=C("DmH"), scalar=-sl, in1=C("causalH"),
                                                                         op0=ALU.mult, op1=ALU.add)), reads=[b_cst], writes=[b_hd[cur]])
        S.op("act", (lambda cur=cur, sl=sl: nc.scalar.activation(out=cq[cur][:], in_=C("irowX"), func=AF.Exp, scale=-sl)),
             reads=[b_cst], writes=[b_hd[cur]])
        groups = []
        for l in range(NLB):
            groups.append((l * NQ, 128, l, l))
            groups.append((l * NQ + 128, 128, l, l))
            groups.append((l * NQ + 256, 32, 16 + l, l))

        def g_s1(gi, cur=cur, h=h, groups=groups):
            q0, nr, row, qmi = groups[gi]

            def mmg():
                nc.tensor.matmul(ps[6][0:nr, 0:64], lhsT=Qa[cur][0:64, q0:q0 + nr], rhs=kbh[:, h * 64:(h + 1) * 64], start=True, stop=False)
                return nc.tensor.matmul(ps[6][0:nr, 0:64], lhsT=Qa[cur][0:64, q0:q0 + nr], rhs=kbl[:, h * 64:(h + 1) * 64], start=False, stop=True)
            S.op("pe", mmg, reads=[b_Qa[cur], b_kb], writes=[bp[6]])

        def g_s2(gi, groups=groups):
            q0, nr, row, qmi = groups[gi]
            g = gi % 2
            pb = C("past")[0:nr, row * 64:(row + 1) * 64]
            S.op("dve", (lambda: nc.vector.tensor_tensor(out=gmt[g][0:nr, :], in0=ps[6][0:nr, 0:64], in1=pb, op=ALU.add)),
                 reads=[bp[6], b_cst], writes=[b_gmt[g]])
            S.op("dve", (lambda: nc.vector.max(out=t8[g][0:nr, :], in_=gmt[g][0:nr, :])), reads=[b_gmt[g]], writes=[b_gmt[g]])
            S.op("dve", (lambda: nc.vector.tensor_scalar(out=gmt[g][0:nr, :], in0=gmt[g][0:nr, :], scalar1=t8[g][0:nr, 2:3], scalar2=-NEG,
                                                         op0=ALU.is_ge, op1=ALU.mult)), reads=[b_gmt[g]], writes=[b_gmt[g]])
            S.op("dve", (lambda: nc.vector.scalar_tensor_tensor(out=mbp[g][0:nr, 64:128], in0=gmt[g][0:nr, :], scalar=NEG, in1=pb,
                                                                op0=ALU.add, op1=ALU.add)),
                 reads=[b_gmt[g], b_cst], writes=[b_mbp[g]])

        def g_s3(gi, cur=cur, groups=groups):
            q0, nr, row, qmi = groups[gi]
            g = gi % 2
            S.op("pe", (lambda: nc.tensor.transpose(ps[7][:, 0:nr], mbp[g][0:nr, :], C("ident")[0:nr, 0:nr])),
                 reads=[b_mbp[g], b_cst], writes=[bp[7]])
            S.op("act", (lambda: nc.scalar.copy(out=Qa[cur][64:128, q0:q0 + nr], in_=ps[7][64:128, 0:nr])),
                 reads=[bp[7]], writes=[b_Qm[cur][qmi]])
        g_s1(0)
        for gi in range(len(groups)):
            g_s2(gi)
            if gi + 1 < len(groups):
                g_s1(gi + 1)
            g_s3(gi)
        for i in range(8):
            for k in range(2):
                l = 2 * i + k
                q0 = l * NQ
                po = 4 + k
                if k == 0:
                    nlist = list(range(8 * i)) + [8 * i + 2, 8 * i + 3, 8 * i + 4]
                else:
                    nlist = list(range(8 * i)) + [8 * i] + list(range(8 * i + 2, 8 * i + 8))
                nk = len(nlist)
                sbase = scnt
                scnt += nk

                def emit_S(ni, cur=cur, q0=q0, l=l, sbase=sbase, nlist=nlist):
                    sb_ = (sbase + ni) % 2
                    n = nlist[ni]

                    def mms():
                        for c in range(2):
                            m = nc.tensor.matmul(SX[sb_][:, c * 512:c * 512 + NQ], lhsT=KTa[cur][:, n * 256 + c * 128:n * 256 + (c + 1) * 128],
                                                 rhs=Qa[cur][:, q0:q0 + NQ], start=True, stop=True)
                        return m
                    S.op("pe", mms, reads=[b_KT[cur], b_Qa[cur], b_Qm[cur][l]], writes=b_SX[sb_])

                def emit_E(ni, cur=cur, l=l, sbase=sbase, nlist=nlist):
                    sb_ = (sbase + ni) % 2
                    n = nlist[ni]
                    S.op("act", (lambda: nc.scalar.activation(out=pt[sb_][:], in_=SX[sb_].rearrange("p (c x) -> p c x", c=2)[:, :, 0:NQ], func=AF.Exp,
                                                              bias=bT[cur][:, l * 64 + n:l * 64 + n + 1])),
                         reads=b_SX[sb_] + [b_hd[cur]], writes=[b_pt[sb_]])

                def emit_PV(ni, cur=cur, po=po, nk=nk, sbase=sbase, nlist=nlist):
                    sb_ = (sbase + ni) % 2
                    n = nlist[ni]

                    def mmpv():
                        for c in range(2):
                            m = nc.tensor.matmul(ps[po][:, 0:NQ], lhsT=Va[cur][:, 2 * n + c, :], rhs=pt[sb_][:, c, :],
                                                 start=(ni == 0 and c == 0), stop=(ni == nk - 1 and c == 1))
                        return m
                    S.op("pe", mmpv, reads=[b_Va[cur], b_pt[sb_]], writes=[bp[po]])
                emit_S(0)
                for n in range(nk):
                    emit_E(n)
                    if n + 1 < nk:
                        emit_S(n + 1)
                    emit_PV(n)
                for (Kt, koff, Vt, ch0, bK, bV, dg, qoff, nq, ocol) in (
                        (KTa[cur], (8 * i + k) * 256, Va[cur], 2 * (8 * i + k), b_KT[cur], b_Va[cur], dgb[cur], q0, 256, 0),
                        (Kd[cur], l * 256, Vdd[cur], 2 * l, b_Kd[cur], b_Vdd[cur], dgbH[cur], q0 + 256, 32, 256)):
                    def mmd(Kt=Kt, koff=koff, qoff=qoff, nq=nq, cur=cur):
                        for c in range(2):
                            m = nc.tensor.matmul(ps[6][:, c * nq:(c + 1) * nq], lhsT=Kt[0:64, koff + c * 128:koff + (c + 1) * 128],
                                                 rhs=Qa[cur][0:64, qoff:qoff + nq], start=True, stop=True)
                        return m
                    S.op("pe", mmd, reads=[bK, b_Qa[cur]], writes=[bp[6]])
                    S.op("dve", (lambda dg=dg, nq=nq: nc.vector.tensor_tensor(out=dtmp[:, 0:2 * nq], in0=ps[6][:, 0:2 * nq], in1=dg[:, 0:2 * nq], op=ALU.add)),
                         reads=[bp[6], b_hd[cur]], writes=[b_dtmp])
                    S.op("act", (lambda nq=nq: nc.scalar.activation(out=pt2[:, 0:2 * nq], in_=dtmp[:, 0:2 * nq], func=AF.Exp)), reads=[b_dtmp], writes=[b_pt2])

                    def mmpo(Vt=Vt, ch0=ch0, nq=nq, ocol=ocol):
                        for c in range(2):
                            m = nc.tensor.matmul(ps[7][0:Vt.shape[2], ocol:ocol + nq], lhsT=Vt[:, ch0 + c, :], rhs=pt2[:, c * nq:(c + 1) * nq],
                                                 start=(c == 0), stop=(c == 1))
                        return m
                    S.op("pe", mmpo, reads=[bV, b_pt2], writes=[bp[7]])
                S.op("dve", (lambda cur=cur, po=po: nc.vector.tensor_tensor(out=o1[0:65, :], in0=ps[po][0:65, 0:NQ], in1=cq[cur][0:65, :], op=ALU.mult)),
                     reads=[bp[po], b_hd[cur]], writes=[b_o])
                S.op("dve", (lambda: nc.vector.tensor_tensor(out=o2[0:65, :], in0=o1[0:65, :], in1=ps[7][0:65, 0:NQ], op=ALU.add)),
                     reads=[bp[7], b_o], writes=[b_o])
                S.op("pe", (lambda: nc.tensor.matmul(ps[6][0:64, 0:NQ], lhsT=C("sel65")[0:65, :], rhs=o2[0:65, :], start=True, stop=True)),
                     reads=[b_o, b_cst], writes=[bp[6]])
                S.op("dve", (lambda: nc.vector.reciprocal(out=rd[0:64, :], in_=ps[6][0:64, 0:NQ])), reads=[bp[6]], writes=[b_o])
                kk = ocnt % 2
                ocnt += 1
                S.op("dve", (lambda kk=kk: nc.vector.tensor_tensor(out=ob[kk][0:64, :], in0=o2[0:64, :], in1=rd[0:64, :], op=ALU.mult)),
                     reads=[b_o], writes=[b_ob[kk]])
                S.op("sp", (lambda kk=kk, h=h, l=l: nc.sync.dma_start(out=d["OT"][h, :, l * 256:(l + 1) * 256], in_=ob[kk][0:64, 0:256])),
                     reads=[b_ob[kk]], writes=[d["b_OT"][l // 2]])
                S.op("sp", (lambda kk=kk, h=h, l=l: nc.sync.dma_start(out=d["OT"][h, :, TOK + l * 32:TOK + (l + 1) * 32], in_=ob[kk][0:64, 256:288])),
                     reads=[b_ob[kk]], writes=[d["b_OT"][8]])


def build_fused():
    nc = bass.Bass("TRN2", target_bir_lowering=False)
    din = lambda n, s, dt: nc.dram_tensor(n, s, dt, kind="ExternalInput").ap()
    scr = lambda n, s, dt: nc.dram_tensor(n, s, dt).ap()
    sp_d = din("sp", [128, SP_N], F32)
    ada_w = din("ada_w", [2, 1024, 9216], F32)
    xall = din("xall", [1024, NT_ALL * 512], F32)
    w_in = [din("w_in%d" % i, [1024, 2 * DFF], F32) for i in range(4)]
    w_out = [din("w_out%d" % i, [DFF, 1024], F32) for i in range(4)]
    wqkv = din("wqkv", [1024, 3072], F32)
    w_o = din("w_o", [1024, 1024], F32)
    w_pw1 = din("w_pw1", [1024, 2048], F32)
    w_pw2 = din("w_pw2", [1024, 1024], F32)
    ind_d = din("ind", [64, SEQ], BF16)
    cst_d = din("cst", [128, CST2_N], F32)
    hm_d = din("hmask", [128, 512], F32)
    ident_d = din("ident", [128, 128], F32)
    xo = nc.dram_tensor("xo", [1024, TOK], F32, kind="ExternalOutput").ap()
    x1all = scr("x1all", [1024, NT_ALL * 512], F32)
    x1o = scr("x1o", [1024, TOKX], F32)
    xa = scr("xa", [1024, TOKX], F32)
    xb = scr("xb", [1024, TOKX], F32)
    x4 = scr("x4", [1024, TOKX], F32)
    x5 = scr("x5", [1024, TOK], F32)
    uT = scr("uT", [1024, TOKX], BF16)
    KT = scr("KTs", [NH, DH, NPOS * 256], BF16)
    Vd = scr("Vds", [NH, 128, 2 * NPOS, DH], BF16)
    kbar = scr("kbars", [NH * DH, NPOS], F32)
    QT = scr("QTs", [NH, DH, TOKX], BF16)
    OT = scr("OTs", [NH, DH, TOKX], BF16)
    S = Sched(nc)
    cx = Ctx()
    setup_common(S, cx)
    emit_load_small(S, cx, sp_d)
    hmask = S.sb("hmask", [128, 512], F32)
    b_hm = Buf("hmask")
    S.op("sp", lambda: nc.sync.dma_start(out=hmask[:], in_=hm_d[:, :]), writes=[b_hm])
    modT = [S.sb("modT%d" % i, [128, 72], F32) for i in range(2)]
    b_modT = [Buf("modT%d" % i) for i in range(2)]
    emit_adaln_scoped(S, cx, ada_w, modT, b_modT)
    M = {}
    for i in range(2):
        for j in range(3):
            M[(i, j)] = emit_mod_vectors_i(S, cx, modT[i], b_modT[i], spc(cx, "ng%d%d" % (i, j)), "l%ds%d" % (i, j), j)
    nbase = len(S._stack)
    mk = lambda name, n: [Buf("%s_%d" % (name, t)) for t in range(n)]
    b_xall, b_x1all, b_x1o = mk("xall", NT_ALL), mk("x1all", NT_ALL), mk("x1o", 9)
    b_xa, b_xb, b_x4, b_u, b_x5, b_xo = mk("xa", 9), mk("xb", 9), mk("x4", 16), mk("u", 9), mk("x5", 8), mk("xo", 8)
    b_x4t = b_x4[:9]
    alloc_tile_bufs(S, cx)
    ntile = len(S._stack)
    alloc_ffn_bufs(S, cx)
    m = M[(0, 0)]
    emit_ffn_phase(S, cx, xall, b_xall, x1all, b_x1all, w_in[0], w_out[0], m[0], m[1], m[2], 0.5, m[3], NT_ALL, "f00")
    S.barrier_free(len(S._stack) - ntile)
    m = M[(0, 1)]
    b_kv = Buf("kvq")
    emit_kvq_phase(S, cx, x1all, b_x1all, wqkv, m[0], m[1], m[3], QT, KT, kbar, Vd, x1o, b_x1o, b_kv)
    S.barrier_free(len(S._stack) - nbase)
    b_OT = mk("OT", 9)
    emit_attn2(S, cx, {"KT": KT, "ind": ind_d, "Vd": Vd, "QT": QT, "kbar": kbar, "cst": cst_d, "OT": OT, "b_OT": b_OT, "b_kv": b_kv})
    S.barrier_free(len(S._stack) - nbase)
    alloc_tile_bufs(S, cx)
    emit_proj_res_phase(S, cx, x1o, b_x1o, xa, b_xa, OT, b_OT, w_o, m[2], None, m[3], "wo", True, ntiles=9)
    S.barrier_free(len(S._stack) - ntile)
    alloc_ffn_bufs(S, cx)
    m = M[(0, 2)]
    emit_ffn_phase(S, cx, xa, b_xa, xb, b_xb, w_in[1], w_out[1], m[0], m[1], m[2], 0.5, m[3], 9, "f01")
    m = M[(1, 0)]
    emit_ffn_phase(S, cx, xb, b_xb, x4, b_x4t, w_in[2], w_out[2], m[0], m[1], m[2], 0.5, m[3], 9, "f10")
    S.barrier_free(len(S._stack) - ntile)
    m = M[(1, 1)]
    emit_glu_phase(S, cx, x4, b_x4t, w_pw1, m[0], m[1], m[3], uT, ntiles=9, hmask=hmask[:], b_hmask=b_hm, b_uout=b_u)
    S.barrier_free(len(S._stack) - ntile)
    b_x4c = [Buf("x4c_%d" % t) for t in range(16)]
    emit_conv_phase(S, cx, None, x4, b_x4c, x5, b_x5, w_pw2, ident_d, m[2], m[3], u_scr=uT, b_u=b_u)
    S.barrier_free(len(S._stack) - ntile)
    alloc_ffn_bufs(S, cx)
    m = M[(1, 2)]
    emit_ffn_phase(S, cx, x5, b_x5, xo, b_xo, w_in[3], w_out[3], m[0], m[1], m[2], 0.5, m[3], 8, "f11")
    S.emit()
    return nc


def kernel(**inp):
    import ml_dtypes
    bf = ml_dtypes.bfloat16
    inp = {k: np.asarray(v) for k, v in inp.items()}
    cores = list(range(8))
    ind = np.zeros((64, SEQ), bf)
    for m in range(64):
        ind[m, m * 256:(m + 1) * 256] = 1
    ident = np.eye(128, dtype=np.float32)
    maps = []
    toks = []
    for c in cores:
        b, j = c // 4, c % 4
        real, dup = pos_order(j)
        xb_ = inp["x"][b]
        xall = np.zeros((NPOS * 256, D), np.float32)
        for pi, g in enumerate(real + dup):
            if g is not None:
                xall[pi * 256:(pi + 1) * 256] = xb_[g * 256:(g + 1) * 256]
        cst, hmask = make_cst2(j)
        toks.append(np.concatenate([np.arange(g * 256, (g + 1) * 256) for g in snake_blocks(j)]))
        mp = {"sp": pack_small(inp, b), "ada_w": inp["ada_w"], "xall": np.ascontiguousarray(xall.T),
              "wqkv": inp["attn_w_qkv"][0], "w_o": inp["attn_w_o"][0], "w_pw1": inp["conv_w_pw1"][0], "w_pw2": inp["conv_w_pw2"][0],
              "ind": ind, "cst": cst, "hmask": hmask, "ident": ident}
        for k, (i, w) in enumerate(((0, 0), (0, 1), (1, 0), (1, 1))):
            mp["w_in%d" % k] = inp["ffn_w_in"][i, w]
            mp["w_out%d" % k] = inp["ffn_w_out"][i, w]
        maps.append(mp)
    nc = _get("fused", build_fused)
    res = run_bass_kernel_spmd(nc, maps, core_ids=cores).results
    out = np.zeros((BATCH, SEQ, D), np.float32)
    for c in cores:
        out[c // 4][toks[c]] = np.asarray(res[c]["xo"]).T
    return out
```

```python
import math
import numpy as np
import concourse.bass as bass
import concourse.mybir as mybir
from concourse.bass_utils import run_bass_kernel_spmd

F32 = mybir.dt.float32
BF16 = mybir.dt.bfloat16
AF = mybir.ActivationFunctionType
ALU = mybir.AluOpType
AX = mybir.AxisListType

D = 1024
KC = 8
DFF = 2816
FC = 22
NH = 16
DH = 64
BLK = 256
NB = 64
SEQ = 16384
BATCH = 2
TOK = 4096
NLB = 16
EPS = 1e-6
NEG = -30000.0
CW = 31


class Buf:
    __slots__ = ("name", "last_w", "readers")

    def __init__(self, name):
        self.name = name
        self.last_w = None
        self.readers = []


class Sched:
    DMA_ENGS = ("sp", "pool_dma")

    def __init__(self, nc, n_dma_sems=12):
        self.nc = nc
        self.ops = []
        self.engs = {"pe": nc.tensor, "act": nc.scalar, "dve": nc.vector,
                     "pool": nc.gpsimd, "sp": nc.sync, "pool_dma": nc.gpsimd}
        self.stream = {"pe": "pe", "act": "act", "dve": "dve", "pool": "pool",
                       "sp": "sp", "pool_dma": "pool"}
        self.n_dma_sems = n_dma_sems
        self._stack = []
        self.bar_list = []
        cm = nc.sbuf_tensor("bar_t", [128, 8], F32)
        self.bar_t = cm.__enter__()

    def sb(self, name, shape, dt):
        self._uid = getattr(self, "_uid", 0) + 1
        cm = self.nc.sbuf_tensor("s%d_%s" % (self._uid, name), shape, dt)
        t = cm.__enter__()
        self._stack.append(cm)
        return t

    def ps(self, name, shape, dt=F32):
        cm = self.nc.psum_tensor("p_" + name, shape, dt)
        t = cm.__enter__()
        self._stack.append(cm)
        return t

    def barrier_free(self, n_free):
        nc = self.nc
        bb = Buf("barrier%d" % len(self.ops))
        prev = list(range(len(self.ops)))
        i0 = self.op("pool", lambda: nc.gpsimd.memset(self.bar_t[:, 0:1], 0.0), writes=[bb])
        self.ops[i0]["deps"] = set(prev)
        for e, fn in (("dve", lambda: nc.vector.memset(self.bar_t[:, 1:2], 0.0)),
                      ("act", lambda: nc.scalar.activation(out=self.bar_t[:, 2:3], in_=self.bar_t[:, 0:1], func=AF.Copy)),
                      ("pe", None),
                      ("sp", lambda: nc.sync.dma_start(out=self.bar_t[:, 4:5], in_=self.bar_t[:, 0:1]))):
            if fn is None:
                continue
            self.op(e, fn, reads=[bb])
        self.bar_after = i0
        self.bar_list.append(i0)
        for _ in range(n_free):
            cm = self._stack.pop()
            cm.__exit__(None, None, None)

    def op(self, eng, fn, reads=(), writes=()):
        deps = set()
        idx = len(self.ops)
        for b in reads:
            if b.last_w is not None:
                deps.add(b.last_w)
        for b in writes:
            if b.last_w is not None:
                deps.add(b.last_w)
            for r in b.readers:
                deps.add(r)
        for b in reads:
            b.readers.append(idx)
        for b in writes:
            b.last_w = idx
            b.readers = []
        deps.discard(idx)
        if self.bar_list:
            deps.add(self.bar_list[-1])
        self.ops.append({"eng": eng, "fn": fn, "deps": deps, "dma": eng in self.DMA_ENGS})
        return idx

    def emit(self):
        nc = self.nc
        ops = self.ops
        n = len(ops)
        need = [False] * n
        for i, o in enumerate(ops):
            if o["dma"]:
                need[i] = True
            for d in o["deps"]:
                if ops[d]["dma"]:
                    continue
                sd, si = self.stream[ops[d]["eng"]], self.stream[o["eng"]]
                if sd != si or sd != "pe":
                    need[d] = True
        sem_cm = []

        def mksem(name):
            cm = nc.semaphore(name)
            s = cm.__enter__()
            sem_cm.append(cm)
            return s

        eng_sem = {k: mksem("s_" + k) for k in ("pe", "act", "dve", "pool")}
        eng_cnt = {k: 0 for k in eng_sem}
        dma_sems = {q: [mksem("d_%s_%d" % (q, i)) for i in range(self.n_dma_sems)] for q in self.DMA_ENGS}
        dma_cnt = {q: [0] * self.n_dma_sems for q in self.DMA_ENGS}
        dma_last = {q: [None] * self.n_dma_sems for q in self.DMA_ENGS}
        dma_rr = {q: 0 for q in self.DMA_ENGS}
        ev = [None] * n
        waited = {s: {} for s in ("pe", "act", "dve", "pool", "sp")}
        for i, o in enumerate(ops):
            eng = o["eng"]
            st = self.stream[eng]
            e = self.engs[eng]
            waits = {}
            for d in o["deps"]:
                if ev[d] is None:
                    continue
                sem, val, key = ev[d]
                if key not in waits or waits[key][1] < val:
                    waits[key] = (sem, val)
            my = None
            if o["dma"]:
                k = dma_rr[eng]
                dma_rr[eng] = (k + 1) % self.n_dma_sems
                prev = dma_last[eng][k]
                if prev is not None:
                    sem, val, key = ev[prev]
                    if key not in waits or waits[key][1] < val:
                        waits[key] = (sem, val)
                dma_cnt[eng][k] += 16
                my = (dma_sems[eng][k], dma_cnt[eng][k], (eng, k))
                dma_last[eng][k] = i
            elif need[i]:
                eng_cnt[st] += 1
                my = (eng_sem[st], eng_cnt[st], st)
            for key, (sem, val) in waits.items():
                if waited[st].get(key, 0) >= val:
                    continue
                e.wait_ge(sem, val)
                waited[st][key] = val
            inst = o["fn"]()
            if my is not None:
                inst.then_inc(my[0], 16 if o["dma"] else 1)
                ev[i] = my
            o["fn"] = None
        sp = nc.sync
        for q in self.DMA_ENGS:
            for k in range(self.n_dma_sems):
                if dma_cnt[q][k] > 0:
                    sp.wait_ge(dma_sems[q][k], dma_cnt[q][k])
        for k in eng_sem:
            if eng_cnt[k] > 0:
                sp.wait_ge(eng_sem[k], eng_cnt[k])
        self.max_counts = dict(eng_cnt)


def snake_blocks(j):
    out = []
    for i in range(8):
        out.append(8 * i + j)
        out.append(8 * i + 7 - j)
    return out


class Ctx:
    pass


def setup_common(S, cx):
    nc = S.nc
    cx.ones_bf = S.sb("ones_bf", [128, 128], BF16)
    cx.b_ones = Buf("ones")
    S.op("dve", lambda: nc.vector.memset(cx.ones_bf[:], 1.0), writes=[cx.b_ones])
    cx.psall = S.ps("psall", [128, 8 * 512], F32)
    cx.psum = [cx.psall[:, i * 512:(i + 1) * 512] for i in range(8)]
    cx.b_psum = [Buf("psb%d" % i) for i in range(8)]


def emit_mod_vectors(S, cx, modT, normg, layer_slot, j):
    nc = S.nc
    A = S.sb("modA_%s" % layer_slot, [128, 8], F32)
    b = Buf("modA_%s" % layer_slot)
    base = j * 24
    S.op("dve", lambda: nc.vector.scalar_tensor_tensor(
        out=A[:], in0=modT[:, base + 8:base + 16], scalar=1.0, in1=normg,
        op0=ALU.add, op1=ALU.mult), reads=[cx.b_mod], writes=[b])
    return A, modT[:, base:base + 8], modT[:, base + 16:base + 24], b


def emit_ffn_phase(S, cx, x_in, b_xin, x_out, b_xout, w_in_d, w_out_d, A, Bv, G, gscale, b_mod, ntiles, tag):
    nc = S.nc
    win = cx.win
    wout = cx.wout
    HF = FC // 2
    for kc in range(KC):
        S.op("pool_dma", (lambda kc=kc: nc.gpsimd.dma_start(out=win[:, kc, :], in_=w_in_d[kc * 128:(kc + 1) * 128, :])),
             writes=[cx.b_win[kc]])
    for f in range(FC):
        S.op("pool_dma", (lambda f=f: nc.gpsimd.dma_start(out=wout[:, f, :], in_=w_out_d[f * 128:(f + 1) * 128, :])),
             writes=[cx.b_wout[f]])
    Gs = S.sb("Gs_" + tag, [128, 8], F32)
    b_Gs = Buf("Gs_" + tag)
    S.op("dve", lambda: nc.vector.tensor_scalar(out=Gs[:], in0=G, scalar1=float(gscale), scalar2=None, op0=ALU.mult),
         reads=[b_mod], writes=[b_Gs])
    xv_in = x_in.rearrange("(c p) t -> p c t", p=128)
    xv_out = x_out.rearrange("(c p) t -> p c t", p=128)
    def _ld(t):
        S.op("sp", (lambda: nc.sync.dma_start(out=cx.xt[t % 2][:], in_=xv_in[:, :, t * 512:(t + 1) * 512])),
             reads=[b_xin[t]], writes=[cx.b_xt[t % 2]])
    hbufs = [cx.hA, cx.hB]
    b_hbufs = [cx.b_hA, cx.b_hB]
    _ld(0)
    emit_norm_mod(S, cx, cx.xt[0], cx.b_xt[0], A, Bv, b_mod, hb=hbufs[0], b_hb=b_hbufs[0])
    for t in range(ntiles):
        xt = cx.xt[t % 2]
        bx = cx.b_xt[t % 2]
        hcur = hbufs[t % 2]
        b_hcur = b_hbufs[t % 2]
        if t + 1 < ntiles:
            _ld(t + 1)
        for hf in range(2):
            if hf == 1 and t + 1 < ntiles:
                emit_norm_mod(S, cx, cx.xt[(t + 1) % 2], cx.b_xt[(t + 1) % 2], A, Bv, b_mod,
                              hb=hbufs[(t + 1) % 2], b_hb=b_hbufs[(t + 1) % 2])
            for fl in range(HF):
                f = hf * HF + fl
                pa = 1 + (f % 2) * 2
                pb = pa + 1

                def mm_a(f=f, pa=pa, hcur=hcur):
                    for kc in range(KC):
                        m = nc.tensor.matmul(cx.psum[pa][:], lhsT=win[:, kc, f * 128:(f + 1) * 128], rhs=hcur[:, kc, :],
                                             start=(kc == 0), stop=(kc == KC - 1))
                    return m

                def mm_b(f=f, pb=pb, hcur=hcur):
                    for kc in range(KC):
                        m = nc.tensor.matmul(cx.psum[pb][:], lhsT=win[:, kc, DFF + f * 128:DFF + (f + 1) * 128],
                                             rhs=hcur[:, kc, :], start=(kc == 0), stop=(kc == KC - 1))
                    return m
                S.op("pe", mm_a, reads=cx.b_win + [b_hcur], writes=[cx.b_psum[pa]])
                S.op("pe", mm_b, reads=cx.b_win + [b_hcur], writes=[cx.b_psum[pb]])
                sa = cx.sa[f % 2]
                S.op("act", (lambda sa=sa, pa=pa: nc.scalar.activation(out=sa[:], in_=cx.psum[pa][:], func=AF.Silu)),
                     reads=[cx.b_psum[pa]], writes=[cx.b_sa[f % 2]])
                S.op("dve", (lambda sa=sa, pb=pb, fl=fl: nc.vector.tensor_tensor(out=cx.u[:, fl, :], in0=sa[:], in1=cx.psum[pb][:], op=ALU.mult)),
                     reads=[cx.b_sa[f % 2], cx.b_psum[pb]], writes=[cx.b_u[fl]])
            for oc in range(KC):
                py = 5 + (oc % 2)

                def mm_y(oc=oc, py=py, hf=hf):
                    for fl in range(HF):
                        m = nc.tensor.matmul(cx.psum[py][:], lhsT=wout[:, hf * HF + fl, oc * 128:(oc + 1) * 128], rhs=cx.u[:, fl, :],
                                             start=(fl == 0), stop=(fl == HF - 1))
                    return m
                S.op("pe", mm_y, reads=cx.b_wout + cx.b_u, writes=[cx.b_psum[py]])
                S.op("dve", (lambda oc=oc, py=py, xt=xt: nc.vector.scalar_tensor_tensor(
                    out=xt[:, oc, :], in0=cx.psum[py][:], scalar=Gs[:, oc:oc + 1], in1=xt[:, oc, :],
                    op0=ALU.mult, op1=ALU.add)), reads=[cx.b_psum[py], b_Gs], writes=[bx])
        S.op("sp", (lambda xt=xt, t=t: nc.sync.dma_start(out=xv_out[:, :, t * 512:(t + 1) * 512], in_=xt[:])),
             reads=[bx], writes=[b_xout[t]])


def emit_norm_mod(S, cx, xt, bx, A, Bv, b_mod, n=512, hb=None, b_hb=None):
    nc = S.nc
    for c in range(KC):
        S.op("act", (lambda c=c: nc.scalar.activation(out=cx.sq[c % 2][:, :n], in_=xt[:, c, :n], func=AF.Square)),
             reads=[bx], writes=[cx.b_sq[c % 2]])
        S.op("pe", (lambda c=c: nc.tensor.matmul(cx.psum[0][:, :n], lhsT=cx.ones_bf[:], rhs=cx.sq[c % 2][:, :n],
                                                 start=(c == 0), stop=(c == KC - 1))),
             reads=[cx.b_sq[c % 2], cx.b_ones], writes=[cx.b_psum[0]])
    S.op("dve", lambda: nc.vector.tensor_scalar(out=cx.rstd[:, :n], in0=cx.psum[0][:, :n], scalar1=1.0 / D, scalar2=EPS,
                                                op0=ALU.mult, op1=ALU.add), reads=[cx.b_psum[0]], writes=[cx.b_rstd])
    S.op("act", lambda: nc.scalar.activation(out=cx.rstd[:, :n], in_=cx.rstd[:, :n], func=AF.Sqrt), reads=[cx.b_rstd], writes=[cx.b_rstd])
    S.op("dve", lambda: nc.vector.reciprocal(out=cx.rstd[:, :n], in_=cx.rstd[:, :n]), reads=[cx.b_rstd], writes=[cx.b_rstd])
    for c in range(KC):
        tmp = cx.tmp[c % 2]
        S.op("dve", (lambda c=c, tmp=tmp: nc.vector.tensor_tensor(out=tmp[:, :n], in0=xt[:, c, :n], in1=cx.rstd[:, :n], op=ALU.mult)),
             reads=[bx, cx.b_rstd], writes=[cx.b_tmp[c % 2]])
        if hb is None:
            S.op("act", (lambda c=c, tmp=tmp: nc.scalar.activation(out=cx.h[:, c, :n], in_=tmp[:, :n], func=AF.Identity,
                                                                   bias=Bv[:, c:c + 1], scale=A[:, c:c + 1])),
                 reads=[cx.b_tmp[c % 2], b_mod], writes=[cx.b_h])
        else:
            S.op("act", (lambda c=c, tmp=tmp, hb=hb: nc.scalar.activation(out=hb[:, c, :n], in_=tmp[:, :n], func=AF.Identity,
                                                                          bias=Bv[:, c:c + 1], scale=A[:, c:c + 1])),
                 reads=[cx.b_tmp[c % 2], b_mod], writes=[b_hb])


def alloc_tile_bufs(S, cx):
    cx.xt = [S.sb("xt%d" % i, [128, KC, 512], F32) for i in range(2)]
    cx.b_xt = [Buf("xt%d" % i) for i in range(2)]
    cx.sq = [S.sb("sq%d" % i, [128, 512], BF16) for i in range(2)]
    cx.b_sq = [Buf("sq%d" % i) for i in range(2)]
    cx.h = S.sb("h", [128, KC, 512], BF16)
    cx.b_h = Buf("h")
    cx.rstd = S.sb("rstd", [128, 512], F32)
    cx.b_rstd = Buf("rstd")
    cx.tmp = [S.sb("tmp%d" % i, [128, 512], F32) for i in range(2)]
    cx.b_tmp = [Buf("tmp%d" % i) for i in range(2)]
    cx.sa = [S.sb("sa%d" % i, [128, 512], F32) for i in range(2)]
    cx.b_sa = [Buf("sa%d" % i) for i in range(2)]


def alloc_ffn_bufs(S, cx):
    cx.win = S.sb("win", [128, KC, 2 * DFF], BF16)
    cx.wout = S.sb("wout", [128, FC, D], BF16)
    cx.b_win = [Buf("win%d" % i) for i in range(KC)]
    cx.b_wout = [Buf("wout%d" % i) for i in range(FC)]
    cx.u = S.sb("u", [128, FC // 2, 512], BF16)
    cx.b_u = [Buf("u%d" % i) for i in range(FC // 2)]
    cx.hA = cx.h
    cx.b_hA = cx.b_h
    cx.hB = S.sb("hB", [128, KC, 512], BF16)
    cx.b_hB = Buf("hB")


def _fm(v, n=None):
    v = np.asarray(v, np.float32).reshape(-1, 128)
    return np.ascontiguousarray(v.T)


SP_OFF = {}


def _sp_layout():
    off = 0
    for name, w in (("c", 8), ("ada_b0", 72), ("ada_b1", 72),
                    ("ng00", 8), ("ng01", 8), ("ng02", 8), ("ng10", 8), ("ng11", 8), ("ng12", 8),
                    ("gq", 1), ("gk", 1), ("b_pw1", 16), ("b_dw", 8), ("ln_g", 8), ("ln_b", 8),
                    ("b_pw2", 8), ("w_dw", 8 * CW)):
        SP_OFF[name] = (off, w)
        off += w
    return off


SP_N = _sp_layout()


def pack_small(inp, b):
    sp = np.zeros((128, SP_N), np.float32)

    def put(name, arr):
        o, w = SP_OFF[name]
        assert arr.shape == (128, w), (name, arr.shape, w)
        sp[:, o:o + w] = arr
    put("c", _fm(inp["c"][b]))
    put("ada_b0", _fm(inp["ada_b"][0]))
    put("ada_b1", _fm(inp["ada_b"][1]))
    for i in range(2):
        for j in range(3):
            put("ng%d%d" % (i, j), _fm(inp["norm_g"][i, j]))
    put("gq", np.tile(np.asarray(inp["attn_g_q"][0], np.float32), 2).reshape(128, 1))
    put("gk", np.tile(np.asarray(inp["attn_g_k"][0], np.float32), 2).reshape(128, 1))
    put("b_pw1", _fm(inp["conv_b_pw1"][0]))
    put("b_dw", _fm(inp["conv_b_dw"][0]))
    put("ln_g", _fm(inp["conv_ln_g"][0]))
    put("ln_b", _fm(inp["conv_ln_b"][0]))
    put("b_pw2", _fm(inp["conv_b_pw2"][0]))
    wd = np.asarray(inp["conv_w_dw"][0], np.float32)
    put("w_dw", np.ascontiguousarray(wd.reshape(CW, 8, 128).transpose(2, 1, 0)).reshape(128, 8 * CW))
    return sp


def spc(cx, name):
    o, w = SP_OFF[name]
    return cx.spt[:, o:o + w]


def emit_load_small(S, cx, sp_d):
    nc = S.nc
    cx.spt = S.sb("spt", [128, SP_N], F32)
    cx.b_sp = Buf("spt")
    S.op("sp", lambda: nc.sync.dma_start(out=cx.spt[:], in_=sp_d[:, :]), writes=[cx.b_sp])


def emit_adaln(S, cx, ada_w_d, modT, b_modT):
    nc = S.nc
    NCOL = 1152
    stg = [S.sb("adastg%d" % i, [128, KC, NCOL], F32) for i in range(2)]
    b_stg = [Buf("adastg%d" % i) for i in range(2)]
    cact = S.sb("cact", [128, 8], F32)
    b_cact = Buf("cact")
    S.op("act", lambda: nc.scalar.activation(out=cact[:], in_=spc(cx, "c"), func=AF.Silu), reads=[cx.b_sp], writes=[b_cact])
    k = 0
    for i in range(2):
        wv = ada_w_d[i].rearrange("(kc p) n -> p kc n", p=128)
        for pc in range(9216 // NCOL):
            st = stg[k % 2]
            bs = b_stg[k % 2]
            k += 1
            S.op("sp", (lambda st=st, wv=wv, pc=pc: nc.sync.dma_start(out=st[:], in_=wv[:, :, pc * NCOL:(pc + 1) * NCOL])), writes=[bs])

            def mm(st=st, pc=pc, i=i):
                for m in range(NCOL // 128):
                    col = pc * (NCOL // 128) + m
                    for kc in range(KC):
                        r = nc.tensor.matmul(cx.psum[7][:, col:col + 1], lhsT=st[:, kc, m * 128:(m + 1) * 128], rhs=cact[:, kc:kc + 1],
                                             start=(kc == 0), stop=(kc == KC - 1))
                return r
            S.op("pe", mm, reads=[bs, b_cact], writes=[cx.b_psum[7]])
        S.op("dve", (lambda i=i: nc.vector.tensor_tensor(out=modT[i][:], in0=cx.psum[7][:, 0:72], in1=spc(cx, "ada_b%d" % i), op=ALU.add)),
             reads=[cx.b_psum[7], cx.b_sp], writes=[b_modT[i]])
    return stg


def joint_mod(S, cx, i, j, modT, b_modT):
    nc = S.nc
    A, Bv, G, bA = emit_mod_vectors_i(S, cx, modT[i], b_modT[i], spc(cx, "ng%d%d" % (i, j)), "l%ds%d" % (i, j), j)
    return A, Bv, G, bA


def emit_mod_vectors_i(S, cx, modT, b_modT, normg, tag, j):
    nc = S.nc
    AB = S.sb("modAB_" + tag, [128, 24], F32)
    b = Buf("modAB_" + tag)
    base = j * 24
    S.op("dve", lambda: nc.vector.tensor_copy(out=AB[:], in_=modT[:, base:base + 24]), reads=[b_modT, cx.b_sp], writes=[b])
    S.op("dve", lambda: nc.vector.scalar_tensor_tensor(
        out=AB[:, 8:16], in0=modT[:, base + 8:base + 16], scalar=1.0, in1=normg,
        op0=ALU.add, op1=ALU.mult), reads=[b_modT, cx.b_sp], writes=[b])
    return AB[:, 8:16], AB[:, 0:8], AB[:, 16:24], b


def load_x_tile(S, cx, xv, b_x, t, n=512):
    nc = S.nc
    xt = cx.xt[t % 2]
    bx = cx.b_xt[t % 2]
    S.op("sp", (lambda: nc.sync.dma_start(out=xt[:, :, :n], in_=xv[:, :, t * n:(t + 1) * n])), reads=[b_x[t]], writes=[bx])
    return xt, bx


def emit_qkv_phase(S, cx, x_d, b_x, wqkv_d, A, Bv, b_mod, QT_d, KT_d, kbar_d, V_d):
    nc = S.nc
    wq = S.sb("wqkv_sb", [128, KC, 3072], BF16)
    b_wq = [Buf("wqkv%d" % k) for k in range(KC)]
    for kc in range(KC):
        S.op("pool_dma", (lambda kc=kc: nc.gpsimd.dma_start(out=wq[:, kc, :], in_=wqkv_d[kc * 128:(kc + 1) * 128, :])), writes=[b_wq[kc]])
    bo = S.sb("blkones", [128, 128], BF16)
    b_bo = Buf("blkones")
    S.op("dve", lambda: nc.vector.memset(bo[:], 0.0), writes=[b_bo])
    S.op("dve", lambda: nc.vector.memset(bo[0:64, 0:64], 1.0), writes=[b_bo])
    S.op("dve", lambda: nc.vector.memset(bo[64:128, 64:128], 1.0), writes=[b_bo])
    g8 = S.sb("g8", [128, 2], F32)
    b_g8 = Buf("g8")
    S.op("dve", lambda: nc.vector.tensor_scalar(out=g8[:, 0:1], in0=spc(cx, "gq"), scalar1=0.125, scalar2=None, op0=ALU.mult),
         reads=[cx.b_sp], writes=[b_g8])
    S.op("dve", lambda: nc.vector.tensor_copy(out=g8[:, 1:2], in_=spc(cx, "gk")), reads=[cx.b_sp], writes=[b_g8])
    qo = [S.sb("qo%d" % i, [128, 512], BF16) for i in range(2)]
    b_qo = [Buf("qo%d" % i) for i in range(2)]
    kb = [S.sb("kb%d" % i, [128, 2], F32) for i in range(2)]
    b_kb = [Buf("kb%d" % i) for i in range(2)]
    vt = [S.sb("vt%d" % i, [128, 1024], BF16) for i in range(2)]
    b_vt = [Buf("vt%d" % i) for i in range(2)]
    xv = x_d.rearrange("(c p) t -> p c t", p=128)
    QTv = QT_d.rearrange("h d t -> (h d) t")
    KTv = KT_d.rearrange("h d t -> (h d) t")
    kbv = kbar_d.rearrange("h d l -> (h d) l")
    cnt = 0
    for t in range(8):
        xt, bx = load_x_tile(S, cx, xv, b_x, t)
        emit_norm_mod(S, cx, xt, bx, A, Bv, b_mod)
        for which in range(2):
            goff = which * 1024
            outv = QTv if which == 0 else KTv
            for hp in range(8):
                k = cnt % 2
                cnt += 1
                pq = cx.psum[1 + k]
                pss = cx.psum[3 + k]

                def mm(hp=hp, pq=pq, goff=goff):
                    for kc in range(KC):
                        m = nc.tensor.matmul(pq[:], lhsT=wq[:, kc, goff + hp * 128:goff + (hp + 1) * 128], rhs=cx.h[:, kc, :],
                                             start=(kc == 0), stop=(kc == KC - 1))
                    return m
                S.op("pe", mm, reads=b_wq + [cx.b_h], writes=[cx.b_psum[1 + k]])
                S.op("act", (lambda k=k, pq=pq: nc.scalar.activation(out=cx.sq[k][:], in_=pq[:], func=AF.Square)),
                     reads=[cx.b_psum[1 + k]], writes=[cx.b_sq[k]])
                S.op("pe", (lambda k=k, pss=pss: nc.tensor.matmul(pss[:], lhsT=bo[:], rhs=cx.sq[k][:], start=True, stop=True)),
                     reads=[cx.b_sq[k], b_bo], writes=[cx.b_psum[3 + k]])
                r1 = cx.tmp[k]
                S.op("dve", (lambda r1=r1, pss=pss: nc.vector.tensor_scalar(out=r1[:], in0=pss[:], scalar1=1.0 / DH, scalar2=EPS,
                                                                            op0=ALU.mult, op1=ALU.add)),
                     reads=[cx.b_psum[3 + k]], writes=[cx.b_tmp[k]])
                S.op("act", (lambda r1=r1: nc.scalar.activation(out=r1[:], in_=r1[:], func=AF.Sqrt)), reads=[cx.b_tmp[k]], writes=[cx.b_tmp[k]])
                S.op("dve", (lambda r1=r1: nc.vector.reciprocal(out=r1[:], in_=r1[:])), reads=[cx.b_tmp[k]], writes=[cx.b_tmp[k]])
                S.op("dve", (lambda r1=r1, pq=pq: nc.vector.tensor_tensor(out=r1[:], in0=pq[:], in1=r1[:], op=ALU.mult)),
                     reads=[cx.b_tmp[k], cx.b_psum[1 + k]], writes=[cx.b_tmp[k]])
                sa = cx.sa[k]
                S.op("act", (lambda r1=r1, sa=sa, which=which: nc.scalar.activation(out=sa[:], in_=r1[:], func=AF.Copy,
                                                                                  scale=g8[:, which:which + 1])),
                     reads=[cx.b_tmp[k], b_g8], writes=[cx.b_sa[k]])
                S.op("pool", (lambda k=k, sa=sa: nc.gpsimd.tensor_copy(out=qo[k][:], in_=sa[:])), reads=[cx.b_sa[k]], writes=[b_qo[k]])
                S.op("sp", (lambda k=k, hp=hp, t=t, outv=outv: nc.sync.dma_start(out=outv[hp * 128:(hp + 1) * 128, t * 512:(t + 1) * 512], in_=qo[k][:])),
                     reads=[b_qo[k]])
                if which == 1:
                    S.op("dve", (lambda k=k, sa=sa: nc.vector.tensor_reduce(out=kb[k][:], in_=sa[:].rearrange("p (b t) -> p b t", b=2),
                                                                          axis=AX.X, op=ALU.add)),
                         reads=[cx.b_sa[k]], writes=[b_kb[k]])
                    S.op("dve", (lambda k=k: nc.vector.tensor_scalar(out=kb[k][:], in0=kb[k][:], scalar1=1.0 / BLK, scalar2=None, op0=ALU.mult)),
                         reads=[b_kb[k]], writes=[b_kb[k]])
                    S.op("sp", (lambda k=k, hp=hp, t=t: nc.sync.dma_start(out=kbv[hp * 128:(hp + 1) * 128, 2 * t:2 * t + 2], in_=kb[k][:])),
                         reads=[b_kb[k]])
        for tg in range(4):
            v = vt[tg % 2]
            for ns in range(2):
                pv = cx.psum[5 + ns]

                def mmv(tg=tg, ns=ns, pv=pv):
                    for kc in range(KC):
                        m = nc.tensor.matmul(pv[:], lhsT=cx.h[:, kc, tg * 128:(tg + 1) * 128], rhs=wq[:, kc, 2048 + ns * 512:2048 + (ns + 1) * 512],
                                             start=(kc == 0), stop=(kc == KC - 1))
                    return m
                S.op("pe", mmv, reads=b_wq + [cx.b_h], writes=[cx.b_psum[5 + ns]])
                if ns == 0:
                    S.op("act", (lambda v=v, pv=pv: nc.scalar.copy(out=v[:, 0:512], in_=pv[:])), reads=[cx.b_psum[5]], writes=[b_vt[tg % 2]])
                else:
                    S.op("dve", (lambda v=v, pv=pv: nc.vector.tensor_copy(out=v[:, 512:1024], in_=pv[:])), reads=[cx.b_psum[6]], writes=[b_vt[tg % 2]])
            S.op("sp", (lambda v=v, tg=tg, t=t: nc.sync.dma_start(out=V_d[t * 512 + tg * 128:t * 512 + (tg + 1) * 128, :], in_=v[:])),
                 reads=[b_vt[tg % 2]])


def build_L1():
    nc = bass.Bass("TRN2", target_bir_lowering=False)
    sp_d = nc.dram_tensor("sp", [128, SP_N], F32, kind="ExternalInput").ap()
    ada_w = nc.dram_tensor("ada_w", [2, 1024, 9216], F32, kind="ExternalInput").ap()
    xin = nc.dram_tensor("xin", [1024, TOK], F32, kind="ExternalInput").ap()
    w_in = nc.dram_tensor("w_in", [1024, 2 * DFF], F32, kind="ExternalInput").ap()
    w_out = nc.dram_tensor("w_out", [DFF, 1024], F32, kind="ExternalInput").ap()
    wqkv = nc.dram_tensor("wqkv", [1024, 3072], F32, kind="ExternalInput").ap()
    x1 = nc.dram_tensor("x1", [1024, TOK], F32, kind="ExternalOutput").ap()
    modo = nc.dram_tensor("modo", [128, 144], F32, kind="ExternalOutput").ap()
    QT = nc.dram_tensor("QT", [NH, DH, TOK], BF16, kind="ExternalOutput").ap()
    KT = nc.dram_tensor("KT", [NH, DH, TOK], BF16, kind="ExternalOutput").ap()
    kbar = nc.dram_tensor("kbar", [NH, DH, NLB], F32, kind="ExternalOutput").ap()
    V = nc.dram_tensor("V", [TOK, 1024], BF16, kind="ExternalOutput").ap()
    S = Sched(nc)
    cx = Ctx()
    setup_common(S, cx)
    emit_load_small(S, cx, sp_d)
    modT = [S.sb("modT%d" % i, [128, 72], F32) for i in range(2)]
    b_modT = [Buf("modT%d" % i) for i in range(2)]
    A0, B0, G0, bm0 = None, None, None, None
    AB0 = joint_mod
    emit_adaln_scoped(S, cx, ada_w, modT, b_modT)
    for i in range(2):
        S.op("sp", (lambda i=i: nc.sync.dma_start(out=modo[:, i * 72:(i + 1) * 72], in_=modT[i][:])), reads=[b_modT[i]])
    A, Bv, G, bA = emit_mod_vectors_i(S, cx, modT[0], b_modT[0], spc(cx, "ng00"), "l0s0", 0)
    A1, Bv1, G1, bA1 = emit_mod_vectors_i(S, cx, modT[0], b_modT[0], spc(cx, "ng01"), "l0s1", 1)
    alloc_tile_bufs(S, cx)
    alloc_ffn_bufs(S, cx)
    b_xin = [Buf("xin%d" % t) for t in range(8)]
    b_x1 = [Buf("x1_%d" % t) for t in range(8)]
    emit_ffn_phase(S, cx, xin, b_xin, x1, b_x1, w_in, w_out, A, Bv, G, 0.5, bA, 8, "f00")
    S.barrier_free(3)
    emit_qkv_phase(S, cx, x1, b_x1, wqkv, A1, Bv1, bA1, QT, KT, kbar, V)
    S.emit()
    return nc


def emit_adaln_scoped(S, cx, ada_w, modT, b_modT):
    n0 = len(S._stack)
    emit_adaln(S, cx, ada_w, modT, b_modT)
    S.barrier_free(len(S._stack) - n0)


SLOPES = [2.0 ** (-8.0 * (h + 1) / NH) for h in range(NH)]


def emit_attn_phase(S, cx, d):
    nc = S.nc
    KTa = [S.sb("KTa%d" % i, [128, SEQ], BF16) for i in range(2)]
    Va = [S.sb("Va%d" % i, [128, 128, 65], BF16) for i in range(2)]
    Qa = [S.sb("Qa%d" % i, [128, TOK], BF16) for i in range(2)]
    Ko = [S.sb("Ko%d" % i, [128, TOK], BF16) for i in range(2)]
    Vo = [S.sb("Vo%d" % i, [128, 32, 65], BF16) for i in range(2)]
    bT = [S.sb("bT%d" % i, [128, 1024], F32) for i in range(2)]
    dgb = [S.sb("dgb%d" % i, [128, 512], F32) for i in range(2)]
    cq = [S.sb("cq%d" % i, [128, 256], F32) for i in range(2)]
    b_KT = [Buf("KTa%d" % i) for i in range(2)]
    b_Va = [Buf("Va%d" % i) for i in range(2)]
    b_Qa = [Buf("Qa%d" % i) for i in range(2)]
    b_Qm = [[Buf("Qm%d_%d" % (i, l)) for l in range(NLB)] for i in range(2)]
    b_Ko = [Buf("Ko%d" % i) for i in range(2)]
    b_Vo = [Buf("Vo%d" % i) for i in range(2)]
    b_hd = [Buf("hd%d" % i) for i in range(2)]
    cst = S.sb("cst", [128, CST_N], F32)
    b_cst = Buf("cst")
    S.op("sp", lambda: nc.sync.dma_start(out=cst[:], in_=d["cst"][:, :]), writes=[b_cst])

    def C(name):
        o, w = CST_OFF[name]
        return cst[:, o:o + w]
    kb32 = S.sb("kb32", [64, NH * NB], F32)
    kbh = S.sb("kbh", [64, NH * NB], BF16)
    kbl = S.sb("kbl", [64, NH * NB], BF16)
    kbt = S.sb("kbt", [64, NH * NB], F32)
    b_kb = Buf("kbhl")
    S.op("sp", lambda: nc.sync.dma_start(out=kb32[:], in_=d["kbar"].rearrange("d h m -> d (h m)")), writes=[b_kb])
    S.op("dve", lambda: nc.vector.tensor_copy(out=kbh[:], in_=kb32[:]), reads=[b_kb], writes=[b_kb])
    S.op("dve", lambda: nc.vector.tensor_copy(out=kbt[:], in_=kbh[:]), reads=[b_kb], writes=[b_kb])
    S.op("dve", lambda: nc.vector.tensor_tensor(out=kbt[:], in0=kb32[:], in1=kbt[:], op=ALU.subtract), reads=[b_kb], writes=[b_kb])
    S.op("dve", lambda: nc.vector.tensor_copy(out=kbl[:], in_=kbt[:]), reads=[b_kb], writes=[b_kb])
    mbp = [S.sb("mbp%d" % i, [128, 128], F32) for i in range(2)]
    b_mbp = [Buf("mbp%d" % i) for i in range(2)]
    gmt = [S.sb("gmt%d" % i, [128, 64], F32) for i in range(2)]
    b_gmt = [Buf("gmt%d" % i) for i in range(2)]
    t8 = [S.sb("t8_%d" % i, [128, 8], F32) for i in range(2)]
    pt = [S.sb("pt%d" % i, [128, 512], BF16) for i in range(2)]
    b_pt = [Buf("pt%d" % i) for i in range(2)]
    pt2 = S.sb("pt2", [128, 512], BF16)
    b_pt2 = Buf("pt2")
    dtmp = S.sb("dtmp", [128, 512], F32)
    b_dtmp = Buf("dtmp")
    o1 = S.sb("o1", [128, 256], F32)
    o2 = S.sb("o2", [128, 256], F32)
    rd = S.sb("rd", [128, 256], F32)
    ob = [S.sb("ob%d" % i, [128, 256], BF16) for i in range(2)]
    b_o = Buf("o12")
    b_ob = [Buf("ob%d" % i) for i in range(2)]
    for i in range(2):
        S.op("sp", (lambda i=i: nc.sync.dma_start(out=KTa[i][64:128, :], in_=d["ind"][:, :])), writes=[b_KT[i]])
        S.op("dve", (lambda i=i: nc.vector.memset(Va[i][:, :, 64:65], 1.0)), writes=[b_Va[i]])
        S.op("dve", (lambda i=i: nc.vector.memset(Vo[i][:, :, 64:65], 1.0)), writes=[b_Vo[i]])
        S.op("dve", (lambda i=i: nc.vector.memset(mbp[i][:], 0.0)), writes=[b_mbp[i]])
    ps = cx.psum
    bp = cx.b_psum
    scnt = 0
    gcnt = 0
    for h in range(NH):
        cur = h % 2
        sl = SLOPES[h]
        es = math.exp(sl)
        S.op("sp", (lambda h=h, cur=cur: nc.sync.dma_start(out=KTa[cur][0:64, :], in_=d["KT"][h])), writes=[b_KT[cur]])
        for q8 in range(8):
            S.op("sp", (lambda h=h, cur=cur, q8=q8: nc.sync.dma_start(out=Va[cur][:, q8 * 16:(q8 + 1) * 16, 0:64], in_=d["Vd"][h, :, q8 * 16:(q8 + 1) * 16, :])),
                 writes=[b_Va[cur]])
        S.op("sp", (lambda h=h, cur=cur: nc.sync.dma_start(out=Qa[cur][0:64, :], in_=d["QT"][h])), writes=[b_Qa[cur]] + b_Qm[cur])
        S.op("sp", (lambda h=h, cur=cur: nc.sync.dma_start(out=Ko[cur][0:64, :], in_=d["KTo"][h])), writes=[b_Ko[cur]])
        for q2 in range(2):
            S.op("sp", (lambda h=h, cur=cur, q2=q2: nc.sync.dma_start(out=Vo[cur][:, q2 * 16:(q2 + 1) * 16, 0:64], in_=d["Vo"][h, :, q2 * 16:(q2 + 1) * 16, :])),
                 writes=[b_Vo[cur]])
        for (VV, bV) in ((Va[cur], b_Va[cur]), (Vo[cur], b_Vo[cur])):
            Vodd = VV[:].rearrange("p (n two) e -> p n two e", two=2)[:, :, 1, :]
            S.op("dve", (lambda Vodd=Vodd, es=es: nc.vector.tensor_scalar(out=Vodd[:, :, 0:64], in0=Vodd[:, :, 0:64], scalar1=es, scalar2=None, op0=ALU.mult)),
                 reads=[bV], writes=[bV])
            S.op("dve", (lambda Vodd=Vodd, es=es: nc.vector.memset(Vodd[:, :, 64:65], es)), reads=[bV], writes=[bV])
        S.op("dve", (lambda cur=cur, sl=sl: nc.vector.tensor_scalar(out=bT[cur][:], in0=C("base"), scalar1=sl, scalar2=None, op0=ALU.mult)),
             reads=[b_cst], writes=[b_hd[cur]])
        S.op("dve", (lambda cur=cur, sl=sl: nc.vector.scalar_tensor_tensor(out=dgb[cur][:], in0=C("Dm"), scalar=-sl, in1=C("causal"),
                                                                         op0=ALU.mult, op1=ALU.add)), reads=[b_cst], writes=[b_hd[cur]])
        S.op("act", (lambda cur=cur, sl=sl: nc.scalar.activation(out=cq[cur][:], in_=C("irow"), func=AF.Exp, scale=-sl)),
             reads=[b_cst], writes=[b_hd[cur]])
        for l in range(NLB):
            for hq in range(2):
                g = gcnt % 2
                gcnt += 1
                q0 = l * 256 + hq * 128

                def mmg(cur=cur, q0=q0, h=h):
                    nc.tensor.matmul(ps[6][:, 0:64], lhsT=Qa[cur][0:64, q0:q0 + 128], rhs=kbh[:, h * 64:(h + 1) * 64], start=True, stop=False)
                    return nc.tensor.matmul(ps[6][:, 0:64], lhsT=Qa[cur][0:64, q0:q0 + 128], rhs=kbl[:, h * 64:(h + 1) * 64], start=False, stop=True)
                S.op("pe", mmg, reads=[b_Qa[cur], b_kb], writes=[bp[6]])
                pb = C("past")[:, l * 64:(l + 1) * 64]
                S.op("dve", (lambda g=g, pb=pb: nc.vector.tensor_tensor(out=gmt[g][:], in0=ps[6][:, 0:64], in1=pb, op=ALU.add)),
                     reads=[bp[6], b_cst], writes=[b_gmt[g]])
                S.op("dve", (lambda g=g: nc.vector.max(out=t8[g][:], in_=gmt[g][:])), reads=[b_gmt[g]], writes=[b_gmt[g]])
                S.op("dve", (lambda g=g: nc.vector.tensor_scalar(out=gmt[g][:], in0=gmt[g][:], scalar1=t8[g][:, 2:3], scalar2=-NEG,
                                                                 op0=ALU.is_ge, op1=ALU.mult)), reads=[b_gmt[g]], writes=[b_gmt[g]])
                S.op("dve", (lambda g=g, pb=pb: nc.vector.scalar_tensor_tensor(out=mbp[g][:, 64:128], in0=gmt[g][:], scalar=NEG, in1=pb,
                                                                             op0=ALU.add, op1=ALU.add)),
                     reads=[b_gmt[g], b_cst], writes=[b_mbp[g]])
                S.op("pe", (lambda g=g: nc.tensor.transpose(ps[7][:, 0:128], mbp[g][:], C("ident"))), reads=[b_mbp[g], b_cst], writes=[bp[7]])
                S.op("act", (lambda cur=cur, q0=q0: nc.scalar.copy(out=Qa[cur][64:128, q0:q0 + 128], in_=ps[7][64:128, 0:128])),
                     reads=[bp[7]], writes=[b_Qm[cur][l]])
        for i in range(8):
            nk = 8 * i + 7
            for X in (2 * i, 2 * i + 1):
                po = 2 + (X % 2)
                for n in range(nk):
                    sb_ = scnt % 2
                    scnt += 1

                    def mms(cur=cur, X=X, n=n, sb_=sb_):
                        for c in range(2):
                            m = nc.tensor.matmul(ps[sb_][:, c * 256:(c + 1) * 256], lhsT=KTa[cur][:, n * 256 + c * 128:n * 256 + (c + 1) * 128],
                                                 rhs=Qa[cur][:, X * 256:(X + 1) * 256], start=True, stop=True)
                        return m
                    S.op("pe", mms, reads=[b_KT[cur], b_Qa[cur], b_Qm[cur][X]], writes=[bp[sb_]])
                    S.op("act", (lambda cur=cur, X=X, n=n, sb_=sb_: nc.scalar.activation(out=pt[sb_][:], in_=ps[sb_][:], func=AF.Exp,
                                                                                        bias=bT[cur][:, X * 64 + n:X * 64 + n + 1])),
                         reads=[bp[sb_], b_hd[cur]], writes=[b_pt[sb_]])

                    def mmpv(cur=cur, n=n, sb_=sb_, po=po, nk=nk):
                        for c in range(2):
                            m = nc.tensor.matmul(ps[po][0:65, 0:256], lhsT=Va[cur][:, 2 * n + c, :], rhs=pt[sb_][:, c * 256:(c + 1) * 256],
                                                 start=(n == 0 and c == 0), stop=(n == nk - 1 and c == 1))
                        return m
                    S.op("pe", mmpv, reads=[b_Va[cur], b_pt[sb_]], writes=[bp[po]])

                def mmd(cur=cur, X=X):
                    for c in range(2):
                        m = nc.tensor.matmul(ps[4][:, c * 256:(c + 1) * 256], lhsT=Ko[cur][0:64, X * 256 + c * 128:X * 256 + (c + 1) * 128],
                                             rhs=Qa[cur][0:64, X * 256:(X + 1) * 256], start=True, stop=True)
                    return m
                S.op("pe", mmd, reads=[b_Ko[cur], b_Qa[cur]], writes=[bp[4]])
                S.op("dve", (lambda cur=cur: nc.vector.tensor_tensor(out=dtmp[:], in0=ps[4][:], in1=dgb[cur][:], op=ALU.add)),
                     reads=[bp[4], b_hd[cur]], writes=[b_dtmp])
                S.op("act", (lambda: nc.scalar.activation(out=pt2[:], in_=dtmp[:], func=AF.Exp)), reads=[b_dtmp], writes=[b_pt2])

                def mmpo(cur=cur, X=X):
                    for c in range(2):
                        m = nc.tensor.matmul(ps[5][0:65, 0:256], lhsT=Vo[cur][:, 2 * X + c, :], rhs=pt2[:, c * 256:(c + 1) * 256],
                                             start=(c == 0), stop=(c == 1))
                    return m
                S.op("pe", mmpo, reads=[b_Vo[cur], b_pt2], writes=[bp[5]])
                S.op("dve", (lambda cur=cur, po=po: nc.vector.tensor_tensor(out=o1[0:65, :], in0=ps[po][0:65, 0:256], in1=cq[cur][0:65, :], op=ALU.mult)),
                     reads=[bp[po], b_hd[cur]], writes=[b_o])
                S.op("dve", (lambda: nc.vector.tensor_tensor(out=o2[0:65, :], in0=o1[0:65, :], in1=ps[5][0:65, 0:256], op=ALU.add)),
                     reads=[bp[5], b_o], writes=[b_o])
                S.op("pe", (lambda: nc.tensor.matmul(ps[6][0:64, 0:256], lhsT=C("sel65")[0:65, :], rhs=o2[0:65, :], start=True, stop=True)),
                     reads=[b_o, b_cst], writes=[bp[6]])
                S.op("dve", (lambda: nc.vector.reciprocal(out=rd[0:64, :], in_=ps[6][0:64, 0:256])), reads=[bp[6]], writes=[b_o])
                k = X % 2
                S.op("dve", (lambda k=k: nc.vector.tensor_tensor(out=ob[k][0:64, :], in0=o2[0:64, :], in1=rd[0:64, :], op=ALU.mult)),
                     reads=[b_o], writes=[b_ob[k]])
                S.op("sp", (lambda k=k, h=h, X=X: nc.sync.dma_start(out=d["OT"][h, :, X * 256:(X + 1) * 256], in_=ob[k][0:64, :])),
                     reads=[b_ob[k]], writes=[d["b_OT"][X // 2]])


CST_OFF = {}


def _cst_layout():
    off = 0
    for name, w in (("past", 1024), ("base", 1024), ("irow", 256), ("Dm", 512), ("causal", 512), ("ident", 128), ("sel65", 64)):
        CST_OFF[name] = (off, w)
        off += w
    return off


CST_N = _cst_layout()


def make_cst(j):
    blocks = snake_blocks(j)
    c = np.zeros((128, CST_N), np.float32)
    p = np.arange(128, dtype=np.float32)[:, None]
    past = np.zeros((16, 64), np.float32)
    base = np.zeros((128, 16, 64), np.float32)
    for l, own in enumerate(blocks):
        m = np.arange(64)
        past[l] = np.where(m < own, 0.0, NEG)
        dist = np.where(m < own, own - m, 1).astype(np.float32)
        base[:, l, :] = 2.0 * p - 256.0 * dist[None, :]
    o, w = CST_OFF["past"]
    c[:, o:o + w] = np.tile(past.reshape(1, 1024), (128, 1))
    o, w = CST_OFF["base"]
    c[:, o:o + w] = base.reshape(128, 1024)
    o, w = CST_OFF["irow"]
    c[:, o:o + w] = np.arange(256, dtype=np.float32)[None, :]
    i = np.arange(256, dtype=np.float32)[None, None, :]
    pp = np.arange(128, dtype=np.float32)[:, None, None]
    cc = np.arange(2, dtype=np.float32)[None, :, None]
    Dm = np.broadcast_to(i - 2 * pp, (128, 2, 256))
    causal = np.where(i >= 2 * pp + cc, 0.0, NEG)
    o, w = CST_OFF["Dm"]
    c[:, o:o + w] = Dm.reshape(128, 512)
    o, w = CST_OFF["causal"]
    c[:, o:o + w] = causal.reshape(128, 512)
    o, w = CST_OFF["ident"]
    c[:, o:o + w] = np.eye(128, dtype=np.float32)
    o, w = CST_OFF["sel65"]
    c[64, o:o + w] = 1.0
    return c


def emit_proj_res_phase(S, cx, x_in, b_xin, x_out, b_xout, src_d, b_src, w_d, G, bias, b_mod, tag, src_is_heads, ntiles=8):
    nc = S.nc
    w = S.sb("w_" + tag, [128, KC, D], BF16)
    b_w = Buf("w_" + tag)
    for kc in range(KC):
        S.op("pool_dma", (lambda kc=kc: nc.gpsimd.dma_start(out=w[:, kc, :], in_=w_d[kc * 128:(kc + 1) * 128, :])), writes=[b_w])
    sv = src_d.rearrange("h d t -> (h d) t") if src_is_heads else src_d
    sv = sv.rearrange("(c p) t -> p c t", p=128)
    xv_in = x_in.rearrange("(c p) t -> p c t", p=128)
    xv_out = x_out.rearrange("(c p) t -> p c t", p=128)
    for t in range(ntiles):
        xt, bx = load_x_tile(S, cx, xv_in, b_xin, t)
        S.op("sp", (lambda t=t: nc.sync.dma_start(out=cx.h[:], in_=sv[:, :, t * 512:(t + 1) * 512])), reads=[b_src[t]], writes=[cx.b_h])
        for oc in range(KC):
            py = 5 + (oc % 2)

            def mm(oc=oc, py=py):
                for kc in range(KC):
                    m = nc.tensor.matmul(cx.psum[py][:], lhsT=w[:, kc, oc * 128:(oc + 1) * 128], rhs=cx.h[:, kc, :], start=(kc == 0), stop=(kc == KC - 1))
                return m
            S.op("pe", mm, reads=[b_w, cx.b_h], writes=[cx.b_psum[py]])
            if bias is None:
                S.op("dve", (lambda oc=oc, py=py, xt=xt: nc.vector.scalar_tensor_tensor(
                    out=xt[:, oc, :], in0=cx.psum[py][:], scalar=G[:, oc:oc + 1], in1=xt[:, oc, :], op0=ALU.mult, op1=ALU.add)),
                    reads=[cx.b_psum[py], b_mod], writes=[bx])
            else:
                tmp = cx.tmp[oc % 2]
                S.op("act", (lambda oc=oc, py=py, tmp=tmp: nc.scalar.activation(out=tmp[:], in_=cx.psum[py][:], func=AF.Identity, bias=bias[:, oc:oc + 1])),
                     reads=[cx.b_psum[py], cx.b_sp], writes=[cx.b_tmp[oc % 2]])
                S.op("dve", (lambda oc=oc, tmp=tmp, xt=xt: nc.vector.scalar_tensor_tensor(
                    out=xt[:, oc, :], in0=tmp[:], scalar=G[:, oc:oc + 1], in1=xt[:, oc, :], op0=ALU.mult, op1=ALU.add)),
                    reads=[cx.b_tmp[oc % 2], b_mod], writes=[bx])
        S.op("sp", (lambda xt=xt, t=t: nc.sync.dma_start(out=xv_out[:, :, t * 512:(t + 1) * 512], in_=xt[:])), reads=[bx], writes=[b_xout[t]])


def emit_glu_phase(S, cx, x_in, b_xin, w_d, A, Bv, b_mod, u_out, ntiles=8, hmask=None, b_hmask=None, b_uout=None):
    nc = S.nc
    w = S.sb("w_pw1", [128, KC, 2 * D], BF16)
    b_w = Buf("w_pw1")
    for kc in range(KC):
        S.op("pool_dma", (lambda kc=kc: nc.gpsimd.dma_start(out=w[:, kc, :], in_=w_d[kc * 128:(kc + 1) * 128, :])), writes=[b_w])
    uo = [S.sb("uo%d" % i, [128, 512], BF16) for i in range(2)]
    b_uo = [Buf("uo%d" % i) for i in range(2)]
    xv_in = x_in.rearrange("(c p) t -> p c t", p=128)
    uv = u_out.rearrange("(c p) t -> p c t", p=128)
    bp1 = spc(cx, "b_pw1")
    for t in range(ntiles):
        xt, bx = load_x_tile(S, cx, xv_in, b_xin, t)
        emit_norm_mod(S, cx, xt, bx, A, Bv, b_mod)
        for oc in range(KC):
            k = oc % 2
            pa = 1 + k * 2
            pg = pa + 1

            def mma(oc=oc, pa=pa):
                for kc in range(KC):
                    m = nc.tensor.matmul(cx.psum[pa][:], lhsT=w[:, kc, oc * 128:(oc + 1) * 128], rhs=cx.h[:, kc, :], start=(kc == 0), stop=(kc == KC - 1))
                return m

            def mmg(oc=oc, pg=pg):
                for kc in range(KC):
                    m = nc.tensor.matmul(cx.psum[pg][:], lhsT=w[:, kc, D + oc * 128:D + (oc + 1) * 128], rhs=cx.h[:, kc, :], start=(kc == 0), stop=(kc == KC - 1))
                return m
            S.op("pe", mma, reads=[b_w, cx.b_h], writes=[cx.b_psum[pa]])
            S.op("pe", mmg, reads=[b_w, cx.b_h], writes=[cx.b_psum[pg]])
            sa = cx.sa[k]
            S.op("act", (lambda oc=oc, pg=pg, sa=sa: nc.scalar.activation(out=sa[:], in_=cx.psum[pg][:], func=AF.Sigmoid, bias=bp1[:, 8 + oc:9 + oc])),
                 reads=[cx.b_psum[pg], cx.b_sp], writes=[cx.b_sa[k]])
            S.op("dve", (lambda oc=oc, pa=pa, sa=sa, k=k: nc.vector.scalar_tensor_tensor(
                out=uo[k][:], in0=cx.psum[pa][:], scalar=bp1[:, oc:oc + 1], in1=sa[:], op0=ALU.add, op1=ALU.mult)),
                reads=[cx.b_psum[pa], cx.b_sa[k], cx.b_sp], writes=[b_uo[k]])
            if hmask is not None and t == 8:
                S.op("pool", (lambda k=k: nc.gpsimd.tensor_tensor(out=uo[k][:], in0=uo[k][:], in1=hmask, op=ALU.mult)),
                     reads=[b_uo[k], b_hmask], writes=[b_uo[k]])
            S.op("sp", (lambda oc=oc, k=k, t=t: nc.sync.dma_start(out=uv[:, oc, t * 512:(t + 1) * 512], in_=uo[k][:])), reads=[b_uo[k]],
                 writes=([b_uout[t]] if b_uout is not None else []))


def build_L2():
    nc = bass.Bass("TRN2", target_bir_lowering=False)
    din = lambda n, s, dt: nc.dram_tensor(n, s, dt, kind="ExternalInput").ap()
    sp_d = din("sp", [128, SP_N], F32)
    mod_d = din("modi", [128, 144], F32)
    x1 = din("x1", [1024, TOK], F32)
    d = {"KT": din("KTf", [NH, DH, SEQ], BF16), "ind": din("ind", [64, SEQ], BF16), "Vd": din("Vd", [NH, 128, 128, DH], BF16),
         "QT": din("QT", [NH, DH, TOK], BF16), "KTo": din("KTo", [NH, DH, TOK], BF16), "Vo": din("Vo", [NH, 128, 32, DH], BF16),
         "kbar": din("kbarf", [DH, NH, NB], F32), "cst": din("cst", [128, CST_N], F32)}
    w_o = din("w_o", [1024, 1024], F32)
    w_in1 = din("w_in1", [1024, 2 * DFF], F32)
    w_out1 = din("w_out1", [DFF, 1024], F32)
    w_in2 = din("w_in2", [1024, 2 * DFF], F32)
    w_out2 = din("w_out2", [DFF, 1024], F32)
    w_pw1 = din("w_pw1", [1024, 2048], F32)
    OT = nc.dram_tensor("OT", [NH, DH, TOK], BF16).ap()
    xa = nc.dram_tensor("xa", [1024, TOK], F32).ap()
    xb = nc.dram_tensor("xb", [1024, TOK], F32).ap()
    x4 = nc.dram_tensor("x4", [1024, TOK], F32, kind="ExternalOutput").ap()
    uT = nc.dram_tensor("uT", [1024, TOK], BF16, kind="ExternalOutput").ap()
    d["OT"] = OT
    d["b_OT"] = [Buf("OT%d" % t) for t in range(8)]
    S = Sched(nc)
    cx = Ctx()
    setup_common(S, cx)
    emit_load_small(S, cx, sp_d)
    modT = [S.sb("modT%d" % i, [128, 72], F32) for i in range(2)]
    b_modT = [Buf("modT%d" % i) for i in range(2)]
    for i in range(2):
        S.op("sp", (lambda i=i: nc.sync.dma_start(out=modT[i][:], in_=mod_d[:, i * 72:(i + 1) * 72])), writes=[b_modT[i]])
    m01 = emit_mod_vectors_i(S, cx, modT[0], b_modT[0], spc(cx, "ng01"), "l0s1", 1)
    m02 = emit_mod_vectors_i(S, cx, modT[0], b_modT[0], spc(cx, "ng02"), "l0s2", 2)
    m10 = emit_mod_vectors_i(S, cx, modT[1], b_modT[1], spc(cx, "ng10"), "l1s0", 0)
    m11 = emit_mod_vectors_i(S, cx, modT[1], b_modT[1], spc(cx, "ng11"), "l1s1", 1)
    n0 = len(S._stack)
    emit_attn_phase(S, cx, d)
    S.barrier_free(len(S._stack) - n0)
    alloc_tile_bufs(S, cx)
    b_x1 = [Buf("x1_%d" % t) for t in range(8)]
    b_xa = [Buf("xa_%d" % t) for t in range(8)]
    b_xb = [Buf("xb_%d" % t) for t in range(8)]
    b_x4 = [Buf("x4_%d" % t) for t in range(8)]
    n1 = len(S._stack)
    emit_proj_res_phase(S, cx, x1, b_x1, xa, b_xa, OT, d["b_OT"], w_o, m01[2], None, m01[3], "wo", True)
    S.barrier_free(len(S._stack) - n1)
    alloc_ffn_bufs(S, cx)
    emit_ffn_phase(S, cx, xa, b_xa, xb, b_xb, w_in1, w_out1, m02[0], m02[1], m02[2], 0.5, m02[3], 8, "f01")
    emit_ffn_phase(S, cx, xb, b_xb, x4, b_x4, w_in2, w_out2, m10[0], m10[1], m10[2], 0.5, m10[3], 8, "f10")
    S.barrier_free(len(S._stack) - n1)
    emit_glu_phase(S, cx, x4, b_x4, w_pw1, m11[0], m11[1], m11[3], uT)
    S.emit()
    return nc


def emit_conv_phase(S, cx, uext_d, x_in, b_xin, x_out, b_xout, w2_d, ident_d, G, b_mod, u_scr=None, b_u=None):
    nc = S.nc
    w2 = S.sb("w_pw2", [128, KC, D], BF16)
    b_w2 = Buf("w_pw2")
    for kc in range(KC):
        S.op("pool_dma", (lambda kc=kc: nc.gpsimd.dma_start(out=w2[:, kc, :], in_=w2_d[kc * 128:(kc + 1) * 128, :])), writes=[b_w2])
    ident = S.sb("identc", [128, 128], F32)
    b_id = Buf("identc")
    S.op("sp", lambda: nc.sync.dma_start(out=ident[:], in_=ident_d[:, :]), writes=[b_id])
    dg = S.sb("dg", [128, KC * CW, 128], BF16)
    b_dg = Buf("dg")
    wdw = spc(cx, "w_dw")
    for ck in range(KC * CW):
        S.op("dve", (lambda ck=ck: nc.vector.tensor_scalar(out=dg[:, ck, :], in0=ident[:], scalar1=wdw[:, ck:ck + 1], scalar2=None, op0=ALU.mult)),
             reads=[b_id, cx.b_sp], writes=[b_dg])
    ue = [S.sb("ue%d" % i, [128, KC, 288], BF16) for i in range(2)]
    b_ue = [Buf("ue%d" % i) for i in range(2)]
    vs = S.sb("vs", [128, KC, 256], F32)
    b_vs = [Buf("vs%d" % c) for c in range(KC)]
    vb = [S.sb("vb%d" % i, [128, 256], BF16) for i in range(2)]
    b_vb = [Buf("vb%d" % i) for i in range(2)]
    vq = [S.sb("vq%d" % i, [128, 256], BF16) for i in range(2)]
    b_vq = [Buf("vq%d" % i) for i in range(2)]
    mu = S.sb("mu", [128, 256], F32)
    rs = S.sb("rs", [128, 256], F32)
    b_st = Buf("lnstat")
    uv = uext_d.rearrange("(c p) l e -> p c l e", p=128) if uext_d is not None else None
    usv = u_scr.rearrange("(c p) t -> p c t", p=128) if u_scr is not None else None
    xv_in = x_in.rearrange("(c p) t -> p c t", p=128)
    xv_out = x_out.rearrange("(c p) t -> p c t", p=128)
    for l in range(NLB):
        u_ = ue[l % 2]
        if usv is None:
            S.op("sp", (lambda u_=u_, l=l: nc.sync.dma_start(out=u_[:], in_=uv[:, :, l, :])), writes=[b_ue[l % 2]])
        else:
            S.op("sp", (lambda u_=u_, l=l: nc.sync.dma_start(out=u_[:, :, 32:288], in_=usv[:, :, l * 256:(l + 1) * 256])),
                 reads=[b_u[l // 2]], writes=[b_ue[l % 2]])
            S.op("sp", (lambda u_=u_, l=l: nc.sync.dma_start(out=u_[:, :, 0:32], in_=usv[:, :, TOK + l * 32:TOK + (l + 1) * 32])),
                 reads=[b_u[8]], writes=[b_ue[l % 2]])
        xt, bx = load_x_tile(S, cx, xv_in, b_xin, l, n=256)
        for c in range(KC):
            k2 = c % 2
            pc = cx.psum[1 + k2]

            def mmc(c=c, pc=pc, u_=u_):
                for k in range(CW):
                    m = nc.tensor.matmul(pc[:, 0:256], lhsT=dg[:, c * CW + k, :], rhs=u_[:, c, 2 + k:2 + k + 256], start=(k == 0), stop=(k == CW - 1))
                return m
            S.op("pe", mmc, reads=[b_dg, b_ue[l % 2]], writes=[cx.b_psum[1 + k2]])
            S.op("act", (lambda c=c, pc=pc: nc.scalar.activation(out=vs[:, c, :], in_=pc[:, 0:256], func=AF.Identity, bias=spc(cx, "b_dw")[:, c:c + 1])),
                 reads=[cx.b_psum[1 + k2], cx.b_sp], writes=[b_vs[c]])
            S.op("pool", (lambda c=c, k2=k2: nc.gpsimd.tensor_copy(out=vb[k2][:], in_=vs[:, c, :])), reads=[b_vs[c]], writes=[b_vb[k2]])
            S.op("act", (lambda c=c, k2=k2: nc.scalar.activation(out=vq[k2][:], in_=vs[:, c, :], func=AF.Square)), reads=[b_vs[c]], writes=[b_vq[k2]])
            S.op("pe", (lambda c=c, k2=k2: nc.tensor.matmul(cx.psum[3][:, 0:256], lhsT=cx.ones_bf[:], rhs=vb[k2][:], start=(c == 0), stop=(c == KC - 1))),
                 reads=[b_vb[k2], cx.b_ones], writes=[cx.b_psum[3]])
            S.op("pe", (lambda c=c, k2=k2: nc.tensor.matmul(cx.psum[4][:, 0:256], lhsT=cx.ones_bf[:], rhs=vq[k2][:], start=(c == 0), stop=(c == KC - 1))),
                 reads=[b_vq[k2], cx.b_ones], writes=[cx.b_psum[4]])
        S.op("dve", lambda: nc.vector.tensor_scalar(out=mu[:], in0=cx.psum[3][:, 0:256], scalar1=1.0 / D, scalar2=None, op0=ALU.mult),
             reads=[cx.b_psum[3]], writes=[b_st])
        S.op("dve", lambda: nc.vector.tensor_tensor(out=rs[:], in0=mu[:], in1=mu[:], op=ALU.mult), reads=[b_st], writes=[b_st])
        S.op("dve", lambda: nc.vector.scalar_tensor_tensor(out=rs[:], in0=cx.psum[4][:, 0:256], scalar=1.0 / D, in1=rs[:], op0=ALU.mult, op1=ALU.subtract),
             reads=[cx.b_psum[4], b_st], writes=[b_st])
        S.op("dve", lambda: nc.vector.tensor_scalar(out=rs[:], in0=rs[:], scalar1=EPS, scalar2=None, op0=ALU.add), reads=[b_st], writes=[b_st])
        S.op("act", lambda: nc.scalar.activation(out=rs[:], in_=rs[:], func=AF.Sqrt), reads=[b_st], writes=[b_st])
        S.op("dve", lambda: nc.vector.reciprocal(out=rs[:], in_=rs[:]), reads=[b_st], writes=[b_st])
        for c in range(KC):
            tmp = cx.tmp[c % 2]
            S.op("dve", (lambda c=c, tmp=tmp: nc.vector.tensor_tensor(out=tmp[:, 0:256], in0=vs[:, c, :], in1=mu[:], op=ALU.subtract)),
                 reads=[b_vs[c], b_st], writes=[cx.b_tmp[c % 2]])
            S.op("dve", (lambda c=c, tmp=tmp: nc.vector.tensor_tensor(out=tmp[:, 0:256], in0=tmp[:, 0:256], in1=rs[:], op=ALU.mult)),
                 reads=[b_st, cx.b_tmp[c % 2]], writes=[cx.b_tmp[c % 2]])
            S.op("act", (lambda c=c, tmp=tmp: nc.scalar.activation(out=cx.h[:, c, 0:256], in_=tmp[:, 0:256], func=AF.Silu,
                                                                   bias=spc(cx, "ln_b")[:, c:c + 1], scale=spc(cx, "ln_g")[:, c:c + 1])),
                 reads=[cx.b_tmp[c % 2], cx.b_sp], writes=[cx.b_h])
        for oc in range(KC):
            py = 5 + (oc % 2)

            def mm(oc=oc, py=py):
                for kc in range(KC):
                    m = nc.tensor.matmul(cx.psum[py][:, 0:256], lhsT=w2[:, kc, oc * 128:(oc + 1) * 128], rhs=cx.h[:, kc, 0:256], start=(kc == 0), stop=(kc == KC - 1))
                return m
            S.op("pe", mm, reads=[b_w2, cx.b_h], writes=[cx.b_psum[py]])
            sa = cx.sa[oc % 2]
            S.op("act", (lambda oc=oc, py=py, sa=sa: nc.scalar.activation(out=sa[:, 0:256], in_=cx.psum[py][:, 0:256], func=AF.Identity,
                                                                        bias=spc(cx, "b_pw2")[:, oc:oc + 1])),
                 reads=[cx.b_psum[py], cx.b_sp], writes=[cx.b_sa[oc % 2]])
            S.op("dve", (lambda oc=oc, sa=sa, xt=xt: nc.vector.scalar_tensor_tensor(
                out=xt[:, oc, 0:256], in0=sa[:, 0:256], scalar=G[:, oc:oc + 1], in1=xt[:, oc, 0:256], op0=ALU.mult, op1=ALU.add)),
                reads=[cx.b_sa[oc % 2], b_mod], writes=[bx])
        S.op("sp", (lambda xt=xt, l=l: nc.sync.dma_start(out=xv_out[:, :, l * 256:(l + 1) * 256], in_=xt[:, :, 0:256])),
             reads=[bx], writes=[b_xout[l // 2]])


def build_L3():
    nc = bass.Bass("TRN2", target_bir_lowering=False)
    din = lambda n, s, dt: nc.dram_tensor(n, s, dt, kind="ExternalInput").ap()
    sp_d = din("sp", [128, SP_N], F32)
    mod_d = din("modi", [128, 144], F32)
    x4 = din("x4", [1024, TOK], F32)
    uext = din("uext", [1024, NLB, 288], BF16)
    ident_d = din("ident", [128, 128], F32)
    w_pw2 = din("w_pw2", [1024, 1024], F32)
    w_in3 = din("w_in3", [1024, 2 * DFF], F32)
    w_out3 = din("w_out3", [DFF, 1024], F32)
    x5 = nc.dram_tensor("x5", [1024, TOK], F32).ap()
    xo = nc.dram_tensor("xo", [1024, TOK], F32, kind="ExternalOutput").ap()
    S = Sched(nc)
    cx = Ctx()
    setup_common(S, cx)
    emit_load_small(S, cx, sp_d)
    modT = [S.sb("modT%d" % i, [128, 72], F32) for i in range(2)]
    b_modT = [Buf("modT%d" % i) for i in range(2)]
    for i in range(2):
        S.op("sp", (lambda i=i: nc.sync.dma_start(out=modT[i][:], in_=mod_d[:, i * 72:(i + 1) * 72])), writes=[b_modT[i]])
    m11 = emit_mod_vectors_i(S, cx, modT[1], b_modT[1], spc(cx, "ng11"), "l1s1", 1)
    m12 = emit_mod_vectors_i(S, cx, modT[1], b_modT[1], spc(cx, "ng12"), "l1s2", 2)
    alloc_tile_bufs(S, cx)
    b_x4 = [Buf("x4_%d" % t) for t in range(16)]
    b_x5 = [Buf("x5_%d" % t) for t in range(8)]
    b_xo = [Buf("xo_%d" % t) for t in range(8)]
    n1 = len(S._stack)
    emit_conv_phase(S, cx, uext, x4, b_x4, x5, b_x5, w_pw2, ident_d, m11[2], m11[3])
    S.barrier_free(len(S._stack) - n1)
    alloc_ffn_bufs(S, cx)
    emit_ffn_phase(S, cx, x5, b_x5, xo, b_xo, w_in3, w_out3, m12[0], m12[1], m12[2], 0.5, m12[3], 8, "f11")
    S.emit()
    return nc


_NC = {}


def _get(name, fn):
    if name not in _NC:
        _NC[name] = fn()
    return _NC[name]


def _deint(a):
    sh = a.shape
    return np.ascontiguousarray(a.reshape(sh[:-1] + (sh[-1] // 256, 128, 2)).swapaxes(-1, -2)).reshape(sh)


def kernel_unfused(**inp):
    import ml_dtypes
    bf = ml_dtypes.bfloat16
    inp = {k: np.asarray(v) for k, v in inp.items()}
    cores = list(range(8))
    toks = []
    for core in cores:
        blocks = snake_blocks(core % 4)
        toks.append(np.concatenate([np.arange(g * 256, (g + 1) * 256) for g in blocks]))
    sps = [pack_small(inp, c // 4) for c in cores]
    nc1 = _get("L1", build_L1)
    maps = [{"sp": sps[c], "ada_w": inp["ada_w"], "xin": np.ascontiguousarray(inp["x"][c // 4][toks[c]].T),
             "w_in": inp["ffn_w_in"][0, 0], "w_out": inp["ffn_w_out"][0, 0], "wqkv": inp["attn_w_qkv"][0]} for c in cores]
    r1 = run_bass_kernel_spmd(nc1, maps, core_ids=cores).results
    ind = np.zeros((64, SEQ), bf)
    for m in range(64):
        ind[m, m * 256:(m + 1) * 256] = 1
    ident = np.eye(128, dtype=np.float32)
    maps2 = []
    per_batch = {}
    for b in range(2):
        KTf = np.zeros((NH, DH, SEQ), bf)
        Vf = np.zeros((SEQ, D), bf)
        kbf = np.zeros((DH, NH, NB), np.float32)
        for j in range(4):
            c = 4 * b + j
            blocks = snake_blocks(j)
            KT = np.asarray(r1[c]["KT"])
            V = np.asarray(r1[c]["V"])
            kb = np.asarray(r1[c]["kbar"])
            for l, g in enumerate(blocks):
                KTf[:, :, g * 256:(g + 1) * 256] = KT[:, :, l * 256:(l + 1) * 256]
                Vf[g * 256:(g + 1) * 256] = V[l * 256:(l + 1) * 256]
                kbf[:, :, g] = kb[:, :, l].T
        KTf = _deint(KTf)
        Vd = np.ascontiguousarray(Vf.reshape(NB, 128, 2, NH, DH).transpose(3, 1, 0, 2, 4)).reshape(NH, 128, 128, DH)
        per_batch[b] = (KTf, Vd, kbf)
    for c in cores:
        b = c // 4
        KTf, Vd, kbf = per_batch[b]
        KTo = _deint(np.asarray(r1[c]["KT"]))
        V = np.asarray(r1[c]["V"])
        Vo = np.ascontiguousarray(V.reshape(NLB, 128, 2, NH, DH).transpose(3, 1, 0, 2, 4)).reshape(NH, 128, 32, DH)
        maps2.append({"sp": sps[c], "modi": r1[c]["modo"], "x1": r1[c]["x1"], "KTf": KTf, "ind": ind, "Vd": Vd,
                      "QT": r1[c]["QT"], "KTo": KTo, "Vo": Vo, "kbarf": kbf, "cst": make_cst(c % 4),
                      "w_o": inp["attn_w_o"][0], "w_in1": inp["ffn_w_in"][0, 1], "w_out1": inp["ffn_w_out"][0, 1],
                      "w_in2": inp["ffn_w_in"][1, 0], "w_out2": inp["ffn_w_out"][1, 0], "w_pw1": inp["conv_w_pw1"][0]})
    nc2 = _get("L2", build_L2)
    r2 = run_bass_kernel_spmd(nc2, maps2, core_ids=cores).results
    maps3 = []
    for b in range(2):
        Uf = np.zeros((D, SEQ), bf)
        for j in range(4):
            c = 4 * b + j
            Uf[:, toks[c]] = np.asarray(r2[c]["uT"])
        for j in range(4):
            c = 4 * b + j
            ue = np.zeros((D, NLB, 288), bf)
            for l, g in enumerate(snake_blocks(j)):
                ue[:, l, 32:] = Uf[:, g * 256:(g + 1) * 256]
                if g > 0:
                    ue[:, l, :32] = Uf[:, g * 256 - 32:g * 256]
            maps3.append({"sp": sps[c], "modi": r1[c]["modo"], "x4": r2[c]["x4"], "uext": ue, "ident": ident,
                          "w_pw2": inp["conv_w_pw2"][0], "w_in3": inp["ffn_w_in"][1, 1], "w_out3": inp["ffn_w_out"][1, 1]})
    nc3 = _get("L3", build_L3)
    r3 = run_bass_kernel_spmd(nc3, maps3, core_ids=cores).results
    out = np.zeros((BATCH, SEQ, D), np.float32)
    for c in cores:
        out[c // 4][toks[c]] = np.asarray(r3[c]["xo"]).T
    return out


NPOS = 80
NT_ALL = 40
TOKX = TOK + 512


def pos_order(j):
    real = []
    own = snake_blocks(j)
    for i in range(8):
        A, B = 8 * i + j, 8 * i + 7 - j
        real += [A, B] + [g for g in range(8 * i, 8 * i + 8) if g not in (A, B)]
    dup = [(g - 1 if g > 0 else None) for g in own]
    return real, dup


def emit_kvq_phase(S, cx, x_d, b_x, wqkv_d, A, Bv, b_mod, QT_d, KT_d, kbar_d, V_d, x1o_d, b_x1o, b_kv):
    nc = S.nc
    wq = S.sb("wqkv_sb", [128, KC, 3072], BF16)
    b_wq = [Buf("wqkv%d" % k) for k in range(KC)]
    for kc in range(KC):
        S.op("pool_dma", (lambda kc=kc: nc.gpsimd.dma_start(out=wq[:, kc, :], in_=wqkv_d[kc * 128:(kc + 1) * 128, :])), writes=[b_wq[kc]])
    bo = S.sb("blkones", [128, 128], BF16)
    b_bo = Buf("blkones")
    S.op("dve", lambda: nc.vector.memset(bo[:], 0.0), writes=[b_bo])
    S.op("dve", lambda: nc.vector.memset(bo[0:64, 0:64], 1.0), writes=[b_bo])
    S.op("dve", lambda: nc.vector.memset(bo[64:128, 64:128], 1.0), writes=[b_bo])
    g8 = S.sb("g8", [128, 2], F32)
    b_g8 = Buf("g8")
    S.op("dve", lambda: nc.vector.tensor_scalar(out=g8[:, 0:1], in0=spc(cx, "gq"), scalar1=0.125, scalar2=None, op0=ALU.mult),
         reads=[cx.b_sp], writes=[b_g8])
    S.op("dve", lambda: nc.vector.tensor_copy(out=g8[:, 1:2], in_=spc(cx, "gk")), reads=[cx.b_sp], writes=[b_g8])
    epst = S.sb("epst", [128, 1], F32)
    b_eps = Buf("epst")
    S.op("dve", lambda: nc.vector.memset(epst[:], EPS), writes=[b_eps])
    qo = [S.sb("qo%d" % i, [128, 512], BF16) for i in range(2)]
    b_qo = [Buf("qo%d" % i) for i in range(2)]
    kb = [S.sb("kb%d" % i, [128, 2], F32) for i in range(2)]
    b_kb = [Buf("kb%d" % i) for i in range(2)]
    vt = [S.sb("vt%d" % i, [128, 1024], BF16) for i in range(2)]
    b_vt = [Buf("vt%d" % i) for i in range(2)]
    xv = x_d.rearrange("(c p) t -> p c t", p=128)
    xov = x1o_d.rearrange("(c p) t -> p c t", p=128)
    QTv = QT_d.rearrange("h d t -> (h d) t")
    KTv = KT_d.rearrange("h d t -> (h d) t")
    cnt = 0
    PQB = (1, 2, 4, 7)
    for t in range(NT_ALL):
        is_own = (t < 32 and t % 4 == 0)
        is_dup = t >= 32
        xt, bx = cx.xt[t % 2], cx.b_xt[t % 2]
        if t == 0:
            load_x_tile(S, cx, xv, b_x, 0)
        if is_own:
            S.op("sp", (lambda xt=xt, t=t: nc.sync.dma_start(out=xov[:, :, (t // 4) * 512:(t // 4 + 1) * 512], in_=xt[:])),
                 reads=[bx], writes=[b_x1o[t // 4]])
        if is_dup:
            i2 = t - 32
            for k2 in range(2):
                S.op("sp", (lambda xt=xt, i2=i2, k2=k2: nc.sync.dma_start(
                    out=xov[:, :, TOK + (2 * i2 + k2) * 32:TOK + (2 * i2 + k2 + 1) * 32], in_=xt[:, :, k2 * 256 + 224:k2 * 256 + 256])),
                    reads=[bx], writes=[b_x1o[8]])
        emit_norm_mod(S, cx, xt, bx, A, Bv, b_mod)
        if t + 1 < NT_ALL:
            load_x_tile(S, cx, xv, b_x, t + 1)
        items = [(w_, hp) for w_ in range(2) for hp in range(8) if not (w_ == 0 and not (is_own or is_dup))]
        base_cnt = cnt
        cnt += len(items)

        def st_A(ii, base_cnt=base_cnt, items=items):
            which, hp = items[ii]
            k = (base_cnt + ii) % 2
            goff = which * 1024
            pqi = PQB[(base_cnt + ii) % 4]
            pq = cx.psum[pqi]

            def mm():
                for kc in range(KC):
                    m = nc.tensor.matmul(pq[:], lhsT=wq[:, kc, goff + hp * 128:goff + (hp + 1) * 128], rhs=cx.h[:, kc, :],
                                         start=(kc == 0), stop=(kc == KC - 1))
                return m
            S.op("pe", mm, reads=b_wq + [cx.b_h], writes=[cx.b_psum[pqi]])
            S.op("act", (lambda: nc.scalar.activation(out=cx.sq[k][:], in_=pq[:], func=AF.Square)),
                 reads=[cx.b_psum[pqi]], writes=[cx.b_sq[k]])

        def st_B(ii, base_cnt=base_cnt, items=items, t=t, is_own=is_own):
            which, hp = items[ii]
            k = (base_cnt + ii) % 2
            pqi = PQB[(base_cnt + ii) % 4]
            pq = cx.psum[pqi]
            pss = cx.psum[3]
            S.op("pe", (lambda: nc.tensor.matmul(pss[:], lhsT=bo[:], rhs=cx.sq[k][:], start=True, stop=True)),
                 reads=[cx.b_sq[k], b_bo], writes=[cx.b_psum[3]])
            r1 = cx.tmp[k]
            S.op("act", (lambda: nc.scalar.activation(out=r1[:], in_=pss[:], func=AF.Ln, bias=epst[:, 0:1], scale=1.0 / DH)),
                 reads=[cx.b_psum[3], b_eps], writes=[cx.b_tmp[k]])
            S.op("act", (lambda: nc.scalar.activation(out=r1[:], in_=r1[:], func=AF.Exp, scale=-0.5)), reads=[cx.b_tmp[k]], writes=[cx.b_tmp[k]])
            if which == 0:
                S.op("dve", (lambda: nc.vector.scalar_tensor_tensor(out=qo[k][:], in0=pq[:], scalar=g8[:, 0:1], in1=r1[:], op0=ALU.mult, op1=ALU.mult)),
                     reads=[cx.b_tmp[k], cx.b_psum[pqi], b_g8], writes=[b_qo[k]])
                if is_own:
                    for k2 in range(2):
                        lb = 2 * (t // 4) + k2
                        S.op("sp", (lambda k2=k2, lb=lb: nc.sync.dma_start(out=QTv[hp * 128:(hp + 1) * 128, lb * 288:lb * 288 + 256],
                                                                         in_=qo[k][:, k2 * 256:(k2 + 1) * 256])),
                             reads=[b_qo[k]], writes=[b_kv])
                else:
                    i2 = t - 32
                    for k2 in range(2):
                        lb = 2 * i2 + k2
                        S.op("sp", (lambda k2=k2, lb=lb: nc.sync.dma_start(
                            out=QTv[hp * 128:(hp + 1) * 128, lb * 288 + 256:lb * 288 + 288],
                            in_=qo[k][:, k2 * 256 + 224:k2 * 256 + 256])), reads=[b_qo[k]], writes=[b_kv])
            else:
                for bb in range(2):
                    S.op("dve", (lambda bb=bb: nc.vector.scalar_tensor_tensor(
                        out=qo[k][:, bb * 256:(bb + 1) * 256].rearrange("q (c p) -> q c p", c=2),
                        in0=pq[:, bb * 256:(bb + 1) * 256].rearrange("q (p c) -> q c p", c=2), scalar=g8[:, 1:2],
                        in1=r1[:, bb * 256:(bb + 1) * 256].rearrange("q (p c) -> q c p", c=2), op0=ALU.mult, op1=ALU.mult)),
                        reads=[cx.b_tmp[k], cx.b_psum[pqi], b_g8], writes=[b_qo[k]])
                S.op("sp", (lambda: nc.sync.dma_start(out=KTv[hp * 128:(hp + 1) * 128, t * 512:(t + 1) * 512], in_=qo[k][:])),
                     reads=[b_qo[k]], writes=[b_kv])
                S.op("dve", (lambda: nc.vector.tensor_reduce(out=kb[k][:], in_=qo[k][:].rearrange("p (b t) -> p b t", b=2), axis=AX.X, op=ALU.add)),
                     reads=[b_qo[k]], writes=[b_kb[k]])
                S.op("dve", (lambda: nc.vector.tensor_scalar(out=kb[k][:], in0=kb[k][:], scalar1=1.0 / BLK, scalar2=None, op0=ALU.mult)),
                     reads=[b_kb[k]], writes=[b_kb[k]])
                S.op("sp", (lambda: nc.sync.dma_start(out=kbar_d[hp * 128:(hp + 1) * 128, 2 * t:2 * t + 2], in_=kb[k][:])),
                     reads=[b_kb[k]], writes=[b_kv])
        vgroups = [(tg, ns) for tg in range(4) for ns in range(2)]

        def st_V(gi, t=t):
            tg, ns = vgroups[gi]
            v = vt[tg % 2]
            pv = cx.psum[5 + ns]

            def mmv():
                for kc in range(KC):
                    m = nc.tensor.matmul(pv[:], lhsT=cx.h[:, kc, tg * 128:(tg + 1) * 128], rhs=wq[:, kc, 2048 + ns * 512:2048 + (ns + 1) * 512],
                                         start=(kc == 0), stop=(kc == KC - 1))
                return m
            S.op("pe", mmv, reads=b_wq + [cx.b_h], writes=[cx.b_psum[5 + ns]])
            if ns == 0:
                S.op("act", (lambda: nc.scalar.copy(out=v[:, 0:512], in_=pv[:])), reads=[cx.b_psum[5]], writes=[b_vt[tg % 2]])
            else:
                S.op("dve", (lambda: nc.vector.tensor_copy(out=v[:, 512:1024], in_=pv[:])), reads=[cx.b_psum[6]], writes=[b_vt[tg % 2]])
                blk = 2 * t + tg // 2
                hf = tg % 2
                for c2 in range(2):
                    dst = V_d[:, hf * 64:(hf + 1) * 64, 2 * blk + c2, :].rearrange("h pp d -> pp h d")
                    S.op("sp", (lambda dst=dst, c2=c2: nc.sync.dma_start(out=dst, in_=v[c2:128:2, :].rearrange("p (h d) -> p h d", h=NH))),
                         reads=[b_vt[tg % 2]], writes=[b_kv])
        vper = len(items) // 8
        st_A(0)
        for ii in range(len(items)):
            if ii + 1 < len(items):
                st_A(ii + 1)
            st_B(ii)
            if (ii + 1) % vper == 0:
                st_V((ii + 1) // vper - 1)


CST2_OFF = {}


def _cst2_layout():
    off = 0
    for name, w in (("past", 2048), ("base", 1024), ("irowX", 288), ("Dm", 512), ("causal", 512),
                    ("DmH", 64), ("causalH", 64), ("ident", 128), ("sel65", 64)):
        CST2_OFF[name] = (off, w)
        off += w
    return off


CST2_N = _cst2_layout()


def make_cst2(j):
    real, dup = pos_order(j)
    own = snake_blocks(j)
    c = np.zeros((128, CST2_N), np.float32)
    p = np.arange(128, dtype=np.float32)[:, None]
    gpos = np.array(real)
    past = np.zeros((32, 64), np.float32)
    base = np.zeros((128, 16, 64), np.float32)
    for row in range(32):
        o = own[row] if row < 16 else own[row - 16] - 1
        valid = gpos < o
        past[row] = np.where(valid, 0.0, NEG)
        if row < 16:
            dist = np.where(valid, o - gpos, 1).astype(np.float32)
            base[:, row, :] = 2.0 * p - 256.0 * dist[None, :]

    def put(name, arr):
        o_, w = CST2_OFF[name]
        c[:, o_:o_ + w] = arr.reshape(arr.shape[0], -1)
    put("past", np.tile(past.reshape(1, 2048), (128, 1)))
    put("base", base.reshape(128, 1024))
    irx = np.concatenate([np.arange(256, dtype=np.float32), -(32.0 - np.arange(32, dtype=np.float32))])
    put("irowX", np.tile(irx[None, :], (128, 1)))
    pp = np.arange(128, dtype=np.float32)[:, None, None]
    cc = np.arange(2, dtype=np.float32)[None, :, None]
    i = np.arange(256, dtype=np.float32)[None, None, :]
    put("Dm", np.broadcast_to(i - 2 * pp, (128, 2, 256)).copy())
    put("causal", np.where(i >= 2 * pp + cc, 0.0, NEG).astype(np.float32))
    ih = 224 + np.arange(32, dtype=np.float32)[None, None, :]
    put("DmH", np.broadcast_to(ih - 2 * pp, (128, 2, 32)).copy())
    put("causalH", np.where(ih >= 2 * pp + cc, 0.0, NEG).astype(np.float32))
    put("ident", np.eye(128, dtype=np.float32))
    s65 = np.zeros((128, 64), np.float32)
    s65[64] = 1.0
    put("sel65", s65)
    hmask = np.ones((128, 512), np.float32)
    for l in range(16):
        if dup[l] is None:
            hmask[:, l * 32:(l + 1) * 32] = 0.0
    return c, hmask


def emit_attn2(S, cx, d):
    nc = S.nc
    cst = S.sb("cst2", [128, CST2_N], F32)
    b_cst = Buf("cst2")
    S.op("sp", lambda: nc.sync.dma_start(out=cst[:], in_=d["cst"][:, :]), writes=[b_cst])

    def C(name):
        o, w = CST2_OFF[name]
        return cst[:, o:o + w]
    kbh = S.sb("kbh", [64, NH * NB], BF16)
    kbl = S.sb("kbl", [64, NH * NB], BF16)
    b_kb = Buf("kbhl")
    n0 = len(S._stack)
    kb32 = S.sb("kb32", [64, NH * NB], F32)
    kbt = S.sb("kbt", [64, NH * NB], F32)
    S.op("sp", lambda: nc.sync.dma_start(out=kb32[:].rearrange("d (h m) -> d h m", h=NH),
                                         in_=d["kbar"].rearrange("(h d) m -> d h m", h=NH)[:, :, 0:NB]), reads=[d["b_kv"]], writes=[b_kb])
    S.op("dve", lambda: nc.vector.tensor_copy(out=kbh[:], in_=kb32[:]), reads=[b_kb], writes=[b_kb])
    S.op("dve", lambda: nc.vector.tensor_copy(out=kbt[:], in_=kbh[:]), reads=[b_kb], writes=[b_kb])
    S.op("dve", lambda: nc.vector.tensor_tensor(out=kbt[:], in0=kb32[:], in1=kbt[:], op=ALU.subtract), reads=[b_kb], writes=[b_kb])
    S.op("dve", lambda: nc.vector.tensor_copy(out=kbl[:], in_=kbt[:]), reads=[b_kb], writes=[b_kb])
    S.barrier_free(len(S._stack) - n0)
    KTa = [S.sb("KTa%d" % i, [128, SEQ], BF16) for i in range(2)]
    Va = [S.sb("Va%d" % i, [128, 128, 128], BF16) for i in range(2)]
    Qa = [S.sb("Qa%d" % i, [128, TOKX], BF16) for i in range(2)]
    Kd = [S.sb("Kd0", [128, TOK], BF16)] * 2
    Vdd = [S.sb("Vdd0", [128, 32, 65], BF16)] * 2
    bT = [S.sb("bT0", [128, 1024], F32)] * 2
    dgb = [S.sb("dgb0", [128, 512], F32)] * 2
    dgbH = [S.sb("dgbH%d" % i, [128, 64], F32) for i in range(2)]
    cq = [S.sb("cq%d" % i, [128, 288], F32) for i in range(2)]
    b_KT = [Buf("KTa%d" % i) for i in range(2)]
    b_Va = [Buf("Va%d" % i) for i in range(2)]
    b_Qa = [Buf("Qa%d" % i) for i in range(2)]
    b_Qm = [[Buf("Qm%d_%d" % (i, l)) for l in range(16)] for i in range(2)]
    b_Kd = [Buf("Kd0")] * 2
    b_Vdd = [Buf("Vdd0")] * 2
    b_hd = [Buf("hd0")] * 2
    mbp = [S.sb("mbp%d" % i, [128, 128], F32) for i in range(2)]
    b_mbp = [Buf("mbp%d" % i) for i in range(2)]
    gmt = [S.sb("gmt%d" % i, [128, 64], F32) for i in range(2)]
    b_gmt = [Buf("gmt%d" % i) for i in range(2)]
    t8 = [S.sb("t8_%d" % i, [128, 8], F32) for i in range(2)]
    pt = [S.sb("pt%d" % i, [128, 2, 288], BF16) for i in range(2)]
    b_pt = [Buf("pt%d" % i) for i in range(2)]
    pt2 = S.sb("pt2", [128, 512], BF16)
    b_pt2 = Buf("pt2")
    dtmp = S.sb("dtmp", [128, 512], F32)
    b_dtmp = Buf("dtmp")
    o1 = S.sb("o1", [128, 288], F32)
    o2 = S.sb("o2", [128, 288], F32)
    rd = S.sb("rd", [128, 288], F32)
    ob = [S.sb("ob%d" % i, [128, 288], BF16) for i in range(2)]
    b_o = Buf("o12")
    b_ob = [Buf("ob%d" % i) for i in range(2)]
    for i in range(2):
        S.op("sp", (lambda i=i: nc.sync.dma_start(out=KTa[i][64:128, :], in_=d["ind"][:, :])), writes=[b_KT[i]])
        S.op("pool", (lambda i=i: nc.gpsimd.memset(Va[i][:, :, 64:128], 0.0)), writes=[b_Va[i]])
        S.op("dve", (lambda i=i: nc.vector.memset(Va[i][:, :, 64:65], 1.0)), writes=[b_Va[i]])
        S.op("dve", (lambda i=i: nc.vector.memset(Vdd[i][:, :, 64:65], 1.0)), writes=[b_Vdd[i]])
        S.op("dve", (lambda i=i: nc.vector.memset(mbp[i][:], 0.0)), writes=[b_mbp[i]])
    ps = cx.psum
    bp = cx.b_psum
    SX = [cx.psall[:, 0:1024], cx.psall[:, 1024:2048]]
    b_SX = [[bp[0], bp[1]], [bp[2], bp[3]]]
    scnt = 0
    ocnt = 0
    NQ = 288
    for h in range(NH):
        cur = h % 2
        sl = SLOPES[h]
        es = math.exp(sl)
        S.op("sp", (lambda h=h, cur=cur: nc.sync.dma_start(out=KTa[cur][0:64, :], in_=d["KT"][h, :, 0:SEQ])), reads=[d["b_kv"]], writes=[b_KT[cur]])
        for q8 in range(8):
            S.op("sp", (lambda h=h, cur=cur, q8=q8: nc.sync.dma_start(out=Va[cur][:, q8 * 16:(q8 + 1) * 16, 0:64], in_=d["Vd"][h, :, q8 * 16:(q8 + 1) * 16, :])),
                 reads=[d["b_kv"]], writes=[b_Va[cur]])
        S.op("sp", (lambda h=h, cur=cur: nc.sync.dma_start(out=Qa[cur][0:64, :], in_=d["QT"][h])), reads=[d["b_kv"]], writes=[b_Qa[cur]] + b_Qm[cur])
        S.op("sp", (lambda h=h, cur=cur: nc.sync.dma_start(out=Kd[cur][0:64, :], in_=d["KT"][h, :, SEQ:SEQ + TOK])), reads=[d["b_kv"]], writes=[b_Kd[cur]])
        for q2 in range(2):
            S.op("sp", (lambda h=h, cur=cur, q2=q2: nc.sync.dma_start(out=Vdd[cur][:, q2 * 16:(q2 + 1) * 16, 0:64],
                                                                     in_=d["Vd"][h, :, 128 + q2 * 16:128 + (q2 + 1) * 16, :])),
                 reads=[d["b_kv"]], writes=[b_Vdd[cur]])
        for (VV, bV) in ((Va[cur], b_Va[cur]), (Vdd[cur], b_Vdd[cur])):
            Vodd = VV[:].rearrange("p (n two) e -> p n two e", two=2)[:, :, 1, :]
            S.op("dve", (lambda Vodd=Vodd, es=es: nc.vector.tensor_scalar(out=Vodd[:, :, 0:64], in0=Vodd[:, :, 0:64], scalar1=es, scalar2=None, op0=ALU.mult)),
                 reads=[bV], writes=[bV])
            S.op("dve", (lambda Vodd=Vodd, es=es: nc.vector.memset(Vodd[:, :, 64:65], es)), reads=[bV], writes=[bV])
        S.op("dve", (lambda cur=cur, sl=sl: nc.vector.tensor_scalar(out=bT[cur][:], in0=C("base"), scalar1=sl, scalar2=None, op0=ALU.mult)),
             reads=[b_cst], writes=[b_hd[cur]])
        S.op("dve", (lambda cur=cur, sl=sl: nc.vector.scalar_tensor_tensor(out=dgb[cur][:], in0=C("Dm"), scalar=-sl, in1=C("causal"),
                                                                         op0=ALU.mult, op1=ALU.add)), reads=[b_cst], writes=[b_hd[cur]])
        S.op("dve", (lambda cur=cur, sl=sl: nc.vector.scalar_tensor_tensor(out=dgbH[cur][:], in0=C("DmH"), scalar=-sl, in1=C("causalH"),
                                                                         op0=ALU.mult, op1=ALU.add)), reads=[b_cst], writes=[b_hd[cur]])
        S.op("act", (lambda cur=cur, sl=sl: nc.scalar.activation(out=cq[cur][:], in_=C("irowX"), func=AF.Exp, scale=-sl)),
             reads=[b_cst], writes=[b_hd[cur]])
        groups = []
        for l in range(NLB):
            groups.append((l * NQ, 128, l, l))
            groups.append((l * NQ + 128, 128, l, l))
            groups.append((l * NQ + 256, 32, 16 + l, l))

        def g_s1(gi, cur=cur, h=h, groups=groups):
            q0, nr, row, qmi = groups[gi]

            def mmg():
                gb = (6, 4)[gi % 2]
                nc.tensor.matmul(ps[gb][0:nr, 0:64], lhsT=Qa[cur][0:64, q0:q0 + nr], rhs=kbh[:, h * 64:(h + 1) * 64], start=True, stop=False)
                return nc.tensor.matmul(ps[gb][0:nr, 0:64], lhsT=Qa[cur][0:64, q0:q0 + nr], rhs=kbl[:, h * 64:(h + 1) * 64], start=False, stop=True)
            S.op("pe", mmg, reads=[b_Qa[cur], b_kb], writes=[bp[(6, 4)[gi % 2]]])

        def g_s2(gi, groups=groups):
            q0, nr, row, qmi = groups[gi]
            g = gi % 2
            pb = C("past")[0:nr, row * 64:(row + 1) * 64]
            gb = (6, 4)[g]
            S.op("dve", (lambda: nc.vector.tensor_tensor(out=gmt[g][0:nr, :], in0=ps[gb][0:nr, 0:64], in1=pb, op=ALU.add)),
                 reads=[bp[gb], b_cst], writes=[b_gmt[g]])
            S.op("dve", (lambda: nc.vector.max(out=t8[g][0:nr, :], in_=gmt[g][0:nr, :])), reads=[b_gmt[g]], writes=[b_gmt[g]])
            S.op("dve", (lambda: nc.vector.tensor_scalar(out=gmt[g][0:nr, :], in0=gmt[g][0:nr, :], scalar1=t8[g][0:nr, 2:3], scalar2=-NEG,
                                                         op0=ALU.is_ge, op1=ALU.mult)), reads=[b_gmt[g]], writes=[b_gmt[g]])
            S.op("dve", (lambda: nc.vector.scalar_tensor_tensor(out=mbp[g][0:nr, 64:128], in0=gmt[g][0:nr, :], scalar=NEG, in1=pb,
                                                                op0=ALU.add, op1=ALU.add)),
                 reads=[b_gmt[g], b_cst], writes=[b_mbp[g]])

        def g_s3(gi, cur=cur, groups=groups):
            q0, nr, row, qmi = groups[gi]
            g = gi % 2
            tb = (7, 5)[g]
            S.op("pe", (lambda: nc.tensor.transpose(ps[tb][:, 0:nr], mbp[g][0:nr, :], C("ident")[0:nr, 0:nr])),
                 reads=[b_mbp[g], b_cst], writes=[bp[tb]])
            S.op("act", (lambda: nc.scalar.copy(out=Qa[cur][64:128, q0:q0 + nr], in_=ps[tb][64:128, 0:nr])),
                 reads=[bp[tb]], writes=[b_Qm[cur][qmi]])
        g_s1(0)
        for gi in range(len(groups)):
            g_s2(gi)
            if gi + 1 < len(groups):
                g_s1(gi + 1)
            g_s3(gi)
        for i in range(8):
            for k in range(2):
                l = 2 * i + k
                q0 = l * NQ
                po = 4 + k
                if k == 0:
                    nlist = list(range(8 * i)) + [8 * i + 2, 8 * i + 3, 8 * i + 4]
                else:
                    nlist = list(range(8 * i)) + [8 * i] + list(range(8 * i + 2, 8 * i + 8))
                nk = len(nlist)
                sbase = scnt
                scnt += nk

                def emit_S(ni, cur=cur, q0=q0, l=l, sbase=sbase, nlist=nlist):
                    sb_ = (sbase + ni) % 2
                    n = nlist[ni]

                    def mms():
                        for c in range(2):
                            m = nc.tensor.matmul(SX[sb_][:, c * 512:c * 512 + NQ], lhsT=KTa[cur][:, n * 256 + c * 128:n * 256 + (c + 1) * 128],
                                                 rhs=Qa[cur][:, q0:q0 + NQ], start=True, stop=True)
                        return m
                    S.op("pe", mms, reads=[b_KT[cur], b_Qa[cur], b_Qm[cur][l]], writes=b_SX[sb_])

                def emit_E(ni, cur=cur, l=l, sbase=sbase, nlist=nlist):
                    sb_ = (sbase + ni) % 2
                    n = nlist[ni]
                    S.op("act", (lambda: nc.scalar.activation(out=pt[sb_][:], in_=SX[sb_].rearrange("p (c x) -> p c x", c=2)[:, :, 0:NQ], func=AF.Exp,
                                                              bias=bT[cur][:, l * 64 + n:l * 64 + n + 1])),
                         reads=b_SX[sb_] + [b_hd[cur]], writes=[b_pt[sb_]])

                def emit_PV(ni, cur=cur, po=po, nk=nk, sbase=sbase, nlist=nlist):
                    sb_ = (sbase + ni) % 2
                    n = nlist[ni]

                    def mmpv():
                        for c in range(2):
                            m = nc.tensor.matmul(ps[po][:, 0:NQ], lhsT=Va[cur][:, 2 * n + c, :], rhs=pt[sb_][:, c, :],
                                                 start=(ni == 0 and c == 0), stop=(ni == nk - 1 and c == 1))
                        return m
                    S.op("pe", mmpv, reads=[b_Va[cur], b_pt[sb_]], writes=[bp[po]])
                emit_S(0)
                for n in range(nk):
                    emit_E(n)
                    if n + 1 < nk:
                        emit_S(n + 1)
                    emit_PV(n)
                for (Kt, koff, Vt, ch0, bK, bV, dg, qoff, nq, ocol) in (
                        (KTa[cur], (8 * i + k) * 256, Va[cur], 2 * (8 * i + k), b_KT[cur], b_Va[cur], dgb[cur], q0, 256, 0),
                        (Kd[cur], l * 256, Vdd[cur], 2 * l, b_Kd[cur], b_Vdd[cur], dgbH[cur], q0 + 256, 32, 256)):
                    def mmd(Kt=Kt, koff=koff, qoff=qoff, nq=nq, cur=cur):
                        for c in range(2):
                            m = nc.tensor.matmul(ps[6][:, c * nq:(c + 1) * nq], lhsT=Kt[0:64, koff + c * 128:koff + (c + 1) * 128],
                                                 rhs=Qa[cur][0:64, qoff:qoff + nq], start=True, stop=True)
                        return m
                    S.op("pe", mmd, reads=[bK, b_Qa[cur]], writes=[bp[6]])
                    S.op("dve", (lambda dg=dg, nq=nq: nc.vector.tensor_tensor(out=dtmp[:, 0:2 * nq], in0=ps[6][:, 0:2 * nq], in1=dg[:, 0:2 * nq], op=ALU.add)),
                         reads=[bp[6], b_hd[cur]], writes=[b_dtmp])
                    S.op("act", (lambda nq=nq: nc.scalar.activation(out=pt2[:, 0:2 * nq], in_=dtmp[:, 0:2 * nq], func=AF.Exp)), reads=[b_dtmp], writes=[b_pt2])

                    def mmpo(Vt=Vt, ch0=ch0, nq=nq, ocol=ocol):
                        for c in range(2):
                            m = nc.tensor.matmul(ps[7][0:Vt.shape[2], ocol:ocol + nq], lhsT=Vt[:, ch0 + c, :], rhs=pt2[:, c * nq:(c + 1) * nq],
                                                 start=(c == 0), stop=(c == 1))
                        return m
                    S.op("pe", mmpo, reads=[bV, b_pt2], writes=[bp[7]])
                S.op("dve", (lambda cur=cur, po=po: nc.vector.tensor_tensor(out=o1[0:65, :], in0=ps[po][0:65, 0:NQ], in1=cq[cur][0:65, :], op=ALU.mult)),
                     reads=[bp[po], b_hd[cur]], writes=[b_o])
                S.op("dve", (lambda: nc.vector.tensor_tensor(out=o2[0:65, :], in0=o1[0:65, :], in1=ps[7][0:65, 0:NQ], op=ALU.add)),
                     reads=[bp[7], b_o], writes=[b_o])
                S.op("pe", (lambda: nc.tensor.matmul(ps[6][0:64, 0:NQ], lhsT=C("sel65")[0:65, :], rhs=o2[0:65, :], start=True, stop=True)),
                     reads=[b_o, b_cst], writes=[bp[6]])
                S.op("dve", (lambda: nc.vector.reciprocal(out=rd[0:64, :], in_=ps[6][0:64, 0:NQ])), reads=[bp[6]], writes=[b_o])
                kk = ocnt % 2
                ocnt += 1
                S.op("dve", (lambda kk=kk: nc.vector.tensor_tensor(out=ob[kk][0:64, :], in0=o2[0:64, :], in1=rd[0:64, :], op=ALU.mult)),
                     reads=[b_o], writes=[b_ob[kk]])
                S.op("sp", (lambda kk=kk, h=h, l=l: nc.sync.dma_start(out=d["OT"][h, :, l * 256:(l + 1) * 256], in_=ob[kk][0:64, 0:256])),
                     reads=[b_ob[kk]], writes=[d["b_OT"][l // 2]])
                S.op("sp", (lambda kk=kk, h=h, l=l: nc.sync.dma_start(out=d["OT"][h, :, TOK + l * 32:TOK + (l + 1) * 32], in_=ob[kk][0:64, 256:288])),
                     reads=[b_ob[kk]], writes=[d["b_OT"][8]])


def build_fused():
    nc = bass.Bass("TRN2", target_bir_lowering=False)
    din = lambda n, s, dt: nc.dram_tensor(n, s, dt, kind="ExternalInput").ap()
    scr = lambda n, s, dt: nc.dram_tensor(n, s, dt).ap()
    sp_d = din("sp", [128, SP_N], F32)
    ada_w = din("ada_w", [2, 1024, 9216], F32)
    xall = din("xall", [1024, NT_ALL * 512], F32)
    w_in = [din("w_in%d" % i, [1024, 2 * DFF], F32) for i in range(4)]
    w_out = [din("w_out%d" % i, [DFF, 1024], F32) for i in range(4)]
    wqkv = din("wqkv", [1024, 3072], F32)
    w_o = din("w_o", [1024, 1024], F32)
    w_pw1 = din("w_pw1", [1024, 2048], F32)
    w_pw2 = din("w_pw2", [1024, 1024], F32)
    ind_d = din("ind", [64, SEQ], BF16)
    cst_d = din("cst", [128, CST2_N], F32)
    hm_d = din("hmask", [128, 512], BF16)
    ident_d = din("ident", [128, 128], F32)
    xo = nc.dram_tensor("xo", [1024, TOK], F32, kind="ExternalOutput").ap()
    x1all = scr("x1all", [1024, NT_ALL * 512], F32)
    x1o = scr("x1o", [1024, TOKX], F32)
    xa = scr("xa", [1024, TOKX], F32)
    xb = scr("xb", [1024, TOKX], F32)
    x4 = scr("x4", [1024, TOKX], F32)
    x5 = scr("x5", [1024, TOK], F32)
    uT = scr("uT", [1024, TOKX], BF16)
    KT = scr("KTs", [NH, DH, NPOS * 256], BF16)
    Vd = scr("Vds", [NH, 128, 2 * NPOS, DH], BF16)
    kbar = scr("kbars", [NH * DH, NPOS], F32)
    QT = scr("QTs", [NH, DH, TOKX], BF16)
    OT = scr("OTs", [NH, DH, TOKX], BF16)
    S = Sched(nc)
    cx = Ctx()
    setup_common(S, cx)
    emit_load_small(S, cx, sp_d)
    hmask = S.sb("hmask", [128, 512], BF16)
    b_hm = Buf("hmask")
    S.op("sp", lambda: nc.sync.dma_start(out=hmask[:], in_=hm_d[:, :]), writes=[b_hm])
    modT = [S.sb("modT%d" % i, [128, 72], F32) for i in range(2)]
    b_modT = [Buf("modT%d" % i) for i in range(2)]
    emit_adaln_scoped(S, cx, ada_w, modT, b_modT)
    M = {}
    for i in range(2):
        for j in range(3):
            M[(i, j)] = emit_mod_vectors_i(S, cx, modT[i], b_modT[i], spc(cx, "ng%d%d" % (i, j)), "l%ds%d" % (i, j), j)
    nbase = len(S._stack)
    mk = lambda name, n: [Buf("%s_%d" % (name, t)) for t in range(n)]
    b_xall, b_x1all, b_x1o = mk("xall", NT_ALL), mk("x1all", NT_ALL), mk("x1o", 9)
    b_xa, b_xb, b_x4, b_u, b_x5, b_xo = mk("xa", 9), mk("xb", 9), mk("x4", 16), mk("u", 9), mk("x5", 8), mk("xo", 8)
    b_x4t = b_x4[:9]
    alloc_tile_bufs(S, cx)
    ntile = len(S._stack)
    alloc_ffn_bufs(S, cx)
    m = M[(0, 0)]
    emit_ffn_phase(S, cx, xall, b_xall, x1all, b_x1all, w_in[0], w_out[0], m[0], m[1], m[2], 0.5, m[3], NT_ALL, "f00")
    S.barrier_free(len(S._stack) - ntile)
    m = M[(0, 1)]
    b_kv = Buf("kvq")
    emit_kvq_phase(S, cx, x1all, b_x1all, wqkv, m[0], m[1], m[3], QT, KT, kbar, Vd, x1o, b_x1o, b_kv)
    S.barrier_free(len(S._stack) - nbase)
    b_OT = mk("OT", 9)
    emit_attn2(S, cx, {"KT": KT, "ind": ind_d, "Vd": Vd, "QT": QT, "kbar": kbar, "cst": cst_d, "OT": OT, "b_OT": b_OT, "b_kv": b_kv})
    S.barrier_free(len(S._stack) - nbase)
    alloc_tile_bufs(S, cx)
    emit_proj_res_phase(S, cx, x1o, b_x1o, xa, b_xa, OT, b_OT, w_o, m[2], None, m[3], "wo", True, ntiles=9)
    S.barrier_free(len(S._stack) - ntile)
    alloc_ffn_bufs(S, cx)
    m = M[(0, 2)]
    emit_ffn_phase(S, cx, xa, b_xa, xb, b_xb, w_in[1], w_out[1], m[0], m[1], m[2], 0.5, m[3], 9, "f01")
    m = M[(1, 0)]
    emit_ffn_phase(S, cx, xb, b_xb, x4, b_x4t, w_in[2], w_out[2], m[0], m[1], m[2], 0.5, m[3], 9, "f10")
    S.barrier_free(len(S._stack) - ntile)
    m = M[(1, 1)]
    emit_glu_phase(S, cx, x4, b_x4t, w_pw1, m[0], m[1], m[3], uT, ntiles=9, hmask=hmask[:], b_hmask=b_hm, b_uout=b_u)
    S.barrier_free(len(S._stack) - ntile)
    b_x4c = [Buf("x4c_%d" % t) for t in range(16)]
    emit_conv_phase(S, cx, None, x4, b_x4c, x5, b_x5, w_pw2, ident_d, m[2], m[3], u_scr=uT, b_u=b_u)
    S.barrier_free(len(S._stack) - ntile)
    alloc_ffn_bufs(S, cx)
    m = M[(1, 2)]
    emit_ffn_phase(S, cx, x5, b_x5, xo, b_xo, w_in[3], w_out[3], m[0], m[1], m[2], 0.5, m[3], 8, "f11")
    S.emit()
    return nc


def kernel(**inp):
    import ml_dtypes
    bf = ml_dtypes.bfloat16
    inp = {k: np.asarray(v) for k, v in inp.items()}
    cores = list(range(8))
    ind = np.zeros((64, SEQ), bf)
    for m in range(64):
        ind[m, m * 256:(m + 1) * 256] = 1
    ident = np.eye(128, dtype=np.float32)
    maps = []
    toks = []
    for c in cores:
        b, j = c // 4, c % 4
        real, dup = pos_order(j)
        xb_ = inp["x"][b]
        xall = np.zeros((NPOS * 256, D), np.float32)
        for pi, g in enumerate(real + dup):
            if g is not None:
                xall[pi * 256:(pi + 1) * 256] = xb_[g * 256:(g + 1) * 256]
        cst, hmask = make_cst2(j)
        toks.append(np.concatenate([np.arange(g * 256, (g + 1) * 256) for g in snake_blocks(j)]))
        mp = {"sp": pack_small(inp, b), "ada_w": inp["ada_w"], "xall": np.ascontiguousarray(xall.T),
              "wqkv": inp["attn_w_qkv"][0], "w_o": inp["attn_w_o"][0], "w_pw1": inp["conv_w_pw1"][0], "w_pw2": inp["conv_w_pw2"][0],
              "ind": ind, "cst": cst, "hmask": hmask.astype(bf), "ident": ident}
        for k, (i, w) in enumerate(((0, 0), (0, 1), (1, 0), (1, 1))):
            mp["w_in%d" % k] = inp["ffn_w_in"][i, w]
            mp["w_out%d" % k] = inp["ffn_w_out"][i, w]
        maps.append(mp)
    nc = _get("fused", build_fused)
    res = run_bass_kernel_spmd(nc, maps, core_ids=cores).results
    out = np.zeros((BATCH, SEQ, D), np.float32)
    for c in cores:
        out[c // 4][toks[c]] = np.asarray(res[c]["xo"]).T
    return out
```
